# Optimizing a Trainium2 kernel written in Bass

```python
import math
import jax
import jax.numpy as jnp
from jax import lax
import numpy as np

D_MODEL = 1024
BATCH = 8
SEQ = 4096
DEPTH = 2

GRID_W = 64
CTX_LEN = 256
N_MIXERS = 2
N_Q_HEADS = 16
N_KV_HEADS = 4
HEAD_DIM = D_MODEL // N_Q_HEADS
WINDOW = 128
BLOCK = 128
ROPE_THETA = 10000.0
DN_HEADS = 8
DN_DK = D_MODEL // DN_HEADS
DN_DV = D_MODEL // DN_HEADS
DN_WIDTH = DN_HEADS * DN_DV
CONV_K = 3
CHUNK = 64
N_EXPERTS = 16
EXPERT_FF = 2 * D_MODEL
CAPACITY_FACTOR = 2
ADA_SCALE = 0.5
EPS = 1e-6

kernel_name = 'hybrid_swa_gdn_ecmoe_dit'


def rms_norm(x, g):
    xf = x.astype(jnp.float32)
    y = xf * lax.rsqrt(jnp.mean(xf * xf, axis=-1, keepdims=True) + EPS)
    return (y * g.astype(jnp.float32)).astype(x.dtype)


def modulate(x, g, shift, scale):
    return rms_norm(x, g) * (1 + scale) + shift


def ada_modulation(cond, w, b):
    m = jax.nn.silu(cond) @ w + b
    return jnp.split(m, 6, axis=-1)


def axial_rope_angles(n_tokens):
    rows = n_tokens // GRID_W
    row = jnp.repeat(jnp.arange(rows), GRID_W)
    col = jnp.tile(jnp.arange(GRID_W), rows)
    quarter = HEAD_DIM // 4
    inv_freq = ROPE_THETA ** (-jnp.arange(quarter, dtype=jnp.float32) / quarter)
    ang = jnp.stack([row[:, None] * inv_freq, col[:, None] * inv_freq], axis=1)
    return jnp.cos(ang), jnp.sin(ang)


def apply_axial_rope(x, cos, sin):
    shp = x.shape
    quarter = HEAD_DIM // 4
    xr = x.reshape(shp[:-1] + (2, 2, quarter)).astype(jnp.float32)
    bshape = (shp[1],) + (1,) * (len(shp) - 3) + (2, quarter)
    c = cos.reshape(bshape)
    s = sin.reshape(bshape)
    x1 = xr[..., 0, :]
    x2 = xr[..., 1, :]
    out = jnp.stack([x1 * c - x2 * s, x2 * c + x1 * s], axis=-2)
    return out.reshape(shp).astype(x.dtype)


def window_sink_attention(h_lat, h_ctx, w_qkv, sink, w_o, need_ctx_out):
    B, N, _ = h_lat.shape
    L = h_ctx.shape[1]
    G = N_Q_HEADS // N_KV_HEADS
    dq = N_Q_HEADS * HEAD_DIM
    dkv = N_KV_HEADS * HEAD_DIM

    def project(h):
        p = h @ w_qkv
        lead = h.shape[:2]
        q = p[..., :dq].reshape(lead + (N_KV_HEADS, G, HEAD_DIM))
        k = p[..., dq:dq + dkv].reshape(lead + (N_KV_HEADS, HEAD_DIM))
        v = p[..., dq + dkv:].reshape(lead + (N_KV_HEADS, HEAD_DIM))
        return q, k, v

    ql, kl, vl = project(h_lat)
    qc, kc, vc = project(h_ctx)
    cos, sin = axial_rope_angles(N)
    ql = apply_axial_rope(ql, cos, sin)
    kl = apply_axial_rope(kl, cos, sin)
    scale = HEAD_DIM ** -0.5
    sink_b = sink.astype(jnp.float32).reshape(N_KV_HEADS, G)[None, :, :, None, None]

    def sink_softmax(s):
        m = jnp.maximum(jnp.max(s, axis=-1, keepdims=True), sink_b)
        p = jnp.exp(s - m)
        return p / (jnp.sum(p, axis=-1, keepdims=True) + jnp.exp(sink_b - m))

    def ctx_scores(q):
        return jnp.einsum('bqhgd,bkhd->bhgqk', q, kc).astype(jnp.float32) * scale

    kpad = jnp.pad(kl, ((0, 0), (BLOCK, BLOCK), (0, 0), (0, 0)))
    vpad = jnp.pad(vl, ((0, 0), (BLOCK, BLOCK), (0, 0), (0, 0)))
    offs = jnp.arange(3 * BLOCK) - BLOCK
    rel = offs[None, :] - jnp.arange(BLOCK)[:, None]
    band = jnp.abs(rel) <= WINDOW

    def block(i):
        start = i * BLOCK
        q_i = lax.dynamic_slice_in_dim(ql, start, BLOCK, axis=1)
        k_i = lax.dynamic_slice_in_dim(kpad, start, 3 * BLOCK, axis=1)
        v_i = lax.dynamic_slice_in_dim(vpad, start, 3 * BLOCK, axis=1)
        kpos = start + offs
        mask = band & ((kpos >= 0) & (kpos < N))[None, :]
        s_loc = jnp.einsum('bqhgd,bkhd->bhgqk', q_i, k_i).astype(jnp.float32) * scale
        s_loc = jnp.where(mask, s_loc, -jnp.inf)
        p = sink_softmax(jnp.concatenate([s_loc, ctx_scores(q_i)], axis=-1)).astype(v_i.dtype)
        return (jnp.einsum('bhgqk,bkhd->bqhgd', p[..., :3 * BLOCK], v_i)
                + jnp.einsum('bhgqk,bkhd->bqhgd', p[..., 3 * BLOCK:], vc))

    o_lat = lax.map(block, jnp.arange(N // BLOCK))
    o_lat = jnp.moveaxis(o_lat, 0, 1).reshape(B, N, dq) @ w_o
    o_ctx = None
    if need_ctx_out:
        p = sink_softmax(ctx_scores(qc)).astype(vc.dtype)
        o_ctx = jnp.einsum('bhgqk,bkhd->bqhgd', p, vc).reshape(B, L, dq) @ w_o
    return o_lat, o_ctx


def centred_depthwise_conv(x, w):
    K, C = w.shape
    return lax.conv_general_dilated(x, w.reshape(K, 1, C).astype(x.dtype), window_strides=(1,),
                                    padding=[(K // 2, K // 2)], dimension_numbers=('NWC', 'WIO', 'NWC'),
                                    feature_group_count=C)


def l2_normalise(t):
    tf = t.astype(jnp.float32)
    return tf * lax.rsqrt(jnp.sum(tf * tf, axis=-1, keepdims=True) + EPS)


def chunk_gated_delta(q, k, v, g, beta, s0):
    B, N, H, _ = q.shape
    dv = v.shape[-1]
    nc = N // CHUNK

    def chunks(t):
        return t.reshape(B, nc, CHUNK, H, -1).transpose(1, 0, 3, 2, 4)

    qc, kc, vc = chunks(q), chunks(k), chunks(v)
    gc = jnp.cumsum(g.reshape(B, nc, CHUNK, H).transpose(1, 0, 3, 2), axis=-1)
    bc = beta.reshape(B, nc, CHUNK, H).transpose(1, 0, 3, 2)[..., None]
    tril = jnp.tril(jnp.ones((CHUNK, CHUNK), bool))
    strict = jnp.tril(jnp.ones((CHUNK, CHUNK), bool), -1)
    diff = gc[..., :, None] - gc[..., None, :]
    decay = jnp.where(tril, jnp.exp(jnp.where(tril, diff, 0.0)), 0.0)
    kb = kc * bc
    a_mat = jnp.where(strict, jnp.einsum('nbhid,nbhjd->nbhij', kb, kc) * decay, 0.0)
    eye = jnp.broadcast_to(jnp.eye(CHUNK, dtype=a_mat.dtype), a_mat.shape)
    t_mat = lax.linalg.triangular_solve(eye + a_mat, eye, left_side=True, lower=True)
    u = t_mat @ (vc * bc)
    w = t_mat @ (kb * jnp.exp(gc)[..., None])
    qk = jnp.einsum('nbhid,nbhjd->nbhij', qc, kc) * decay

    def step(state, xs):
        q_i, k_i, u_i, w_i, qk_i, g_i = xs
        v_new = u_i - w_i @ state
        o_i = (q_i * jnp.exp(g_i)[..., None]) @ state + qk_i @ v_new
        g_last = g_i[..., -1:]
        state = state * jnp.exp(g_last)[..., None] + jnp.einsum(
            'bhcd,bhce->bhde', k_i * jnp.exp(g_last - g_i)[..., None], v_new)
        return state, o_i

    s_final, o = lax.scan(step, s0, (qc, kc, u, w, qk, gc))
    return o.transpose(1, 0, 3, 2, 4).reshape(B, N, H, dv), s_final


def gated_deltanet_bidir(h_lat, h_ctx, w_in, conv_w, a_log, dt_bias, o_norm, w_o, need_ctx_out):
    def project(h):
        B, N, _ = h.shape
        p = h @ w_in
        qkv = jax.nn.silu(centred_depthwise_conv(p[..., :3 * DN_WIDTH], conv_w))
        q, k, v = jnp.split(qkv, 3, axis=-1)
        q = l2_normalise(q.reshape(B, N, DN_HEADS, DN_DK)) * (DN_DK ** -0.5)
        k = l2_normalise(k.reshape(B, N, DN_HEADS, DN_DK))
        v = v.reshape(B, N, DN_HEADS, DN_DV).astype(jnp.float32)
        z = p[..., 3 * DN_WIDTH:4 * DN_WIDTH]
        a = p[..., 4 * DN_WIDTH:4 * DN_WIDTH + 2 * DN_HEADS].astype(jnp.float32).reshape(B, N, 2, DN_HEADS)
        b = p[..., 4 * DN_WIDTH + 2 * DN_HEADS:].astype(jnp.float32).reshape(B, N, 2, DN_HEADS)
        g = -jnp.exp(a_log.astype(jnp.float32)) * jax.nn.softplus(a + dt_bias.astype(jnp.float32))
        beta = jax.nn.sigmoid(b)
        return q, k, v, z, g, beta

    ql, kl, vl, zl, gl, bl = project(h_lat)
    qc, kc, vc, zc, gc, bc = project(h_ctx)
    s0 = jnp.zeros((h_lat.shape[0], DN_HEADS, DN_DK, DN_DV), jnp.float32)

    def flip(t):
        return jnp.flip(t, axis=1)

    oc_f, sc_f = chunk_gated_delta(qc, kc, vc, gc[:, :, 0], bc[:, :, 0], s0)
    ol_f, _ = chunk_gated_delta(ql, kl, vl, gl[:, :, 0], bl[:, :, 0], sc_f)
    oc_b, sc_b = chunk_gated_delta(flip(qc), flip(kc), flip(vc), flip(gc[:, :, 1]), flip(bc[:, :, 1]), s0)
    ol_b, _ = chunk_gated_delta(flip(ql), flip(kl), flip(vl), flip(gl[:, :, 1]), flip(bl[:, :, 1]), sc_b)

    def readout(o, z):
        B, N = o.shape[:2]
        o = o * lax.rsqrt(jnp.mean(o * o, axis=-1, keepdims=True) + EPS) * o_norm.astype(jnp.float32)
        o = o.astype(z.dtype) * jax.nn.silu(z).reshape(B, N, DN_HEADS, DN_DV)
        return o.reshape(B, N, DN_WIDTH) @ w_o

    o_lat = readout(ol_f + flip(ol_b), zl)
    o_ctx = readout(oc_f + flip(oc_b), zc) if need_ctx_out else None
    return o_lat, o_ctx


def expert_choice_moe(h, w_router, w_gate, w_up, w_down):
    B, N, D = h.shape
    cap = CAPACITY_FACTOR * N // N_EXPERTS
    aff = jax.nn.softmax((h @ w_router).astype(jnp.float32), axis=-1)
    gates, idx = lax.top_k(jnp.swapaxes(aff, 1, 2), cap)
    xs = jax.vmap(lambda hb, ib: hb[ib])(h, idx)
    hid = jax.nn.silu(jnp.einsum('becd,edf->becf', xs, w_gate)) * jnp.einsum('becd,edf->becf', xs, w_up)
    y = jnp.einsum('becf,efd->becd', hid, w_down) * gates[..., None].astype(h.dtype)
    return jax.vmap(lambda ib, yb: jnp.zeros((N, D), h.dtype).at[ib.reshape(-1)].add(yb.reshape(-1, D)))(idx, y)


def setup_inputs(seed: int = 0) -> dict:
    key = jax.random.key(seed)
    ks = jax.random.split(key, 30)
    D = D_MODEL

    def nrm(i, shape, scale):
        return jax.random.normal(ks[i], shape, jnp.float32) * scale

    def gain(i, n):
        return 1.0 + nrm(i, (n,), 0.02)

    a_log = jnp.log(jax.random.uniform(ks[20], (2, DN_HEADS), jnp.float32, minval=1.0, maxval=16.0))
    dt = jnp.exp(jax.random.uniform(ks[21], (2, DN_HEADS), jnp.float32,
                                    minval=math.log(1e-3), maxval=math.log(1e-1)))
    dt_bias = dt + jnp.log(-jnp.expm1(-dt))
    return {
        'x': nrm(0, (BATCH, SEQ, D), 1.0),
        'c': nrm(1, (BATCH, D), 1.0),
        'ctx': nrm(2, (BATCH, CTX_LEN, D), 1.0),
        'c_ctx': nrm(3, (D,), 1.0),
        'l0_ada_w': nrm(4, (D, 6 * D), ADA_SCALE * D ** -0.5),
        'l0_ada_b': nrm(5, (6 * D,), 0.02),
        'l0_norm_mix': gain(6, D),
        'l0_w_qkv': nrm(7, (D, (N_Q_HEADS + 2 * N_KV_HEADS) * HEAD_DIM), D ** -0.5),
        'l0_sink': nrm(8, (N_Q_HEADS,), 0.5),
        'l0_w_o': nrm(9, (N_Q_HEADS * HEAD_DIM, D), (N_Q_HEADS * HEAD_DIM) ** -0.5),
        'l0_norm_ffn': gain(10, D),
        'l0_router': nrm(11, (D, N_EXPERTS), D ** -0.5),
        'l0_w_gate': nrm(12, (N_EXPERTS, D, EXPERT_FF), D ** -0.5),
        'l0_w_up': nrm(13, (N_EXPERTS, D, EXPERT_FF), D ** -0.5),
        'l0_w_down': nrm(14, (N_EXPERTS, EXPERT_FF, D), EXPERT_FF ** -0.5),
        'l1_ada_w': nrm(15, (D, 6 * D), ADA_SCALE * D ** -0.5),
        'l1_ada_b': nrm(16, (6 * D,), 0.02),
        'l1_norm_mix': gain(17, D),
        'l1_w_in': nrm(18, (D, 4 * DN_WIDTH + 4 * DN_HEADS), D ** -0.5),
        'l1_conv': nrm(19, (CONV_K, 3 * DN_WIDTH), CONV_K ** -0.5),
        'l1_a_log': a_log,
        'l1_dt_bias': dt_bias,
        'l1_o_norm': gain(22, DN_DV),
        'l1_w_o': nrm(23, (DN_WIDTH, D), DN_WIDTH ** -0.5),
        'l1_norm_ffn': gain(24, D),
        'l1_router': nrm(25, (D, N_EXPERTS), D ** -0.5),
        'l1_w_gate': nrm(26, (N_EXPERTS, D, EXPERT_FF), D ** -0.5),
        'l1_w_up': nrm(27, (N_EXPERTS, D, EXPERT_FF), D ** -0.5),
        'l1_w_down': nrm(28, (N_EXPERTS, EXPERT_FF, D), EXPERT_FF ** -0.5),
        'final_norm': gain(29, D),
    }


def reference(x, c, ctx, c_ctx,
              l0_ada_w, l0_ada_b, l0_norm_mix, l0_w_qkv, l0_sink, l0_w_o, l0_norm_ffn,
              l0_router, l0_w_gate, l0_w_up, l0_w_down,
              l1_ada_w, l1_ada_b, l1_norm_mix, l1_w_in, l1_conv, l1_a_log, l1_dt_bias, l1_o_norm, l1_w_o,
              l1_norm_ffn, l1_router, l1_w_gate, l1_w_up, l1_w_down,
              final_norm):
    layer_params = (
        ((l0_ada_w, l0_ada_b, l0_norm_mix, l0_norm_ffn, l0_router, l0_w_gate, l0_w_up, l0_w_down),
         (l0_w_qkv, l0_sink, l0_w_o)),
        ((l1_ada_w, l1_ada_b, l1_norm_mix, l1_norm_ffn, l1_router, l1_w_gate, l1_w_up, l1_w_down),
         (l1_w_in, l1_conv, l1_a_log, l1_dt_bias, l1_o_norm, l1_w_o)),
    )
    x_lat, x_ctx = x, ctx
    for i in range(DEPTH):
        (ada_w, ada_b, norm_mix, norm_ffn, router, w_gate, w_up, w_down), mix_params = layer_params[i]
        last = i == DEPTH - 1
        sh1, sc1, gt1, sh2, sc2, gt2 = [t[:, None, :] for t in ada_modulation(c, ada_w, ada_b)]
        csh1, csc1, cgt1, csh2, csc2, cgt2 = ada_modulation(c_ctx, ada_w, ada_b)
        h_lat = modulate(x_lat, norm_mix, sh1, sc1)
        h_ctx = modulate(x_ctx, norm_mix, csh1, csc1)
        if i % N_MIXERS == 0:
            m_lat, m_ctx = window_sink_attention(h_lat, h_ctx, *mix_params, need_ctx_out=not last)
        else:
            m_lat, m_ctx = gated_deltanet_bidir(h_lat, h_ctx, *mix_params, need_ctx_out=not last)
        x_lat = x_lat + gt1 * m_lat
        x_lat = x_lat + gt2 * expert_choice_moe(modulate(x_lat, norm_ffn, sh2, sc2), router, w_gate, w_up, w_down)
        if not last:
            x_ctx = x_ctx + cgt1 * m_ctx
            x_ctx = x_ctx + cgt2 * expert_choice_moe(modulate(x_ctx, norm_ffn, csh2, csc2),
                                                     router, w_gate, w_up, w_down)
    return rms_norm(x_lat, final_norm)
```

```python
import numpy as np
import concourse.bass as bass
import concourse.mybir as mybir
from concourse.bass_utils import run_bass_kernel_spmd

F32 = mybir.dt.float32
F32R = mybir.dt.float32r
U32 = mybir.dt.uint32
ALU = mybir.AluOpType
AF = mybir.ActivationFunctionType
AX = mybir.AxisListType

D = 1024
NLAT = 4096
NCTX = 256
NT_LAT = 32
NT_CTX = 2
NT = 34
NTOK = NLAT + NCTX
NE = 16
FF = 2048
CAP_LAT = 512
CAP_CTX = 32
NDUMMY = 640
EPS = 1e-6
NBIS = 30

C_ID, C_ONES, C_U, C_MP, C_MN, C_IOTA, C_PIDX, C_DMY, C_END = 0, 128, 256, 384, 896, 1408, 1920, 1921, 1926


C2_LF, C2_LB, C2_LT, C2_GT, C2_GE, C2_LE, C2_B64, C2_NB64, C2_OFF, C2_END = 0, 128, 256, 384, 512, 640, 768, 896, 1024, 1152
NEG = -30000.0


class Sched:
    def __init__(self, nc, n_dma_sems=24):
        self.nc = nc
        self.engs = {"pe": nc.tensor, "act": nc.scalar, "dve": nc.vector,
                     "pool": nc.gpsimd, "sp": nc.sync}
        self.csem = {e: nc.alloc_semaphore("c_" + e) for e in ("pe", "act", "dve", "pool")}
        self.ccnt = {e: 0 for e in self.csem}
        self.known = {e: {} for e in self.engs}
        self.dsems = [nc.alloc_semaphore("d%d" % i) for i in range(2 * n_dma_sems)]
        self.dcnt = [0] * (2 * n_dma_sems)
        self.dpool = {"sp": list(range(0, n_dma_sems)), "pool": list(range(n_dma_sems, 2 * n_dma_sems))}
        self.dnext = {"sp": 0, "pool": 0}
        self.state = {}
        self.out_events = []
        self.n_wait = 0
        self.n_inst = 0

    def _need(self, eng, ev):
        sem, val = ev
        k = self.known[eng]
        if k.get(sem.num, 0) >= val:
            return
        self.engs[eng].wait_ge(sem, val)
        self.n_wait += 1
        k[sem.num] = val

    def _deps(self, eng, reads, writes, skip_self=False):
        evs = {}

        def add(ev):
            if ev is None:
                return
            sem, val = ev
            if skip_self and sem.num == self.csem[eng].num:
                return
            if evs.get(sem.num, (None, 0))[1] < val:
                evs[sem.num] = ev

        own = self.csem[eng].num if eng in self.csem else -1
        for k in reads:
            st = self.state.get(k)
            if st:
                add(st["w"])
                if isinstance(k, tuple) and k[0] == "ps":
                    for r in st["r"]:
                        if r[0].num != own:
                            add(r)
        for k in writes:
            st = self.state.get(k)
            if st:
                add(st["w"])
                for r in st["r"]:
                    add(r)
        for ev in evs.values():
            self._need(eng, ev)

    def _commit(self, ev, reads, writes):
        for k in reads:
            st = self.state.setdefault(k, {"w": None, "r": []})
            st["r"] = [r for r in st["r"] if r[0].num != ev[0].num] + [ev]
        for k in writes:
            self.state[k] = {"w": ev, "r": []}

    def op(self, eng, fn, reads=(), writes=()):
        self._deps(eng, reads, writes, skip_self=(eng == "pe"))
        ins = fn(self.engs[eng])
        self.ccnt[eng] += 1
        ins.then_inc(self.csem[eng], 1)
        ev = (self.csem[eng], self.ccnt[eng])
        self._commit(ev, reads, writes)
        self.n_inst += 1
        return ev

    def dma(self, q, fn, reads=(), writes=(), is_output=False):
        self._deps(q, reads, writes)
        pool = self.dpool[q]
        i = pool[self.dnext[q]]
        self.dnext[q] = (self.dnext[q] + 1) % len(pool)
        sem = self.dsems[i]
        if self.dcnt[i] > 0:
            self._need(q, (sem, 16 * self.dcnt[i]))
        ins = fn(self.engs[q])
        self.dcnt[i] += 1
        ins.then_inc(sem, 16)
        ev = (sem, 16 * self.dcnt[i])
        self._commit(ev, reads, writes)
        if is_output:
            self.out_events.append(ev)
        self.n_inst += 1
        return ev

    def barrier(self):
        for eng in self.engs:
            for i, sem in enumerate(self.dsems):
                if self.dcnt[i] > 0:
                    self._need(eng, (sem, 16 * self.dcnt[i]))
            for e, sem in self.csem.items():
                if self.ccnt[e] > 0:
                    self._need(eng, (sem, self.ccnt[e]))

    def finish(self, eng="sp"):
        for i, sem in enumerate(self.dsems):
            if self.dcnt[i] > 0:
                self._need(eng, (sem, 16 * self.dcnt[i]))
        for e, sem in self.csem.items():
            if self.ccnt[e] > 0:
                self._need(eng, (sem, self.ccnt[e]))


class Scope:
    def __init__(self, builder):
        from contextlib import ExitStack
        self.b = builder
        self.st = ExitStack()

    def sb(self, name, shape, dtype=F32):
        return self.st.enter_context(self.b.nc.sbuf_tensor(name, list(shape), dtype))

    def close(self):
        self.b.S.barrier()
        self.st.close()


class Ring:
    def __init__(self, sc, name, shape, dtype, n):
        self.t = [sc.sb("%s%d" % (name, i), shape, dtype) for i in range(n)]
        self.k = [("%s" % name, i) for i in range(n)]
        self.i = 0

    def next(self):
        r = (self.t[self.i], self.k[self.i])
        self.i = (self.i + 1) % len(self.t)
        return r


class Builder:
    def __init__(self, stage=99, debug=False, n_exp=None, start_layer=0, n_tiles=None):
        self.n_exp = n_exp
        self.start_layer = start_layer
        self.n_tiles = n_tiles
        self.stage = stage
        self.debug = debug
        nc = bass.Bass("TRN2", target_bir_lowering=False)
        self.nc = nc
        self.S = Sched(nc)
        self.inp = {}
        self.ps = [nc.alloc_psum_tensor("psb%d" % i, [128, 512], F32) for i in range(8)]
        self.psk = [("ps", i) for i in range(8)]

    def din(self, name, shape, dtype=F32):
        t = self.nc.dram_tensor(name, list(shape), dtype, kind="ExternalInput").ap()
        self.inp[name] = t
        return t

    def dscratch(self, name, shape, dtype=F32, out=False):
        kind = "ExternalOutput" if (out or self.debug) else "Internal"
        return self.nc.dram_tensor(name, list(shape), dtype, kind=kind).ap()

    def sb(self, name, shape, dtype=F32):
        return self.nc.alloc_sbuf_tensor(name, list(shape), dtype)

    def mm(self, out, lhsT, rhs, start, stop, reads, writes):
        return self.S.op("pe", lambda e: e.matmul(out, lhsT=lhsT, rhs=rhs, start=start, stop=stop),
                         reads, writes)

    def tr(self, out, in_, reads, writes, kp=128):
        ident = self.cst[0:kp, C_ID:C_ID + kp]
        return self.S.op("pe", lambda e: e.transpose(out, in_, ident), list(reads) + ["cst"], writes)

    def act(self, out, in_, func, reads, writes, **kw):
        return self.S.op("act", lambda e: e.activation(out=out, in_=in_, func=func, **kw), reads, writes)

    def tt(self, eng, out, in0, in1, op, reads, writes):
        return self.S.op(eng, lambda e: e.tensor_tensor(out=out, in0=in0, in1=in1, op=op), reads, writes)

    def ts(self, eng, out, in0, s1, s2, op0, op1, reads, writes, **kw):
        if s2 is None:
            return self.S.op(eng, lambda e: e.tensor_scalar(out=out, in0=in0, scalar1=s1, scalar2=None,
                                                            op0=op0, **kw), reads, writes)
        return self.S.op(eng, lambda e: e.tensor_scalar(out=out, in0=in0, scalar1=s1, scalar2=s2,
                                                        op0=op0, op1=op1, **kw), reads, writes)

    def stt(self, eng, out, in0, scalar, in1, op0, op1, reads, writes):
        return self.S.op(eng, lambda e: e.scalar_tensor_tensor(out=out, in0=in0, scalar=scalar, in1=in1,
                                                               op0=op0, op1=op1), reads, writes)

    def cp(self, eng, out, in_, reads, writes):
        if eng == "act":
            return self.act(out, in_, AF.Copy, reads, writes)
        return self.S.op(eng, lambda e: e.tensor_copy(out=out, in_=in_), reads, writes)

    def ld(self, out, in_, reads, writes, q="sp"):
        return self.S.dma(q, lambda e: e.dma_start(out=out, in_=in_), reads, writes)

    def rstd_of(self, x_ap, xk, junk, junkk, ss, ssk):
        self.act(junk, x_ap, AF.Square, [xk], [junkk, ssk], accum_out=ss)
        self.ts("dve", ss, ss, 1.0 / D, EPS, ALU.mult, ALU.add, [ssk], [ssk])
        self.act(ss, ss, AF.Sqrt, [ssk], [ssk])
        self.S.op("dve", lambda e: e.reciprocal(out=ss, in_=ss), [ssk], [ssk])

    def build(self):
        nc, S = self.nc, self.S
        st, sl = self.stage, self.start_layer
        x = self.din("x", [NLAT, D]) if sl == 0 else None
        ctx = self.din("ctx", [NCTX, D]) if sl == 0 else None
        cvecT = self.din("cvecT", [128, 8, 2])
        cpack = self.din("cpack", [128, C_END])
        rope = self.din("rope", [NLAT, 64]) if sl == 0 else None
        W = {}
        for l in (0, 1):
            if l == 0 and sl > 0:
                continue
            if l == 1 and st < 30:
                continue
            W[l] = dict(
                ada_w=self.din("l%d_ada_w" % l, [D, 6 * D]),
                ada_b=self.din("l%d_ada_b" % l, [6 * D]),
                ada_bT=self.din("l%d_ada_bT" % l, [128, 48]),
                nmixT=self.din("l%d_nmixT" % l, [128, 8]),
                nffnT=self.din("l%d_nffnT" % l, [128, 8]),
                router=self.din("l%d_router" % l, [D, NE]),
            )
            if (l == 0 and st >= 2) or (l == 1 and st >= 40):
                W[l].update(w_gate=self.din("l%d_w_gate" % l, [NE, D, FF]),
                            w_up=self.din("l%d_w_up" % l, [NE, D, FF]),
                            w_down=self.din("l%d_w_down" % l, [NE, FF, D]))
        if 0 in W:
            W[0].update(w_qkv=self.din("l0_w_qkv", [D, 1536]), sink=self.din("l0_sink", [16]),
                        w_o=self.din("l0_w_o", [D, D]))
        if 1 in W:
            W[1].update(w_in=self.din("l1_w_in", [D, 4128]), convT=self.din("l1_convT", [128, 24, 3]),
                        a_log=self.din("l1_a_log", [16]), dt_bias=self.din("l1_dt_bias", [16]),
                        o_norm=self.din("l1_o_norm", [128]), w_o=self.din("l1_w_o", [D, D]))
            cpack2 = self.din("cpack2", [128, C2_END])
        if st >= 50:
            self.din("final_norm", [D])
        self.W = W
        self.x, self.ctx, self.rope = x, ctx, rope
        self.out = self.nc.dram_tensor("out", [NLAT, D], F32, kind="ExternalOutput").ap()
        self.qs = self.dscratch("qs", [NTOK, D])
        if sl == 0:
            xa = self.dscratch("xresA", [NTOK + NDUMMY, D])
        else:
            xa = self.din("xresA_in", [NTOK + NDUMMY, D])
        self.xres = [xa, self.dscratch("xresB", [NTOK + NDUMMY, D])]
        self.xn2 = self.dscratch("xn2", [NTOK + NDUMMY, D])

        self.cst = self.sb("cst", [128, C_END])
        self.ld(self.cst[:], cpack, [], ["cst"])
        self.scT = self.sb("scT", [128, 8, 2], F32R)
        self.cols = self.sb("cols", [128, 48, 2])
        self.A1 = self.sb("A1", [128, 8, 2]); self.A2 = self.sb("A2", [128, 8, 2])
        self.Gbc = self.sb("Gbc", [128, 2, 2, D])
        self.aff = self.sb("aff", [128, NT, NE])
        if self.debug:
            S.op("dve", lambda e: e.memset(self.aff[:], 0.0), [], [("aff", i) for i in range(NT)])
        sc0 = Scope(self)
        zero_t = sc0.sb("zero_t", [128, D])
        S.op("dve", lambda e: e.memset(zero_t[:], 0.0), [], ["zero_t"])
        bufs = [(self.xres[1], "xres1"), (self.xn2, "xn2")] + ([(self.xres[0], "xres0")] if sl == 0 else [])
        for buf, k in bufs:
            for r in range(NDUMMY // 128):
                self.ld(buf[NTOK + r * 128: NTOK + (r + 1) * 128, :], zero_t[:], ["zero_t"], [(k, "dummy", r)])
        sc0.close()
        if sl == 0:
            self.modulation(0)
            if st >= 1:
                self.attention_layer()
            if st >= 2:
                self.moe(0, self.xres[0], "xres0", with_ctx=True)
        if st >= 30:
            self.cst2 = self.sb("cst2", [128, C2_END])
            self.ld(self.cst2[:], cpack2, [], ["cst2"])
            self.modulation(1)
            self.deltanet_layer()
        if st >= 40:
            self.moe(1, self.xres[1], "xres1", with_ctx=False)
        if st >= 50:
            self.final_norm()
        if self.debug:
            d_aff = self.nc.dram_tensor("d_aff", [128, NT * NE], F32, kind="ExternalOutput").ap()
            self.ld(d_aff, self.aff[:, :, :].rearrange("p j e -> p (j e)"), [("aff", i) for i in range(NT)], ["d_aff"])
            d_cols = self.nc.dram_tensor("d_cols", [128, 96], F32, kind="ExternalOutput").ap()
            self.ld(d_cols, self.cols[:, :, :].rearrange("p c t -> p (c t)"), ["cols"], ["d_cols"])
            d_g = self.nc.dram_tensor("d_g", [128, 4 * D], F32, kind="ExternalOutput").ap()
            self.ld(d_g, self.Gbc[:, :, :, :].rearrange("p a b d -> p (a b d)"),
                    [("Gbc", a, b) for a in range(2) for b in range(2)], ["d_g"])
        S.finish()
        return nc

    def modulation(self, l):
        nc, S, W = self.nc, self.S, self.W[l]
        sfx = "m%d" % l
        sc = Scope(self)
        self.wring = Ring(sc, "wst" + sfx, [128, 8, 512], F32R, 4)
        cv = sc.sb("cv" + sfx, [128, 8, 2])
        self.ld(cv[:], self.inp["cvecT"], [], ["cv"])
        self.act(self.scT[:], cv[:], AF.Silu, ["cv"], ["scT"])
        screp = [sc.sb("screp%d%s" % (c, sfx), [128, 8, 128], F32R) for c in range(2)]
        for c in range(2):
            self.cp("dve", screp[c][:], self.scT[:, :, c:c + 1].to_broadcast([128, 8, 128]), ["scT"], [("screp", c)])
        abT = sc.sb("abT" + sfx, [128, 48])
        self.ld(abT[:], W["ada_bT"], [], ["abT"])
        nmix = sc.sb("nmix" + sfx, [128, 8]); nffn = sc.sb("nffn" + sfx, [128, 8])
        self.ld(nmix[:], W["nmixT"], [], ["nmix"])
        self.ld(nffn[:], W["nffnT"], [], ["nffn"])
        abbc = sc.sb("abbc" + sfx, [128, 2, D])
        self.ld(abbc[:, 0, :], W["ada_b"][2 * D:3 * D].partition_broadcast(128), [], [("abbc", 0)])
        self.ld(abbc[:, 1, :], W["ada_b"][5 * D:6 * D].partition_broadcast(128), [], [("abbc", 1)])
        pcol = self.ps[0]
        pcv = pcol[:, 0:96].rearrange("p (c t) -> p c t", t=2)
        for cg in range(12):
            wt, wk = self.wring.next()
            self.ld(wt[:], W["ada_w"][:, cg * 512:(cg + 1) * 512].rearrange("(k p) n -> p k n", p=128),
                    [], [wk], q="pool")
            for c4 in range(4):
                cc = cg * 4 + c4
                for kc in range(8):
                    self.mm(pcv[:, cc, :], wt[:, kc, c4 * 128:(c4 + 1) * 128], self.scT[:, kc, :],
                            kc == 0, kc == 7, [wk, "scT"], [self.psk[0]])
            if cg in (4, 5, 10, 11):
                gi = 0 if cg < 6 else 1
                half = cg % 2
                for c in range(2):
                    pb, pbk = self.ps[1 + c], self.psk[1 + c]
                    for kc in range(8):
                        self.mm(pb[:, :], screp[c][:, kc, :], wt[:, kc, :], kc == 0, kc == 7,
                                [wk, ("screp", c)], [pbk])
                    self.tt("dve", self.Gbc[:, gi, c, half * 512:(half + 1) * 512], pb[:, :],
                            abbc[:, gi, half * 512:(half + 1) * 512], ALU.add,
                            [pbk, ("abbc", gi)], [("Gbc", gi, c)])
        self.tt("dve", self.cols[:], pcv, abT[:].unsqueeze(2).to_broadcast([128, 48, 2]), ALU.add,
                [self.psk[0], "abT"], ["cols"])
        for (A, nrm, nk, v) in ((self.A1, nmix, "nmix", 1), (self.A2, nffn, "nffn", 4)):
            self.stt("dve", A[:], self.cols[:, v * 8:(v + 1) * 8, :], 1.0,
                     nrm[:].unsqueeze(2).to_broadcast([128, 8, 2]), ALU.add, ALU.mult,
                     ["cols", nk], [("A", v)])
        sc.close()

    def B1(self, kc, c):
        return self.cols[:, 0 + kc, c:c + 1]

    def B2(self, kc, c):
        return self.cols[:, 24 + kc, c:c + 1]

    def attention_layer(self):
        nc, S, W = self.nc, self.S, self.W[0]
        cst = self.cst
        sca = Scope(self)
        KT = sca.sb("KT", [128, 2, NTOK], F32R)
        V = sca.sb("Vaug", [128, NT, 4, 66], F32R)
        esink = sca.sb("esink", [128, 16])
        wr = sca.sb("wr", [128, 8, NE])
        xr = Ring(sca, "xt", [128, D], F32, 2)
        xnr = Ring(sca, "xnb", [128, D], F32, 3)
        ssr = Ring(sca, "ss", [128, 1], F32, 6)
        junk = sca.sb("junk", [128, D])
        sc1 = Scope(self)
        wbig = sc1.sb("wbig", [128, 8, 1536], F32R)
        hTr = Ring(sc1, "hT", [128, 8, 128], F32R, 3)
        qkr = Ring(sc1, "qk", [128, 1280], F32, 2)
        csr = Ring(sc1, "cs", [128, 64], F32, 6)
        tmpr = Ring(sc1, "rt", [128, 4, 256], F32, 1)
        for cg in range(3):
            self.ld(wbig[:, :, cg * 512:(cg + 1) * 512],
                    W["w_qkv"][:, cg * 512:(cg + 1) * 512].rearrange("(k p) n -> p k n", p=128),
                    [], [("wbig", cg)], q="pool")
        Vf = V[:, :, :, :].rearrange("p j h c -> p (j h) c")
        self.cp("dve", Vf[:, :, 64:65], self.cst[:, C_ONES:C_ONES + 1].unsqueeze(1).to_broadcast([128, NT * 4, 1]), ["cst"], [("V1",)])
        self.cp("dve", Vf[:, :, 65:66], self.cst[:, C_U:C_U + 1].unsqueeze(1).to_broadcast([128, NT * 4, 1]), ["cst"], [("V0",)])
        self.ld(esink[:], W["sink"].partition_broadcast(128), [], ["esink"])
        self.act(esink[:], esink[:], AF.Exp, ["esink"], ["esink"])
        self.ld(wr[:], W["router"].rearrange("(k p) n -> p k n", p=128), [], ["wr"])

        def src_rows(j):
            if j < NT_LAT:
                return self.x[j * 128:(j + 1) * 128, :]
            return self.ctx[(j - NT_LAT) * 128:(j - NT_LAT + 1) * 128, :]

        cx = [dict() for _ in range(NT)]

        def p1_s0(j):
            c = cx[j]
            c["c"] = 0 if j < NT_LAT else 1
            xt, xk = xr.next()
            self.ld(xt[:], src_rows(j), [], [xk])
            if c["c"] == 0:
                c["cs"], c["ck"] = csr.next()
                self.ld(c["cs"][:], self.rope[j * 128:(j + 1) * 128, :], [], [c["ck"]])
            ss, ssk = ssr.next()
            self.rstd_of(xt[:], xk, junk[:], "junk", ss[:], ssk)
            c["xn"], c["xnk"] = xnr.next()
            self.ts("dve", c["xn"][:], xt[:], ss[:, 0:1], None, ALU.mult, None, [xk, ssk], [c["xnk"]])

        def p1_s1(j):
            c = cx[j]
            xn, xnk, cc = c["xn"], c["xnk"], c["c"]
            c["hT"], c["hk"] = hTr.next()
            hT, hk = c["hT"], c["hk"]
            for kc in range(8):
                pt, ptk = self.ps[kc // 4], self.psk[kc // 4]
                self.tr(pt[:, (kc % 4) * 128:(kc % 4 + 1) * 128], xn[:, kc * 128:(kc + 1) * 128], [xnk], [ptk])
            for kc in range(8):
                pt, ptk = self.ps[kc // 4], self.psk[kc // 4]
                self.act(hT[:, kc, :], pt[:, (kc % 4) * 128:(kc % 4 + 1) * 128], AF.Identity,
                         [ptk, ("A", 1), "cols"], [hk], scale=self.A1[:, kc, cc:cc + 1], bias=self.B1(kc, cc))

        def p1_s2(j):
            c = cx[j]
            hT, hk = c["hT"], c["hk"]
            c["pb"] = 2 + 3 * (j % 2)
            for cg in range(3):
                pq, pqk = self.ps[c["pb"] + cg], self.psk[c["pb"] + cg]
                for kc in range(8):
                    self.mm(pq[:, :], hT[:, kc, :], wbig[:, kc, cg * 512:(cg + 1) * 512], kc == 0, kc == 7,
                            [hk, ("wbig", cg)], [pqk])

        def p1_s3(j):
            c = cx[j]
            pb = c["pb"]
            c["qk"], c["qkk"] = qkr.next()
            qk, qkk = c["qk"], c["qkk"]
            if c["c"] == 0:
                cs, ck = c["cs"], c["ck"]
                cosb = lambda nh: cs[:, 0:32].rearrange("p (a f) -> p a f", a=2).unsqueeze(1).to_broadcast([128, nh, 2, 16])
                sinb = lambda nh: cs[:, 32:64].rearrange("p (a f) -> p a f", a=2).unsqueeze(1).to_broadcast([128, nh, 2, 16])
                for cg in range(3):
                    nh = 8 if cg < 2 else 4
                    pq, pqk = self.ps[pb + cg], self.psk[pb + cg]
                    pv = pq[:, 0:nh * 64].rearrange("p (h a b f) -> p h a b f", h=nh, a=2, b=2, f=16)
                    ov = qk[:, cg * 512:cg * 512 + nh * 64].rearrange("p (h a b f) -> p h a b f", h=nh, a=2, b=2, f=16)
                    x1, x2 = pv[:, :, :, 0, :], pv[:, :, :, 1, :]
                    tm, tmk = tmpr.next()
                    t = [tm[:, i, 0:nh * 32].rearrange("p (h a f) -> p h a f", h=nh, a=2, f=16) for i in range(4)]
                    self.tt("dve", t[0], x1, cosb(nh), ALU.mult, [pqk, ck], [tmk])
                    self.tt("dve", t[1], x2, sinb(nh), ALU.mult, [pqk, ck], [tmk])
                    self.tt("dve", t[2], x2, cosb(nh), ALU.mult, [pqk, ck], [tmk])
                    self.tt("dve", t[3], x1, sinb(nh), ALU.mult, [pqk, ck], [tmk])
                    self.tt("pool", ov[:, :, :, 0, :], t[0], t[1], ALU.subtract, [tmk], [qkk])
                    self.tt("pool", ov[:, :, :, 1, :], t[2], t[3], ALU.add, [tmk], [qkk])
            else:
                for cg in range(3):
                    ncol = 512 if cg < 2 else 256
                    self.cp("act", qk[:, cg * 512:cg * 512 + ncol], self.ps[pb + cg][:, 0:ncol], [self.psk[pb + cg]], [qkk])
            self.cp("act", V[:, j, :, 0:64], self.ps[pb + 2][:, 256:512].rearrange("p (h d) -> p h d", h=4),
                    [self.psk[pb + 2]], [("V", j)])
            self.ld(self.qs[j * 128:(j + 1) * 128, :], qk[:, 0:1024], [qkk], [("qs", j)])

        def p1_s4(j):
            c = cx[j]
            qk, qkk = c["qk"], c["qkk"]
            for pr in range(2):
                self.tr(self.ps[1][:, pr * 128:(pr + 1) * 128], qk[:, 1024 + pr * 128:1024 + (pr + 1) * 128], [qkk], [self.psk[1]])
            self.cp("act", KT[:, :, j * 128:(j + 1) * 128], self.ps[1][:, 0:256].rearrange("p (a t) -> p a t", a=2),
                    [self.psk[1]], [("KT", j)])

        stages = [p1_s0, p1_s1, p1_s2, p1_s3, p1_s4]
        for t_ in range(NT + len(stages) - 1):
            for st_ in range(len(stages) - 1, -1, -1):
                i_ = t_ - st_
                if 0 <= i_ < NT:
                    stages[st_](i_)

        sc1.close()
        sca_outer, sca = sca, Scope(self)
        wo = sca.sb("wo", [128, 8, 1024], F32R)
        self.ld(wo[:], W["w_o"].rearrange("(k p) n -> p k n", p=128), [], ["wo"], q="pool")
        wok = ["wo"]
        QTr = Ring(sca, "QT", [128, 2, 4, 128], F32R, 2)
        PTr = Ring(sca, "PT", [128, 5, 512], F32R, 2)
        osb = sca.sb("osb", [128, 16, 64])
        oT = sca.sb("oT", [128, 8, 128], F32R)
        den = sca.sb("den", [128, 16])
        xmr = Ring(sca, "xm", [128, D], F32, 3)
        h2T = sca.sb("h2T", [128, 8, 128])
        lg = sca.sb("lg", [128, NE]); mx = sca.sb("mx", [128, 1]); sm = sca.sb("sm", [128, 1])
        pend = [None]
        for i in range(NT):
            c = 0 if i < NT_LAT else 1
            qt, qtk = xr.next()
            self.ld(qt[:], self.qs[i * 128:(i + 1) * 128, :], [("qs", i)], [qtk])
            xt, xk = xnr.next()
            self.ld(xt[:], src_rows(i), [], [xk])
            QT, QTk = QTr.next()
            for pr in range(2):
                for g in range(4):
                    self.tr(self.ps[pr][:, g * 128:(g + 1) * 128], qt[:, (pr * 4 + g) * 128:(pr * 4 + g + 1) * 128], [qtk], [self.psk[pr]])
                self.cp("act", QT[:, pr, :, :], self.ps[pr][:, :].rearrange("p (g t) -> p g t", g=4), [self.psk[pr]], [QTk])
            if c == 0:
                kbs = ([i - 1] if i > 0 else []) + [i] + ([i + 1] if i < NT_LAT - 1 else []) + [32, 33]
                kmask = ([C_MP] if i > 0 else []) + [None] + ([C_MN] if i < NT_LAT - 1 else []) + [None, None]
            else:
                kbs = [32, 33]
                kmask = [None, None]
            if pend[0] is not None:
                self.moe_prep_a(*pend[0])

            def st_phase(kvh):
                pr, base = kvh // 2, (kvh % 2) * 64
                PT, PTk = PTr.next()
                for kbi, kb in enumerate(kbs):
                    pS, pSk = self.ps[2 + kbi % 2], self.psk[2 + kbi % 2]
                    self.mm(pS[:, :], KT[base:base + 64, pr, kb * 128:(kb + 1) * 128],
                            QT[base:base + 64, pr, :, :], True, True, [("KT", kb), QTk], [pSk])
                    self.act(PT[:, kbi, :], pS[:, :], AF.Exp, [pSk], [PTk], scale=0.125)
                    if kmask[kbi] is not None:
                        mo = kmask[kbi]
                        self.tt("pool", PT[:, kbi, :], PT[:, kbi, :], cst[:, mo:mo + 512], ALU.mult, [PTk, "cst"], [PTk])
                return PT, PTk

            def pv_phase(kvh, PT, PTk):
                pO, pOk = self.ps[4 + kvh], self.psk[4 + kvh]
                for g in range(4):
                    for kbi, kb in enumerate(kbs):
                        self.mm(pO[:, g * 128:g * 128 + 66], PT[:, kbi, g * 128:(g + 1) * 128], V[:, kb, kvh, :],
                                kbi == 0, kbi == len(kbs) - 1, [PTk, ("V", kb), ("V1",), ("V0",)], [pOk])

            pts = {0: st_phase(0)}
            for kvh in range(4):
                if kvh + 1 < 4:
                    pts[kvh + 1] = st_phase(kvh + 1)
                if kvh == 1 and pend[0] is not None:
                    self.moe_prep_b(*pend[0])
                    pend[0] = None
                pv_phase(kvh, *pts[kvh])
            for kvh in range(4):
                pOv = self.ps[4 + kvh][:, :].rearrange("p (g t) -> p g t", g=4)
                self.tt("dve", den[:, kvh * 4:(kvh + 1) * 4], pOv[:, :, 64], esink[:, kvh * 4:(kvh + 1) * 4], ALU.add,
                        [self.psk[4 + kvh], "esink"], ["den"])
            self.S.op("dve", lambda e: e.reciprocal(out=den[:], in_=den[:]), ["den"], ["den"])
            for kvh in range(4):
                pOv = self.ps[4 + kvh][:, :].rearrange("p (g t) -> p g t", g=4)
                self.tt("dve", osb[:, kvh * 4:(kvh + 1) * 4, :], pOv[:, :, 0:64],
                        den[:, kvh * 4:(kvh + 1) * 4].unsqueeze(2).to_broadcast([128, 4, 64]), ALU.mult,
                        [self.psk[4 + kvh], "den"], ["osb"])
            osf = osb[:, :, :].rearrange("p h d -> p (h d)")
            for kc in range(8):
                self.tr(self.ps[kc // 4][:, (kc % 4) * 128:(kc % 4 + 1) * 128], osf[:, kc * 128:(kc + 1) * 128], ["osb"], [self.psk[kc // 4]])
            for b in range(2):
                self.cp("act", oT[:, b * 4:(b + 1) * 4, :], self.ps[b][:, :].rearrange("p (k t) -> p k t", k=4), [self.psk[b]], ["oT"])
            xm, xmk = xmr.next()
            for dh in range(2):
                pM, pMk = self.ps[2 + dh], self.psk[2 + dh]
                for kc in range(8):
                    self.mm(pM[:, :], oT[:, kc, :], wo[:, kc, dh * 512:(dh + 1) * 512], kc == 0, kc == 7, ["oT"] + wok, [pMk])
                self.tt("dve", xm[:, dh * 512:(dh + 1) * 512], pM[:, :], self.Gbc[:, 0, c, dh * 512:(dh + 1) * 512], ALU.mult,
                        [pMk, ("Gbc", 0, c)], [xmk])
            self.tt("dve", xm[:], xm[:], xt[:], ALU.add, [xmk, xk], [xmk])
            self.ld(self.xres[0][i * 128:(i + 1) * 128, :], xm[:], [xmk], [("xres0", i)])
            pend[0] = (i, c, xm, xmk, junk, ssr, wr, h2T, lg, mx, sm)
        self.moe_prep(*pend[0])
        sca.close()
        sca_outer.close()

    def moe_prep_a(self, i, c, xm, xmk, junk, ssr, wr, h2T, lg, mx, sm):
        ss, ssk = ssr.next()
        self.rstd_of(xm[:], xmk, junk[:], "junk", ss[:], ssk)
        self.ts("dve", junk[:], xm[:], ss[:, 0:1], None, ALU.mult, None, [xmk, ssk], ["junk"])
        self.ld(self.xn2[i * 128:(i + 1) * 128, :], junk[:], ["junk"], [("xn2", i)])
        for kc in range(8):
            self.tr(self.ps[kc // 4][:, (kc % 4) * 128:(kc % 4 + 1) * 128], junk[:, kc * 128:(kc + 1) * 128], ["junk"], [self.psk[kc // 4]])
        for kc in range(8):
            self.act(h2T[:, kc, :], self.ps[kc // 4][:, (kc % 4) * 128:(kc % 4 + 1) * 128], AF.Identity,
                     [self.psk[kc // 4], ("A", 4), "cols"], ["h2T"], scale=self.A2[:, kc, c:c + 1], bias=self.B2(kc, c))

    def moe_prep_b(self, i, c, xm, xmk, junk, ssr, wr, h2T, lg, mx, sm):
        pL, pLk = self.ps[1], self.psk[1]
        for kc in range(8):
            self.mm(pL[:, 0:NE], h2T[:, kc, :], wr[:, kc, :], kc == 0, kc == 7, ["h2T", "wr"], [pLk])
        self.S.op("dve", lambda e: e.reduce_max(out=mx[:], in_=pL[:, 0:NE], axis=AX.X), [pLk], ["mx"])
        self.ts("dve", mx[:], mx[:], -1.0, None, ALU.mult, None, ["mx"], ["mx"])
        self.act(lg[:], pL[:, 0:NE], AF.Exp, [pLk, "mx"], ["lg", "sm"], bias=mx[:, 0:1], accum_out=sm[:])
        self.S.op("dve", lambda e: e.reciprocal(out=sm[:], in_=sm[:]), ["sm"], ["sm"])
        self.ts("dve", self.aff[:, i, :], lg[:], sm[:, 0:1], None, ALU.mult, None, ["lg", "sm"], [("aff", i)])

    def moe_prep(self, *args):
        self.moe_prep_a(*args)
        self.moe_prep_b(*args)

    def pk(self, b, h):
        return ("ps", b)

    def deltanet_layer(self):
        nc, S, W = self.nc, self.S, self.W[1]
        cst, c2 = self.cst, self.cst2
        xin = self.xres[0]
        qT_d = self.dscratch("qT_d", [D, NTOK]); kT_d = self.dscratch("kT_d", [D, NTOK])
        ktok_d = self.dscratch("ktok_d", [NTOK, D]); vtok_d = self.dscratch("vtok_d", [NTOK, D])
        sz_d = self.dscratch("sz_d", [NTOK, D]); of_d = self.dscratch("of_d", [NLAT, D])
        self.dn_dbg = dict(qT_d=qT_d, kT_d=kT_d, ktok_d=ktok_d, vtok_d=vtok_d, sz_d=sz_d, of_d=of_d)
        ident = cst[:, C_ID:C_ID + 128]
        ones = cst[:, C_ONES:C_ONES + 128]
        zcol = cst[:, C_U:C_U + 1]
        scL = Scope(self)
        g_all = scL.sb("g_all", [128, NT, 16]); beta_all = scL.sb("beta_all", [128, NT, 16])
        lnb_all = scL.sb("lnb_all", [128, NT, 16]); ab_all = scL.sb("ab_all", [128, NT, 32])
        onesR = scL.sb("onesR", [128, 128], F32R)
        self.cp("dve", onesR[:], ones, ["cst"], ["onesR"])
        convT = scL.sb("convT", [128, 24, 3])
        self.ld(convT[:], W["convT"], [], ["convT"])
        allps = [self.pk(b, h) for b in range(8) for h in range(2)]

        def bankk(b):
            return [self.pk(b, 0), self.pk(b, 1)]

        scp = Scope(self)
        win = scp.sb("winqkv", [128, 8, 3072], F32R)
        wink = [("win", i) for i in range(6)]
        for i in range(6):
            self.ld(win[:, :, i * 512:(i + 1) * 512], W["w_in"][:, i * 512:(i + 1) * 512].rearrange("(k p) n -> p k n", p=128),
                    [], [wink[i]], q="pool")
        wnd = [scp.sb("wnd%d" % i, [128, 8, 258], F32R) for i in range(2)]
        xr = Ring(scp, "pxt", [128, D], F32, 2)
        ssr = Ring(scp, "pss", [128, 1], F32, 4)
        junk = scp.sb("pjunk", [128, D])
        c1r = Ring(scp, "pc1", [128, 256], F32, 3)
        sr = Ring(scp, "psl", [128, 256], F32, 8)
        sqr = Ring(scp, "psq", [128, 256], F32R, 3)
        rnr = Ring(scp, "prn", [128, 256], F32, 4)
        qnr = Ring(scp, "pqn", [128, 256], F32, 4)
        tkr = Ring(scp, "ptk", [128, 128], F32, 6)
        groups = [[32, 33]] + [[2 * g, 2 * g + 1] for g in range(16)]

        def tile_hT(jj, dst_fn, xr_, ssr_, junk_):
            c = 0 if jj < NT_LAT else 1
            xt, xk = xr_.next()
            self.ld(xt[:], xin[jj * 128:(jj + 1) * 128, :], [], [xk])
            ss, ssk = ssr_.next()
            self.rstd_of(xt[:], xk, junk_[:], "pjunk", ss[:], ssk)
            self.ts("dve", xt[:], xt[:], ss[:, 0:1], None, ALU.mult, None, [xk, ssk], [xk])
            for kc in range(8):
                b = kc // 4
                self.tr(self.ps[b][:, (kc % 4) * 128:(kc % 4 + 1) * 128], xt[:, kc * 128:(kc + 1) * 128], [xk], bankk(b))
            for kc in range(8):
                b = kc // 4
                dst, dk = dst_fn(kc)
                self.act(dst, self.ps[b][:, (kc % 4) * 128:(kc % 4 + 1) * 128], AF.Identity,
                         bankk(b) + [("A", 1), "cols"], [dk], scale=self.A1[:, kc, c:c + 1], bias=self.B1(kc, c))

        def prep_window(gi):
            buf = gi % 2
            for ti, jj in enumerate(groups[gi]):
                tile_hT(jj, lambda kc: (wnd[buf][:, kc, 1 + 128 * ti:1 + 128 * (ti + 1)], ("wnd", buf)), xr, ssr, junk)
            same_prev = gi >= 2
            if same_prev:
                self.cp("dve", wnd[buf][:, :, 0:1], wnd[1 - buf][:, :, 256:257], [("wnd", 1 - buf)], [("wnd", buf)])
                self.cp("dve", wnd[1 - buf][:, :, 257:258], wnd[buf][:, :, 1:2], [("wnd", buf)], [("wnd", 1 - buf)])
            else:
                self.cp("dve", wnd[buf][:, :, 0:1], zcol.unsqueeze(1).to_broadcast([128, 8, 1]), ["cst"], [("wnd", buf)])
                if gi >= 1:
                    self.cp("dve", wnd[1 - buf][:, :, 257:258], zcol.unsqueeze(1).to_broadcast([128, 8, 1]), ["cst"], [("wnd", 1 - buf)])

        pb_i = [0]
        pn_i = [0]

        def skew(n_items, stages):
            k = len(stages)
            for t in range(n_items + k - 1):
                for st_ in range(k - 1, -1, -1):
                    i_ = t - st_
                    if 0 <= i_ < n_items:
                        stages[st_](i_)

        def project(gi):
            buf = gi % 2
            wv, wvk = wnd[buf], ("wnd", buf)
            tok0 = groups[gi][0] * 128
            ctxs = [dict() for _ in range(24)]

            def s0(ch):
                c = ctxs[ch]
                b = pb_i[0] % 3
                pb_i[0] += 1
                c["pb"] = 2 + b
                pP = self.ps[2 + b]
                for kc in range(8):
                    self.mm(pP[:, 0:258], win[:, kc, ch * 128:(ch + 1) * 128], wv[:, kc, 0:258], kc == 0, kc == 7,
                            [wink[ch // 4], wvk], bankk(2 + b))

            def s1(ch):
                c = ctxs[ch]
                pb = c["pb"]
                pP = self.ps[pb]
                c1, c1k = c1r.next()
                self.ts("dve", c1[:], pP[:, 0:256], convT[:, ch, 0:1], None, ALU.mult, None, bankk(pb) + ["convT"], [c1k])
                self.stt("dve", c1[:], pP[:, 1:257], convT[:, ch, 1:2], c1[:], ALU.mult, ALU.add, bankk(pb) + ["convT", c1k], [c1k])
                self.stt("dve", c1[:], pP[:, 2:258], convT[:, ch, 2:3], c1[:], ALU.mult, ALU.add, bankk(pb) + ["convT", c1k], [c1k])
                c["sl"], c["slk"] = sr.next()
                self.act(c["sl"][:], c1[:], AF.Silu, [c1k], [c["slk"]])

            def s2(ch):
                c = ctxs[ch]
                if ch // 8 < 2:
                    sq, sqk = sqr.next()
                    self.act(sq[:], c["sl"][:], AF.Square, [c["slk"]], [sqk])
                    nb = 5 if (pn_i[0] % 2 == 0) else 7
                    pn_i[0] += 1
                    c["nb"] = nb
                    self.mm(self.ps[nb][:, 0:256], onesR[:], sq[:], True, True, ["onesR", sqk], bankk(nb))

            def s3(ch):
                c = ctxs[ch]
                if ch // 8 < 2:
                    c["rn"], c["rnk"] = rnr.next()
                    rn, rnk = c["rn"], c["rnk"]
                    self.ts("dve", rn[:], self.ps[c["nb"]][:, 0:256], EPS, None, ALU.add, None, bankk(c["nb"]), [rnk])
                    self.act(rn[:], rn[:], AF.Sqrt, [rnk], [rnk])

            def s4(ch):
                c = ctxs[ch]
                kind, h = ch // 8, ch % 8
                if kind < 2:
                    rn, rnk = c["rn"], c["rnk"]
                    S.op("dve", lambda e: e.reciprocal(out=rn[:], in_=rn[:]), [rnk], [rnk])
                    qn, qnk = qnr.next()
                    self.stt("dve", qn[:], c["sl"][:], (128.0 ** -0.5) if kind == 0 else 1.0, rn[:], ALU.mult, ALU.mult, [c["slk"], rnk], [qnk])
                    dst = qT_d if kind == 0 else kT_d
                    self.ld(dst[h * 128:(h + 1) * 128, tok0:tok0 + 256], qn[:], [qnk], [("qkT_d", kind, gi, h)])
                    c["src"], c["srck"] = qn, qnk
                else:
                    c["src"], c["srck"] = c["sl"], c["slk"]

            def s5(ch):
                c = ctxs[ch]
                if ch // 8 >= 1:
                    for ti in range(2):
                        self.tr(self.ps[6][:, ti * 128:(ti + 1) * 128], c["src"][:, ti * 128:(ti + 1) * 128], [c["srck"]], bankk(6))

            def s6(ch):
                c = ctxs[ch]
                kind, h = ch // 8, ch % 8
                if kind >= 1:
                    dstd = ktok_d if kind == 1 else vtok_d
                    for ti in range(2):
                        tk, tkk = tkr.next()
                        self.cp("act", tk[:], self.ps[6][:, ti * 128:(ti + 1) * 128], bankk(6), [tkk])
                        self.ld(dstd[tok0 + ti * 128:tok0 + (ti + 1) * 128, h * 128:(h + 1) * 128], tk[:], [tkk], [("tok_d", kind, gi, h, ti)])

            skew(24, [s0, s1, s2, s3, s4, s5, s6])

        for gi in range(len(groups)):
            prep_window(gi)
            if gi >= 1:
                project(gi - 1)
        lastb = (len(groups) - 1) % 2
        self.cp("dve", wnd[lastb][:, :, 257:258], zcol.unsqueeze(1).to_broadcast([128, 8, 1]), ["cst"], [("wnd", lastb)])
        project(len(groups) - 1)
        scp.close()

        scz = Scope(self)
        wz = scz.sb("winz", [128, 8, 1056], F32R)
        self.ld(wz[:, :, 0:528], W["w_in"][:, 3072:3600].rearrange("(k p) n -> p k n", p=128), [], [("wz", 0)], q="pool")
        self.ld(wz[:, :, 528:1056], W["w_in"][:, 3600:4128].rearrange("(k p) n -> p k n", p=128), [], [("wz", 1)], q="pool")
        wzk = [("wz", 0), ("wz", 1)]
        xr = Ring(scz, "zxt", [128, D], F32, 2)
        ssr = Ring(scz, "zss", [128, 1], F32, 4)
        junk = scz.sb("zjunk", [128, D])
        hTr = Ring(scz, "zhT", [128, 8, 128], F32R, 2)
        zr = Ring(scz, "zst", [128, D], F32, 2)
        for jj in range(NT):
            hT, hk = hTr.next()
            tile_hT(jj, lambda kc: (hT[:, kc, :], hk), xr, ssr, junk)
            for zh in range(2):
                for kc in range(8):
                    self.mm(self.ps[2 + zh][:, :], hT[:, kc, :], wz[:, kc, zh * 512:(zh + 1) * 512], kc == 0, kc == 7, [hk] + wzk, bankk(2 + zh))
            for kc in range(8):
                self.mm(self.ps[4][:, 0:32], hT[:, kc, :], wz[:, kc, 1024:1056], kc == 0, kc == 7, [hk] + wzk, bankk(4))
            zt, ztk = zr.next()
            for zh in range(2):
                self.act(zt[:, zh * 512:(zh + 1) * 512], self.ps[2 + zh][:, :], AF.Silu, bankk(2 + zh), [ztk])
            self.ld(sz_d[jj * 128:(jj + 1) * 128, :], zt[:], [ztk], [("sz_d", jj)])
            self.cp("dve", ab_all[:, jj, :], self.ps[4][:, 0:32], bankk(4), [("ab", jj)])
        abk = [("ab", jj) for jj in range(NT)]
        dtb = scz.sb("dtb", [128, 16]); nea = scz.sb("nea", [128, 16])
        self.ld(dtb[:], W["dt_bias"].partition_broadcast(128), [], ["dtb"])
        self.ld(nea[:], W["a_log"].partition_broadcast(128), [], ["nea"])
        self.act(nea[:], nea[:], AF.Exp, ["nea"], ["nea"])
        self.ts("dve", nea[:], nea[:], -1.0, None, ALU.mult, None, ["nea"], ["nea"])
        self.tt("dve", g_all[:], ab_all[:, :, 0:16], dtb[:].unsqueeze(1).to_broadcast([128, NT, 16]), ALU.add, abk + ["dtb"], ["g_all"])
        uu = scz.sb("sp_u", [128, NT, 16]); la = scz.sb("sp_la", [128, NT, 16])
        qq = scz.sb("sp_q", [128, NT, 16]); mk = scz.sb("sp_mk", [128, NT, 16])
        self.act(uu[:], g_all[:], AF.Exp, ["g_all"], ["sp_u"])
        self.act(la[:], uu[:], AF.Ln, ["sp_u"], ["sp_la"], bias=1.0)
        self.ts("dve", qq[:], uu[:], 1.0 / 7, None, ALU.mult, None, ["sp_u"], ["sp_q"])
        for cc_ in (-1.0 / 6, 1.0 / 5, -1.0 / 4, 1.0 / 3, -1.0 / 2, 1.0):
            self.stt("dve", qq[:], qq[:], cc_, uu[:], ALU.add, ALU.mult, ["sp_q", "sp_u"], ["sp_q"])
        self.ts("dve", mk[:], uu[:], 0.25, None, ALU.is_lt, None, ["sp_u"], ["sp_mk"])
        self.tt("dve", qq[:], qq[:], la[:], ALU.subtract, ["sp_q", "sp_la"], ["sp_q"])
        self.tt("dve", qq[:], qq[:], mk[:], ALU.mult, ["sp_q", "sp_mk"], ["sp_q"])
        self.tt("dve", g_all[:], la[:], qq[:], ALU.add, ["sp_la", "sp_q"], ["g_all"])
        self.tt("dve", g_all[:], g_all[:], nea[:].unsqueeze(1).to_broadcast([128, NT, 16]), ALU.mult, ["g_all", "nea"], ["g_all"])
        self.act(beta_all[:], ab_all[:, :, 16:32], AF.Sigmoid, abk, ["beta_all"])
        self.act(lnb_all[:], beta_all[:], AF.Ln, ["beta_all"], ["lnb_all"])
        scz.close()
        if self.stage >= 31:
            self.dn_scan(0, dict(qT_d=qT_d, kT_d=kT_d, ktok_d=ktok_d, vtok_d=vtok_d, sz_d=sz_d, of_d=of_d), g_all, beta_all, lnb_all)
        if self.stage >= 32:
            self.dn_scan(1, dict(qT_d=qT_d, kT_d=kT_d, ktok_d=ktok_d, vtok_d=vtok_d, sz_d=sz_d, of_d=of_d), g_all, beta_all, lnb_all)
        if self.debug:
            d_gates = self.nc.dram_tensor("d_gates", [128, 3, NT * 16], F32, kind="ExternalOutput").ap()
            for i, (t, k) in enumerate(((g_all, "g_all"), (beta_all, "beta_all"), (lnb_all, "lnb_all"))):
                self.ld(d_gates[:, i, :], t[:, :, :].rearrange("p j e -> p (j e)"), [k], [("d_gates", i)])
        scL.close()
        if self.stage >= 33:
            self.dn_out(of_d)

    def dn_scan(self, dr, dd, g_all, beta_all, lnb_all):
        nc, S, W = self.nc, self.S, self.W[1]
        cst, c2 = self.cst, self.cst2
        ident = cst[:, C_ID:C_ID + 128]
        ones = cst[:, C_ONES:C_ONES + 128]
        zcol = cst[:, C_U:C_U + 1]
        Ltri = c2[:, C2_LF:C2_LF + 128] if dr == 0 else c2[:, C2_LB:C2_LB + 128]
        LT, GT = c2[:, C2_LT:C2_LT + 128], c2[:, C2_GT:C2_GT + 128]
        GE, LE = c2[:, C2_GE:C2_GE + 128], c2[:, C2_LE:C2_LE + 128]
        m_db, m_dbt, m_dt = (LT, GT, GE) if dr == 0 else (GT, LT, LE)
        order = ([32, 33] + list(range(32))) if dr == 0 else ([33, 32] + list(range(31, -1, -1)))
        if self.n_tiles is not None:
            order = order[:self.n_tiles]
        sc = Scope(self)
        sfx = "_%d" % dr

        def bankk(b):
            return [self.pk(b, 0), self.pk(b, 1)]

        def zfill(t, k):
            sh = list(t.shape)
            self.cp("dve", t[:], zcol.to_broadcast(sh) if len(sh) == 2 else zcol.unsqueeze(1).to_broadcast(sh), ["cst"], [k])


        def z3(name, w, dt=F32R, fill=True):
            t = sc.sb(name + sfx, [128, 8, w], dt)
            if fill:
                for b_ in range(4):
                    self.cp("dve", t[:, 2 * b_:2 * b_ + 2, :], zcol.unsqueeze(1).to_broadcast([128, 2, w]), ["cst"], [(name, b_)])
            return t

        Sst = z3("S", 256)
        Xb = [z3("X0", 256)]
        qkT = z3("qkT", 128, fill=False)
        vb = z3("vb", 256); kbe = z3("kbe", 128, fill=False); kd = z3("kd", 128, fill=False)
        qeT = z3("qeT", 128, fill=False); usb = z3("usb", 128, F32, fill=False)
        wT = z3("wT", 128, fill=False); vnew = z3("vn", 256); Sdec = z3("Sdec", 128, F32, fill=False)
        Mf = z3("Mf", 128, F32, fill=False); Ao = z3("Ao", 128, F32, fill=False)
        Xp = [z3("Xa", 128, F32, fill=False), z3("Xb2", 128, F32, fill=False)]
        XPN = ["Xa", "Xb2"]
        ETf = z3("ETf", 128, F32, fill=False)
        Td = z3("Td", 128, F32, fill=False); Uf = z3("Uf", 128, F32, fill=False)
        negIf = sc.sb("negIf" + sfx, [128, 128])
        self.ts("dve", negIf[:], ident, -1.0, None, ALU.mult, None, ["cst"], ["negIf"])
        m64b = c2[:, C2_B64:C2_B64 + 128].unsqueeze(1).to_broadcast([128, 8, 128])
        nm64b = c2[:, C2_NB64:C2_NB64 + 128].unsqueeze(1).to_broadcast([128, 8, 128])
        offb = c2[:, C2_OFF:C2_OFF + 128].unsqueeze(1).to_broadcast([128, 8, 128])
        kqr = Ring(sc, "kq" + sfx, [128, 8, 256], F32R, 2)
        ktr = Ring(sc, "kt" + sfx, [128, D], F32, 1)
        vtr = Ring(sc, "vt" + sfx, [128, D], F32, 1)
        otr = Ring(sc, "ot" + sfx, [128, D], F32, 2)
        Dg = sc.sb("Dg" + sfx, [128, 2, 8, 128])
        DB = sc.sb("DB" + sfx, [128, 8, 128]); DBT = sc.sb("DBT" + sfx, [128, 8, 128])
        DT = sc.sb("DT" + sfx, [128, 8, 128]); Ec = sc.sb("Ec" + sfx, [128, 8, 128])
        gsm = sc.sb("gsm" + sfx, [128, 6, 8])
        if dr == 1:
            ofr = Ring(sc, "of" + sfx, [128, D], F32, 1)
            szr = Ring(sc, "sz" + sfx, [128, D], F32, 1)
            junk = sc.sb("bjunk", [128, D])
            ssq = sc.sb("ssq", [128, 8])
            onb = sc.sb("onb", [128, 128])
            self.ld(onb[:], W["o_norm"].partition_broadcast(128), [], ["onb"])
        NIT = 5
        P4 = range(4)

        def A(b_):
            return self.ps[b_][:, :].rearrange("p (s c) -> p s c", s=2), [("ps", b_)]

        def B(b_):
            return self.ps[4 + b_][:, :].rearrange("p (s c) -> p s c", s=2), [("ps", 4 + b_)]

        def keys(name):
            return [(name, b_) for b_ in P4]

        idb8 = ident.unsqueeze(1).to_broadcast([128, 8, 128])
        idb2 = ident.unsqueeze(1).to_broadcast([128, 2, 128])
        for jj in order:
            lat = jj < NT_LAT
            tsl = slice(jj * 128, (jj + 1) * 128)
            kq, kqk = kqr.next()
            self.ld(kq[:, :, 0:128], dd["kT_d"][:, tsl].rearrange("(h p) t -> p h t", p=128), [], [kqk + ("k",)], q="pool")
            kqkeys = [kqk + ("k",)]
            if lat:
                self.ld(kq[:, :, 128:256], dd["qT_d"][:, tsl].rearrange("(h p) t -> p h t", p=128), [], [kqk + ("q",)], q="pool")
                kqkeys.append(kqk + ("q",))
            kt, ktk = ktr.next()
            self.ld(kt[:], dd["ktok_d"][tsl, :], [], [ktk])
            vt, vtk = vtr.next()
            self.ld(vt[:], dd["vtok_d"][tsl, :], [], [vtk])
            kt3 = kt[:, :].rearrange("p (h d) -> p h d", h=8)
            vt3 = vt[:, :].rearrange("p (h d) -> p h d", h=8)
            gj = g_all[:, jj, dr * 8:(dr + 1) * 8]
            bj = beta_all[:, jj, dr * 8:(dr + 1) * 8]
            lbj = lnb_all[:, jj, dr * 8:(dr + 1) * 8]
            gk = ["g_all", "beta_all", "lnb_all"]
            pg = self.ps[0]
            self.mm(pg[:, 0:8], Ltri, gj, True, True, ["cst2"] + gk, bankk(0))
            self.mm(pg[:, 8:16], ones, gj, True, True, ["cst"] + gk, bankk(0))
            gc, gb, ebg, ekd, egl, tmpg = (gsm[:, i, :] for i in range(6))
            self.cp("act", gc, pg[:, 0:8], bankk(0), ["gsm"])
            self.tt("dve", gb, gc, lbj, ALU.add, ["gsm"] + gk, ["gsm"])
            self.act(ebg, gb, AF.Exp, ["gsm"], ["gsm"])
            self.tt("dve", tmpg, pg[:, 8:16], gc, ALU.subtract, bankk(0) + ["gsm"], ["gsm"])
            self.act(ekd, tmpg, AF.Exp, ["gsm"], ["gsm"])
            self.act(egl, pg[:, 8:16], AF.Exp, bankk(0), ["gsm"])
            self.tt("pool", Dg[:, 0, :, :], idb8, gc.unsqueeze(2).to_broadcast([128, 8, 128]), ALU.mult, ["cst", "gsm"], ["Dg0"])
            self.tt("pool", Dg[:, 1, :, :], idb8, gb.unsqueeze(2).to_broadcast([128, 8, 128]), ALU.mult, ["cst", "gsm"], ["Dg1"])
            for half in range(2):
                self.mm(self.ps[4 + half][:, :], ones, Dg[:, 0, half * 4:(half + 1) * 4, :], True, True, ["cst", "Dg0"], bankk(4 + half))
                self.mm(self.ps[6 + half][:, :], ones, Dg[:, 1, half * 4:(half + 1) * 4, :], True, True, ["cst", "Dg1"], bankk(6 + half))
            for half in range(2):
                hs = slice(half * 4, half * 4 + 4)
                pRc = self.ps[4 + half][:, :].rearrange("p (h f) -> p h f", h=4)
                pRb = self.ps[6 + half][:, :].rearrange("p (h f) -> p h f", h=4)
                gbb = gb[:, hs].unsqueeze(2).to_broadcast([128, 4, 128])
                gcb = gc[:, hs].unsqueeze(2).to_broadcast([128, 4, 128])
                self.tt("dve", DB[:, hs, :], gbb, pRc, ALU.subtract, ["gsm"] + bankk(4 + half), ["DB"])
                self.tt("pool", DB[:, hs, :], DB[:, hs, :], m_db.unsqueeze(1).to_broadcast([128, 4, 128]), ALU.add, ["DB", "cst2"], ["DB"])
                self.tt("dve", DBT[:, hs, :], pRb, gcb, ALU.subtract, ["gsm"] + bankk(6 + half), ["DBT"])
                self.tt("pool", DBT[:, hs, :], DBT[:, hs, :], m_dbt.unsqueeze(1).to_broadcast([128, 4, 128]), ALU.add, ["DBT", "cst2"], ["DBT"])
                if lat:
                    self.tt("dve", DT[:, hs, :], pRc, gcb, ALU.subtract, ["gsm"] + bankk(4 + half), ["DT"])
                    self.tt("pool", DT[:, hs, :], DT[:, hs, :], m_dt.unsqueeze(1).to_broadcast([128, 4, 128]), ALU.add, ["DT", "cst2"], ["DT"])
                    self.act(Ec[:, hs, :], pRc, AF.Exp, bankk(4 + half), ["Ec"])
            self.act(DB[:], DB[:], AF.Exp, ["DB"], ["DB"])
            self.act(DBT[:], DBT[:], AF.Exp, ["DBT"], ["DBT"])
            if lat:
                self.act(DT[:], DT[:], AF.Exp, ["DT"], ["DT"])
            ncol = 256 if lat else 128
            for b_ in P4:
                ap_, apk = A(b_)
                for s_ in range(2):
                    h = 2 * b_ + s_
                    self.mm(ap_[:, s_, 0:ncol], kq[:, h, 0:128], kq[:, h, 0:ncol], True, True, kqkeys, apk)
            for b_ in P4:
                ap_, apk = A(b_)
                hp = slice(2 * b_, 2 * b_ + 2)
                self.tt("dve", Mf[:, hp, :], ap_[:, :, 0:128], DB[:, hp, :], ALU.mult, apk + ["DB"], [("Mf", b_)])
                self.tt("dve", Xp[0][:, hp, :], ap_[:, :, 0:128], DBT[:, hp, :], ALU.mult, apk + ["DBT"], [("Xa", b_)])
                if lat:
                    self.tt("dve", qkT[:, hp, :], ap_[:, :, 128:256], DT[:, hp, :], ALU.mult, apk + ["DT"], [("qkT", b_)])
            self.tt("pool", Ao[:, :, :], Mf[:, :, :], offb, ALU.mult, keys("Mf") + ["cst2"], keys("Ao"))
            self.tt("pool", Mf[:, :, :], Mf[:, :, :], m64b, ALU.mult, keys("Mf") + keys("Ao") + ["cst2"], keys("Mf"))
            self.tt("pool", Mf[:, :, :], Mf[:, :, :], idb8, ALU.add, keys("Mf") + ["cst"], keys("Mf"))
            self.tt("pool", Xp[0][:, :, :], Xp[0][:, :, :], nm64b, ALU.mult, keys("Xa") + ["cst2"], keys("Xa"))
            self.tt("pool", Xp[0][:, :, :], Xp[0][:, :, :], idb8, ALU.add, keys("Xa") + ["cst"], keys("Xa"))
            cur = 0
            for it in range(NIT):
                src, srck = Xp[cur], XPN[cur]
                dst, dstk = Xp[1 - cur], XPN[1 - cur]
                for b_ in P4:
                    ap_, apk = A(b_)
                    for s_ in range(2):
                        h = 2 * b_ + s_
                        self.mm(ap_[:, s_, 0:128], src[:, h, :], Mf[:, h, :], True, True, [(srck, b_), ("Mf", b_)], apk)
                for b_ in P4:
                    ap_, apk = A(b_)
                    self.stt("dve", ETf[:, 2 * b_:2 * b_ + 2, :], ap_[:, :, 0:128], -1.0, idb2, ALU.mult, ALU.add, apk + ["cst"], [("ETf", b_)])
                for b_ in P4:
                    bp_, bpk = B(b_)
                    for s_ in range(2):
                        h = 2 * b_ + s_
                        self.mm(bp_[:, s_, 0:128], ETf[:, h, :], src[:, h, :], True, True, [("ETf", b_), (srck, b_)], bpk)
                for b_ in P4:
                    bp_, bpk = B(b_)
                    hp = slice(2 * b_, 2 * b_ + 2)
                    self.tt("dve", dst[:, hp, :], src[:, hp, :], bp_[:, :, 0:128], ALU.add, [(srck, b_)] + bpk, [(dstk, b_)])
                cur = 1 - cur
            Xd, Xdk = Xp[cur], XPN[cur]
            for b_ in P4:
                ap_, apk = A(b_)
                bp_, bpk = B(b_)
                for s_ in range(2):
                    h = 2 * b_ + s_
                    self.tr(ap_[:, s_, 0:128], Xd[:, h, :], [(Xdk, b_)], apk)
                    self.mm(bp_[:, s_, 0:128], Ao[:, h, :], Xd[:, h, :], True, True, [("Ao", b_), (Xdk, b_)], bpk)
            for b_ in P4:
                ap_, apk = A(b_)
                bp_, bpk = B(b_)
                hp = slice(2 * b_, 2 * b_ + 2)
                self.cp("act", Td[:, hp, :], ap_[:, :, 0:128], apk, [("Td", b_)])
                self.cp("act", Uf[:, hp, :], bp_[:, :, 0:128], bpk, [("Uf", b_)])
            for b_ in P4:
                ap_, apk = A(b_)
                for s_ in range(2):
                    h = 2 * b_ + s_
                    self.mm(ap_[:, s_, 0:128], Td[:, h, :], Uf[:, h, :], True, True, [("Td", b_), ("Uf", b_)], apk)
            for b_ in P4:
                ap_, apk = A(b_)
                hp = slice(2 * b_, 2 * b_ + 2)
                self.tt("dve", Xb[0][:, hp, 0:128], Xd[:, hp, :], ap_[:, :, 0:128], ALU.subtract, [(Xdk, b_)] + apk, [("X0", b_)])
            cur = 0
            XN = ["X0"]
            Xf, kf_ = Xb[cur], XN[cur]
            self.tt("dve", vb[:, :, 0:128], vt3, bj.unsqueeze(2).to_broadcast([128, 8, 128]), ALU.mult, [vtk] + gk, keys("vb"))
            self.tt("pool", kbe[:, :, :], kt3, ebg.unsqueeze(2).to_broadcast([128, 8, 128]), ALU.mult, [ktk, "gsm"], keys("kbe"))
            self.tt("pool", kd[:, :, :], kt3, ekd.unsqueeze(2).to_broadcast([128, 8, 128]), ALU.mult, [ktk, "gsm"], keys("kd"))
            if lat:
                self.tt("pool", qeT[:, :, :], kq[:, :, 128:256], Ec[:, :, :], ALU.mult, kqkeys + ["Ec"], keys("qeT"))
            for b_ in P4:
                ap_, apk = A(b_)
                bp_, bpk = B(b_)
                for s_ in range(2):
                    h = 2 * b_ + s_
                    self.mm(ap_[:, s_, :], Xf[:, h, 0:128], vb[:, h, :], True, True, [(kf_, b_), ("vb", b_)], apk)
                    self.mm(bp_[:, s_, :], kbe[:, h, :], Xf[:, h, :], True, True, [(kf_, b_), ("kbe", b_)], bpk)
            for b_ in P4:
                ap_, apk = A(b_)
                bp_, bpk = B(b_)
                hp = slice(2 * b_, 2 * b_ + 2)
                self.cp("act", usb[:, hp, :], ap_[:, :, 0:128], apk, [("usb", b_)])
                self.cp("act", wT[:, hp, :], bp_[:, :, 0:128], bpk, [("wT", b_)])
            ot, otk = otr.next()
            ot3 = ot[:, :].rearrange("p (h d) -> p h d", h=8)
            for b_ in P4:
                ap_, apk = A(b_)
                for s_ in range(2):
                    h = 2 * b_ + s_
                    self.mm(ap_[:, s_, :], wT[:, h, :], Sst[:, h, :], True, True, [("wT", b_), ("S", b_)], apk)
            for b_ in P4:
                ap_, apk = A(b_)
                hp = slice(2 * b_, 2 * b_ + 2)
                self.tt("dve", vnew[:, hp, 0:128], usb[:, hp, :], ap_[:, :, 0:128], ALU.subtract, [("usb", b_)] + apk, [("vn", b_)])
            for b_ in P4:
                ap_, apk = A(b_)
                bp_, bpk = B(b_)
                for s_ in range(2):
                    h = 2 * b_ + s_
                    if lat:
                        self.mm(bp_[:, s_, :], qeT[:, h, :], Sst[:, h, :], True, False, [("qeT", b_), ("S", b_)], bpk)
                        self.mm(bp_[:, s_, :], qkT[:, h, :], vnew[:, h, :], False, True, [("qkT", b_), ("vn", b_)], bpk)
                    self.mm(ap_[:, s_, :], kd[:, h, :], vnew[:, h, :], True, True, [("kd", b_), ("vn", b_)], apk)
            self.tt("pool", Sdec[:, :, :], Sst[:, :, 0:128], egl.unsqueeze(2).to_broadcast([128, 8, 128]), ALU.mult,
                    keys("S") + ["gsm"], keys("Sdec"))
            for b_ in P4:
                ap_, apk = A(b_)
                bp_, bpk = B(b_)
                hp = slice(2 * b_, 2 * b_ + 2)
                if lat:
                    self.cp("act", ot3[:, hp, :], bp_[:, :, 0:128], bpk, [otk])
                self.tt("dve", Sst[:, hp, 0:128], Sdec[:, hp, :], ap_[:, :, 0:128], ALU.add, [("Sdec", b_)] + apk, [("S", b_)])
            if not lat:
                continue
            if dr == 0:
                self.ld(dd["of_d"][tsl, :], ot[:], [otk], [("of_d", jj)])
            else:
                if self.debug:
                    if not hasattr(self, "ob_d"):
                        self.ob_d = self.nc.dram_tensor("ob_d", [NLAT, D], F32, kind="ExternalOutput").ap()
                    self.ld(self.ob_d[tsl, :], ot[:], [otk], [("ob_d", jj)])
                of, ofk = ofr.next()
                self.ld(of[:], dd["of_d"][tsl, :], [("of_d", jj)], [ofk])
                szt, szk = szr.next()
                self.ld(szt[:], dd["sz_d"][tsl, :], [], [szk])
                self.tt("pool", ot[:], ot[:], of[:], ALU.add, [otk, ofk], [otk])
                self.act(junk[:], ot[:], AF.Square, [otk], ["bjunk"])
                S.op("dve", lambda e: e.tensor_reduce(out=ssq[:], in_=junk[:, :].rearrange("p (h d) -> p h d", h=8), axis=AX.X, op=ALU.add),
                     ["bjunk"], ["ssq"])
                self.ts("dve", ssq[:], ssq[:], 1.0 / 128, EPS, ALU.mult, ALU.add, ["ssq"], ["ssq"])
                self.act(ssq[:], ssq[:], AF.Sqrt, ["ssq"], ["ssq"])
                S.op("dve", lambda e: e.reciprocal(out=ssq[:], in_=ssq[:]), ["ssq"], ["ssq"])
                o3 = ot[:, :].rearrange("p (h d) -> p h d", h=8)
                self.tt("dve", o3, o3, ssq[:].unsqueeze(2).to_broadcast([128, 8, 128]), ALU.mult, [otk, "ssq"], [otk])
                self.tt("pool", o3, o3, onb[:].unsqueeze(1).to_broadcast([128, 8, 128]), ALU.mult, [otk, "onb"], [otk])
                self.tt("pool", ot[:], ot[:], szt[:], ALU.mult, [otk, szk], [otk])
                self.ld(dd["of_d"][tsl, :], ot[:], [otk, ofk], [("of_d", jj)])
        sc.close()

    def dn_out(self, of_d):
        nc, S, W = self.nc, self.S, self.W[1]
        sc = Scope(self)
        wo = sc.sb("wo1", [128, 8, D], F32R)
        self.ld(wo[:], W["w_o"].rearrange("(k p) n -> p k n", p=128), [], ["wo1"], q="pool")
        wr = sc.sb("wr1", [128, 8, NE])
        self.ld(wr[:], W["router"].rearrange("(k p) n -> p k n", p=128), [], ["wr"])
        yr = Ring(sc, "oy", [128, D], F32, 2)
        xr = Ring(sc, "ox", [128, D], F32, 2)
        xmr = Ring(sc, "oxm", [128, D], F32, 3)
        ssr = Ring(sc, "oss", [128, 1], F32, 4)
        junk = sc.sb("ojunk", [128, D])
        yT = sc.sb("oyT", [128, 8, 128], F32R)
        h2T = sc.sb("oh2T", [128, 8, 128])
        lg = sc.sb("olg", [128, NE]); mx = sc.sb("omx", [128, 1]); sm = sc.sb("osm", [128, 1])
        pend = [None]
        for i in range(NT_LAT):
            yt, ytk = yr.next()
            self.ld(yt[:], of_d[i * 128:(i + 1) * 128, :], [], [ytk])
            xt, xk = xr.next()
            self.ld(xt[:], self.xres[0][i * 128:(i + 1) * 128, :], [], [xk])
            for kc in range(8):
                self.tr(self.ps[kc // 4][:, (kc % 4) * 128:(kc % 4 + 1) * 128], yt[:, kc * 128:(kc + 1) * 128], [ytk], [self.psk[kc // 4]])
            for b in range(2):
                self.cp("act", yT[:, b * 4:(b + 1) * 4, :], self.ps[b][:, :].rearrange("p (k t) -> p k t", k=4), [self.psk[b]], ["yT"])
            if pend[0] is not None:
                self.moe_prep(*pend[0])
                pend[0] = None
            xm, xmk = xmr.next()
            for dh in range(2):
                pM, pMk = self.ps[2 + dh], self.psk[2 + dh]
                for kc in range(8):
                    self.mm(pM[:, :], yT[:, kc, :], wo[:, kc, dh * 512:(dh + 1) * 512], kc == 0, kc == 7, ["yT", "wo1"], [pMk])
                self.tt("dve", xm[:, dh * 512:(dh + 1) * 512], pM[:, :], self.Gbc[:, 0, 0, dh * 512:(dh + 1) * 512], ALU.mult,
                        [pMk, ("Gbc", 0, 0)], [xmk])
            self.tt("dve", xm[:], xm[:], xt[:], ALU.add, [xmk, xk], [xmk])
            self.ld(self.xres[1][i * 128:(i + 1) * 128, :], xm[:], [xmk], [("xres1", i)])
            pend[0] = (i, 0, xm, xmk, junk, ssr, wr, h2T, lg, mx, sm)
        self.moe_prep(*pend[0])
        sc.close()

    def final_norm(self):
        nc, S = self.nc, self.S
        sc = Scope(self)
        fn = sc.sb("fnb", [128, D])
        self.ld(fn[:], self.inp["final_norm"].partition_broadcast(128), [], ["fnb"])
        xr = Ring(sc, "fx", [128, D], F32, 3)
        ssr = Ring(sc, "fss", [128, 1], F32, 4)
        junk = sc.sb("fjunk", [128, D])
        for i in range(NT_LAT):
            xt, xk = xr.next()
            self.ld(xt[:], self.xres[1][i * 128:(i + 1) * 128, :], [], [xk])
            ss, ssk = ssr.next()
            self.rstd_of(xt[:], xk, junk[:], "fjunk", ss[:], ssk)
            self.stt("dve", xt[:], xt[:], ss[:, 0:1], fn[:], ALU.mult, ALU.mult, [xk, ssk, "fnb"], [xk])
            S.dma("sp", lambda e: e.dma_start(out=self.out[i * 128:(i + 1) * 128, :], in_=xt[:]), [xk], [("out", i)], is_output=True)
        sc.close()

    def moe(self, l, xres, xresk, with_ctx):
        nc, S, W = self.nc, self.S, self.W[l]
        cst = self.cst
        ones = cst[:, C_ONES:C_ONES + 128]
        Umat = cst[:, C_U:C_U + 128]
        iota = cst[:, C_IOTA:C_IOTA + 512]
        sets = [(0, 0, NT_LAT, CAP_LAT)] + ([(1, NT_LAT, NT_CTX, CAP_CTX)] if with_ctx else [])
        sc = Scope(self)
        slot_m = {}; meta = {}
        for (si, j0, nj, cap) in sets:
            slot_m[si] = sc.sb("slotm%d_%d" % (si, l), [128, nj, NE])
            meta[si] = sc.sb("meta%d_%d" % (si, l), [128, nj, NE, 4], F32R)
        scr = Scope(self)
        for (si, j0, nj, cap) in sets:
            sfx = "%d_%d" % (si, l)
            affv = self.aff[:, j0:j0 + nj, :]
            affk = [("aff", j) for j in range(j0, j0 + nj)]
            lo = scr.sb("lo" + sfx, [128, NE]); mid = scr.sb("mid" + sfx, [128, NE])
            cmpt = scr.sb("cmp" + sfx, [128, nj, NE]); cnt = scr.sb("cnt" + sfx, [128, NE])
            tq = scr.sb("tq" + sfx, [128, NE])
            offs = scr.sb("offs" + sfx, [128, nj, NE]); slot = scr.sb("slot" + sfx, [128, nj, NE])
            S.op("dve", lambda e: e.memset(lo[:], 0.0), [], ["lo"])
            S.op("dve", lambda e: e.memset(mid[:], 0.5), [], ["mid"])
            pC, pCk = self.ps[0], self.psk[0]
            for it in range(NBIS):
                w = 2.0 ** -(it + 1)
                self.tt("dve", cmpt[:], affv, mid[:].unsqueeze(1).to_broadcast([128, nj, NE]), ALU.is_ge, affk + ["mid"], ["cmp"])
                S.op("dve", lambda e: e.tensor_reduce(out=cnt[:], in_=cmpt[:, :, :].rearrange("p j e -> p e j"), axis=AX.X, op=ALU.add),
                     ["cmp"], ["cnt"])
                self.mm(pC[:, 0:NE], ones, cnt[:], True, True, ["cst", "cnt"], [pCk])
                self.ts("dve", tq[:], pC[:, 0:NE], cap - 0.5, w, ALU.is_ge, ALU.mult, [pCk], ["tq"])
                self.tt("dve", lo[:], lo[:], tq[:], ALU.add, ["lo", "tq"], ["lo"])
                self.ts("dve", mid[:], lo[:], w * 0.5, None, ALU.add, None, ["lo"], ["mid"])
            self.tt("dve", cmpt[:], affv, lo[:].unsqueeze(1).to_broadcast([128, nj, NE]), ALU.is_ge, affk + ["lo"], ["cmp"])
            pP, pPk = self.ps[1], self.psk[1]
            pT, pTk = self.ps[2], self.psk[2]
            mflat = cmpt[:, :, :].rearrange("p j e -> p (j e)")
            self.mm(pP[:, 0:nj * NE], Umat, mflat, True, True, ["cst", "cmp"], [pPk])
            self.mm(pT[:, 0:nj * NE], ones, mflat, True, True, ["cst", "cmp"], [pTk])
            pTv = pT[:, 0:nj * NE].rearrange("p (j e) -> p j e", e=NE)
            pPv = pP[:, 0:nj * NE].rearrange("p (j e) -> p j e", e=NE)
            S.op("dve", lambda e: e.memset(offs[:, 0, :], 0.0), [], ["offs"])
            for j in range(1, nj):
                self.tt("dve", offs[:, j, :], pTv[:, j - 1, :], offs[:, j - 1, :], ALU.add, [pTk, "offs"], ["offs"])
            self.tt("dve", slot[:], pPv, offs[:], ALU.add, [pPk, "offs"], ["slot"])
            self.ts("dve", cmpt[:], cmpt[:], -1.0e6, 1.0e6, ALU.mult, ALU.add, ["cmp"], ["cmp"])
            self.tt("dve", slot_m[si][:], slot[:], cmpt[:], ALU.add, ["slot", "cmp"], [("slotm", si)])
            mt = meta[si]
            for j in range(nj):
                self.cp("dve", mt[:, j, :, 0:1], cst[:, C_U:C_U + 1].unsqueeze(1).to_broadcast([128, NE, 1]), ["cst"], [("meta", si)])
                self.ts("dve", mt[:, j, :, 0:1], mt[:, j, :, 0:1], float(j0 + j), None, ALU.add, None, [("meta", si)], [("meta", si)])
            mtf = mt[:, :, :, :].rearrange("p j e c -> p (j e) c")
            self.cp("dve", mtf[:, :, 1:2], cst[:, C_PIDX:C_PIDX + 1].unsqueeze(1).to_broadcast([128, nj * NE, 1]), ["cst"], [("meta", si)])
            self.cp("dve", mtf[:, :, 2:3], affv.rearrange("p j e -> p (j e)").unsqueeze(2), affk, [("meta", si)])
            self.cp("dve", mtf[:, :, 3:4], cst[:, C_ONES:C_ONES + 1].unsqueeze(1).to_broadcast([128, nj * NE, 1]), ["cst"], [("meta", si)])
        scr.close()

        NW = 8
        wring = Ring(sc, "wm%d" % l, [128, 8, 256], F32R, NW)
        xsT = sc.sb("xsT%d" % l, [128, 8, 640], F32R)
        hidT = sc.sb("hidT%d" % l, [128, 16, 544], F32R)
        ysb = sc.sb("ysb%d" % l, [128, 5, D])
        xsr = Ring(sc, "xstok%d" % l, [128, D], F32, 2)
        selr = Ring(sc, "sel%d" % l, [128, 512], F32R, 2)
        selc = sc.sb("selc%d" % l, [128, 128], F32R)
        sgr = Ring(sc, "sg%d" % l, [128, 512], F32, 2)
        hcr = Ring(sc, "hc%d" % l, [32, 256], F32, 2)
        idxrow = sc.sb("idxrow%d" % l, [4, 640])
        metac = sc.sb("metac%d" % l, [128, 5, 4])
        tmp5 = sc.sb("tmp5%d" % l, [128, 5]); idxf = sc.sb("idxf%d" % l, [128, 5])
        idur = Ring(sc, "idu%d" % l, [128, 5], U32, 3)
        gcr = Ring(sc, "gc%d" % l, [128, 5], F32, 3)
        self.cp("dve", selc[:], cst[:, C_U:C_U + 1].to_broadcast([128, 128]), ["cst"], ["selc"])
        S.op("dve", lambda e: e.memset(ysb[:], 0.0), [], ["ysb"])
        nk = 5 if with_ctx else 4
        c2 = {0: 0, 1: 1}

        def idx_phase(e):
            pI, pIk = self.ps[7], self.psk[7]
            for (si, j0, nj, cap) in sets:
                ncol = 512 if si == 0 else 128
                for j in range(nj):
                    if si == 0:
                        sel, selk = selr.next()
                        self.ts("dve", sel[:], iota, slot_m[si][:, j, e:e + 1], None, ALU.is_equal, None, ["cst", ("slotm", si)], [selk])
                        rhs = sel[:]
                    else:
                        selk = "selc"
                        self.ts("dve", selc[:, 0:CAP_CTX], iota[:, 0:CAP_CTX], slot_m[si][:, j, e:e + 1], None, ALU.is_equal, None,
                                ["cst", ("slotm", si)], [selk])
                        rhs = selc[:]
                    self.mm(pI[0:4, 0:ncol], meta[si][:, j, e, :], rhs, j == 0, j == nj - 1, [("meta", si), selk], [pIk])
                off = 0 if si == 0 else 512
                self.cp("act", idxrow[0:4, off:off + ncol], pI[0:4, 0:ncol], [pIk], ["idxrow"])
            pX, pXk = self.ps[7], self.psk[7]
            for k in range(nk):
                self.tr(pX[:, k * 4:(k + 1) * 4], idxrow[0:4, k * 128:(k + 1) * 128], ["idxrow"], [pXk], kp=4)
            self.cp("act", metac[:, 0:nk, :], pX[:, 0:nk * 4].rearrange("p (k c) -> p k c", c=4), [pXk], ["metac"])
            idu, iduk = idur.next()
            gc, gck = gcr.next()
            self.stt("dve", idxf[:, 0:nk], metac[:, 0:nk, 0], 128.0, metac[:, 0:nk, 1], ALU.mult, ALU.add, ["metac"], ["idxf"])
            self.ts("dve", tmp5[:, 0:nk], metac[:, 0:nk, 3], -1.0, 1.0, ALU.mult, ALU.add, ["metac"], ["tmp5"])
            self.tt("dve", tmp5[:, 0:nk], tmp5[:, 0:nk], cst[:, C_DMY:C_DMY + nk], ALU.mult, ["tmp5", "cst"], ["tmp5"])
            self.tt("dve", idxf[:, 0:nk], idxf[:, 0:nk], tmp5[:, 0:nk], ALU.add, ["idxf", "tmp5"], ["idxf"])
            self.cp("dve", idu[:, 0:nk], idxf[:, 0:nk], ["idxf"], [iduk])
            self.cp("dve", gc[:, 0:nk], metac[:, 0:nk, 2], ["metac"], [gck])
            return (idu, iduk, gc, gck)

        gt_i = [0]

        def gather_phase(ix):
            idu, iduk, gc, gck = ix
            for k in range(nk):
                xs, xsk = xsr.next()
                S.dma("pool", lambda e: e.indirect_dma_start(out=xs[:], out_offset=None, in_=self.xn2[:, :],
                                                              in_offset=bass.IndirectOffsetOnAxis(ap=idu[:, k:k + 1], axis=0)),
                      [iduk], [xsk])
                c = 1 if k == 4 else 0
                for half in range(2):
                    bnk = gt_i[0] % 4
                    gt_i[0] += 1
                    pt, ptk = self.ps[bnk], self.psk[bnk]
                    for kc in range(half * 4, half * 4 + 4):
                        self.tr(pt[:, (kc % 4) * 128:(kc % 4 + 1) * 128], xs[:, kc * 128:(kc + 1) * 128], [xsk], [ptk])
                    for kc in range(half * 4, half * 4 + 4):
                        self.act(xsT[:, kc, k * 128:(k + 1) * 128], pt[:, (kc % 4) * 128:(kc % 4 + 1) * 128], AF.Identity,
                                 [ptk, ("A", 4), "cols"], ["xsT"], scale=self.A2[:, kc, c:c + 1], bias=self.B2(kc, c))

        def wload(src_ap):
            wt, wk = wring.next()
            self.ld(wt[:], src_ap, [], [wk], q="pool")
            return wt, wk

        gu_i = [0]

        cpend = [None]

        def ctx_tr(hc, hck, fq):
            pt, ptk = self.ps[7], self.psk[7]
            for fc in range(2):
                self.tr(pt[:, fc * 32:(fc + 1) * 32], hc[0:32, fc * 128:(fc + 1) * 128], [hck], [ptk], kp=32)
            self.cp("act", hidT[:, fq * 2:fq * 2 + 2, 512:544], pt[:, 0:64].rearrange("p (a t) -> p a t", a=2), [ptk],
                    [("hidT", fq * 2), ("hidT", fq * 2 + 1)])

        def ffn1(e):
            for fq in range(8):
                wg, wgk = wload(W["w_gate"][e, :, fq * 256:(fq + 1) * 256].rearrange("(k p) n -> p k n", p=128))
                wu, wuk = wload(W["w_up"][e, :, fq * 256:(fq + 1) * 256].rearrange("(k p) n -> p k n", p=128))
                for fc in range(2):
                    fcc = fq * 2 + fc
                    b = gu_i[0] % 2
                    gu_i[0] += 1
                    pG, pGk = self.ps[2 * b], self.psk[2 * b]
                    pU, pUk = self.ps[2 * b + 1], self.psk[2 * b + 1]
                    for kc in range(8):
                        self.mm(pG[:, :], wg[:, kc, fc * 128:(fc + 1) * 128], xsT[:, kc, 0:512], kc == 0, kc == 7, [wgk, "xsT"], [pGk])
                    for kc in range(8):
                        self.mm(pU[:, :], wu[:, kc, fc * 128:(fc + 1) * 128], xsT[:, kc, 0:512], kc == 0, kc == 7, [wuk, "xsT"], [pUk])
                    sg, sgk = sgr.next()
                    self.act(sg[:, 0:512], pG[:, :], AF.Silu, [pGk], [sgk])
                    self.tt("dve", hidT[:, fcc, 0:512], sg[:, 0:512], pU[:, :], ALU.mult, [sgk, pUk], [("hidT", fcc)])
                if with_ctx:
                    pc, pck = self.ps[4], self.psk[4]
                    for kc in range(8):
                        self.mm(pc[0:32, 0:256], xsT[:, kc, 512:544], wg[:, kc, :], kc == 0, kc == 7, [wgk, "xsT"], [pck])
                    for kc in range(8):
                        self.mm(pc[0:32, 256:512], xsT[:, kc, 512:544], wu[:, kc, :], kc == 0, kc == 7, [wuk, "xsT"], [pck])
                    hc, hck = hcr.next()
                    self.act(hc[0:32, 0:256], pc[0:32, 0:256], AF.Silu, [pck], [hck])
                    self.tt("dve", hc[0:32, 0:256], hc[0:32, 0:256], pc[0:32, 256:512], ALU.mult, [hck, pck], [hck])
                    if cpend[0] is not None:
                        ctx_tr(*cpend[0])
                    cpend[0] = (hc, hck, fq)
            if cpend[0] is not None:
                ctx_tr(*cpend[0])
                cpend[0] = None

        y_i = [0]

        def ffn2(e, ix):
            idu, iduk, gc, gck = ix
            hk = [("hidT", f) for f in range(16)]
            for dq in range(4):
                wd = []
                for fh in range(2):
                    wd.append(wload(W["w_down"][e, fh * 1024:(fh + 1) * 1024, dq * 256:(dq + 1) * 256].rearrange("(k p) n -> p k n", p=128)))
                for k in range(nk):
                    if k < 4:
                        b = y_i[0] % 2
                        y_i[0] += 1
                        pY, pYk = self.ps[5 + b], self.psk[5 + b]
                        rows = 128
                        lsl = slice(k * 128, (k + 1) * 128)
                    else:
                        pY, pYk = self.ps[4], self.psk[4]
                        rows = 32
                        lsl = slice(512, 544)
                    for fcc in range(16):
                        wt, wk = wd[fcc // 8]
                        self.mm(pY[0:rows, 0:256], hidT[:, fcc, lsl], wt[:, fcc % 8, :], fcc == 0, fcc == 15, [("hidT", fcc), wk], [pYk])
                    c = 1 if k == 4 else 0
                    self.stt("dve", ysb[0:rows, k, dq * 256:(dq + 1) * 256], pY[0:rows, 0:256], gc[0:rows, k:k + 1],
                             self.Gbc[0:rows, 1, c, dq * 256:(dq + 1) * 256], ALU.mult, ALU.mult,
                             [pYk, gck, ("Gbc", 1, c)], [("ysb", k)])

        def scatter_phase(ix):
            idu, iduk, gc, gck = ix
            for k in range(nk):
                S.dma("pool", lambda e: e.indirect_dma_start(out=xres[:, :], out_offset=bass.IndirectOffsetOnAxis(ap=idu[:, k:k + 1], axis=0),
                                                              in_=ysb[:, k, :], in_offset=None, compute_op=ALU.add),
                      [iduk, ("ysb", k), "ysb"], ["xacc"])

        n_exp = NE if self.n_exp is None else self.n_exp
        ixs = {0: idx_phase(0)}
        gather_phase(ixs[0])
        for e in range(n_exp):
            if e + 1 < n_exp:
                ixs[e + 1] = idx_phase(e + 1)
            ffn1(e)
            if e > 0:
                scatter_phase(ixs[e - 1])
            if e + 1 < n_exp:
                gather_phase(ixs[e + 1])
            ffn2(e, ixs[e])
        scatter_phase(ixs[n_exp - 1])
        sc.close()


def _host_consts():
    cp = np.zeros((128, C_END), np.float32)
    cp[:, C_ID:C_ID + 128] = np.eye(128, dtype=np.float32)
    cp[:, C_ONES:C_ONES + 128] = 1.0
    pi = np.arange(128)
    cp[:, C_U:C_U + 128] = (pi[:, None] < pi[None, :]).astype(np.float32)
    mp = (pi[:, None] >= pi[None, :]).astype(np.float32)
    mn = (pi[:, None] <= pi[None, :]).astype(np.float32)
    cp[:, C_MP:C_MP + 512] = np.tile(mp, (1, 4))
    cp[:, C_MN:C_MN + 512] = np.tile(mn, (1, 4))
    cp[:, C_IOTA:C_IOTA + 512] = np.arange(512, dtype=np.float32)[None, :]
    cp[:, C_PIDX] = pi
    for k in range(5):
        cp[:, C_DMY + k] = NTOK + k * 128 + pi
    t = np.arange(NLAT)
    row = (t // 64).astype(np.float32)
    col = (t % 64).astype(np.float32)
    inv = (np.float32(10000.0) ** (-np.arange(16, dtype=np.float32) / np.float32(16))).astype(np.float32)
    ar = (row[:, None] * inv[None, :]).astype(np.float32)
    ac = (col[:, None] * inv[None, :]).astype(np.float32)
    rope = np.concatenate([np.cos(ar), np.cos(ac), np.sin(ar), np.sin(ac)], axis=1).astype(np.float32)
    return cp, rope


def _host_consts2():
    c2 = np.zeros((128, C2_END), np.float32)
    p = np.arange(128)[:, None]
    f = np.arange(128)[None, :]
    c2[:, C2_LF:C2_LF + 128] = (p <= f)
    c2[:, C2_LB:C2_LB + 128] = (p >= f)
    c2[:, C2_LT:C2_LT + 128] = np.where(f < p, 0.0, NEG)
    c2[:, C2_GT:C2_GT + 128] = np.where(f > p, 0.0, NEG)
    c2[:, C2_GE:C2_GE + 128] = np.where(f >= p, 0.0, NEG)
    c2[:, C2_LE:C2_LE + 128] = np.where(f <= p, 0.0, NEG)
    same = ((p // 64) == (f // 64)).astype(np.float32)
    c2[:, C2_B64:C2_B64 + 128] = same
    c2[:, C2_NB64:C2_NB64 + 128] = -same
    c2[:, C2_OFF:C2_OFF + 128] = 1.0 - same
    return c2


def _colT(v, n):
    return np.ascontiguousarray(np.asarray(v, np.float32).reshape(n, 128).T)


def make_in_maps(inputs, cores):
    cp, rope = _host_consts()
    shared = {"cpack": cp, "rope": rope}
    for l in (0, 1):
        p = "l%d_" % l
        shared[p + "ada_w"] = np.asarray(inputs[p + "ada_w"], np.float32)
        shared[p + "ada_b"] = np.asarray(inputs[p + "ada_b"], np.float32)
        shared[p + "ada_bT"] = _colT(inputs[p + "ada_b"], 48)
        shared[p + "nmixT"] = _colT(inputs[p + "norm_mix"], 8)
        shared[p + "nffnT"] = _colT(inputs[p + "norm_ffn"], 8)
        for n in ("router", "w_gate", "w_up", "w_down"):
            shared[p + n] = np.asarray(inputs[p + n], np.float32)
    for n in ("l0_sink", "l0_w_o", "l1_w_in", "l1_o_norm", "l1_w_o", "final_norm"):
        shared[n] = np.asarray(inputs[n], np.float32)
    shared["l1_a_log"] = np.asarray(inputs["l1_a_log"], np.float32).reshape(16)
    shared["l1_dt_bias"] = np.asarray(inputs["l1_dt_bias"], np.float32).reshape(16)
    cv = np.asarray(inputs["l1_conv"], np.float32)
    shared["l1_convT"] = np.ascontiguousarray(cv.reshape(3, 24, 128).transpose(2, 1, 0))
    shared["cpack2"] = _host_consts2()
    wq = np.asarray(inputs["l0_w_qkv"], np.float32)
    perm = [pr * 8 + s_ * 4 + g for pr in range(2) for g in range(4) for s_ in range(2)]
    cols = np.concatenate([np.arange(h * 64, (h + 1) * 64) for h in perm] + [np.arange(1024, 1536)])
    shared["l0_w_qkv"] = np.ascontiguousarray(wq[:, cols])
    maps = []
    for b in cores:
        m = dict(shared)
        m["x"] = np.ascontiguousarray(inputs["x"][b], dtype=np.float32)
        m["ctx"] = np.ascontiguousarray(inputs["ctx"][b], dtype=np.float32)
        cv = np.stack([np.asarray(inputs["c"][b], np.float32), np.asarray(inputs["c_ctx"], np.float32)], axis=1)
        m["cvecT"] = np.ascontiguousarray(cv.reshape(8, 128, 2).transpose(1, 0, 2))
        maps.append(m)
    return maps


def kernel(**inputs):
    b = Builder()
    nc = b.build()
    maps = make_in_maps(inputs, list(range(8)))
    maps = [{k: v for k, v in m.items() if k in b.inp} for m in maps]
    res = run_bass_kernel_spmd(nc, maps, core_ids=list(range(8)))
    return np.stack([r["out"] for r in res.results], axis=0).astype(np.float32)
```

```python
import numpy as np
import concourse.bass as bass
import concourse.mybir as mybir
from concourse.bass_utils import run_bass_kernel_spmd

F32 = mybir.dt.float32
F32R = mybir.dt.float32r
U32 = mybir.dt.uint32
ALU = mybir.AluOpType
AF = mybir.ActivationFunctionType
AX = mybir.AxisListType

D = 1024
NLAT = 4096
NCTX = 256
NT_LAT = 32
NT_CTX = 2
NT = 34
NTOK = NLAT + NCTX
NE = 16
FF = 2048
CAP_LAT = 512
CAP_CTX = 32
NDUMMY = 640
EPS = 1e-6
NBIS = 30

C_ID, C_ONES, C_U, C_MP, C_MN, C_IOTA, C_PIDX, C_DMY, C_END = 0, 128, 256, 384, 896, 1408, 1920, 1921, 1926


C2_LF, C2_LB, C2_LT, C2_GT, C2_GE, C2_LE, C2_B64, C2_NB64, C2_OFF, C2_END = 0, 128, 256, 384, 512, 640, 768, 896, 1024, 1152
NEG = -30000.0


class Sched:
    def __init__(self, nc, n_dma_sems=24):
        self.nc = nc
        self.engs = {"pe": nc.tensor, "act": nc.scalar, "dve": nc.vector,
                     "pool": nc.gpsimd, "sp": nc.sync}
        self.csem = {e: nc.alloc_semaphore("c_" + e) for e in ("pe", "act", "dve", "pool")}
        self.ccnt = {e: 0 for e in self.csem}
        self.known = {e: {} for e in self.engs}
        self.dsems = [nc.alloc_semaphore("d%d" % i) for i in range(2 * n_dma_sems)]
        self.dcnt = [0] * (2 * n_dma_sems)
        self.dpool = {"sp": list(range(0, n_dma_sems)), "pool": list(range(n_dma_sems, 2 * n_dma_sems))}
        self.dnext = {"sp": 0, "pool": 0}
        self.state = {}
        self.out_events = []
        self.n_wait = 0
        self.n_inst = 0

    def _need(self, eng, ev):
        sem, val = ev
        k = self.known[eng]
        if k.get(sem.num, 0) >= val:
            return
        self.engs[eng].wait_ge(sem, val)
        self.n_wait += 1
        k[sem.num] = val

    def _deps(self, eng, reads, writes, skip_self=False):
        evs = {}

        def add(ev):
            if ev is None:
                return
            sem, val = ev
            if skip_self and sem.num == self.csem[eng].num:
                return
            if evs.get(sem.num, (None, 0))[1] < val:
                evs[sem.num] = ev

        own = self.csem[eng].num if eng in self.csem else -1
        for k in reads:
            st = self.state.get(k)
            if st:
                add(st["w"])
                if isinstance(k, tuple) and k[0] == "ps":
                    for r in st["r"]:
                        if r[0].num != own:
                            add(r)
        for k in writes:
            st = self.state.get(k)
            if st:
                add(st["w"])
                for r in st["r"]:
                    add(r)
        for ev in evs.values():
            self._need(eng, ev)

    def _commit(self, ev, reads, writes):
        for k in reads:
            st = self.state.setdefault(k, {"w": None, "r": []})
            st["r"] = [r for r in st["r"] if r[0].num != ev[0].num] + [ev]
        for k in writes:
            self.state[k] = {"w": ev, "r": []}

    def op(self, eng, fn, reads=(), writes=()):
        self._deps(eng, reads, writes, skip_self=(eng == "pe"))
        ins = fn(self.engs[eng])
        self.ccnt[eng] += 1
        ins.then_inc(self.csem[eng], 1)
        ev = (self.csem[eng], self.ccnt[eng])
        self._commit(ev, reads, writes)
        self.n_inst += 1
        return ev

    def dma(self, q, fn, reads=(), writes=(), is_output=False):
        self._deps(q, reads, writes)
        pool = self.dpool[q]
        i = pool[self.dnext[q]]
        self.dnext[q] = (self.dnext[q] + 1) % len(pool)
        sem = self.dsems[i]
        if self.dcnt[i] > 0:
            self._need(q, (sem, 16 * self.dcnt[i]))
        ins = fn(self.engs[q])
        self.dcnt[i] += 1
        ins.then_inc(sem, 16)
        ev = (sem, 16 * self.dcnt[i])
        self._commit(ev, reads, writes)
        if is_output:
            self.out_events.append(ev)
        self.n_inst += 1
        return ev

    def barrier(self):
        for eng in self.engs:
            for i, sem in enumerate(self.dsems):
                if self.dcnt[i] > 0:
                    self._need(eng, (sem, 16 * self.dcnt[i]))
            for e, sem in self.csem.items():
                if self.ccnt[e] > 0:
                    self._need(eng, (sem, self.ccnt[e]))

    def finish(self, eng="sp"):
        for i, sem in enumerate(self.dsems):
            if self.dcnt[i] > 0:
                self._need(eng, (sem, 16 * self.dcnt[i]))
        for e, sem in self.csem.items():
            if self.ccnt[e] > 0:
                self._need(eng, (sem, self.ccnt[e]))


class Scope:
    def __init__(self, builder):
        from contextlib import ExitStack
        self.b = builder
        self.st = ExitStack()

    def sb(self, name, shape, dtype=F32):
        return self.st.enter_context(self.b.nc.sbuf_tensor(name, list(shape), dtype))

    def close(self):
        self.b.S.barrier()
        self.st.close()


class Ring:
    def __init__(self, sc, name, shape, dtype, n):
        self.t = [sc.sb("%s%d" % (name, i), shape, dtype) for i in range(n)]
        self.k = [("%s" % name, i) for i in range(n)]
        self.i = 0

    def next(self):
        r = (self.t[self.i], self.k[self.i])
        self.i = (self.i + 1) % len(self.t)
        return r


class Builder:
    def __init__(self, stage=99, debug=False, n_exp=None, start_layer=0, n_tiles=None):
        self.n_exp = n_exp
        self.start_layer = start_layer
        self.n_tiles = n_tiles
        self.stage = stage
        self.debug = debug
        nc = bass.Bass("TRN2", target_bir_lowering=False)
        self.nc = nc
        self.S = Sched(nc)
        self.inp = {}
        self.ps = [nc.alloc_psum_tensor("psb%d" % i, [128, 512], F32) for i in range(8)]
        self.psk = [("ps", i) for i in range(8)]

    def din(self, name, shape, dtype=F32):
        t = self.nc.dram_tensor(name, list(shape), dtype, kind="ExternalInput").ap()
        self.inp[name] = t
        return t

    def dscratch(self, name, shape, dtype=F32, out=False):
        kind = "ExternalOutput" if (out or self.debug) else "Internal"
        return self.nc.dram_tensor(name, list(shape), dtype, kind=kind).ap()

    def sb(self, name, shape, dtype=F32):
        return self.nc.alloc_sbuf_tensor(name, list(shape), dtype)

    def mm(self, out, lhsT, rhs, start, stop, reads, writes):
        return self.S.op("pe", lambda e: e.matmul(out, lhsT=lhsT, rhs=rhs, start=start, stop=stop),
                         reads, writes)

    def tr(self, out, in_, reads, writes, kp=128):
        ident = self.cst[0:kp, C_ID:C_ID + kp]
        return self.S.op("pe", lambda e: e.transpose(out, in_, ident), list(reads) + ["cst"], writes)

    def act(self, out, in_, func, reads, writes, **kw):
        return self.S.op("act", lambda e: e.activation(out=out, in_=in_, func=func, **kw), reads, writes)

    def tt(self, eng, out, in0, in1, op, reads, writes):
        return self.S.op(eng, lambda e: e.tensor_tensor(out=out, in0=in0, in1=in1, op=op), reads, writes)

    def ts(self, eng, out, in0, s1, s2, op0, op1, reads, writes, **kw):
        if s2 is None:
            return self.S.op(eng, lambda e: e.tensor_scalar(out=out, in0=in0, scalar1=s1, scalar2=None,
                                                            op0=op0, **kw), reads, writes)
        return self.S.op(eng, lambda e: e.tensor_scalar(out=out, in0=in0, scalar1=s1, scalar2=s2,
                                                        op0=op0, op1=op1, **kw), reads, writes)

    def stt(self, eng, out, in0, scalar, in1, op0, op1, reads, writes):
        return self.S.op(eng, lambda e: e.scalar_tensor_tensor(out=out, in0=in0, scalar=scalar, in1=in1,
                                                               op0=op0, op1=op1), reads, writes)

    def cp(self, eng, out, in_, reads, writes):
        if eng == "act":
            return self.act(out, in_, AF.Copy, reads, writes)
        return self.S.op(eng, lambda e: e.tensor_copy(out=out, in_=in_), reads, writes)

    def ld(self, out, in_, reads, writes, q="sp"):
        return self.S.dma(q, lambda e: e.dma_start(out=out, in_=in_), reads, writes)

    def rstd_of(self, x_ap, xk, junk, junkk, ss, ssk):
        self.act(junk, x_ap, AF.Square, [xk], [junkk, ssk], accum_out=ss)
        self.ts("dve", ss, ss, 1.0 / D, EPS, ALU.mult, ALU.add, [ssk], [ssk])
        self.act(ss, ss, AF.Sqrt, [ssk], [ssk])
        self.S.op("dve", lambda e: e.reciprocal(out=ss, in_=ss), [ssk], [ssk])

    def build(self):
        nc, S = self.nc, self.S
        st, sl = self.stage, self.start_layer
        x = self.din("x", [NLAT, D]) if sl == 0 else None
        ctx = self.din("ctx", [NCTX, D]) if sl == 0 else None
        cvecT = self.din("cvecT", [128, 8, 2])
        cpack = self.din("cpack", [128, C_END])
        rope = self.din("rope", [NLAT, 64]) if sl == 0 else None
        W = {}
        for l in (0, 1):
            if l == 0 and sl > 0:
                continue
            if l == 1 and st < 30:
                continue
            W[l] = dict(
                ada_w=self.din("l%d_ada_w" % l, [D, 6 * D]),
                ada_b=self.din("l%d_ada_b" % l, [6 * D]),
                ada_bT=self.din("l%d_ada_bT" % l, [128, 48]),
                nmixT=self.din("l%d_nmixT" % l, [128, 8]),
                nffnT=self.din("l%d_nffnT" % l, [128, 8]),
                router=self.din("l%d_router" % l, [D, NE]),
            )
            if (l == 0 and st >= 2) or (l == 1 and st >= 40):
                W[l].update(w_gate=self.din("l%d_w_gate" % l, [NE, D, FF]),
                            w_up=self.din("l%d_w_up" % l, [NE, D, FF]),
                            w_down=self.din("l%d_w_down" % l, [NE, FF, D]))
        if 0 in W:
            W[0].update(w_qkv=self.din("l0_w_qkv", [D, 1536]), sink=self.din("l0_sink", [16]),
                        w_o=self.din("l0_w_o", [D, D]))
        if 1 in W:
            W[1].update(w_in=self.din("l1_w_in", [D, 4128]), convT=self.din("l1_convT", [128, 24, 3]),
                        a_log=self.din("l1_a_log", [16]), dt_bias=self.din("l1_dt_bias", [16]),
                        o_norm=self.din("l1_o_norm", [128]), w_o=self.din("l1_w_o", [D, D]))
            cpack2 = self.din("cpack2", [128, C2_END])
        if st >= 50:
            self.din("final_norm", [D])
        self.W = W
        self.x, self.ctx, self.rope = x, ctx, rope
        self.out = self.nc.dram_tensor("out", [NLAT, D], F32, kind="ExternalOutput").ap()
        self.qs = self.dscratch("qs", [NTOK, D])
        if sl == 0:
            xa = self.dscratch("xresA", [NTOK + NDUMMY, D])
        else:
            xa = self.din("xresA_in", [NTOK + NDUMMY, D])
        self.xres = [xa, self.dscratch("xresB", [NTOK + NDUMMY, D])]
        self.xn2 = self.dscratch("xn2", [NTOK + NDUMMY, D])

        self.cst = self.sb("cst", [128, C_END])
        self.ld(self.cst[:], cpack, [], ["cst"])
        self.scT = self.sb("scT", [128, 8, 2], F32R)
        self.cols = self.sb("cols", [128, 48, 2])
        self.A1 = self.sb("A1", [128, 8, 2]); self.A2 = self.sb("A2", [128, 8, 2])
        self.Gbc = self.sb("Gbc", [128, 2, 2, D])
        self.aff = self.sb("aff", [128, NT, NE])
        if self.debug:
            S.op("dve", lambda e: e.memset(self.aff[:], 0.0), [], [("aff", i) for i in range(NT)])
        sc0 = Scope(self)
        zero_t = sc0.sb("zero_t", [128, D])
        S.op("dve", lambda e: e.memset(zero_t[:], 0.0), [], ["zero_t"])
        bufs = [(self.xres[1], "xres1"), (self.xn2, "xn2")] + ([(self.xres[0], "xres0")] if sl == 0 else [])
        for buf, k in bufs:
            for r in range(NDUMMY // 128):
                self.ld(buf[NTOK + r * 128: NTOK + (r + 1) * 128, :], zero_t[:], ["zero_t"], [(k, "dummy", r)])
        sc0.close()
        if sl == 0:
            self.modulation(0)
            if st >= 1:
                self.attention_layer()
            if st >= 2:
                self.moe(0, self.xres[0], "xres0", with_ctx=True)
        if st >= 30:
            self.cst2 = self.sb("cst2", [128, C2_END])
            self.ld(self.cst2[:], cpack2, [], ["cst2"])
            self.modulation(1)
            self.deltanet_layer()
        if st >= 40:
            self.moe(1, self.xres[1], "xres1", with_ctx=False)
        if st >= 50:
            self.final_norm()
        if self.debug:
            d_aff = self.nc.dram_tensor("d_aff", [128, NT * NE], F32, kind="ExternalOutput").ap()
            self.ld(d_aff, self.aff[:, :, :].rearrange("p j e -> p (j e)"), [("aff", i) for i in range(NT)], ["d_aff"])
            d_cols = self.nc.dram_tensor("d_cols", [128, 96], F32, kind="ExternalOutput").ap()
            self.ld(d_cols, self.cols[:, :, :].rearrange("p c t -> p (c t)"), ["cols"], ["d_cols"])
            d_g = self.nc.dram_tensor("d_g", [128, 4 * D], F32, kind="ExternalOutput").ap()
            self.ld(d_g, self.Gbc[:, :, :, :].rearrange("p a b d -> p (a b d)"),
                    [("Gbc", a, b) for a in range(2) for b in range(2)], ["d_g"])
        S.finish()
        return nc

    def modulation(self, l):
        nc, S, W = self.nc, self.S, self.W[l]
        sfx = "m%d" % l
        sc = Scope(self)
        self.wring = Ring(sc, "wst" + sfx, [128, 8, 512], F32R, 4)
        cv = sc.sb("cv" + sfx, [128, 8, 2])
        self.ld(cv[:], self.inp["cvecT"], [], ["cv"])
        self.act(self.scT[:], cv[:], AF.Silu, ["cv"], ["scT"])
        screp = [sc.sb("screp%d%s" % (c, sfx), [128, 8, 128], F32R) for c in range(2)]
        for c in range(2):
            self.cp("dve", screp[c][:], self.scT[:, :, c:c + 1].to_broadcast([128, 8, 128]), ["scT"], [("screp", c)])
        abT = sc.sb("abT" + sfx, [128, 48])
        self.ld(abT[:], W["ada_bT"], [], ["abT"])
        nmix = sc.sb("nmix" + sfx, [128, 8]); nffn = sc.sb("nffn" + sfx, [128, 8])
        self.ld(nmix[:], W["nmixT"], [], ["nmix"])
        self.ld(nffn[:], W["nffnT"], [], ["nffn"])
        abbc = sc.sb("abbc" + sfx, [128, 2, D])
        self.ld(abbc[:, 0, :], W["ada_b"][2 * D:3 * D].partition_broadcast(128), [], [("abbc", 0)])
        self.ld(abbc[:, 1, :], W["ada_b"][5 * D:6 * D].partition_broadcast(128), [], [("abbc", 1)])
        pcol = self.ps[0]
        pcv = pcol[:, 0:96].rearrange("p (c t) -> p c t", t=2)
        for cg in range(12):
            wt, wk = self.wring.next()
            self.ld(wt[:], W["ada_w"][:, cg * 512:(cg + 1) * 512].rearrange("(k p) n -> p k n", p=128),
                    [], [wk], q="pool")
            for c4 in range(4):
                cc = cg * 4 + c4
                for kc in range(8):
                    self.mm(pcv[:, cc, :], wt[:, kc, c4 * 128:(c4 + 1) * 128], self.scT[:, kc, :],
                            kc == 0, kc == 7, [wk, "scT"], [self.psk[0]])
            if cg in (4, 5, 10, 11):
                gi = 0 if cg < 6 else 1
                half = cg % 2
                for c in range(2):
                    pb, pbk = self.ps[1 + c], self.psk[1 + c]
                    for kc in range(8):
                        self.mm(pb[:, :], screp[c][:, kc, :], wt[:, kc, :], kc == 0, kc == 7,
                                [wk, ("screp", c)], [pbk])
                    self.tt("dve", self.Gbc[:, gi, c, half * 512:(half + 1) * 512], pb[:, :],
                            abbc[:, gi, half * 512:(half + 1) * 512], ALU.add,
                            [pbk, ("abbc", gi)], [("Gbc", gi, c)])
        self.tt("dve", self.cols[:], pcv, abT[:].unsqueeze(2).to_broadcast([128, 48, 2]), ALU.add,
                [self.psk[0], "abT"], ["cols"])
        for (A, nrm, nk, v) in ((self.A1, nmix, "nmix", 1), (self.A2, nffn, "nffn", 4)):
            self.stt("dve", A[:], self.cols[:, v * 8:(v + 1) * 8, :], 1.0,
                     nrm[:].unsqueeze(2).to_broadcast([128, 8, 2]), ALU.add, ALU.mult,
                     ["cols", nk], [("A", v)])
        sc.close()

    def B1(self, kc, c):
        return self.cols[:, 0 + kc, c:c + 1]

    def B2(self, kc, c):
        return self.cols[:, 24 + kc, c:c + 1]

    def attention_layer(self):
        nc, S, W = self.nc, self.S, self.W[0]
        cst = self.cst
        sca = Scope(self)
        KT = sca.sb("KT", [128, 2, NTOK], F32R)
        V = sca.sb("Vaug", [128, NT, 4, 66], F32R)
        esink = sca.sb("esink", [128, 16])
        wr = sca.sb("wr", [128, 8, NE])
        xr = Ring(sca, "xt", [128, D], F32, 2)
        xnr = Ring(sca, "xnb", [128, D], F32, 3)
        ssr = Ring(sca, "ss", [128, 1], F32, 6)
        junk = sca.sb("junk", [128, D])
        sc1 = Scope(self)
        wbig = sc1.sb("wbig", [128, 8, 1536], F32R)
        hTr = Ring(sc1, "hT", [128, 8, 128], F32R, 3)
        qkr = Ring(sc1, "qk", [128, 1280], F32, 2)
        csr = Ring(sc1, "cs", [128, 64], F32, 6)
        tmpr = Ring(sc1, "rt", [128, 4, 256], F32, 1)
        for cg in range(3):
            self.ld(wbig[:, :, cg * 512:(cg + 1) * 512],
                    W["w_qkv"][:, cg * 512:(cg + 1) * 512].rearrange("(k p) n -> p k n", p=128),
                    [], [("wbig", cg)], q="pool")
        Vf = V[:, :, :, :].rearrange("p j h c -> p (j h) c")
        self.cp("dve", Vf[:, :, 64:65], self.cst[:, C_ONES:C_ONES + 1].unsqueeze(1).to_broadcast([128, NT * 4, 1]), ["cst"], [("V1",)])
        self.cp("dve", Vf[:, :, 65:66], self.cst[:, C_U:C_U + 1].unsqueeze(1).to_broadcast([128, NT * 4, 1]), ["cst"], [("V0",)])
        self.ld(esink[:], W["sink"].partition_broadcast(128), [], ["esink"])
        self.act(esink[:], esink[:], AF.Exp, ["esink"], ["esink"])
        self.ld(wr[:], W["router"].rearrange("(k p) n -> p k n", p=128), [], ["wr"])

        def src_rows(j):
            if j < NT_LAT:
                return self.x[j * 128:(j + 1) * 128, :]
            return self.ctx[(j - NT_LAT) * 128:(j - NT_LAT + 1) * 128, :]

        cx = [dict() for _ in range(NT)]

        def p1_s0(j):
            c = cx[j]
            c["c"] = 0 if j < NT_LAT else 1
            xt, xk = xr.next()
            self.ld(xt[:], src_rows(j), [], [xk])
            if c["c"] == 0:
                c["cs"], c["ck"] = csr.next()
                self.ld(c["cs"][:], self.rope[j * 128:(j + 1) * 128, :], [], [c["ck"]])
            ss, ssk = ssr.next()
            self.rstd_of(xt[:], xk, junk[:], "junk", ss[:], ssk)
            c["xn"], c["xnk"] = xnr.next()
            self.ts("dve", c["xn"][:], xt[:], ss[:, 0:1], None, ALU.mult, None, [xk, ssk], [c["xnk"]])

        def p1_s1(j):
            c = cx[j]
            xn, xnk, cc = c["xn"], c["xnk"], c["c"]
            c["hT"], c["hk"] = hTr.next()
            hT, hk = c["hT"], c["hk"]
            for kc in range(8):
                pt, ptk = self.ps[kc // 4], self.psk[kc // 4]
                self.tr(pt[:, (kc % 4) * 128:(kc % 4 + 1) * 128], xn[:, kc * 128:(kc + 1) * 128], [xnk], [ptk])
            for kc in range(8):
                pt, ptk = self.ps[kc // 4], self.psk[kc // 4]
                self.act(hT[:, kc, :], pt[:, (kc % 4) * 128:(kc % 4 + 1) * 128], AF.Identity,
                         [ptk, ("A", 1), "cols"], [hk], scale=self.A1[:, kc, cc:cc + 1], bias=self.B1(kc, cc))

        def p1_s2(j):
            c = cx[j]
            hT, hk = c["hT"], c["hk"]
            c["pb"] = 2 + 3 * (j % 2)
            for cg in range(3):
                pq, pqk = self.ps[c["pb"] + cg], self.psk[c["pb"] + cg]
                for kc in range(8):
                    self.mm(pq[:, :], hT[:, kc, :], wbig[:, kc, cg * 512:(cg + 1) * 512], kc == 0, kc == 7,
                            [hk, ("wbig", cg)], [pqk])

        def p1_s3(j):
            c = cx[j]
            pb = c["pb"]
            c["qk"], c["qkk"] = qkr.next()
            qk, qkk = c["qk"], c["qkk"]
            if c["c"] == 0:
                cs, ck = c["cs"], c["ck"]
                cosb = lambda nh: cs[:, 0:32].rearrange("p (a f) -> p a f", a=2).unsqueeze(1).to_broadcast([128, nh, 2, 16])
                sinb = lambda nh: cs[:, 32:64].rearrange("p (a f) -> p a f", a=2).unsqueeze(1).to_broadcast([128, nh, 2, 16])
                for cg in range(3):
                    nh = 8 if cg < 2 else 4
                    pq, pqk = self.ps[pb + cg], self.psk[pb + cg]
                    pv = pq[:, 0:nh * 64].rearrange("p (h a b f) -> p h a b f", h=nh, a=2, b=2, f=16)
                    ov = qk[:, cg * 512:cg * 512 + nh * 64].rearrange("p (h a b f) -> p h a b f", h=nh, a=2, b=2, f=16)
                    x1, x2 = pv[:, :, :, 0, :], pv[:, :, :, 1, :]
                    tm, tmk = tmpr.next()
                    t = [tm[:, i, 0:nh * 32].rearrange("p (h a f) -> p h a f", h=nh, a=2, f=16) for i in range(4)]
                    self.tt("dve", t[0], x1, cosb(nh), ALU.mult, [pqk, ck], [tmk])
                    self.tt("dve", t[1], x2, sinb(nh), ALU.mult, [pqk, ck], [tmk])
                    self.tt("dve", t[2], x2, cosb(nh), ALU.mult, [pqk, ck], [tmk])
                    self.tt("dve", t[3], x1, sinb(nh), ALU.mult, [pqk, ck], [tmk])
                    self.tt("pool", ov[:, :, :, 0, :], t[0], t[1], ALU.subtract, [tmk], [qkk])
                    self.tt("pool", ov[:, :, :, 1, :], t[2], t[3], ALU.add, [tmk], [qkk])
            else:
                for cg in range(3):
                    ncol = 512 if cg < 2 else 256
                    self.cp("act", qk[:, cg * 512:cg * 512 + ncol], self.ps[pb + cg][:, 0:ncol], [self.psk[pb + cg]], [qkk])
            self.cp("act", V[:, j, :, 0:64], self.ps[pb + 2][:, 256:512].rearrange("p (h d) -> p h d", h=4),
                    [self.psk[pb + 2]], [("V", j)])
            self.ld(self.qs[j * 128:(j + 1) * 128, :], qk[:, 0:1024], [qkk], [("qs", j)])

        def p1_s4(j):
            c = cx[j]
            qk, qkk = c["qk"], c["qkk"]
            for pr in range(2):
                self.tr(self.ps[1][:, pr * 128:(pr + 1) * 128], qk[:, 1024 + pr * 128:1024 + (pr + 1) * 128], [qkk], [self.psk[1]])
            self.cp("act", KT[:, :, j * 128:(j + 1) * 128], self.ps[1][:, 0:256].rearrange("p (a t) -> p a t", a=2),
                    [self.psk[1]], [("KT", j)])

        stages = [p1_s0, p1_s1, p1_s2, p1_s3, p1_s4]
        for t_ in range(NT + len(stages) - 1):
            for st_ in range(len(stages) - 1, -1, -1):
                i_ = t_ - st_
                if 0 <= i_ < NT:
                    stages[st_](i_)

        sc1.close()
        sca_outer, sca = sca, Scope(self)
        wo = sca.sb("wo", [128, 8, 1024], F32R)
        self.ld(wo[:], W["w_o"].rearrange("(k p) n -> p k n", p=128), [], ["wo"], q="pool")
        wok = ["wo"]
        QTr = Ring(sca, "QT", [128, 2, 4, 128], F32R, 2)
        PTr = Ring(sca, "PT", [128, 5, 512], F32R, 2)
        osb = sca.sb("osb", [128, 16, 64])
        otsr = Ring(sca, "ots", [66, 512], F32, 1)
        oT = sca.sb("oT", [128, 8, 128], F32R)
        den = sca.sb("den", [128, 16])
        xmr = Ring(sca, "xm", [128, D], F32, 3)
        h2T = sca.sb("h2T", [128, 8, 128])
        lg = sca.sb("lg", [128, NE]); mx = sca.sb("mx", [128, 1]); sm = sca.sb("sm", [128, 1])
        pend = [None]

        def p2_loads(i_):
            qt_, qtk_ = xr.next()
            self.ld(qt_[:], self.qs[i_ * 128:(i_ + 1) * 128, :], [("qs", i_)], [qtk_])
            xt_, xk_ = xnr.next()
            self.ld(xt_[:], src_rows(i_), [], [xk_])
            return qt_, qtk_, xt_, xk_

        nxt = p2_loads(0)
        for i in range(NT):
            c = 0 if i < NT_LAT else 1
            qt, qtk, xt, xk = nxt
            if i + 1 < NT:
                nxt = p2_loads(i + 1)
            QT, QTk = QTr.next()
            for pr in range(2):
                for g in range(4):
                    self.tr(self.ps[pr][:, g * 128:(g + 1) * 128], qt[:, (pr * 4 + g) * 128:(pr * 4 + g + 1) * 128], [qtk], [self.psk[pr]])
                self.cp("act", QT[:, pr, :, :], self.ps[pr][:, :].rearrange("p (g t) -> p g t", g=4), [self.psk[pr]], [QTk])
            if c == 0:
                kbs = ([i - 1] if i > 0 else []) + [i] + ([i + 1] if i < NT_LAT - 1 else []) + [32, 33]
                kmask = ([C_MP] if i > 0 else []) + [None] + ([C_MN] if i < NT_LAT - 1 else []) + [None, None]
            else:
                kbs = [32, 33]
                kmask = [None, None]
            if pend[0] is not None:
                self.moe_prep_a(*pend[0], skip0=True)

            def st_phase(kvh):
                pr, base = kvh // 2, (kvh % 2) * 64
                PT, PTk = PTr.next()
                for kbi, kb in enumerate(kbs):
                    pS, pSk = self.ps[2 + kbi % 2], self.psk[2 + kbi % 2]
                    self.mm(pS[:, :], KT[base:base + 64, pr, kb * 128:(kb + 1) * 128],
                            QT[base:base + 64, pr, :, :], True, True, [("KT", kb), QTk], [pSk])
                    self.act(PT[:, kbi, :], pS[:, :], AF.Exp, [pSk], [PTk], scale=0.125)
                    if kmask[kbi] is not None:
                        mo = kmask[kbi]
                        self.tt("pool", PT[:, kbi, :], PT[:, kbi, :], cst[:, mo:mo + 512], ALU.mult, [PTk, "cst"], [PTk])
                return PT, PTk

            def pv_phase(kvh, PT, PTk):
                pO, pOk = self.ps[4 + kvh], self.psk[4 + kvh]
                for kbi, kb in enumerate(kbs):
                    self.mm(pO[0:66, :], V[:, kb, kvh, :], PT[:, kbi, :], kbi == 0, kbi == len(kbs) - 1,
                            [PTk, ("V", kb), ("V1",), ("V0",)], [pOk])
                ots, otsk = otsr.next()
                self.cp("act", ots[0:66, :], pO[0:66, :], [pOk], [otsk])
                for g in range(4):
                    self.tr(pO[:, g * 128:g * 128 + 66], ots[0:66, g * 128:(g + 1) * 128], [otsk], [pOk], kp=66)

            pts = {0: st_phase(0)}
            for kvh in range(4):
                if kvh + 1 < 4:
                    pts[kvh + 1] = st_phase(kvh + 1)
                if kvh == 1 and pend[0] is not None:
                    self.moe_prep_b(*pend[0])
                    pend[0] = None
                pv_phase(kvh, *pts[kvh])
            for kvh in range(4):
                pOv = self.ps[4 + kvh][:, :].rearrange("p (g t) -> p g t", g=4)
                self.tt("dve", den[:, kvh * 4:(kvh + 1) * 4], pOv[:, :, 64], esink[:, kvh * 4:(kvh + 1) * 4], ALU.add,
                        [self.psk[4 + kvh], "esink"], ["den"])
            self.S.op("dve", lambda e: e.reciprocal(out=den[:], in_=den[:]), ["den"], ["den"])
            for kvh in range(4):
                pOv = self.ps[4 + kvh][:, :].rearrange("p (g t) -> p g t", g=4)
                self.tt("dve", osb[:, kvh * 4:(kvh + 1) * 4, :], pOv[:, :, 0:64],
                        den[:, kvh * 4:(kvh + 1) * 4].unsqueeze(2).to_broadcast([128, 4, 64]), ALU.mult,
                        [self.psk[4 + kvh], "den"], ["osb"])
            osf = osb[:, :, :].rearrange("p h d -> p (h d)")
            for kc in range(8):
                self.tr(self.ps[kc // 4][:, (kc % 4) * 128:(kc % 4 + 1) * 128], osf[:, kc * 128:(kc + 1) * 128], ["osb"], [self.psk[kc // 4]])
            for b in range(2):
                self.cp("act", oT[:, b * 4:(b + 1) * 4, :], self.ps[b][:, :].rearrange("p (k t) -> p k t", k=4), [self.psk[b]], ["oT"])
            xm, xmk = xmr.next()
            for dh in range(2):
                pM, pMk = self.ps[2 + dh], self.psk[2 + dh]
                for kc in range(8):
                    self.mm(pM[:, :], oT[:, kc, :], wo[:, kc, dh * 512:(dh + 1) * 512], kc == 0, kc == 7, ["oT"] + wok, [pMk])
                self.tt("dve", xm[:, dh * 512:(dh + 1) * 512], pM[:, :], self.Gbc[:, 0, c, dh * 512:(dh + 1) * 512], ALU.mult,
                        [pMk, ("Gbc", 0, c)], [xmk])
            self.tt("dve", xm[:], xm[:], xt[:], ALU.add, [xmk, xk], [xmk])
            self.ld(self.xres[0][i * 128:(i + 1) * 128, :], xm[:], [xmk], [("xres0", i)])
            pend[0] = (i, c, xm, xmk, junk, ssr, wr, h2T, lg, mx, sm)
            self.moe_prep_a0(*pend[0])
        self.moe_prep_a(*pend[0], skip0=True)
        self.moe_prep_b(*pend[0])
        sca.close()
        sca_outer.close()

    def moe_prep_a0(self, i, c, xm, xmk, junk, ssr, wr, h2T, lg, mx, sm):
        ss, ssk = ssr.next()
        self.rstd_of(xm[:], xmk, junk[:], "junk", ss[:], ssk)
        self.ts("dve", junk[:], xm[:], ss[:, 0:1], None, ALU.mult, None, [xmk, ssk], ["junk"])
        self.ld(self.xn2[i * 128:(i + 1) * 128, :], junk[:], ["junk"], [("xn2", i)])

    def moe_prep_a(self, i, c, xm, xmk, junk, ssr, wr, h2T, lg, mx, sm, skip0=False):
        if not skip0:
            self.moe_prep_a0(i, c, xm, xmk, junk, ssr, wr, h2T, lg, mx, sm)
        for kc in range(8):
            self.tr(self.ps[kc // 4][:, (kc % 4) * 128:(kc % 4 + 1) * 128], junk[:, kc * 128:(kc + 1) * 128], ["junk"], [self.psk[kc // 4]])
        for kc in range(8):
            self.act(h2T[:, kc, :], self.ps[kc // 4][:, (kc % 4) * 128:(kc % 4 + 1) * 128], AF.Identity,
                     [self.psk[kc // 4], ("A", 4), "cols"], ["h2T"], scale=self.A2[:, kc, c:c + 1], bias=self.B2(kc, c))

    def moe_prep_b(self, i, c, xm, xmk, junk, ssr, wr, h2T, lg, mx, sm):
        pL, pLk = self.ps[1], self.psk[1]
        for kc in range(8):
            self.mm(pL[:, 0:NE], h2T[:, kc, :], wr[:, kc, :], kc == 0, kc == 7, ["h2T", "wr"], [pLk])
        self.S.op("dve", lambda e: e.reduce_max(out=mx[:], in_=pL[:, 0:NE], axis=AX.X), [pLk], ["mx"])
        self.ts("dve", mx[:], mx[:], -1.0, None, ALU.mult, None, ["mx"], ["mx"])
        self.act(lg[:], pL[:, 0:NE], AF.Exp, [pLk, "mx"], ["lg", "sm"], bias=mx[:, 0:1], accum_out=sm[:])
        self.S.op("dve", lambda e: e.reciprocal(out=sm[:], in_=sm[:]), ["sm"], ["sm"])
        self.ts("dve", self.aff[:, i, :], lg[:], sm[:, 0:1], None, ALU.mult, None, ["lg", "sm"], [("aff", i)])

    def moe_prep(self, *args):
        self.moe_prep_a(*args)
        self.moe_prep_b(*args)

    def pk(self, b, h):
        return ("ps", b)

    def deltanet_layer(self):
        nc, S, W = self.nc, self.S, self.W[1]
        cst, c2 = self.cst, self.cst2
        xin = self.xres[0]
        qT_d = self.dscratch("qT_d", [D, NTOK]); kT_d = self.dscratch("kT_d", [D, NTOK])
        ktok_d = self.dscratch("ktok_d", [NTOK, D]); vtok_d = self.dscratch("vtok_d", [NTOK, D])
        sz_d = self.dscratch("sz_d", [NTOK, D]); of_d = self.dscratch("of_d", [NLAT, D])
        self.dn_dbg = dict(qT_d=qT_d, kT_d=kT_d, ktok_d=ktok_d, vtok_d=vtok_d, sz_d=sz_d, of_d=of_d)
        ident = cst[:, C_ID:C_ID + 128]
        ones = cst[:, C_ONES:C_ONES + 128]
        zcol = cst[:, C_U:C_U + 1]
        scL = Scope(self)
        g_all = scL.sb("g_all", [128, NT, 16]); beta_all = scL.sb("beta_all", [128, NT, 16])
        lnb_all = scL.sb("lnb_all", [128, NT, 16]); ab_all = scL.sb("ab_all", [128, NT, 32])
        onesR = scL.sb("onesR", [128, 128], F32R)
        self.cp("dve", onesR[:], ones, ["cst"], ["onesR"])
        convT = scL.sb("convT", [128, 24, 3])
        self.ld(convT[:], W["convT"], [], ["convT"])
        allps = [self.pk(b, h) for b in range(8) for h in range(2)]

        def bankk(b):
            return [self.pk(b, 0), self.pk(b, 1)]

        scp = Scope(self)
        win = scp.sb("winqkv", [128, 8, 3072], F32R)
        wink = [("win", i) for i in range(6)]
        for i in range(6):
            self.ld(win[:, :, i * 512:(i + 1) * 512], W["w_in"][:, i * 512:(i + 1) * 512].rearrange("(k p) n -> p k n", p=128),
                    [], [wink[i]], q="pool")
        wnd = [scp.sb("wnd%d" % i, [128, 8, 258], F32R) for i in range(2)]
        xr = Ring(scp, "pxt", [128, D], F32, 2)
        ssr = Ring(scp, "pss", [128, 1], F32, 4)
        junk = scp.sb("pjunk", [128, D])
        c1r = Ring(scp, "pc1", [128, 256], F32, 3)
        sr = Ring(scp, "psl", [128, 256], F32, 8)
        sqr = Ring(scp, "psq", [128, 256], F32R, 3)
        rnr = Ring(scp, "prn", [128, 256], F32, 4)
        qnr = Ring(scp, "pqn", [128, 256], F32, 4)
        tkr = Ring(scp, "ptk", [128, 128], F32, 6)
        groups = [[32, 33]] + [[2 * g, 2 * g + 1] for g in range(16)]

        def tile_hT(jj, dst_fn, xr_, ssr_, junk_):
            c = 0 if jj < NT_LAT else 1
            xt, xk = xr_.next()
            self.ld(xt[:], xin[jj * 128:(jj + 1) * 128, :], [], [xk])
            ss, ssk = ssr_.next()
            self.rstd_of(xt[:], xk, junk_[:], "pjunk", ss[:], ssk)
            self.ts("dve", xt[:], xt[:], ss[:, 0:1], None, ALU.mult, None, [xk, ssk], [xk])
            for kc in range(8):
                b = kc // 4
                self.tr(self.ps[b][:, (kc % 4) * 128:(kc % 4 + 1) * 128], xt[:, kc * 128:(kc + 1) * 128], [xk], bankk(b))
            for kc in range(8):
                b = kc // 4
                dst, dk = dst_fn(kc)
                self.act(dst, self.ps[b][:, (kc % 4) * 128:(kc % 4 + 1) * 128], AF.Identity,
                         bankk(b) + [("A", 1), "cols"], [dk], scale=self.A1[:, kc, c:c + 1], bias=self.B1(kc, c))

        def prep_window(gi):
            buf = gi % 2
            for ti, jj in enumerate(groups[gi]):
                tile_hT(jj, lambda kc: (wnd[buf][:, kc, 1 + 128 * ti:1 + 128 * (ti + 1)], ("wnd", buf)), xr, ssr, junk)
            same_prev = gi >= 2
            if same_prev:
                self.cp("dve", wnd[buf][:, :, 0:1], wnd[1 - buf][:, :, 256:257], [("wnd", 1 - buf)], [("wnd", buf)])
                self.cp("dve", wnd[1 - buf][:, :, 257:258], wnd[buf][:, :, 1:2], [("wnd", buf)], [("wnd", 1 - buf)])
            else:
                self.cp("dve", wnd[buf][:, :, 0:1], zcol.unsqueeze(1).to_broadcast([128, 8, 1]), ["cst"], [("wnd", buf)])
                if gi >= 1:
                    self.cp("dve", wnd[1 - buf][:, :, 257:258], zcol.unsqueeze(1).to_broadcast([128, 8, 1]), ["cst"], [("wnd", 1 - buf)])

        pb_i = [0]
        pn_i = [0]

        def skew(n_items, stages):
            k = len(stages)
            for t in range(n_items + k - 1):
                for st_ in range(k - 1, -1, -1):
                    i_ = t - st_
                    if 0 <= i_ < n_items:
                        stages[st_](i_)

        def project(gi):
            buf = gi % 2
            wv, wvk = wnd[buf], ("wnd", buf)
            tok0 = groups[gi][0] * 128
            ctxs = [dict() for _ in range(24)]

            def s0(ch):
                c = ctxs[ch]
                b = pb_i[0] % 3
                pb_i[0] += 1
                c["pb"] = 2 + b
                pP = self.ps[2 + b]
                for kc in range(8):
                    self.mm(pP[:, 0:258], win[:, kc, ch * 128:(ch + 1) * 128], wv[:, kc, 0:258], kc == 0, kc == 7,
                            [wink[ch // 4], wvk], bankk(2 + b))

            def s1(ch):
                c = ctxs[ch]
                pb = c["pb"]
                pP = self.ps[pb]
                c1, c1k = c1r.next()
                self.ts("dve", c1[:], pP[:, 0:256], convT[:, ch, 0:1], None, ALU.mult, None, bankk(pb) + ["convT"], [c1k])
                self.stt("dve", c1[:], pP[:, 1:257], convT[:, ch, 1:2], c1[:], ALU.mult, ALU.add, bankk(pb) + ["convT", c1k], [c1k])
                self.stt("dve", c1[:], pP[:, 2:258], convT[:, ch, 2:3], c1[:], ALU.mult, ALU.add, bankk(pb) + ["convT", c1k], [c1k])
                c["sl"], c["slk"] = sr.next()
                self.act(c["sl"][:], c1[:], AF.Silu, [c1k], [c["slk"]])

            def s2(ch):
                c = ctxs[ch]
                if ch // 8 < 2:
                    sq, sqk = sqr.next()
                    self.act(sq[:], c["sl"][:], AF.Square, [c["slk"]], [sqk])
                    nb = 5 if (pn_i[0] % 2 == 0) else 7
                    pn_i[0] += 1
                    c["nb"] = nb
                    self.mm(self.ps[nb][:, 0:256], onesR[:], sq[:], True, True, ["onesR", sqk], bankk(nb))

            def s3(ch):
                c = ctxs[ch]
                if ch // 8 < 2:
                    c["rn"], c["rnk"] = rnr.next()
                    rn, rnk = c["rn"], c["rnk"]
                    self.ts("dve", rn[:], self.ps[c["nb"]][:, 0:256], EPS, None, ALU.add, None, bankk(c["nb"]), [rnk])
                    self.act(rn[:], rn[:], AF.Sqrt, [rnk], [rnk])

            def s4(ch):
                c = ctxs[ch]
                kind, h = ch // 8, ch % 8
                if kind < 2:
                    rn, rnk = c["rn"], c["rnk"]
                    S.op("dve", lambda e: e.reciprocal(out=rn[:], in_=rn[:]), [rnk], [rnk])
                    qn, qnk = qnr.next()
                    self.stt("dve", qn[:], c["sl"][:], (128.0 ** -0.5) if kind == 0 else 1.0, rn[:], ALU.mult, ALU.mult, [c["slk"], rnk], [qnk])
                    dst = qT_d if kind == 0 else kT_d
                    self.ld(dst[h * 128:(h + 1) * 128, tok0:tok0 + 256], qn[:], [qnk], [("qkT_d", kind, gi, h)])
                    c["src"], c["srck"] = qn, qnk
                else:
                    c["src"], c["srck"] = c["sl"], c["slk"]

            def s5(ch):
                c = ctxs[ch]
                if ch // 8 >= 1:
                    for ti in range(2):
                        self.tr(self.ps[6][:, ti * 128:(ti + 1) * 128], c["src"][:, ti * 128:(ti + 1) * 128], [c["srck"]], bankk(6))

            def s6(ch):
                c = ctxs[ch]
                kind, h = ch // 8, ch % 8
                if kind >= 1:
                    dstd = ktok_d if kind == 1 else vtok_d
                    for ti in range(2):
                        tk, tkk = tkr.next()
                        self.cp("act", tk[:], self.ps[6][:, ti * 128:(ti + 1) * 128], bankk(6), [tkk])
                        self.ld(dstd[tok0 + ti * 128:tok0 + (ti + 1) * 128, h * 128:(h + 1) * 128], tk[:], [tkk], [("tok_d", kind, gi, h, ti)])

            skew(24, [s0, s1, s2, s3, s4, s5, s6])

        for gi in range(len(groups)):
            prep_window(gi)
            if gi >= 1:
                project(gi - 1)
        lastb = (len(groups) - 1) % 2
        self.cp("dve", wnd[lastb][:, :, 257:258], zcol.unsqueeze(1).to_broadcast([128, 8, 1]), ["cst"], [("wnd", lastb)])
        project(len(groups) - 1)
        scp.close()

        scz = Scope(self)
        wz = scz.sb("winz", [128, 8, 1056], F32R)
        self.ld(wz[:, :, 0:528], W["w_in"][:, 3072:3600].rearrange("(k p) n -> p k n", p=128), [], [("wz", 0)], q="pool")
        self.ld(wz[:, :, 528:1056], W["w_in"][:, 3600:4128].rearrange("(k p) n -> p k n", p=128), [], [("wz", 1)], q="pool")
        wzk = [("wz", 0), ("wz", 1)]
        xr = Ring(scz, "zxt", [128, D], F32, 2)
        ssr = Ring(scz, "zss", [128, 1], F32, 4)
        junk = scz.sb("zjunk", [128, D])
        hTr = Ring(scz, "zhT", [128, 8, 128], F32R, 2)
        zr = Ring(scz, "zst", [128, D], F32, 2)
        for jj in range(NT):
            hT, hk = hTr.next()
            tile_hT(jj, lambda kc: (hT[:, kc, :], hk), xr, ssr, junk)
            for zh in range(2):
                for kc in range(8):
                    self.mm(self.ps[2 + zh][:, :], hT[:, kc, :], wz[:, kc, zh * 512:(zh + 1) * 512], kc == 0, kc == 7, [hk] + wzk, bankk(2 + zh))
            for kc in range(8):
                self.mm(self.ps[4][:, 0:32], hT[:, kc, :], wz[:, kc, 1024:1056], kc == 0, kc == 7, [hk] + wzk, bankk(4))
            zt, ztk = zr.next()
            for zh in range(2):
                self.act(zt[:, zh * 512:(zh + 1) * 512], self.ps[2 + zh][:, :], AF.Silu, bankk(2 + zh), [ztk])
            self.ld(sz_d[jj * 128:(jj + 1) * 128, :], zt[:], [ztk], [("sz_d", jj)])
            self.cp("dve", ab_all[:, jj, :], self.ps[4][:, 0:32], bankk(4), [("ab", jj)])
        abk = [("ab", jj) for jj in range(NT)]
        dtb = scz.sb("dtb", [128, 16]); nea = scz.sb("nea", [128, 16])
        self.ld(dtb[:], W["dt_bias"].partition_broadcast(128), [], ["dtb"])
        self.ld(nea[:], W["a_log"].partition_broadcast(128), [], ["nea"])
        self.act(nea[:], nea[:], AF.Exp, ["nea"], ["nea"])
        self.ts("dve", nea[:], nea[:], -1.0, None, ALU.mult, None, ["nea"], ["nea"])
        self.tt("dve", g_all[:], ab_all[:, :, 0:16], dtb[:].unsqueeze(1).to_broadcast([128, NT, 16]), ALU.add, abk + ["dtb"], ["g_all"])
        uu = scz.sb("sp_u", [128, NT, 16]); la = scz.sb("sp_la", [128, NT, 16])
        qq = scz.sb("sp_q", [128, NT, 16]); mk = scz.sb("sp_mk", [128, NT, 16])
        self.act(uu[:], g_all[:], AF.Exp, ["g_all"], ["sp_u"])
        self.act(la[:], uu[:], AF.Ln, ["sp_u"], ["sp_la"], bias=1.0)
        self.ts("dve", qq[:], uu[:], 1.0 / 7, None, ALU.mult, None, ["sp_u"], ["sp_q"])
        for cc_ in (-1.0 / 6, 1.0 / 5, -1.0 / 4, 1.0 / 3, -1.0 / 2, 1.0):
            self.stt("dve", qq[:], qq[:], cc_, uu[:], ALU.add, ALU.mult, ["sp_q", "sp_u"], ["sp_q"])
        self.ts("dve", mk[:], uu[:], 0.25, None, ALU.is_lt, None, ["sp_u"], ["sp_mk"])
        self.tt("dve", qq[:], qq[:], la[:], ALU.subtract, ["sp_q", "sp_la"], ["sp_q"])
        self.tt("dve", qq[:], qq[:], mk[:], ALU.mult, ["sp_q", "sp_mk"], ["sp_q"])
        self.tt("dve", g_all[:], la[:], qq[:], ALU.add, ["sp_la", "sp_q"], ["g_all"])
        self.tt("dve", g_all[:], g_all[:], nea[:].unsqueeze(1).to_broadcast([128, NT, 16]), ALU.mult, ["g_all", "nea"], ["g_all"])
        self.act(beta_all[:], ab_all[:, :, 16:32], AF.Sigmoid, abk, ["beta_all"])
        self.act(lnb_all[:], beta_all[:], AF.Ln, ["beta_all"], ["lnb_all"])
        scz.close()
        if self.stage >= 31:
            self.dn_scan(0, dict(qT_d=qT_d, kT_d=kT_d, ktok_d=ktok_d, vtok_d=vtok_d, sz_d=sz_d, of_d=of_d), g_all, beta_all, lnb_all)
        if self.stage >= 32:
            self.dn_scan(1, dict(qT_d=qT_d, kT_d=kT_d, ktok_d=ktok_d, vtok_d=vtok_d, sz_d=sz_d, of_d=of_d), g_all, beta_all, lnb_all)
        if self.debug:
            d_gates = self.nc.dram_tensor("d_gates", [128, 3, NT * 16], F32, kind="ExternalOutput").ap()
            for i, (t, k) in enumerate(((g_all, "g_all"), (beta_all, "beta_all"), (lnb_all, "lnb_all"))):
                self.ld(d_gates[:, i, :], t[:, :, :].rearrange("p j e -> p (j e)"), [k], [("d_gates", i)])
        scL.close()
        if self.stage >= 33:
            self.dn_out(of_d)

    def dn_scan(self, dr, dd, g_all, beta_all, lnb_all):
        nc, S, W = self.nc, self.S, self.W[1]
        cst, c2 = self.cst, self.cst2
        ident = cst[:, C_ID:C_ID + 128]
        ones = cst[:, C_ONES:C_ONES + 128]
        zcol = cst[:, C_U:C_U + 1]
        Ltri = c2[:, C2_LF:C2_LF + 128] if dr == 0 else c2[:, C2_LB:C2_LB + 128]
        LT, GT = c2[:, C2_LT:C2_LT + 128], c2[:, C2_GT:C2_GT + 128]
        GE, LE = c2[:, C2_GE:C2_GE + 128], c2[:, C2_LE:C2_LE + 128]
        m_db, m_dbt, m_dt = (LT, GT, GE) if dr == 0 else (GT, LT, LE)
        order = ([32, 33] + list(range(32))) if dr == 0 else ([33, 32] + list(range(31, -1, -1)))
        if self.n_tiles is not None:
            order = order[:self.n_tiles]
        sc = Scope(self)
        sfx = "_%d" % dr

        def bankk(b):
            return [self.pk(b, 0), self.pk(b, 1)]

        def zfill(t, k):
            sh = list(t.shape)
            self.cp("dve", t[:], zcol.to_broadcast(sh) if len(sh) == 2 else zcol.unsqueeze(1).to_broadcast(sh), ["cst"], [k])


        def z3(name, w, dt=F32R, fill=True):
            t = sc.sb(name + sfx, [128, 8, w], dt)
            if fill:
                for b_ in range(4):
                    self.cp("dve", t[:, 2 * b_:2 * b_ + 2, :], zcol.unsqueeze(1).to_broadcast([128, 2, w]), ["cst"], [(name, b_)])
            return t

        Sst = z3("S", 256)
        Xb = [z3("X0", 256)]
        qkT = z3("qkT", 128, fill=False)
        vb = z3("vb", 256); kbe = z3("kbe", 128, fill=False); kd = z3("kd", 128, fill=False)
        qeT = z3("qeT", 128, fill=False); usb = z3("usb", 128, F32, fill=False)
        wT = z3("wT", 128, fill=False); vnew = z3("vn", 256); Sdec = z3("Sdec", 128, F32, fill=False)
        Mf = z3("Mf", 128, F32, fill=False); Ao = z3("Ao", 128, F32, fill=False)
        Xp = [z3("Xa", 128, F32, fill=False), z3("Xb2", 128, F32, fill=False)]
        XPN = ["Xa", "Xb2"]
        ETf = z3("ETf", 128, F32, fill=False)
        Td = z3("Td", 128, F32, fill=False); Uf = z3("Uf", 128, F32, fill=False)
        negIf = sc.sb("negIf" + sfx, [128, 128])
        self.ts("dve", negIf[:], ident, -1.0, None, ALU.mult, None, ["cst"], ["negIf"])
        m64b = c2[:, C2_B64:C2_B64 + 128].unsqueeze(1).to_broadcast([128, 8, 128])
        nm64b = c2[:, C2_NB64:C2_NB64 + 128].unsqueeze(1).to_broadcast([128, 8, 128])
        offb = c2[:, C2_OFF:C2_OFF + 128].unsqueeze(1).to_broadcast([128, 8, 128])
        kqr = Ring(sc, "kq" + sfx, [128, 8, 256], F32R, 2)
        ktr = Ring(sc, "kt" + sfx, [128, D], F32, 1)
        vtr = Ring(sc, "vt" + sfx, [128, D], F32, 1)
        otr = Ring(sc, "ot" + sfx, [128, D], F32, 2)
        Dg = sc.sb("Dg" + sfx, [128, 2, 8, 128])
        DB = sc.sb("DB" + sfx, [128, 8, 128]); DBT = sc.sb("DBT" + sfx, [128, 8, 128])
        DT = sc.sb("DT" + sfx, [128, 8, 128]); Ec = sc.sb("Ec" + sfx, [128, 8, 128])
        gsm = sc.sb("gsm" + sfx, [128, 6, 8])
        if dr == 1:
            ofr = Ring(sc, "of" + sfx, [128, D], F32, 1)
            szr = Ring(sc, "sz" + sfx, [128, D], F32, 1)
            junk = sc.sb("bjunk", [128, D])
            ssq = sc.sb("ssq", [128, 8])
            onb = sc.sb("onb", [128, 128])
            self.ld(onb[:], W["o_norm"].partition_broadcast(128), [], ["onb"])
        NIT = 5
        P4 = range(4)

        def A(b_):
            return self.ps[b_][:, :].rearrange("p (s c) -> p s c", s=2), [("ps", b_)]

        def B(b_):
            return self.ps[4 + b_][:, :].rearrange("p (s c) -> p s c", s=2), [("ps", 4 + b_)]

        def keys(name):
            return [(name, b_) for b_ in P4]

        idb8 = ident.unsqueeze(1).to_broadcast([128, 8, 128])
        idb2 = ident.unsqueeze(1).to_broadcast([128, 2, 128])
        for jj in order:
            lat = jj < NT_LAT
            tsl = slice(jj * 128, (jj + 1) * 128)
            kq, kqk = kqr.next()
            self.ld(kq[:, :, 0:128], dd["kT_d"][:, tsl].rearrange("(h p) t -> p h t", p=128), [], [kqk + ("k",)], q="pool")
            kqkeys = [kqk + ("k",)]
            if lat:
                self.ld(kq[:, :, 128:256], dd["qT_d"][:, tsl].rearrange("(h p) t -> p h t", p=128), [], [kqk + ("q",)], q="pool")
                kqkeys.append(kqk + ("q",))
            kt, ktk = ktr.next()
            self.ld(kt[:], dd["ktok_d"][tsl, :], [], [ktk])
            vt, vtk = vtr.next()
            self.ld(vt[:], dd["vtok_d"][tsl, :], [], [vtk])
            kt3 = kt[:, :].rearrange("p (h d) -> p h d", h=8)
            vt3 = vt[:, :].rearrange("p (h d) -> p h d", h=8)
            gj = g_all[:, jj, dr * 8:(dr + 1) * 8]
            bj = beta_all[:, jj, dr * 8:(dr + 1) * 8]
            lbj = lnb_all[:, jj, dr * 8:(dr + 1) * 8]
            gk = ["g_all", "beta_all", "lnb_all"]
            pg = self.ps[0]
            self.mm(pg[:, 0:8], Ltri, gj, True, True, ["cst2"] + gk, bankk(0))
            self.mm(pg[:, 8:16], ones, gj, True, True, ["cst"] + gk, bankk(0))
            gc, gb, ebg, ekd, egl, tmpg = (gsm[:, i, :] for i in range(6))
            self.cp("act", gc, pg[:, 0:8], bankk(0), ["gsm"])
            self.tt("dve", gb, gc, lbj, ALU.add, ["gsm"] + gk, ["gsm"])
            self.act(ebg, gb, AF.Exp, ["gsm"], ["gsm"])
            self.tt("dve", tmpg, pg[:, 8:16], gc, ALU.subtract, bankk(0) + ["gsm"], ["gsm"])
            self.act(ekd, tmpg, AF.Exp, ["gsm"], ["gsm"])
            self.act(egl, pg[:, 8:16], AF.Exp, bankk(0), ["gsm"])
            self.tt("pool", Dg[:, 0, :, :], idb8, gc.unsqueeze(2).to_broadcast([128, 8, 128]), ALU.mult, ["cst", "gsm"], ["Dg0"])
            self.tt("pool", Dg[:, 1, :, :], idb8, gb.unsqueeze(2).to_broadcast([128, 8, 128]), ALU.mult, ["cst", "gsm"], ["Dg1"])
            for half in range(2):
                self.mm(self.ps[4 + half][:, :], ones, Dg[:, 0, half * 4:(half + 1) * 4, :], True, True, ["cst", "Dg0"], bankk(4 + half))
                self.mm(self.ps[6 + half][:, :], ones, Dg[:, 1, half * 4:(half + 1) * 4, :], True, True, ["cst", "Dg1"], bankk(6 + half))
            for half in range(2):
                hs = slice(half * 4, half * 4 + 4)
                pRc = self.ps[4 + half][:, :].rearrange("p (h f) -> p h f", h=4)
                pRb = self.ps[6 + half][:, :].rearrange("p (h f) -> p h f", h=4)
                gbb = gb[:, hs].unsqueeze(2).to_broadcast([128, 4, 128])
                gcb = gc[:, hs].unsqueeze(2).to_broadcast([128, 4, 128])
                self.tt("dve", DB[:, hs, :], gbb, pRc, ALU.subtract, ["gsm"] + bankk(4 + half), ["DB"])
                self.tt("pool", DB[:, hs, :], DB[:, hs, :], m_db.unsqueeze(1).to_broadcast([128, 4, 128]), ALU.add, ["DB", "cst2"], ["DB"])
                self.tt("dve", DBT[:, hs, :], pRb, gcb, ALU.subtract, ["gsm"] + bankk(6 + half), ["DBT"])
                self.tt("pool", DBT[:, hs, :], DBT[:, hs, :], m_dbt.unsqueeze(1).to_broadcast([128, 4, 128]), ALU.add, ["DBT", "cst2"], ["DBT"])
                if lat:
                    self.tt("dve", DT[:, hs, :], pRc, gcb, ALU.subtract, ["gsm"] + bankk(4 + half), ["DT"])
                    self.tt("pool", DT[:, hs, :], DT[:, hs, :], m_dt.unsqueeze(1).to_broadcast([128, 4, 128]), ALU.add, ["DT", "cst2"], ["DT"])
                    self.act(Ec[:, hs, :], pRc, AF.Exp, bankk(4 + half), ["Ec"])
            self.act(DB[:], DB[:], AF.Exp, ["DB"], ["DB"])
            self.act(DBT[:], DBT[:], AF.Exp, ["DBT"], ["DBT"])
            if lat:
                self.act(DT[:], DT[:], AF.Exp, ["DT"], ["DT"])
            ncol = 256 if lat else 128
            for b_ in P4:
                ap_, apk = A(b_)
                for s_ in range(2):
                    h = 2 * b_ + s_
                    self.mm(ap_[:, s_, 0:ncol], kq[:, h, 0:128], kq[:, h, 0:ncol], True, True, kqkeys, apk)
            for b_ in P4:
                ap_, apk = A(b_)
                hp = slice(2 * b_, 2 * b_ + 2)
                self.tt("dve", Mf[:, hp, :], ap_[:, :, 0:128], DB[:, hp, :], ALU.mult, apk + ["DB"], [("Mf", b_)])
                self.tt("dve", Xp[0][:, hp, :], ap_[:, :, 0:128], DBT[:, hp, :], ALU.mult, apk + ["DBT"], [("Xa", b_)])
                if lat:
                    self.tt("dve", qkT[:, hp, :], ap_[:, :, 128:256], DT[:, hp, :], ALU.mult, apk + ["DT"], [("qkT", b_)])
            self.tt("pool", Ao[:, :, :], Mf[:, :, :], offb, ALU.mult, keys("Mf") + ["cst2"], keys("Ao"))
            self.tt("pool", Mf[:, :, :], Mf[:, :, :], m64b, ALU.mult, keys("Mf") + keys("Ao") + ["cst2"], keys("Mf"))
            self.tt("pool", Mf[:, :, :], Mf[:, :, :], idb8, ALU.add, keys("Mf") + ["cst"], keys("Mf"))
            self.tt("pool", Xp[0][:, :, :], Xp[0][:, :, :], nm64b, ALU.mult, keys("Xa") + ["cst2"], keys("Xa"))
            self.tt("pool", Xp[0][:, :, :], Xp[0][:, :, :], idb8, ALU.add, keys("Xa") + ["cst"], keys("Xa"))
            cur = 0
            for it in range(NIT):
                src, srck = Xp[cur], XPN[cur]
                dst, dstk = Xp[1 - cur], XPN[1 - cur]
                for b_ in P4:
                    ap_, apk = A(b_)
                    for s_ in range(2):
                        h = 2 * b_ + s_
                        self.mm(ap_[:, s_, 0:128], src[:, h, :], Mf[:, h, :], True, True, [(srck, b_), ("Mf", b_)], apk)
                for b_ in P4:
                    ap_, apk = A(b_)
                    self.stt("dve", ETf[:, 2 * b_:2 * b_ + 2, :], ap_[:, :, 0:128], -1.0, idb2, ALU.mult, ALU.add, apk + ["cst"], [("ETf", b_)])
                for b_ in P4:
                    bp_, bpk = B(b_)
                    for s_ in range(2):
                        h = 2 * b_ + s_
                        self.mm(bp_[:, s_, 0:128], ETf[:, h, :], src[:, h, :], True, True, [("ETf", b_), (srck, b_)], bpk)
                for b_ in P4:
                    bp_, bpk = B(b_)
                    hp = slice(2 * b_, 2 * b_ + 2)
                    self.tt("dve", dst[:, hp, :], src[:, hp, :], bp_[:, :, 0:128], ALU.add, [(srck, b_)] + bpk, [(dstk, b_)])
                cur = 1 - cur
            Xd, Xdk = Xp[cur], XPN[cur]
            for b_ in P4:
                ap_, apk = A(b_)
                bp_, bpk = B(b_)
                for s_ in range(2):
                    h = 2 * b_ + s_
                    self.tr(ap_[:, s_, 0:128], Xd[:, h, :], [(Xdk, b_)], apk)
                    self.mm(bp_[:, s_, 0:128], Ao[:, h, :], Xd[:, h, :], True, True, [("Ao", b_), (Xdk, b_)], bpk)
            for b_ in P4:
                ap_, apk = A(b_)
                bp_, bpk = B(b_)
                hp = slice(2 * b_, 2 * b_ + 2)
                self.cp("act", Td[:, hp, :], ap_[:, :, 0:128], apk, [("Td", b_)])
                self.cp("act", Uf[:, hp, :], bp_[:, :, 0:128], bpk, [("Uf", b_)])
            for b_ in P4:
                ap_, apk = A(b_)
                for s_ in range(2):
                    h = 2 * b_ + s_
                    self.mm(ap_[:, s_, 0:128], Td[:, h, :], Uf[:, h, :], True, True, [("Td", b_), ("Uf", b_)], apk)
            for b_ in P4:
                ap_, apk = A(b_)
                hp = slice(2 * b_, 2 * b_ + 2)
                self.tt("dve", Xb[0][:, hp, 0:128], Xd[:, hp, :], ap_[:, :, 0:128], ALU.subtract, [(Xdk, b_)] + apk, [("X0", b_)])
            cur = 0
            XN = ["X0"]
            Xf, kf_ = Xb[cur], XN[cur]
            self.tt("dve", vb[:, :, 0:128], vt3, bj.unsqueeze(2).to_broadcast([128, 8, 128]), ALU.mult, [vtk] + gk, keys("vb"))
            self.tt("pool", kbe[:, :, :], kt3, ebg.unsqueeze(2).to_broadcast([128, 8, 128]), ALU.mult, [ktk, "gsm"], keys("kbe"))
            self.tt("pool", kd[:, :, :], kt3, ekd.unsqueeze(2).to_broadcast([128, 8, 128]), ALU.mult, [ktk, "gsm"], keys("kd"))
            if lat:
                self.tt("pool", qeT[:, :, :], kq[:, :, 128:256], Ec[:, :, :], ALU.mult, kqkeys + ["Ec"], keys("qeT"))
            for b_ in P4:
                ap_, apk = A(b_)
                bp_, bpk = B(b_)
                for s_ in range(2):
                    h = 2 * b_ + s_
                    self.mm(ap_[:, s_, :], Xf[:, h, 0:128], vb[:, h, :], True, True, [(kf_, b_), ("vb", b_)], apk)
                    self.mm(bp_[:, s_, :], kbe[:, h, :], Xf[:, h, :], True, True, [(kf_, b_), ("kbe", b_)], bpk)
            for b_ in P4:
                ap_, apk = A(b_)
                bp_, bpk = B(b_)
                hp = slice(2 * b_, 2 * b_ + 2)
                self.cp("act", usb[:, hp, :], ap_[:, :, 0:128], apk, [("usb", b_)])
                self.cp("act", wT[:, hp, :], bp_[:, :, 0:128], bpk, [("wT", b_)])
            ot, otk = otr.next()
            ot3 = ot[:, :].rearrange("p (h d) -> p h d", h=8)
            for b_ in P4:
                ap_, apk = A(b_)
                for s_ in range(2):
                    h = 2 * b_ + s_
                    self.mm(ap_[:, s_, :], wT[:, h, :], Sst[:, h, :], True, True, [("wT", b_), ("S", b_)], apk)
            for b_ in P4:
                ap_, apk = A(b_)
                hp = slice(2 * b_, 2 * b_ + 2)
                self.tt("dve", vnew[:, hp, 0:128], usb[:, hp, :], ap_[:, :, 0:128], ALU.subtract, [("usb", b_)] + apk, [("vn", b_)])
            for b_ in P4:
                ap_, apk = A(b_)
                bp_, bpk = B(b_)
                for s_ in range(2):
                    h = 2 * b_ + s_
                    if lat:
                        self.mm(bp_[:, s_, :], qeT[:, h, :], Sst[:, h, :], True, False, [("qeT", b_), ("S", b_)], bpk)
                        self.mm(bp_[:, s_, :], qkT[:, h, :], vnew[:, h, :], False, True, [("qkT", b_), ("vn", b_)], bpk)
                    self.mm(ap_[:, s_, :], kd[:, h, :], vnew[:, h, :], True, True, [("kd", b_), ("vn", b_)], apk)
            self.tt("pool", Sdec[:, :, :], Sst[:, :, 0:128], egl.unsqueeze(2).to_broadcast([128, 8, 128]), ALU.mult,
                    keys("S") + ["gsm"], keys("Sdec"))
            for b_ in P4:
                ap_, apk = A(b_)
                bp_, bpk = B(b_)
                hp = slice(2 * b_, 2 * b_ + 2)
                if lat:
                    self.cp("act", ot3[:, hp, :], bp_[:, :, 0:128], bpk, [otk])
                self.tt("dve", Sst[:, hp, 0:128], Sdec[:, hp, :], ap_[:, :, 0:128], ALU.add, [("Sdec", b_)] + apk, [("S", b_)])
            if not lat:
                continue
            if dr == 0:
                self.ld(dd["of_d"][tsl, :], ot[:], [otk], [("of_d", jj)])
            else:
                if self.debug:
                    if not hasattr(self, "ob_d"):
                        self.ob_d = self.nc.dram_tensor("ob_d", [NLAT, D], F32, kind="ExternalOutput").ap()
                    self.ld(self.ob_d[tsl, :], ot[:], [otk], [("ob_d", jj)])
                of, ofk = ofr.next()
                self.ld(of[:], dd["of_d"][tsl, :], [("of_d", jj)], [ofk])
                szt, szk = szr.next()
                self.ld(szt[:], dd["sz_d"][tsl, :], [], [szk])
                self.tt("pool", ot[:], ot[:], of[:], ALU.add, [otk, ofk], [otk])
                self.act(junk[:], ot[:], AF.Square, [otk], ["bjunk"])
                S.op("dve", lambda e: e.tensor_reduce(out=ssq[:], in_=junk[:, :].rearrange("p (h d) -> p h d", h=8), axis=AX.X, op=ALU.add),
                     ["bjunk"], ["ssq"])
                self.ts("dve", ssq[:], ssq[:], 1.0 / 128, EPS, ALU.mult, ALU.add, ["ssq"], ["ssq"])
                self.act(ssq[:], ssq[:], AF.Sqrt, ["ssq"], ["ssq"])
                S.op("dve", lambda e: e.reciprocal(out=ssq[:], in_=ssq[:]), ["ssq"], ["ssq"])
                o3 = ot[:, :].rearrange("p (h d) -> p h d", h=8)
                self.tt("dve", o3, o3, ssq[:].unsqueeze(2).to_broadcast([128, 8, 128]), ALU.mult, [otk, "ssq"], [otk])
                self.tt("pool", o3, o3, onb[:].unsqueeze(1).to_broadcast([128, 8, 128]), ALU.mult, [otk, "onb"], [otk])
                self.tt("pool", ot[:], ot[:], szt[:], ALU.mult, [otk, szk], [otk])
                self.ld(dd["of_d"][tsl, :], ot[:], [otk, ofk], [("of_d", jj)])
        sc.close()

    def dn_out(self, of_d):
        nc, S, W = self.nc, self.S, self.W[1]
        sc = Scope(self)
        wo = sc.sb("wo1", [128, 8, D], F32R)
        self.ld(wo[:], W["w_o"].rearrange("(k p) n -> p k n", p=128), [], ["wo1"], q="pool")
        wr = sc.sb("wr1", [128, 8, NE])
        self.ld(wr[:], W["router"].rearrange("(k p) n -> p k n", p=128), [], ["wr"])
        yr = Ring(sc, "oy", [128, D], F32, 2)
        xr = Ring(sc, "ox", [128, D], F32, 2)
        xmr = Ring(sc, "oxm", [128, D], F32, 3)
        ssr = Ring(sc, "oss", [128, 1], F32, 4)
        junk = sc.sb("ojunk", [128, D])
        yT = sc.sb("oyT", [128, 8, 128], F32R)
        h2T = sc.sb("oh2T", [128, 8, 128])
        lg = sc.sb("olg", [128, NE]); mx = sc.sb("omx", [128, 1]); sm = sc.sb("osm", [128, 1])
        pend = [None]
        for i in range(NT_LAT):
            yt, ytk = yr.next()
            self.ld(yt[:], of_d[i * 128:(i + 1) * 128, :], [], [ytk])
            xt, xk = xr.next()
            self.ld(xt[:], self.xres[0][i * 128:(i + 1) * 128, :], [], [xk])
            for kc in range(8):
                self.tr(self.ps[kc // 4][:, (kc % 4) * 128:(kc % 4 + 1) * 128], yt[:, kc * 128:(kc + 1) * 128], [ytk], [self.psk[kc // 4]])
            for b in range(2):
                self.cp("act", yT[:, b * 4:(b + 1) * 4, :], self.ps[b][:, :].rearrange("p (k t) -> p k t", k=4), [self.psk[b]], ["yT"])
            if pend[0] is not None:
                self.moe_prep(*pend[0])
                pend[0] = None
            xm, xmk = xmr.next()
            for dh in range(2):
                pM, pMk = self.ps[2 + dh], self.psk[2 + dh]
                for kc in range(8):
                    self.mm(pM[:, :], yT[:, kc, :], wo[:, kc, dh * 512:(dh + 1) * 512], kc == 0, kc == 7, ["yT", "wo1"], [pMk])
                self.tt("dve", xm[:, dh * 512:(dh + 1) * 512], pM[:, :], self.Gbc[:, 0, 0, dh * 512:(dh + 1) * 512], ALU.mult,
                        [pMk, ("Gbc", 0, 0)], [xmk])
            self.tt("dve", xm[:], xm[:], xt[:], ALU.add, [xmk, xk], [xmk])
            self.ld(self.xres[1][i * 128:(i + 1) * 128, :], xm[:], [xmk], [("xres1", i)])
            pend[0] = (i, 0, xm, xmk, junk, ssr, wr, h2T, lg, mx, sm)
        self.moe_prep(*pend[0])
        sc.close()

    def final_norm(self):
        nc, S = self.nc, self.S
        sc = Scope(self)
        fn = sc.sb("fnb", [128, D])
        self.ld(fn[:], self.inp["final_norm"].partition_broadcast(128), [], ["fnb"])
        xr = Ring(sc, "fx", [128, D], F32, 3)
        ssr = Ring(sc, "fss", [128, 1], F32, 4)
        junk = sc.sb("fjunk", [128, D])
        for i in range(NT_LAT):
            xt, xk = xr.next()
            self.ld(xt[:], self.xres[1][i * 128:(i + 1) * 128, :], [], [xk])
            ss, ssk = ssr.next()
            self.rstd_of(xt[:], xk, junk[:], "fjunk", ss[:], ssk)
            self.stt("dve", xt[:], xt[:], ss[:, 0:1], fn[:], ALU.mult, ALU.mult, [xk, ssk, "fnb"], [xk])
            S.dma("sp", lambda e: e.dma_start(out=self.out[i * 128:(i + 1) * 128, :], in_=xt[:]), [xk], [("out", i)], is_output=True)
        sc.close()

    def moe(self, l, xres, xresk, with_ctx):
        nc, S, W = self.nc, self.S, self.W[l]
        cst = self.cst
        ones = cst[:, C_ONES:C_ONES + 128]
        Umat = cst[:, C_U:C_U + 128]
        iota = cst[:, C_IOTA:C_IOTA + 512]
        sets = [(0, 0, NT_LAT, CAP_LAT)] + ([(1, NT_LAT, NT_CTX, CAP_CTX)] if with_ctx else [])
        sc = Scope(self)
        slot_m = {}; meta = {}
        for (si, j0, nj, cap) in sets:
            slot_m[si] = sc.sb("slotm%d_%d" % (si, l), [128, nj, NE])
            meta[si] = sc.sb("meta%d_%d" % (si, l), [128, nj, NE, 4], F32R)
        scr = Scope(self)
        for (si, j0, nj, cap) in sets:
            sfx = "%d_%d" % (si, l)
            affv = self.aff[:, j0:j0 + nj, :]
            affk = [("aff", j) for j in range(j0, j0 + nj)]
            lo = scr.sb("lo" + sfx, [128, NE]); mid = scr.sb("mid" + sfx, [128, NE])
            cmpt = scr.sb("cmp" + sfx, [128, nj, NE]); cnt = scr.sb("cnt" + sfx, [128, NE])
            tq = scr.sb("tq" + sfx, [128, NE])
            offs = scr.sb("offs" + sfx, [128, nj, NE]); slot = scr.sb("slot" + sfx, [128, nj, NE])
            S.op("dve", lambda e: e.memset(lo[:], 0.0), [], ["lo"])
            S.op("dve", lambda e: e.memset(mid[:], 0.5), [], ["mid"])
            pC, pCk = self.ps[0], self.psk[0]
            for it in range(NBIS):
                w = 2.0 ** -(it + 1)
                self.tt("dve", cmpt[:], affv, mid[:].unsqueeze(1).to_broadcast([128, nj, NE]), ALU.is_ge, affk + ["mid"], ["cmp"])
                S.op("dve", lambda e: e.tensor_reduce(out=cnt[:], in_=cmpt[:, :, :].rearrange("p j e -> p e j"), axis=AX.X, op=ALU.add),
                     ["cmp"], ["cnt"])
                self.mm(pC[:, 0:NE], ones, cnt[:], True, True, ["cst", "cnt"], [pCk])
                self.ts("dve", tq[:], pC[:, 0:NE], cap - 0.5, w, ALU.is_ge, ALU.mult, [pCk], ["tq"])
                self.tt("dve", lo[:], lo[:], tq[:], ALU.add, ["lo", "tq"], ["lo"])
                self.ts("dve", mid[:], lo[:], w * 0.5, None, ALU.add, None, ["lo"], ["mid"])
            self.tt("dve", cmpt[:], affv, lo[:].unsqueeze(1).to_broadcast([128, nj, NE]), ALU.is_ge, affk + ["lo"], ["cmp"])
            pP, pPk = self.ps[1], self.psk[1]
            pT, pTk = self.ps[2], self.psk[2]
            mflat = cmpt[:, :, :].rearrange("p j e -> p (j e)")
            self.mm(pP[:, 0:nj * NE], Umat, mflat, True, True, ["cst", "cmp"], [pPk])
            self.mm(pT[:, 0:nj * NE], ones, mflat, True, True, ["cst", "cmp"], [pTk])
            pTv = pT[:, 0:nj * NE].rearrange("p (j e) -> p j e", e=NE)
            pPv = pP[:, 0:nj * NE].rearrange("p (j e) -> p j e", e=NE)
            S.op("dve", lambda e: e.memset(offs[:, 0, :], 0.0), [], ["offs"])
            for j in range(1, nj):
                self.tt("dve", offs[:, j, :], pTv[:, j - 1, :], offs[:, j - 1, :], ALU.add, [pTk, "offs"], ["offs"])
            self.tt("dve", slot[:], pPv, offs[:], ALU.add, [pPk, "offs"], ["slot"])
            self.ts("dve", cmpt[:], cmpt[:], -1.0e6, 1.0e6, ALU.mult, ALU.add, ["cmp"], ["cmp"])
            self.tt("dve", slot_m[si][:], slot[:], cmpt[:], ALU.add, ["slot", "cmp"], [("slotm", si)])
            mt = meta[si]
            for j in range(nj):
                self.cp("dve", mt[:, j, :, 0:1], cst[:, C_U:C_U + 1].unsqueeze(1).to_broadcast([128, NE, 1]), ["cst"], [("meta", si)])
                self.ts("dve", mt[:, j, :, 0:1], mt[:, j, :, 0:1], float(j0 + j), None, ALU.add, None, [("meta", si)], [("meta", si)])
            mtf = mt[:, :, :, :].rearrange("p j e c -> p (j e) c")
            self.cp("dve", mtf[:, :, 1:2], cst[:, C_PIDX:C_PIDX + 1].unsqueeze(1).to_broadcast([128, nj * NE, 1]), ["cst"], [("meta", si)])
            self.cp("dve", mtf[:, :, 2:3], affv.rearrange("p j e -> p (j e)").unsqueeze(2), affk, [("meta", si)])
            self.cp("dve", mtf[:, :, 3:4], cst[:, C_ONES:C_ONES + 1].unsqueeze(1).to_broadcast([128, nj * NE, 1]), ["cst"], [("meta", si)])
        scr.close()

        NW = 8
        wring = Ring(sc, "wm%d" % l, [128, 8, 256], F32R, NW)
        xsT = sc.sb("xsT%d" % l, [128, 8, 640], F32R)
        hidT = sc.sb("hidT%d" % l, [128, 16, 544], F32R)
        ysb = sc.sb("ysb%d" % l, [128, 5, D])
        xsr = Ring(sc, "xstok%d" % l, [128, D], F32, 2)
        selr = Ring(sc, "sel%d" % l, [128, 512], F32R, 2)
        selc = sc.sb("selc%d" % l, [128, 128], F32R)
        sgr = Ring(sc, "sg%d" % l, [128, 512], F32, 2)
        hcr = Ring(sc, "hc%d" % l, [32, 256], F32, 2)
        idxrow = sc.sb("idxrow%d" % l, [4, 640])
        metac = sc.sb("metac%d" % l, [128, 5, 4])
        tmp5 = sc.sb("tmp5%d" % l, [128, 5]); idxf = sc.sb("idxf%d" % l, [128, 5])
        idur = Ring(sc, "idu%d" % l, [128, 5], U32, 3)
        gcr = Ring(sc, "gc%d" % l, [128, 5], F32, 3)
        self.cp("dve", selc[:], cst[:, C_U:C_U + 1].to_broadcast([128, 128]), ["cst"], ["selc"])
        S.op("dve", lambda e: e.memset(ysb[:], 0.0), [], ["ysb"])
        nk = 5 if with_ctx else 4
        c2 = {0: 0, 1: 1}

        def idx_phase(e):
            pI, pIk = self.ps[7], self.psk[7]
            for (si, j0, nj, cap) in sets:
                ncol = 512 if si == 0 else 128
                for j in range(nj):
                    if si == 0:
                        sel, selk = selr.next()
                        self.ts("dve", sel[:], iota, slot_m[si][:, j, e:e + 1], None, ALU.is_equal, None, ["cst", ("slotm", si)], [selk])
                        rhs = sel[:]
                    else:
                        selk = "selc"
                        self.ts("dve", selc[:, 0:CAP_CTX], iota[:, 0:CAP_CTX], slot_m[si][:, j, e:e + 1], None, ALU.is_equal, None,
                                ["cst", ("slotm", si)], [selk])
                        rhs = selc[:]
                    self.mm(pI[0:4, 0:ncol], meta[si][:, j, e, :], rhs, j == 0, j == nj - 1, [("meta", si), selk], [pIk])
                off = 0 if si == 0 else 512
                self.cp("act", idxrow[0:4, off:off + ncol], pI[0:4, 0:ncol], [pIk], ["idxrow"])
            pX, pXk = self.ps[7], self.psk[7]
            for k in range(nk):
                self.tr(pX[:, k * 4:(k + 1) * 4], idxrow[0:4, k * 128:(k + 1) * 128], ["idxrow"], [pXk], kp=4)
            self.cp("act", metac[:, 0:nk, :], pX[:, 0:nk * 4].rearrange("p (k c) -> p k c", c=4), [pXk], ["metac"])
            idu, iduk = idur.next()
            gc, gck = gcr.next()
            self.stt("dve", idxf[:, 0:nk], metac[:, 0:nk, 0], 128.0, metac[:, 0:nk, 1], ALU.mult, ALU.add, ["metac"], ["idxf"])
            self.ts("dve", tmp5[:, 0:nk], metac[:, 0:nk, 3], -1.0, 1.0, ALU.mult, ALU.add, ["metac"], ["tmp5"])
            self.tt("dve", tmp5[:, 0:nk], tmp5[:, 0:nk], cst[:, C_DMY:C_DMY + nk], ALU.mult, ["tmp5", "cst"], ["tmp5"])
            self.tt("dve", idxf[:, 0:nk], idxf[:, 0:nk], tmp5[:, 0:nk], ALU.add, ["idxf", "tmp5"], ["idxf"])
            self.cp("dve", idu[:, 0:nk], idxf[:, 0:nk], ["idxf"], [iduk])
            self.cp("dve", gc[:, 0:nk], metac[:, 0:nk, 2], ["metac"], [gck])
            return (idu, iduk, gc, gck)

        gt_i = [0]

        def gather_phase(ix):
            idu, iduk, gc, gck = ix
            for k in range(nk):
                xs, xsk = xsr.next()
                S.dma("pool", lambda e: e.indirect_dma_start(out=xs[:], out_offset=None, in_=self.xn2[:, :],
                                                              in_offset=bass.IndirectOffsetOnAxis(ap=idu[:, k:k + 1], axis=0)),
                      [iduk], [xsk])
                c = 1 if k == 4 else 0
                for half in range(2):
                    bnk = gt_i[0] % 4
                    gt_i[0] += 1
                    pt, ptk = self.ps[bnk], self.psk[bnk]
                    for kc in range(half * 4, half * 4 + 4):
                        self.tr(pt[:, (kc % 4) * 128:(kc % 4 + 1) * 128], xs[:, kc * 128:(kc + 1) * 128], [xsk], [ptk])
                    for kc in range(half * 4, half * 4 + 4):
                        self.act(xsT[:, kc, k * 128:(k + 1) * 128], pt[:, (kc % 4) * 128:(kc % 4 + 1) * 128], AF.Identity,
                                 [ptk, ("A", 4), "cols"], ["xsT"], scale=self.A2[:, kc, c:c + 1], bias=self.B2(kc, c))

        def wload(src_ap):
            wt, wk = wring.next()
            self.ld(wt[:], src_ap, [], [wk], q="pool")
            return wt, wk

        gu_i = [0]

        cpend = [None]

        def ctx_tr(hc, hck, fq):
            pt, ptk = self.ps[7], self.psk[7]
            for fc in range(2):
                self.tr(pt[:, fc * 32:(fc + 1) * 32], hc[0:32, fc * 128:(fc + 1) * 128], [hck], [ptk], kp=32)
            self.cp("act", hidT[:, fq * 2:fq * 2 + 2, 512:544], pt[:, 0:64].rearrange("p (a t) -> p a t", a=2), [ptk],
                    [("hidT", fq * 2), ("hidT", fq * 2 + 1)])

        def ffn1(e):
            for fq in range(8):
                wg, wgk = wload(W["w_gate"][e, :, fq * 256:(fq + 1) * 256].rearrange("(k p) n -> p k n", p=128))
                wu, wuk = wload(W["w_up"][e, :, fq * 256:(fq + 1) * 256].rearrange("(k p) n -> p k n", p=128))
                for fc in range(2):
                    fcc = fq * 2 + fc
                    b = gu_i[0] % 2
                    gu_i[0] += 1
                    pG, pGk = self.ps[2 * b], self.psk[2 * b]
                    pU, pUk = self.ps[2 * b + 1], self.psk[2 * b + 1]
                    for kc in range(8):
                        self.mm(pG[:, :], wg[:, kc, fc * 128:(fc + 1) * 128], xsT[:, kc, 0:512], kc == 0, kc == 7, [wgk, "xsT"], [pGk])
                    for kc in range(8):
                        self.mm(pU[:, :], wu[:, kc, fc * 128:(fc + 1) * 128], xsT[:, kc, 0:512], kc == 0, kc == 7, [wuk, "xsT"], [pUk])
                    sg, sgk = sgr.next()
                    self.act(sg[:, 0:512], pG[:, :], AF.Silu, [pGk], [sgk])
                    self.tt("dve", hidT[:, fcc, 0:512], sg[:, 0:512], pU[:, :], ALU.mult, [sgk, pUk], [("hidT", fcc)])
                if with_ctx:
                    pc, pck = self.ps[4], self.psk[4]
                    for kc in range(8):
                        self.mm(pc[0:32, 0:256], xsT[:, kc, 512:544], wg[:, kc, :], kc == 0, kc == 7, [wgk, "xsT"], [pck])
                    for kc in range(8):
                        self.mm(pc[0:32, 256:512], xsT[:, kc, 512:544], wu[:, kc, :], kc == 0, kc == 7, [wuk, "xsT"], [pck])
                    hc, hck = hcr.next()
                    self.act(hc[0:32, 0:256], pc[0:32, 0:256], AF.Silu, [pck], [hck])
                    self.tt("dve", hc[0:32, 0:256], hc[0:32, 0:256], pc[0:32, 256:512], ALU.mult, [hck, pck], [hck])
                    if cpend[0] is not None:
                        ctx_tr(*cpend[0])
                    cpend[0] = (hc, hck, fq)
            if cpend[0] is not None:
                ctx_tr(*cpend[0])
                cpend[0] = None

        y_i = [0]

        def ffn2(e, ix):
            idu, iduk, gc, gck = ix
            hk = [("hidT", f) for f in range(16)]
            for dq in range(4):
                wd = []
                for fh in range(2):
                    wd.append(wload(W["w_down"][e, fh * 1024:(fh + 1) * 1024, dq * 256:(dq + 1) * 256].rearrange("(k p) n -> p k n", p=128)))
                for k in range(nk):
                    if k < 4:
                        b = y_i[0] % 2
                        y_i[0] += 1
                        pY, pYk = self.ps[5 + b], self.psk[5 + b]
                        rows = 128
                        lsl = slice(k * 128, (k + 1) * 128)
                    else:
                        pY, pYk = self.ps[4], self.psk[4]
                        rows = 32
                        lsl = slice(512, 544)
                    for fcc in range(16):
                        wt, wk = wd[fcc // 8]
                        self.mm(pY[0:rows, 0:256], hidT[:, fcc, lsl], wt[:, fcc % 8, :], fcc == 0, fcc == 15, [("hidT", fcc), wk], [pYk])
                    c = 1 if k == 4 else 0
                    self.stt("dve", ysb[0:rows, k, dq * 256:(dq + 1) * 256], pY[0:rows, 0:256], gc[0:rows, k:k + 1],
                             self.Gbc[0:rows, 1, c, dq * 256:(dq + 1) * 256], ALU.mult, ALU.mult,
                             [pYk, gck, ("Gbc", 1, c)], [("ysb", k)])

        def scatter_phase(ix):
            idu, iduk, gc, gck = ix
            for k in range(nk):
                S.dma("pool", lambda e: e.indirect_dma_start(out=xres[:, :], out_offset=bass.IndirectOffsetOnAxis(ap=idu[:, k:k + 1], axis=0),
                                                              in_=ysb[:, k, :], in_offset=None, compute_op=ALU.add),
                      [iduk, ("ysb", k), "ysb"], ["xacc"])

        n_exp = NE if self.n_exp is None else self.n_exp
        ixs = {0: idx_phase(0)}
        gather_phase(ixs[0])
        for e in range(n_exp):
            if e + 1 < n_exp:
                ixs[e + 1] = idx_phase(e + 1)
            ffn1(e)
            if e > 0:
                scatter_phase(ixs[e - 1])
            if e + 1 < n_exp:
                gather_phase(ixs[e + 1])
            ffn2(e, ixs[e])
        scatter_phase(ixs[n_exp - 1])
        sc.close()


def _host_consts():
    cp = np.zeros((128, C_END), np.float32)
    cp[:, C_ID:C_ID + 128] = np.eye(128, dtype=np.float32)
    cp[:, C_ONES:C_ONES + 128] = 1.0
    pi = np.arange(128)
    cp[:, C_U:C_U + 128] = (pi[:, None] < pi[None, :]).astype(np.float32)
    mp = (pi[:, None] >= pi[None, :]).astype(np.float32)
    mn = (pi[:, None] <= pi[None, :]).astype(np.float32)
    cp[:, C_MP:C_MP + 512] = np.tile(mp, (1, 4))
    cp[:, C_MN:C_MN + 512] = np.tile(mn, (1, 4))
    cp[:, C_IOTA:C_IOTA + 512] = np.arange(512, dtype=np.float32)[None, :]
    cp[:, C_PIDX] = pi
    for k in range(5):
        cp[:, C_DMY + k] = NTOK + k * 128 + pi
    t = np.arange(NLAT)
    row = (t // 64).astype(np.float32)
    col = (t % 64).astype(np.float32)
    inv = (np.float32(10000.0) ** (-np.arange(16, dtype=np.float32) / np.float32(16))).astype(np.float32)
    ar = (row[:, None] * inv[None, :]).astype(np.float32)
    ac = (col[:, None] * inv[None, :]).astype(np.float32)
    rope = np.concatenate([np.cos(ar), np.cos(ac), np.sin(ar), np.sin(ac)], axis=1).astype(np.float32)
    return cp, rope


def _host_consts2():
    c2 = np.zeros((128, C2_END), np.float32)
    p = np.arange(128)[:, None]
    f = np.arange(128)[None, :]
    c2[:, C2_LF:C2_LF + 128] = (p <= f)
    c2[:, C2_LB:C2_LB + 128] = (p >= f)
    c2[:, C2_LT:C2_LT + 128] = np.where(f < p, 0.0, NEG)
    c2[:, C2_GT:C2_GT + 128] = np.where(f > p, 0.0, NEG)
    c2[:, C2_GE:C2_GE + 128] = np.where(f >= p, 0.0, NEG)
    c2[:, C2_LE:C2_LE + 128] = np.where(f <= p, 0.0, NEG)
    same = ((p // 64) == (f // 64)).astype(np.float32)
    c2[:, C2_B64:C2_B64 + 128] = same
    c2[:, C2_NB64:C2_NB64 + 128] = -same
    c2[:, C2_OFF:C2_OFF + 128] = 1.0 - same
    return c2


def _colT(v, n):
    return np.ascontiguousarray(np.asarray(v, np.float32).reshape(n, 128).T)


def make_in_maps(inputs, cores):
    cp, rope = _host_consts()
    shared = {"cpack": cp, "rope": rope}
    for l in (0, 1):
        p = "l%d_" % l
        shared[p + "ada_w"] = np.asarray(inputs[p + "ada_w"], np.float32)
        shared[p + "ada_b"] = np.asarray(inputs[p + "ada_b"], np.float32)
        shared[p + "ada_bT"] = _colT(inputs[p + "ada_b"], 48)
        shared[p + "nmixT"] = _colT(inputs[p + "norm_mix"], 8)
        shared[p + "nffnT"] = _colT(inputs[p + "norm_ffn"], 8)
        for n in ("router", "w_gate", "w_up", "w_down"):
            shared[p + n] = np.asarray(inputs[p + n], np.float32)
    for n in ("l0_sink", "l0_w_o", "l1_w_in", "l1_o_norm", "l1_w_o", "final_norm"):
        shared[n] = np.asarray(inputs[n], np.float32)
    shared["l1_a_log"] = np.asarray(inputs["l1_a_log"], np.float32).reshape(16)
    shared["l1_dt_bias"] = np.asarray(inputs["l1_dt_bias"], np.float32).reshape(16)
    cv = np.asarray(inputs["l1_conv"], np.float32)
    shared["l1_convT"] = np.ascontiguousarray(cv.reshape(3, 24, 128).transpose(2, 1, 0))
    shared["cpack2"] = _host_consts2()
    wq = np.asarray(inputs["l0_w_qkv"], np.float32)
    perm = [pr * 8 + s_ * 4 + g for pr in range(2) for g in range(4) for s_ in range(2)]
    cols = np.concatenate([np.arange(h * 64, (h + 1) * 64) for h in perm] + [np.arange(1024, 1536)])
    shared["l0_w_qkv"] = np.ascontiguousarray(wq[:, cols])
    maps = []
    for b in cores:
        m = dict(shared)
        m["x"] = np.ascontiguousarray(inputs["x"][b], dtype=np.float32)
        m["ctx"] = np.ascontiguousarray(inputs["ctx"][b], dtype=np.float32)
        cv = np.stack([np.asarray(inputs["c"][b], np.float32), np.asarray(inputs["c_ctx"], np.float32)], axis=1)
        m["cvecT"] = np.ascontiguousarray(cv.reshape(8, 128, 2).transpose(1, 0, 2))
        maps.append(m)
    return maps


def kernel(**inputs):
    b = Builder()
    nc = b.build()
    maps = make_in_maps(inputs, list(range(8)))
    maps = [{k: v for k, v in m.items() if k in b.inp} for m in maps]
    res = run_bass_kernel_spmd(nc, maps, core_ids=list(range(8)))
    return np.stack([r["out"] for r in res.results], axis=0).astype(np.float32)
```

```python
import numpy as np
import concourse.bass as bass
import concourse.mybir as mybir
from concourse.bass_utils import run_bass_kernel_spmd

F32 = mybir.dt.float32
F32R = mybir.dt.float32r
U32 = mybir.dt.uint32
ALU = mybir.AluOpType
AF = mybir.ActivationFunctionType
AX = mybir.AxisListType

D = 1024
NLAT = 4096
NCTX = 256
NT_LAT = 32
NT_CTX = 2
NT = 34
NTOK = NLAT + NCTX
NE = 16
FF = 2048
CAP_LAT = 512
CAP_CTX = 32
NDUMMY = 640
EPS = 1e-6
NBIS = 30

C_ID, C_ONES, C_U, C_MP, C_MN, C_IOTA, C_PIDX, C_DMY, C_END = 0, 128, 256, 384, 896, 1408, 1920, 1921, 1926


C2_LF, C2_LB, C2_LT, C2_GT, C2_GE, C2_LE, C2_B64, C2_NB64, C2_OFF, C2_END = 0, 128, 256, 384, 512, 640, 768, 896, 1024, 1152
NEG = -30000.0


class Sched:
    def __init__(self, nc, n_dma_sems=24):
        self.nc = nc
        self.engs = {"pe": nc.tensor, "act": nc.scalar, "dve": nc.vector,
                     "pool": nc.gpsimd, "sp": nc.sync}
        self.csem = {e: nc.alloc_semaphore("c_" + e) for e in ("pe", "act", "dve", "pool")}
        self.ccnt = {e: 0 for e in self.csem}
        self.known = {e: {} for e in self.engs}
        self.dsems = [nc.alloc_semaphore("d%d" % i) for i in range(2 * n_dma_sems)]
        self.dcnt = [0] * (2 * n_dma_sems)
        self.dpool = {"sp": list(range(0, n_dma_sems)), "pool": list(range(n_dma_sems, 2 * n_dma_sems))}
        self.dnext = {"sp": 0, "pool": 0}
        self.state = {}
        self.out_events = []
        self.n_wait = 0
        self.n_inst = 0

    def _need(self, eng, ev):
        sem, val = ev
        k = self.known[eng]
        if k.get(sem.num, 0) >= val:
            return
        self.engs[eng].wait_ge(sem, val)
        self.n_wait += 1
        k[sem.num] = val

    def _deps(self, eng, reads, writes, skip_self=False):
        evs = {}

        def add(ev):
            if ev is None:
                return
            sem, val = ev
            if skip_self and sem.num == self.csem[eng].num:
                return
            if evs.get(sem.num, (None, 0))[1] < val:
                evs[sem.num] = ev

        own = self.csem[eng].num if eng in self.csem else -1
        for k in reads:
            st = self.state.get(k)
            if st:
                add(st["w"])
                if isinstance(k, tuple) and k[0] == "ps":
                    for r in st["r"]:
                        if r[0].num != own:
                            add(r)
        for k in writes:
            st = self.state.get(k)
            if st:
                add(st["w"])
                for r in st["r"]:
                    add(r)
        for ev in evs.values():
            self._need(eng, ev)

    def _commit(self, ev, reads, writes):
        for k in reads:
            st = self.state.setdefault(k, {"w": None, "r": []})
            st["r"] = [r for r in st["r"] if r[0].num != ev[0].num] + [ev]
        for k in writes:
            self.state[k] = {"w": ev, "r": []}

    def op(self, eng, fn, reads=(), writes=()):
        self._deps(eng, reads, writes, skip_self=(eng == "pe"))
        ins = fn(self.engs[eng])
        self.ccnt[eng] += 1
        ins.then_inc(self.csem[eng], 1)
        ev = (self.csem[eng], self.ccnt[eng])
        self._commit(ev, reads, writes)
        self.n_inst += 1
        return ev

    def dma(self, q, fn, reads=(), writes=(), is_output=False):
        self._deps(q, reads, writes)
        pool = self.dpool[q]
        i = pool[self.dnext[q]]
        self.dnext[q] = (self.dnext[q] + 1) % len(pool)
        sem = self.dsems[i]
        if self.dcnt[i] > 0:
            self._need(q, (sem, 16 * self.dcnt[i]))
        ins = fn(self.engs[q])
        self.dcnt[i] += 1
        ins.then_inc(sem, 16)
        ev = (sem, 16 * self.dcnt[i])
        self._commit(ev, reads, writes)
        if is_output:
            self.out_events.append(ev)
        self.n_inst += 1
        return ev

    def barrier(self):
        for eng in self.engs:
            for i, sem in enumerate(self.dsems):
                if self.dcnt[i] > 0:
                    self._need(eng, (sem, 16 * self.dcnt[i]))
            for e, sem in self.csem.items():
                if self.ccnt[e] > 0:
                    self._need(eng, (sem, self.ccnt[e]))

    def finish(self, eng="sp"):
        for i, sem in enumerate(self.dsems):
            if self.dcnt[i] > 0:
                self._need(eng, (sem, 16 * self.dcnt[i]))
        for e, sem in self.csem.items():
            if self.ccnt[e] > 0:
                self._need(eng, (sem, self.ccnt[e]))


class Scope:
    def __init__(self, builder):
        from contextlib import ExitStack
        self.b = builder
        self.st = ExitStack()

    def sb(self, name, shape, dtype=F32):
        return self.st.enter_context(self.b.nc.sbuf_tensor(name, list(shape), dtype))

    def close(self):
        self.b.S.barrier()
        self.st.close()


class Ring:
    def __init__(self, sc, name, shape, dtype, n):
        self.t = [sc.sb("%s%d" % (name, i), shape, dtype) for i in range(n)]
        self.k = [("%s" % name, i) for i in range(n)]
        self.i = 0

    def next(self):
        r = (self.t[self.i], self.k[self.i])
        self.i = (self.i + 1) % len(self.t)
        return r


class Builder:
    def __init__(self, stage=99, debug=False, n_exp=None, start_layer=0, n_tiles=None):
        self.n_exp = n_exp
        self.start_layer = start_layer
        self.n_tiles = n_tiles
        self.stage = stage
        self.debug = debug
        nc = bass.Bass("TRN2", target_bir_lowering=False)
        self.nc = nc
        self.S = Sched(nc)
        self.inp = {}
        self.ps = [nc.alloc_psum_tensor("psb%d" % i, [128, 512], F32) for i in range(8)]
        self.psk = [("ps", i) for i in range(8)]

    def din(self, name, shape, dtype=F32):
        t = self.nc.dram_tensor(name, list(shape), dtype, kind="ExternalInput").ap()
        self.inp[name] = t
        return t

    def dscratch(self, name, shape, dtype=F32, out=False):
        kind = "ExternalOutput" if (out or self.debug) else "Internal"
        return self.nc.dram_tensor(name, list(shape), dtype, kind=kind).ap()

    def sb(self, name, shape, dtype=F32):
        return self.nc.alloc_sbuf_tensor(name, list(shape), dtype)

    def mm(self, out, lhsT, rhs, start, stop, reads, writes):
        return self.S.op("pe", lambda e: e.matmul(out, lhsT=lhsT, rhs=rhs, start=start, stop=stop),
                         reads, writes)

    def tr(self, out, in_, reads, writes, kp=128):
        ident = self.cst[0:kp, C_ID:C_ID + kp]
        return self.S.op("pe", lambda e: e.transpose(out, in_, ident), list(reads) + ["cst"], writes)

    def act(self, out, in_, func, reads, writes, **kw):
        return self.S.op("act", lambda e: e.activation(out=out, in_=in_, func=func, **kw), reads, writes)

    def tt(self, eng, out, in0, in1, op, reads, writes):
        return self.S.op(eng, lambda e: e.tensor_tensor(out=out, in0=in0, in1=in1, op=op), reads, writes)

    def ts(self, eng, out, in0, s1, s2, op0, op1, reads, writes, **kw):
        if s2 is None:
            return self.S.op(eng, lambda e: e.tensor_scalar(out=out, in0=in0, scalar1=s1, scalar2=None,
                                                            op0=op0, **kw), reads, writes)
        return self.S.op(eng, lambda e: e.tensor_scalar(out=out, in0=in0, scalar1=s1, scalar2=s2,
                                                        op0=op0, op1=op1, **kw), reads, writes)

    def stt(self, eng, out, in0, scalar, in1, op0, op1, reads, writes):
        return self.S.op(eng, lambda e: e.scalar_tensor_tensor(out=out, in0=in0, scalar=scalar, in1=in1,
                                                               op0=op0, op1=op1), reads, writes)

    def cp(self, eng, out, in_, reads, writes):
        if eng == "act":
            return self.act(out, in_, AF.Copy, reads, writes)
        return self.S.op(eng, lambda e: e.tensor_copy(out=out, in_=in_), reads, writes)

    def ld(self, out, in_, reads, writes, q="sp"):
        return self.S.dma(q, lambda e: e.dma_start(out=out, in_=in_), reads, writes)

    def rstd_of(self, x_ap, xk, junk, junkk, ss, ssk):
        self.act(junk, x_ap, AF.Square, [xk], [junkk, ssk], accum_out=ss)
        self.ts("dve", ss, ss, 1.0 / D, EPS, ALU.mult, ALU.add, [ssk], [ssk])
        self.act(ss, ss, AF.Sqrt, [ssk], [ssk])
        self.S.op("dve", lambda e: e.reciprocal(out=ss, in_=ss), [ssk], [ssk])

    def build(self):
        nc, S = self.nc, self.S
        st, sl = self.stage, self.start_layer
        x = self.din("x", [NLAT, D]) if sl == 0 else None
        ctx = self.din("ctx", [NCTX, D]) if sl == 0 else None
        cvecT = self.din("cvecT", [128, 8, 2])
        cpack = self.din("cpack", [128, C_END])
        rope = self.din("rope", [NLAT, 64]) if sl == 0 else None
        W = {}
        for l in (0, 1):
            if l == 0 and sl > 0:
                continue
            if l == 1 and st < 30:
                continue
            W[l] = dict(
                ada_w=self.din("l%d_ada_w" % l, [D, 6 * D]),
                ada_b=self.din("l%d_ada_b" % l, [6 * D]),
                ada_bT=self.din("l%d_ada_bT" % l, [128, 48]),
                nmixT=self.din("l%d_nmixT" % l, [128, 8]),
                nffnT=self.din("l%d_nffnT" % l, [128, 8]),
                router=self.din("l%d_router" % l, [D, NE]),
            )
            if (l == 0 and st >= 2) or (l == 1 and st >= 40):
                W[l].update(w_gate=self.din("l%d_w_gate" % l, [NE, D, FF]),
                            w_up=self.din("l%d_w_up" % l, [NE, D, FF]),
                            w_down=self.din("l%d_w_down" % l, [NE, FF, D]))
        if 0 in W:
            W[0].update(w_qkv=self.din("l0_w_qkv", [D, 1536]), sink=self.din("l0_sink", [16]),
                        w_o=self.din("l0_w_o", [D, D]))
        if 1 in W:
            W[1].update(w_in=self.din("l1_w_in", [D, 4128]), convT=self.din("l1_convT", [128, 24, 3]),
                        a_log=self.din("l1_a_log", [16]), dt_bias=self.din("l1_dt_bias", [16]),
                        o_norm=self.din("l1_o_norm", [128]), w_o=self.din("l1_w_o", [D, D]))
            cpack2 = self.din("cpack2", [128, C2_END])
        if st >= 50:
            self.din("final_norm", [D])
        self.W = W
        self.x, self.ctx, self.rope = x, ctx, rope
        self.out = self.nc.dram_tensor("out", [NLAT, D], F32, kind="ExternalOutput").ap()
        self.qs = self.dscratch("qs", [NTOK, D])
        if sl == 0:
            xa = self.dscratch("xresA", [NTOK + NDUMMY, D])
        else:
            xa = self.din("xresA_in", [NTOK + NDUMMY, D])
        self.xres = [xa, self.dscratch("xresB", [NTOK + NDUMMY, D])]
        self.xn2 = self.dscratch("xn2", [NTOK + NDUMMY, D])

        self.cst = self.sb("cst", [128, C_END])
        self.ld(self.cst[:], cpack, [], ["cst"])
        self.scT = self.sb("scT", [128, 8, 2], F32R)
        self.cols = self.sb("cols", [128, 48, 2])
        self.A1 = self.sb("A1", [128, 8, 2]); self.A2 = self.sb("A2", [128, 8, 2])
        self.Gbc = self.sb("Gbc", [128, 2, 2, D])
        self.aff = self.sb("aff", [128, NT, NE])
        if self.debug:
            S.op("dve", lambda e: e.memset(self.aff[:], 0.0), [], [("aff", i) for i in range(NT)])
        sc0 = Scope(self)
        zero_t = sc0.sb("zero_t", [128, D])
        S.op("dve", lambda e: e.memset(zero_t[:], 0.0), [], ["zero_t"])
        bufs = [(self.xres[1], "xres1"), (self.xn2, "xn2")] + ([(self.xres[0], "xres0")] if sl == 0 else [])
        for buf, k in bufs:
            for r in range(NDUMMY // 128):
                self.ld(buf[NTOK + r * 128: NTOK + (r + 1) * 128, :], zero_t[:], ["zero_t"], [(k, "dummy", r)])
        sc0.close()
        if sl == 0:
            self.modulation(0)
            if st >= 1:
                self.attention_layer()
            if st >= 2:
                self.moe(0, self.xres[0], "xres0", with_ctx=True)
        if st >= 30:
            self.cst2 = self.sb("cst2", [128, C2_END])
            self.ld(self.cst2[:], cpack2, [], ["cst2"])
            self.modulation(1)
            self.deltanet_layer()
        if st >= 40:
            self.moe(1, self.xres[1], "xres1", with_ctx=False)
        if st >= 50:
            self.final_norm()
        if self.debug:
            d_aff = self.nc.dram_tensor("d_aff", [128, NT * NE], F32, kind="ExternalOutput").ap()
            self.ld(d_aff, self.aff[:, :, :].rearrange("p j e -> p (j e)"), [("aff", i) for i in range(NT)], ["d_aff"])
            d_cols = self.nc.dram_tensor("d_cols", [128, 96], F32, kind="ExternalOutput").ap()
            self.ld(d_cols, self.cols[:, :, :].rearrange("p c t -> p (c t)"), ["cols"], ["d_cols"])
            d_g = self.nc.dram_tensor("d_g", [128, 4 * D], F32, kind="ExternalOutput").ap()
            self.ld(d_g, self.Gbc[:, :, :, :].rearrange("p a b d -> p (a b d)"),
                    [("Gbc", a, b) for a in range(2) for b in range(2)], ["d_g"])
        S.finish()
        return nc

    def modulation(self, l):
        nc, S, W = self.nc, self.S, self.W[l]
        sfx = "m%d" % l
        sc = Scope(self)
        self.wring = Ring(sc, "wst" + sfx, [128, 8, 512], F32R, 4)
        cv = sc.sb("cv" + sfx, [128, 8, 2])
        self.ld(cv[:], self.inp["cvecT"], [], ["cv"])
        self.act(self.scT[:], cv[:], AF.Silu, ["cv"], ["scT"])
        screp = [sc.sb("screp%d%s" % (c, sfx), [128, 8, 128], F32R) for c in range(2)]
        for c in range(2):
            self.cp("dve", screp[c][:], self.scT[:, :, c:c + 1].to_broadcast([128, 8, 128]), ["scT"], [("screp", c)])
        abT = sc.sb("abT" + sfx, [128, 48])
        self.ld(abT[:], W["ada_bT"], [], ["abT"])
        nmix = sc.sb("nmix" + sfx, [128, 8]); nffn = sc.sb("nffn" + sfx, [128, 8])
        self.ld(nmix[:], W["nmixT"], [], ["nmix"])
        self.ld(nffn[:], W["nffnT"], [], ["nffn"])
        abbc = sc.sb("abbc" + sfx, [128, 2, D])
        self.ld(abbc[:, 0, :], W["ada_b"][2 * D:3 * D].partition_broadcast(128), [], [("abbc", 0)])
        self.ld(abbc[:, 1, :], W["ada_b"][5 * D:6 * D].partition_broadcast(128), [], [("abbc", 1)])
        pcol = self.ps[0]
        pcv = pcol[:, 0:96].rearrange("p (c t) -> p c t", t=2)
        for cg in range(12):
            wt, wk = self.wring.next()
            self.ld(wt[:], W["ada_w"][:, cg * 512:(cg + 1) * 512].rearrange("(k p) n -> p k n", p=128),
                    [], [wk], q="pool")
            for c4 in range(4):
                cc = cg * 4 + c4
                for kc in range(8):
                    self.mm(pcv[:, cc, :], wt[:, kc, c4 * 128:(c4 + 1) * 128], self.scT[:, kc, :],
                            kc == 0, kc == 7, [wk, "scT"], [self.psk[0]])
            if cg in (4, 5, 10, 11):
                gi = 0 if cg < 6 else 1
                half = cg % 2
                for c in range(2):
                    pb, pbk = self.ps[1 + c], self.psk[1 + c]
                    for kc in range(8):
                        self.mm(pb[:, :], screp[c][:, kc, :], wt[:, kc, :], kc == 0, kc == 7,
                                [wk, ("screp", c)], [pbk])
                    self.tt("dve", self.Gbc[:, gi, c, half * 512:(half + 1) * 512], pb[:, :],
                            abbc[:, gi, half * 512:(half + 1) * 512], ALU.add,
                            [pbk, ("abbc", gi)], [("Gbc", gi, c)])
        self.tt("dve", self.cols[:], pcv, abT[:].unsqueeze(2).to_broadcast([128, 48, 2]), ALU.add,
                [self.psk[0], "abT"], ["cols"])
        for (A, nrm, nk, v) in ((self.A1, nmix, "nmix", 1), (self.A2, nffn, "nffn", 4)):
            self.stt("dve", A[:], self.cols[:, v * 8:(v + 1) * 8, :], 1.0,
                     nrm[:].unsqueeze(2).to_broadcast([128, 8, 2]), ALU.add, ALU.mult,
                     ["cols", nk], [("A", v)])
        sc.close()

    def B1(self, kc, c):
        return self.cols[:, 0 + kc, c:c + 1]

    def B2(self, kc, c):
        return self.cols[:, 24 + kc, c:c + 1]

    def attention_layer(self):
        nc, S, W = self.nc, self.S, self.W[0]
        cst = self.cst
        sca = Scope(self)
        KT = sca.sb("KT", [128, 2, NTOK], F32R)
        V = sca.sb("Vaug", [128, NT, 4, 66], F32R)
        esink = sca.sb("esink", [128, 16])
        wr = sca.sb("wr", [128, 8, NE])
        xr = Ring(sca, "xt", [128, D], F32, 2)
        xnr = Ring(sca, "xnb", [128, D], F32, 3)
        ssr = Ring(sca, "ss", [128, 1], F32, 6)
        junk = sca.sb("junk", [128, D])
        sc1 = Scope(self)
        wbig = sc1.sb("wbig", [128, 8, 1536], F32R)
        hTr = Ring(sc1, "hT", [128, 8, 128], F32R, 3)
        qkr = Ring(sc1, "qk", [128, 1280], F32, 2)
        csr = Ring(sc1, "cs", [128, 64], F32, 6)
        tmpr = Ring(sc1, "rt", [128, 4, 256], F32, 1)
        for cg in range(3):
            self.ld(wbig[:, :, cg * 512:(cg + 1) * 512],
                    W["w_qkv"][:, cg * 512:(cg + 1) * 512].rearrange("(k p) n -> p k n", p=128),
                    [], [("wbig", cg)], q="pool")
        Vf = V[:, :, :, :].rearrange("p j h c -> p (j h) c")
        self.cp("dve", Vf[:, :, 64:65], self.cst[:, C_ONES:C_ONES + 1].unsqueeze(1).to_broadcast([128, NT * 4, 1]), ["cst"], [("V1",)])
        self.cp("dve", Vf[:, :, 65:66], self.cst[:, C_U:C_U + 1].unsqueeze(1).to_broadcast([128, NT * 4, 1]), ["cst"], [("V0",)])
        self.ld(esink[:], W["sink"].partition_broadcast(128), [], ["esink"])
        self.act(esink[:], esink[:], AF.Exp, ["esink"], ["esink"])
        self.ld(wr[:], W["router"].rearrange("(k p) n -> p k n", p=128), [], ["wr"])

        def src_rows(j):
            if j < NT_LAT:
                return self.x[j * 128:(j + 1) * 128, :]
            return self.ctx[(j - NT_LAT) * 128:(j - NT_LAT + 1) * 128, :]

        cx = [dict() for _ in range(NT)]

        def p1_s0(j):
            c = cx[j]
            c["c"] = 0 if j < NT_LAT else 1
            xt, xk = xr.next()
            self.ld(xt[:], src_rows(j), [], [xk])
            if c["c"] == 0:
                c["cs"], c["ck"] = csr.next()
                self.ld(c["cs"][:], self.rope[j * 128:(j + 1) * 128, :], [], [c["ck"]])
            ss, ssk = ssr.next()
            self.rstd_of(xt[:], xk, junk[:], "junk", ss[:], ssk)
            c["xn"], c["xnk"] = xnr.next()
            self.ts("dve", c["xn"][:], xt[:], ss[:, 0:1], None, ALU.mult, None, [xk, ssk], [c["xnk"]])

        def p1_s1(j):
            c = cx[j]
            xn, xnk, cc = c["xn"], c["xnk"], c["c"]
            c["hT"], c["hk"] = hTr.next()
            hT, hk = c["hT"], c["hk"]
            for kc in range(8):
                pt, ptk = self.ps[kc // 4], self.psk[kc // 4]
                self.tr(pt[:, (kc % 4) * 128:(kc % 4 + 1) * 128], xn[:, kc * 128:(kc + 1) * 128], [xnk], [ptk])
            for kc in range(8):
                pt, ptk = self.ps[kc // 4], self.psk[kc // 4]
                self.act(hT[:, kc, :], pt[:, (kc % 4) * 128:(kc % 4 + 1) * 128], AF.Identity,
                         [ptk, ("A", 1), "cols"], [hk], scale=self.A1[:, kc, cc:cc + 1], bias=self.B1(kc, cc))

        def p1_s2(j):
            c = cx[j]
            hT, hk = c["hT"], c["hk"]
            c["pb"] = 2 + 3 * (j % 2)
            for cg in range(3):
                pq, pqk = self.ps[c["pb"] + cg], self.psk[c["pb"] + cg]
                for kc in range(8):
                    self.mm(pq[:, :], hT[:, kc, :], wbig[:, kc, cg * 512:(cg + 1) * 512], kc == 0, kc == 7,
                            [hk, ("wbig", cg)], [pqk])

        def p1_s3(j):
            c = cx[j]
            pb = c["pb"]
            c["qk"], c["qkk"] = qkr.next()
            qk, qkk = c["qk"], c["qkk"]
            if c["c"] == 0:
                cs, ck = c["cs"], c["ck"]
                cosb = lambda nh: cs[:, 0:32].rearrange("p (a f) -> p a f", a=2).unsqueeze(1).to_broadcast([128, nh, 2, 16])
                sinb = lambda nh: cs[:, 32:64].rearrange("p (a f) -> p a f", a=2).unsqueeze(1).to_broadcast([128, nh, 2, 16])
                for cg in range(3):
                    nh = 8 if cg < 2 else 4
                    pq, pqk = self.ps[pb + cg], self.psk[pb + cg]
                    pv = pq[:, 0:nh * 64].rearrange("p (h a b f) -> p h a b f", h=nh, a=2, b=2, f=16)
                    ov = qk[:, cg * 512:cg * 512 + nh * 64].rearrange("p (h a b f) -> p h a b f", h=nh, a=2, b=2, f=16)
                    x1, x2 = pv[:, :, :, 0, :], pv[:, :, :, 1, :]
                    tm, tmk = tmpr.next()
                    t = [tm[:, i, 0:nh * 32].rearrange("p (h a f) -> p h a f", h=nh, a=2, f=16) for i in range(4)]
                    self.tt("dve", t[0], x1, cosb(nh), ALU.mult, [pqk, ck], [tmk])
                    self.tt("dve", t[1], x2, sinb(nh), ALU.mult, [pqk, ck], [tmk])
                    self.tt("dve", t[2], x2, cosb(nh), ALU.mult, [pqk, ck], [tmk])
                    self.tt("dve", t[3], x1, sinb(nh), ALU.mult, [pqk, ck], [tmk])
                    self.tt("pool", ov[:, :, :, 0, :], t[0], t[1], ALU.subtract, [tmk], [qkk])
                    self.tt("pool", ov[:, :, :, 1, :], t[2], t[3], ALU.add, [tmk], [qkk])
            else:
                for cg in range(3):
                    ncol = 512 if cg < 2 else 256
                    self.cp("act", qk[:, cg * 512:cg * 512 + ncol], self.ps[pb + cg][:, 0:ncol], [self.psk[pb + cg]], [qkk])
            self.cp("act", V[:, j, :, 0:64], self.ps[pb + 2][:, 256:512].rearrange("p (h d) -> p h d", h=4),
                    [self.psk[pb + 2]], [("V", j)])
            self.ld(self.qs[j * 128:(j + 1) * 128, :], qk[:, 0:1024], [qkk], [("qs", j)])

        def p1_s4(j):
            c = cx[j]
            qk, qkk = c["qk"], c["qkk"]
            for pr in range(2):
                self.tr(self.ps[1][:, pr * 128:(pr + 1) * 128], qk[:, 1024 + pr * 128:1024 + (pr + 1) * 128], [qkk], [self.psk[1]])
            self.cp("act", KT[:, :, j * 128:(j + 1) * 128], self.ps[1][:, 0:256].rearrange("p (a t) -> p a t", a=2),
                    [self.psk[1]], [("KT", j)])

        stages = [p1_s0, p1_s1, p1_s2, p1_s3, p1_s4]
        for t_ in range(NT + len(stages) - 1):
            for st_ in range(len(stages) - 1, -1, -1):
                i_ = t_ - st_
                if 0 <= i_ < NT:
                    stages[st_](i_)

        sc1.close()
        sca_outer, sca = sca, Scope(self)
        wo = sca.sb("wo", [128, 8, 1024], F32R)
        self.ld(wo[:], W["w_o"].rearrange("(k p) n -> p k n", p=128), [], ["wo"], q="pool")
        wok = ["wo"]
        QTr = Ring(sca, "QT", [128, 2, 4, 128], F32R, 2)
        PTr = Ring(sca, "PT", [128, 5, 512], F32R, 2)
        osb = sca.sb("osb", [128, 16, 64])
        otsr = Ring(sca, "ots", [66, 512], F32, 1)
        oT = sca.sb("oT", [128, 8, 128], F32R)
        den = sca.sb("den", [128, 16])
        xmr = Ring(sca, "xm", [128, D], F32, 3)
        h2T = sca.sb("h2T", [128, 8, 128])
        lg = sca.sb("lg", [128, NE]); mx = sca.sb("mx", [128, 1]); sm = sca.sb("sm", [128, 1])
        pend = [None]

        def p2_loads(i_):
            qt_, qtk_ = xr.next()
            self.ld(qt_[:], self.qs[i_ * 128:(i_ + 1) * 128, :], [("qs", i_)], [qtk_])
            xt_, xk_ = xnr.next()
            self.ld(xt_[:], src_rows(i_), [], [xk_])
            return qt_, qtk_, xt_, xk_

        nxt = p2_loads(0)
        for i in range(NT):
            c = 0 if i < NT_LAT else 1
            qt, qtk, xt, xk = nxt
            if i + 1 < NT:
                nxt = p2_loads(i + 1)
            QT, QTk = QTr.next()
            for pr in range(2):
                for g in range(4):
                    self.tr(self.ps[pr][:, g * 128:(g + 1) * 128], qt[:, (pr * 4 + g) * 128:(pr * 4 + g + 1) * 128], [qtk], [self.psk[pr]])
                self.cp("act", QT[:, pr, :, :], self.ps[pr][:, :].rearrange("p (g t) -> p g t", g=4), [self.psk[pr]], [QTk])
            if c == 0:
                kbs = ([i - 1] if i > 0 else []) + [i] + ([i + 1] if i < NT_LAT - 1 else []) + [32, 33]
                kmask = ([C_MP] if i > 0 else []) + [None] + ([C_MN] if i < NT_LAT - 1 else []) + [None, None]
            else:
                kbs = [32, 33]
                kmask = [None, None]
            if pend[0] is not None:
                self.moe_prep_a(*pend[0], skip0=True)

            def st_phase(kvh):
                pr, base = kvh // 2, (kvh % 2) * 64
                PT, PTk = PTr.next()
                for kbi, kb in enumerate(kbs):
                    pS, pSk = self.ps[2 + kbi % 2], self.psk[2 + kbi % 2]
                    self.mm(pS[:, :], KT[base:base + 64, pr, kb * 128:(kb + 1) * 128],
                            QT[base:base + 64, pr, :, :], True, True, [("KT", kb), QTk], [pSk])
                    self.act(PT[:, kbi, :], pS[:, :], AF.Exp, [pSk], [PTk], scale=0.125)
                    if kmask[kbi] is not None:
                        mo = kmask[kbi]
                        self.tt("pool", PT[:, kbi, :], PT[:, kbi, :], cst[:, mo:mo + 512], ALU.mult, [PTk, "cst"], [PTk])
                return PT, PTk

            def pv_phase(kvh, PT, PTk):
                pO, pOk = self.ps[4 + kvh], self.psk[4 + kvh]
                for kbi, kb in enumerate(kbs):
                    self.mm(pO[0:66, :], V[:, kb, kvh, :], PT[:, kbi, :], kbi == 0, kbi == len(kbs) - 1,
                            [PTk, ("V", kb), ("V1",), ("V0",)], [pOk])
                ots, otsk = otsr.next()
                self.cp("act", ots[0:66, :], pO[0:66, :], [pOk], [otsk])
                for g in range(4):
                    self.tr(pO[:, g * 128:g * 128 + 66], ots[0:66, g * 128:(g + 1) * 128], [otsk], [pOk], kp=66)

            pts = {0: st_phase(0)}
            for kvh in range(4):
                if kvh + 1 < 4:
                    pts[kvh + 1] = st_phase(kvh + 1)
                if kvh == 1 and pend[0] is not None:
                    self.moe_prep_b(*pend[0])
                    pend[0] = None
                pv_phase(kvh, *pts[kvh])
            for kvh in range(4):
                pOv = self.ps[4 + kvh][:, :].rearrange("p (g t) -> p g t", g=4)
                self.tt("dve", den[:, kvh * 4:(kvh + 1) * 4], pOv[:, :, 64], esink[:, kvh * 4:(kvh + 1) * 4], ALU.add,
                        [self.psk[4 + kvh], "esink"], ["den"])
            self.S.op("dve", lambda e: e.reciprocal(out=den[:], in_=den[:]), ["den"], ["den"])
            for kvh in range(4):
                pOv = self.ps[4 + kvh][:, :].rearrange("p (g t) -> p g t", g=4)
                self.tt("dve", osb[:, kvh * 4:(kvh + 1) * 4, :], pOv[:, :, 0:64],
                        den[:, kvh * 4:(kvh + 1) * 4].unsqueeze(2).to_broadcast([128, 4, 64]), ALU.mult,
                        [self.psk[4 + kvh], "den"], ["osb"])
            osf = osb[:, :, :].rearrange("p h d -> p (h d)")
            for kc in range(8):
                self.tr(self.ps[kc // 4][:, (kc % 4) * 128:(kc % 4 + 1) * 128], osf[:, kc * 128:(kc + 1) * 128], ["osb"], [self.psk[kc // 4]])
            for b in range(2):
                self.cp("act", oT[:, b * 4:(b + 1) * 4, :], self.ps[b][:, :].rearrange("p (k t) -> p k t", k=4), [self.psk[b]], ["oT"])
            xm, xmk = xmr.next()
            for dh in range(2):
                pM, pMk = self.ps[2 + dh], self.psk[2 + dh]
                for kc in range(8):
                    self.mm(pM[:, :], oT[:, kc, :], wo[:, kc, dh * 512:(dh + 1) * 512], kc == 0, kc == 7, ["oT"] + wok, [pMk])
                self.tt("dve", xm[:, dh * 512:(dh + 1) * 512], pM[:, :], self.Gbc[:, 0, c, dh * 512:(dh + 1) * 512], ALU.mult,
                        [pMk, ("Gbc", 0, c)], [xmk])
            self.tt("dve", xm[:], xm[:], xt[:], ALU.add, [xmk, xk], [xmk])
            self.ld(self.xres[0][i * 128:(i + 1) * 128, :], xm[:], [xmk], [("xres0", i)])
            pend[0] = (i, c, xm, xmk, junk, ssr, wr, h2T, lg, mx, sm)
            self.moe_prep_a0(*pend[0])
        self.moe_prep_a(*pend[0], skip0=True)
        self.moe_prep_b(*pend[0])
        sca.close()
        sca_outer.close()

    def moe_prep_a0(self, i, c, xm, xmk, junk, ssr, wr, h2T, lg, mx, sm):
        ss, ssk = ssr.next()
        self.rstd_of(xm[:], xmk, junk[:], "junk", ss[:], ssk)
        self.ts("dve", junk[:], xm[:], ss[:, 0:1], None, ALU.mult, None, [xmk, ssk], ["junk"])
        self.ld(self.xn2[i * 128:(i + 1) * 128, :], junk[:], ["junk"], [("xn2", i)])

    def moe_prep_a(self, i, c, xm, xmk, junk, ssr, wr, h2T, lg, mx, sm, skip0=False):
        if not skip0:
            self.moe_prep_a0(i, c, xm, xmk, junk, ssr, wr, h2T, lg, mx, sm)
        for kc in range(8):
            self.tr(self.ps[kc // 4][:, (kc % 4) * 128:(kc % 4 + 1) * 128], junk[:, kc * 128:(kc + 1) * 128], ["junk"], [self.psk[kc // 4]])
        for kc in range(8):
            self.act(h2T[:, kc, :], self.ps[kc // 4][:, (kc % 4) * 128:(kc % 4 + 1) * 128], AF.Identity,
                     [self.psk[kc // 4], ("A", 4), "cols"], ["h2T"], scale=self.A2[:, kc, c:c + 1], bias=self.B2(kc, c))

    def moe_prep_b(self, i, c, xm, xmk, junk, ssr, wr, h2T, lg, mx, sm):
        pL, pLk = self.ps[1], self.psk[1]
        for kc in range(8):
            self.mm(pL[:, 0:NE], h2T[:, kc, :], wr[:, kc, :], kc == 0, kc == 7, ["h2T", "wr"], [pLk])
        self.S.op("dve", lambda e: e.reduce_max(out=mx[:], in_=pL[:, 0:NE], axis=AX.X), [pLk], ["mx"])
        self.ts("dve", mx[:], mx[:], -1.0, None, ALU.mult, None, ["mx"], ["mx"])
        self.act(lg[:], pL[:, 0:NE], AF.Exp, [pLk, "mx"], ["lg", "sm"], bias=mx[:, 0:1], accum_out=sm[:])
        self.S.op("dve", lambda e: e.reciprocal(out=sm[:], in_=sm[:]), ["sm"], ["sm"])
        self.ts("dve", self.aff[:, i, :], lg[:], sm[:, 0:1], None, ALU.mult, None, ["lg", "sm"], [("aff", i)])

    def moe_prep(self, *args):
        self.moe_prep_a(*args)
        self.moe_prep_b(*args)

    def pk(self, b, h):
        return ("ps", b)

    def deltanet_layer(self):
        nc, S, W = self.nc, self.S, self.W[1]
        cst, c2 = self.cst, self.cst2
        xin = self.xres[0]
        qT_d = self.dscratch("qT_d", [D, NTOK]); kT_d = self.dscratch("kT_d", [D, NTOK])
        ktok_d = self.dscratch("ktok_d", [NTOK, D]); vtok_d = self.dscratch("vtok_d", [NTOK, D])
        sz_d = self.dscratch("sz_d", [NTOK, D]); of_d = self.dscratch("of_d", [NLAT, D])
        self.dn_dbg = dict(qT_d=qT_d, kT_d=kT_d, ktok_d=ktok_d, vtok_d=vtok_d, sz_d=sz_d, of_d=of_d)
        ident = cst[:, C_ID:C_ID + 128]
        ones = cst[:, C_ONES:C_ONES + 128]
        zcol = cst[:, C_U:C_U + 1]
        scL = Scope(self)
        g_all = scL.sb("g_all", [128, NT, 16]); beta_all = scL.sb("beta_all", [128, NT, 16])
        lnb_all = scL.sb("lnb_all", [128, NT, 16]); ab_all = scL.sb("ab_all", [128, NT, 32])
        onesR = scL.sb("onesR", [128, 128], F32R)
        self.cp("dve", onesR[:], ones, ["cst"], ["onesR"])
        convT = scL.sb("convT", [128, 24, 3])
        self.ld(convT[:], W["convT"], [], ["convT"])
        allps = [self.pk(b, h) for b in range(8) for h in range(2)]

        def bankk(b):
            return [self.pk(b, 0), self.pk(b, 1)]

        scp = Scope(self)
        win = scp.sb("winqkv", [128, 8, 3072], F32R)
        wink = [("win", i) for i in range(6)]
        for i in range(6):
            self.ld(win[:, :, i * 512:(i + 1) * 512], W["w_in"][:, i * 512:(i + 1) * 512].rearrange("(k p) n -> p k n", p=128),
                    [], [wink[i]], q="pool")
        wnd = [scp.sb("wnd%d" % i, [128, 8, 258], F32R) for i in range(2)]
        xr = Ring(scp, "pxt", [128, D], F32, 2)
        ssr = Ring(scp, "pss", [128, 1], F32, 4)
        junk = scp.sb("pjunk", [128, D])
        c1r = Ring(scp, "pc1", [128, 256], F32, 3)
        sr = Ring(scp, "psl", [128, 256], F32, 8)
        sqr = Ring(scp, "psq", [128, 256], F32R, 3)
        rnr = Ring(scp, "prn", [128, 256], F32, 4)
        qnr = Ring(scp, "pqn", [128, 256], F32, 4)
        tkr = Ring(scp, "ptk", [128, 128], F32, 6)
        groups = [[32, 33]] + [[2 * g, 2 * g + 1] for g in range(16)]

        def tile_hT(jj, dst_fn, xr_, ssr_, junk_):
            c = 0 if jj < NT_LAT else 1
            xt, xk = xr_.next()
            self.ld(xt[:], xin[jj * 128:(jj + 1) * 128, :], [], [xk])
            ss, ssk = ssr_.next()
            self.rstd_of(xt[:], xk, junk_[:], "pjunk", ss[:], ssk)
            self.ts("dve", xt[:], xt[:], ss[:, 0:1], None, ALU.mult, None, [xk, ssk], [xk])
            for kc in range(8):
                b = kc // 4
                self.tr(self.ps[b][:, (kc % 4) * 128:(kc % 4 + 1) * 128], xt[:, kc * 128:(kc + 1) * 128], [xk], bankk(b))
            for kc in range(8):
                b = kc // 4
                dst, dk = dst_fn(kc)
                self.act(dst, self.ps[b][:, (kc % 4) * 128:(kc % 4 + 1) * 128], AF.Identity,
                         bankk(b) + [("A", 1), "cols"], [dk], scale=self.A1[:, kc, c:c + 1], bias=self.B1(kc, c))

        def prep_window(gi):
            buf = gi % 2
            for ti, jj in enumerate(groups[gi]):
                tile_hT(jj, lambda kc: (wnd[buf][:, kc, 1 + 128 * ti:1 + 128 * (ti + 1)], ("wnd", buf)), xr, ssr, junk)
            same_prev = gi >= 2
            if same_prev:
                self.cp("dve", wnd[buf][:, :, 0:1], wnd[1 - buf][:, :, 256:257], [("wnd", 1 - buf)], [("wnd", buf)])
                self.cp("dve", wnd[1 - buf][:, :, 257:258], wnd[buf][:, :, 1:2], [("wnd", buf)], [("wnd", 1 - buf)])
            else:
                self.cp("dve", wnd[buf][:, :, 0:1], zcol.unsqueeze(1).to_broadcast([128, 8, 1]), ["cst"], [("wnd", buf)])
                if gi >= 1:
                    self.cp("dve", wnd[1 - buf][:, :, 257:258], zcol.unsqueeze(1).to_broadcast([128, 8, 1]), ["cst"], [("wnd", 1 - buf)])

        pb_i = [0]
        pn_i = [0]

        def skew(n_items, stages):
            k = len(stages)
            for t in range(n_items + k - 1):
                for st_ in range(k - 1, -1, -1):
                    i_ = t - st_
                    if 0 <= i_ < n_items:
                        stages[st_](i_)

        def project(gi):
            buf = gi % 2
            wv, wvk = wnd[buf], ("wnd", buf)
            tok0 = groups[gi][0] * 128
            ctxs = [dict() for _ in range(24)]

            def s0(ch):
                c = ctxs[ch]
                b = pb_i[0] % 3
                pb_i[0] += 1
                c["pb"] = 2 + b
                pP = self.ps[2 + b]
                for kc in range(8):
                    self.mm(pP[:, 0:258], win[:, kc, ch * 128:(ch + 1) * 128], wv[:, kc, 0:258], kc == 0, kc == 7,
                            [wink[ch // 4], wvk], bankk(2 + b))

            def s1(ch):
                c = ctxs[ch]
                pb = c["pb"]
                pP = self.ps[pb]
                c1, c1k = c1r.next()
                self.ts("dve", c1[:], pP[:, 0:256], convT[:, ch, 0:1], None, ALU.mult, None, bankk(pb) + ["convT"], [c1k])
                self.stt("dve", c1[:], pP[:, 1:257], convT[:, ch, 1:2], c1[:], ALU.mult, ALU.add, bankk(pb) + ["convT", c1k], [c1k])
                self.stt("dve", c1[:], pP[:, 2:258], convT[:, ch, 2:3], c1[:], ALU.mult, ALU.add, bankk(pb) + ["convT", c1k], [c1k])
                c["sl"], c["slk"] = sr.next()
                self.act(c["sl"][:], c1[:], AF.Silu, [c1k], [c["slk"]])

            def s2(ch):
                c = ctxs[ch]
                if ch // 8 < 2:
                    sq, sqk = sqr.next()
                    self.act(sq[:], c["sl"][:], AF.Square, [c["slk"]], [sqk])
                    nb = 5 if (pn_i[0] % 2 == 0) else 7
                    pn_i[0] += 1
                    c["nb"] = nb
                    self.mm(self.ps[nb][:, 0:256], onesR[:], sq[:], True, True, ["onesR", sqk], bankk(nb))

            def s3(ch):
                c = ctxs[ch]
                if ch // 8 < 2:
                    c["rn"], c["rnk"] = rnr.next()
                    rn, rnk = c["rn"], c["rnk"]
                    self.ts("dve", rn[:], self.ps[c["nb"]][:, 0:256], EPS, None, ALU.add, None, bankk(c["nb"]), [rnk])
                    self.act(rn[:], rn[:], AF.Sqrt, [rnk], [rnk])

            def s4(ch):
                c = ctxs[ch]
                kind, h = ch // 8, ch % 8
                if kind < 2:
                    rn, rnk = c["rn"], c["rnk"]
                    S.op("dve", lambda e: e.reciprocal(out=rn[:], in_=rn[:]), [rnk], [rnk])
                    qn, qnk = qnr.next()
                    self.stt("dve", qn[:], c["sl"][:], (128.0 ** -0.5) if kind == 0 else 1.0, rn[:], ALU.mult, ALU.mult, [c["slk"], rnk], [qnk])
                    dst = qT_d if kind == 0 else kT_d
                    self.ld(dst[h * 128:(h + 1) * 128, tok0:tok0 + 256], qn[:], [qnk], [("qkT_d", kind, gi, h)])
                    c["src"], c["srck"] = qn, qnk
                else:
                    c["src"], c["srck"] = c["sl"], c["slk"]

            def s5(ch):
                c = ctxs[ch]
                if ch // 8 >= 1:
                    for ti in range(2):
                        self.tr(self.ps[6][:, ti * 128:(ti + 1) * 128], c["src"][:, ti * 128:(ti + 1) * 128], [c["srck"]], bankk(6))

            def s6(ch):
                c = ctxs[ch]
                kind, h = ch // 8, ch % 8
                if kind >= 1:
                    dstd = ktok_d if kind == 1 else vtok_d
                    for ti in range(2):
                        tk, tkk = tkr.next()
                        self.cp("act", tk[:], self.ps[6][:, ti * 128:(ti + 1) * 128], bankk(6), [tkk])
                        self.ld(dstd[tok0 + ti * 128:tok0 + (ti + 1) * 128, h * 128:(h + 1) * 128], tk[:], [tkk], [("tok_d", kind, gi, h, ti)])

            skew(24, [s0, s1, s2, s3, s4, s5, s6])

        for gi in range(len(groups)):
            prep_window(gi)
            if gi >= 1:
                project(gi - 1)
        lastb = (len(groups) - 1) % 2
        self.cp("dve", wnd[lastb][:, :, 257:258], zcol.unsqueeze(1).to_broadcast([128, 8, 1]), ["cst"], [("wnd", lastb)])
        project(len(groups) - 1)
        scp.close()

        scz = Scope(self)
        wz = scz.sb("winz", [128, 8, 1056], F32R)
        self.ld(wz[:, :, 0:528], W["w_in"][:, 3072:3600].rearrange("(k p) n -> p k n", p=128), [], [("wz", 0)], q="pool")
        self.ld(wz[:, :, 528:1056], W["w_in"][:, 3600:4128].rearrange("(k p) n -> p k n", p=128), [], [("wz", 1)], q="pool")
        wzk = [("wz", 0), ("wz", 1)]
        xr = Ring(scz, "zxt", [128, D], F32, 2)
        ssr = Ring(scz, "zss", [128, 1], F32, 4)
        junk = scz.sb("zjunk", [128, D])
        hTr = Ring(scz, "zhT", [128, 8, 128], F32R, 2)
        zr = Ring(scz, "zst", [128, D], F32, 2)
        for jj in range(NT):
            hT, hk = hTr.next()
            tile_hT(jj, lambda kc: (hT[:, kc, :], hk), xr, ssr, junk)
            for zh in range(2):
                for kc in range(8):
                    self.mm(self.ps[2 + zh][:, :], hT[:, kc, :], wz[:, kc, zh * 512:(zh + 1) * 512], kc == 0, kc == 7, [hk] + wzk, bankk(2 + zh))
            for kc in range(8):
                self.mm(self.ps[4][:, 0:32], hT[:, kc, :], wz[:, kc, 1024:1056], kc == 0, kc == 7, [hk] + wzk, bankk(4))
            zt, ztk = zr.next()
            for zh in range(2):
                self.act(zt[:, zh * 512:(zh + 1) * 512], self.ps[2 + zh][:, :], AF.Silu, bankk(2 + zh), [ztk])
            self.ld(sz_d[jj * 128:(jj + 1) * 128, :], zt[:], [ztk], [("sz_d", jj)])
            self.cp("dve", ab_all[:, jj, :], self.ps[4][:, 0:32], bankk(4), [("ab", jj)])
        abk = [("ab", jj) for jj in range(NT)]
        dtb = scz.sb("dtb", [128, 16]); nea = scz.sb("nea", [128, 16])
        self.ld(dtb[:], W["dt_bias"].partition_broadcast(128), [], ["dtb"])
        self.ld(nea[:], W["a_log"].partition_broadcast(128), [], ["nea"])
        self.act(nea[:], nea[:], AF.Exp, ["nea"], ["nea"])
        self.ts("dve", nea[:], nea[:], -1.0, None, ALU.mult, None, ["nea"], ["nea"])
        self.tt("dve", g_all[:], ab_all[:, :, 0:16], dtb[:].unsqueeze(1).to_broadcast([128, NT, 16]), ALU.add, abk + ["dtb"], ["g_all"])
        uu = scz.sb("sp_u", [128, NT, 16]); la = scz.sb("sp_la", [128, NT, 16])
        qq = scz.sb("sp_q", [128, NT, 16]); mk = scz.sb("sp_mk", [128, NT, 16])
        self.act(uu[:], g_all[:], AF.Exp, ["g_all"], ["sp_u"])
        self.act(la[:], uu[:], AF.Ln, ["sp_u"], ["sp_la"], bias=1.0)
        self.ts("dve", qq[:], uu[:], 1.0 / 7, None, ALU.mult, None, ["sp_u"], ["sp_q"])
        for cc_ in (-1.0 / 6, 1.0 / 5, -1.0 / 4, 1.0 / 3, -1.0 / 2, 1.0):
            self.stt("dve", qq[:], qq[:], cc_, uu[:], ALU.add, ALU.mult, ["sp_q", "sp_u"], ["sp_q"])
        self.ts("dve", mk[:], uu[:], 0.25, None, ALU.is_lt, None, ["sp_u"], ["sp_mk"])
        self.tt("dve", qq[:], qq[:], la[:], ALU.subtract, ["sp_q", "sp_la"], ["sp_q"])
        self.tt("dve", qq[:], qq[:], mk[:], ALU.mult, ["sp_q", "sp_mk"], ["sp_q"])
        self.tt("dve", g_all[:], la[:], qq[:], ALU.add, ["sp_la", "sp_q"], ["g_all"])
        self.tt("dve", g_all[:], g_all[:], nea[:].unsqueeze(1).to_broadcast([128, NT, 16]), ALU.mult, ["g_all", "nea"], ["g_all"])
        self.act(beta_all[:], ab_all[:, :, 16:32], AF.Sigmoid, abk, ["beta_all"])
        self.act(lnb_all[:], beta_all[:], AF.Ln, ["beta_all"], ["lnb_all"])
        scz.close()
        if self.stage >= 31:
            self.dn_scan(0, dict(qT_d=qT_d, kT_d=kT_d, ktok_d=ktok_d, vtok_d=vtok_d, sz_d=sz_d, of_d=of_d), g_all, beta_all, lnb_all)
        if self.stage >= 32:
            self.dn_scan(1, dict(qT_d=qT_d, kT_d=kT_d, ktok_d=ktok_d, vtok_d=vtok_d, sz_d=sz_d, of_d=of_d), g_all, beta_all, lnb_all)
        if self.debug:
            d_gates = self.nc.dram_tensor("d_gates", [128, 3, NT * 16], F32, kind="ExternalOutput").ap()
            for i, (t, k) in enumerate(((g_all, "g_all"), (beta_all, "beta_all"), (lnb_all, "lnb_all"))):
                self.ld(d_gates[:, i, :], t[:, :, :].rearrange("p j e -> p (j e)"), [k], [("d_gates", i)])
        scL.close()
        if self.stage >= 33:
            self.dn_out(of_d)

    def dn_scan(self, dr, dd, g_all, beta_all, lnb_all):
        nc, S, W = self.nc, self.S, self.W[1]
        cst, c2 = self.cst, self.cst2
        ident = cst[:, C_ID:C_ID + 128]
        ones = cst[:, C_ONES:C_ONES + 128]
        zcol = cst[:, C_U:C_U + 1]
        Ltri = c2[:, C2_LF:C2_LF + 128] if dr == 0 else c2[:, C2_LB:C2_LB + 128]
        LT, GT = c2[:, C2_LT:C2_LT + 128], c2[:, C2_GT:C2_GT + 128]
        GE, LE = c2[:, C2_GE:C2_GE + 128], c2[:, C2_LE:C2_LE + 128]
        m_db, m_dbt, m_dt = (LT, GT, GE) if dr == 0 else (GT, LT, LE)
        order = ([32, 33] + list(range(32))) if dr == 0 else ([33, 32] + list(range(31, -1, -1)))
        if self.n_tiles is not None:
            order = order[:self.n_tiles]
        sc = Scope(self)
        sfx = "_%d" % dr

        def bankk(b):
            return [self.pk(b, 0), self.pk(b, 1)]

        def zfill(t, k):
            sh = list(t.shape)
            self.cp("dve", t[:], zcol.to_broadcast(sh) if len(sh) == 2 else zcol.unsqueeze(1).to_broadcast(sh), ["cst"], [k])


        def z3(name, w, dt=F32R, fill=True):
            t = sc.sb(name + sfx, [128, 8, w], dt)
            if fill:
                for b_ in range(4):
                    self.cp("dve", t[:, 2 * b_:2 * b_ + 2, :], zcol.unsqueeze(1).to_broadcast([128, 2, w]), ["cst"], [(name, b_)])
            return t

        Sst = z3("S", 256)
        Xb = [z3("X0", 256)]
        qkT = z3("qkT", 128, fill=False)
        vb = z3("vb", 256); kbe = z3("kbe", 128, fill=False); kd = z3("kd", 128, fill=False)
        qeT = z3("qeT", 128, fill=False); usb = z3("usb", 128, F32, fill=False)
        wT = z3("wT", 128, fill=False); vnew = z3("vn", 256); Sdec = z3("Sdec", 128, F32, fill=False)
        Mf = z3("Mf", 128, F32, fill=False); Ao = z3("Ao", 128, F32, fill=False)
        Xp = [z3("Xa", 128, F32, fill=False), z3("Xb2", 128, F32, fill=False)]
        XPN = ["Xa", "Xb2"]
        ETf = z3("ETf", 128, F32, fill=False)
        Td = z3("Td", 128, F32, fill=False); Uf = z3("Uf", 128, F32, fill=False)
        negIf = sc.sb("negIf" + sfx, [128, 128])
        self.ts("dve", negIf[:], ident, -1.0, None, ALU.mult, None, ["cst"], ["negIf"])
        m64b = c2[:, C2_B64:C2_B64 + 128].unsqueeze(1).to_broadcast([128, 8, 128])
        nm64b = c2[:, C2_NB64:C2_NB64 + 128].unsqueeze(1).to_broadcast([128, 8, 128])
        offb = c2[:, C2_OFF:C2_OFF + 128].unsqueeze(1).to_broadcast([128, 8, 128])
        m64b2 = c2[:, C2_B64:C2_B64 + 128].unsqueeze(1).to_broadcast([128, 2, 128])
        nm64b2 = c2[:, C2_NB64:C2_NB64 + 128].unsqueeze(1).to_broadcast([128, 2, 128])
        offb2 = c2[:, C2_OFF:C2_OFF + 128].unsqueeze(1).to_broadcast([128, 2, 128])
        kqr = Ring(sc, "kq" + sfx, [128, 8, 256], F32R, 2)
        ktr = Ring(sc, "kt" + sfx, [128, D], F32, 1)
        vtr = Ring(sc, "vt" + sfx, [128, D], F32, 1)
        otr = Ring(sc, "ot" + sfx, [128, D], F32, 1)
        Dg2 = [sc.sb("Dg%d" % i + sfx, [128, 2, 8, 128]) for i in range(2)]
        gsm2 = [sc.sb("gsm%d" % i + sfx, [128, 6, 8]) for i in range(2)]
        DB = sc.sb("DB" + sfx, [128, 8, 128]); DBT = sc.sb("DBT" + sfx, [128, 8, 128])
        DT = sc.sb("DT" + sfx, [128, 8, 128]); Ec = sc.sb("Ec" + sfx, [128, 8, 128])
        if dr == 1:
            ofr = Ring(sc, "of" + sfx, [128, D], F32, 1)
            szr = Ring(sc, "sz" + sfx, [128, D], F32, 1)
            ssq = sc.sb("ssq", [128, 8])
            onb = sc.sb("onb", [128, 128])
            self.ld(onb[:], W["o_norm"].partition_broadcast(128), [], ["onb"])
        NIT = 5
        P4 = range(4)

        def A(b_):
            return self.ps[b_][:, :].rearrange("p (s c) -> p s c", s=2), [("ps", b_)]

        def B(b_):
            return self.ps[4 + b_][:, :].rearrange("p (s c) -> p s c", s=2), [("ps", 4 + b_)]

        def keys(name):
            return [(name, b_) for b_ in P4]

        idb8 = ident.unsqueeze(1).to_broadcast([128, 8, 128])
        idb2 = ident.unsqueeze(1).to_broadcast([128, 2, 128])
        gk = ["g_all", "beta_all", "lnb_all"]

        def gates(jj_, sl_):
            gj_ = g_all[:, jj_, dr * 8:(dr + 1) * 8]
            lbj_ = lnb_all[:, jj_, dr * 8:(dr + 1) * 8]
            gs_ = gsm2[sl_]
            gsk = ("gsm", sl_)
            pg = self.ps[7]
            self.mm(pg[:, 0:8], Ltri, gj_, True, True, ["cst2"] + gk, bankk(7))
            self.mm(pg[:, 8:16], ones, gj_, True, True, ["cst"] + gk, bankk(7))
            gc_, gb_, ebg_, ekd_, egl_, tmpg_ = (gs_[:, i, :] for i in range(6))
            self.cp("act", gc_, pg[:, 0:8], bankk(7), [gsk])
            self.tt("dve", gb_, gc_, lbj_, ALU.add, [gsk] + gk, [gsk])
            self.act(ebg_, gb_, AF.Exp, [gsk], [gsk])
            self.tt("dve", tmpg_, pg[:, 8:16], gc_, ALU.subtract, bankk(7) + [gsk], [gsk])
            self.act(ekd_, tmpg_, AF.Exp, [gsk], [gsk])
            self.act(egl_, pg[:, 8:16], AF.Exp, bankk(7), [gsk])
            self.tt("pool", Dg2[sl_][:, 0, :, :], idb8, gc_.unsqueeze(2).to_broadcast([128, 8, 128]), ALU.mult, ["cst", gsk], [("Dg0", sl_)])
            self.tt("pool", Dg2[sl_][:, 1, :, :], idb8, gb_.unsqueeze(2).to_broadcast([128, 8, 128]), ALU.mult, ["cst", gsk], [("Dg1", sl_)])

        def kq_load(jj_):
            tsl_ = slice(jj_ * 128, (jj_ + 1) * 128)
            kq_, kqk_ = kqr.next()
            self.ld(kq_[:, :, 0:128], dd["kT_d"][:, tsl_].rearrange("(h p) t -> p h t", p=128), [], [kqk_ + ("k",)], q="pool")
            keys_ = [kqk_ + ("k",)]
            if jj_ < NT_LAT:
                self.ld(kq_[:, :, 128:256], dd["qT_d"][:, tsl_].rearrange("(h p) t -> p h t", p=128), [], [kqk_ + ("q",)], q="pool")
                keys_.append(kqk_ + ("q",))
            return kq_, kqk_, keys_

        gates(order[0], 0)
        kq_nxt = kq_load(order[0])
        for oi, jj in enumerate(order):
            sl = oi % 2
            gsk = ("gsm", sl)
            Dg = Dg2[sl]
            lat = jj < NT_LAT
            tsl = slice(jj * 128, (jj + 1) * 128)
            kq, kqk, kqkeys = kq_nxt
            if oi + 1 < len(order):
                kq_nxt = kq_load(order[oi + 1])
            kt, ktk = ktr.next()
            self.ld(kt[:], dd["ktok_d"][tsl, :], [], [ktk])
            vt, vtk = vtr.next()
            self.ld(vt[:], dd["vtok_d"][tsl, :], [], [vtk])
            kt3 = kt[:, :].rearrange("p (h d) -> p h d", h=8)
            vt3 = vt[:, :].rearrange("p (h d) -> p h d", h=8)
            bj = beta_all[:, jj, dr * 8:(dr + 1) * 8]
            gc, gb, ebg, ekd, egl, tmpg = (gsm2[sl][:, i, :] for i in range(6))
            for half in range(2):
                self.mm(self.ps[4 + half][:, :], ones, Dg[:, 0, half * 4:(half + 1) * 4, :], True, True, ["cst", ("Dg0", sl)], bankk(4 + half))
                self.mm(self.ps[6 + half][:, :], ones, Dg[:, 1, half * 4:(half + 1) * 4, :], True, True, ["cst", ("Dg1", sl)], bankk(6 + half))
            ncol = 256 if lat else 128
            for b_ in P4:
                ap_, apk = A(b_)
                for s_ in range(2):
                    h = 2 * b_ + s_
                    self.mm(ap_[:, s_, 0:ncol], kq[:, h, 0:128], kq[:, h, 0:ncol], True, True, kqkeys, apk)
            if oi + 1 < len(order):
                gates_next = (order[oi + 1], 1 - sl)
            else:
                gates_next = None
            for half in range(2):
                hs = slice(half * 4, half * 4 + 4)
                pRc = self.ps[4 + half][:, :].rearrange("p (h f) -> p h f", h=4)
                pRb = self.ps[6 + half][:, :].rearrange("p (h f) -> p h f", h=4)
                gbb = gb[:, hs].unsqueeze(2).to_broadcast([128, 4, 128])
                gcb = gc[:, hs].unsqueeze(2).to_broadcast([128, 4, 128])
                self.tt("dve", DB[:, hs, :], gbb, pRc, ALU.subtract, [gsk] + bankk(4 + half), ["DB"])
                self.tt("pool", DB[:, hs, :], DB[:, hs, :], m_db.unsqueeze(1).to_broadcast([128, 4, 128]), ALU.add, ["DB", "cst2"], ["DB"])
                self.tt("dve", DBT[:, hs, :], pRb, gcb, ALU.subtract, [gsk] + bankk(6 + half), ["DBT"])
                self.tt("pool", DBT[:, hs, :], DBT[:, hs, :], m_dbt.unsqueeze(1).to_broadcast([128, 4, 128]), ALU.add, ["DBT", "cst2"], ["DBT"])
                if lat:
                    self.tt("dve", DT[:, hs, :], pRc, gcb, ALU.subtract, [gsk] + bankk(4 + half), ["DT"])
                    self.tt("pool", DT[:, hs, :], DT[:, hs, :], m_dt.unsqueeze(1).to_broadcast([128, 4, 128]), ALU.add, ["DT", "cst2"], ["DT"])
                    self.act(Ec[:, hs, :], pRc, AF.Exp, bankk(4 + half), ["Ec"])
            self.act(DB[:], DB[:], AF.Exp, ["DB"], ["DB"])
            self.act(DBT[:], DBT[:], AF.Exp, ["DBT"], ["DBT"])
            if lat:
                self.act(DT[:], DT[:], AF.Exp, ["DT"], ["DT"])
            for b_ in P4:
                ap_, apk = A(b_)
                hp = slice(2 * b_, 2 * b_ + 2)
                self.tt("dve", Mf[:, hp, :], ap_[:, :, 0:128], DB[:, hp, :], ALU.mult, apk + ["DB"], [("Mf", b_)])
                self.tt("dve", Xp[0][:, hp, :], ap_[:, :, 0:128], DBT[:, hp, :], ALU.mult, apk + ["DBT"], [("Xa", b_)])
                if lat:
                    self.tt("dve", qkT[:, hp, :], ap_[:, :, 128:256], DT[:, hp, :], ALU.mult, apk + ["DT"], [("qkT", b_)])
            for b_ in P4:
                hp = slice(2 * b_, 2 * b_ + 2)
                self.tt("pool", Ao[:, hp, :], Mf[:, hp, :], offb2, ALU.mult, [("Mf", b_), "cst2"], [("Ao", b_)])
                self.tt("pool", Mf[:, hp, :], Mf[:, hp, :], m64b2, ALU.mult, [("Mf", b_), ("Ao", b_), "cst2"], [("Mf", b_)])
                self.tt("pool", Mf[:, hp, :], Mf[:, hp, :], idb2, ALU.add, [("Mf", b_), "cst"], [("Mf", b_)])
                self.tt("pool", Xp[0][:, hp, :], Xp[0][:, hp, :], nm64b2, ALU.mult, [("Xa", b_), "cst2"], [("Xa", b_)])
                self.tt("pool", Xp[0][:, hp, :], Xp[0][:, hp, :], idb2, ALU.add, [("Xa", b_), "cst"], [("Xa", b_)])
            if gates_next is not None:
                gates(*gates_next)
            cur = 0
            for it in range(NIT):
                src, srck = Xp[cur], XPN[cur]
                dst, dstk = Xp[1 - cur], XPN[1 - cur]
                for b_ in P4:
                    ap_, apk = A(b_)
                    for s_ in range(2):
                        h = 2 * b_ + s_
                        self.mm(ap_[:, s_, 0:128], src[:, h, :], Mf[:, h, :], True, True, [(srck, b_), ("Mf", b_)], apk)
                for b_ in P4:
                    ap_, apk = A(b_)
                    self.stt("dve", ETf[:, 2 * b_:2 * b_ + 2, :], ap_[:, :, 0:128], -1.0, idb2, ALU.mult, ALU.add, apk + ["cst"], [("ETf", b_)])
                for b_ in P4:
                    bp_, bpk = B(b_)
                    for s_ in range(2):
                        h = 2 * b_ + s_
                        self.mm(bp_[:, s_, 0:128], ETf[:, h, :], src[:, h, :], True, True, [("ETf", b_), (srck, b_)], bpk)
                for b_ in P4:
                    bp_, bpk = B(b_)
                    hp = slice(2 * b_, 2 * b_ + 2)
                    self.tt("dve", dst[:, hp, :], src[:, hp, :], bp_[:, :, 0:128], ALU.add, [(srck, b_)] + bpk, [(dstk, b_)])
                cur = 1 - cur
            Xd, Xdk = Xp[cur], XPN[cur]
            for b_ in P4:
                ap_, apk = A(b_)
                bp_, bpk = B(b_)
                for s_ in range(2):
                    h = 2 * b_ + s_
                    self.tr(ap_[:, s_, 0:128], Xd[:, h, :], [(Xdk, b_)], apk)
                    self.mm(bp_[:, s_, 0:128], Ao[:, h, :], Xd[:, h, :], True, True, [("Ao", b_), (Xdk, b_)], bpk)
            for b_ in P4:
                ap_, apk = A(b_)
                bp_, bpk = B(b_)
                hp = slice(2 * b_, 2 * b_ + 2)
                self.cp("act", Td[:, hp, :], ap_[:, :, 0:128], apk, [("Td", b_)])
                self.cp("act", Uf[:, hp, :], bp_[:, :, 0:128], bpk, [("Uf", b_)])
            for b_ in P4:
                ap_, apk = A(b_)
                for s_ in range(2):
                    h = 2 * b_ + s_
                    self.mm(ap_[:, s_, 0:128], Td[:, h, :], Uf[:, h, :], True, True, [("Td", b_), ("Uf", b_)], apk)
            for b_ in P4:
                ap_, apk = A(b_)
                hp = slice(2 * b_, 2 * b_ + 2)
                self.tt("dve", Xb[0][:, hp, 0:128], Xd[:, hp, :], ap_[:, :, 0:128], ALU.subtract, [(Xdk, b_)] + apk, [("X0", b_)])
            cur = 0
            XN = ["X0"]
            Xf, kf_ = Xb[cur], XN[cur]
            self.tt("dve", vb[:, :, 0:128], vt3, bj.unsqueeze(2).to_broadcast([128, 8, 128]), ALU.mult, [vtk] + gk, keys("vb"))
            self.tt("pool", kbe[:, :, :], kt3, ebg.unsqueeze(2).to_broadcast([128, 8, 128]), ALU.mult, [ktk, gsk], keys("kbe"))
            self.tt("pool", kd[:, :, :], kt3, ekd.unsqueeze(2).to_broadcast([128, 8, 128]), ALU.mult, [ktk, gsk], keys("kd"))
            if lat:
                self.tt("pool", qeT[:, :, :], kq[:, :, 128:256], Ec[:, :, :], ALU.mult, kqkeys + ["Ec"], keys("qeT"))
            for b_ in P4:
                ap_, apk = A(b_)
                bp_, bpk = B(b_)
                for s_ in range(2):
                    h = 2 * b_ + s_
                    self.mm(ap_[:, s_, :], Xf[:, h, 0:128], vb[:, h, :], True, True, [(kf_, b_), ("vb", b_)], apk)
                    self.mm(bp_[:, s_, :], kbe[:, h, :], Xf[:, h, :], True, True, [(kf_, b_), ("kbe", b_)], bpk)
            for b_ in P4:
                ap_, apk = A(b_)
                bp_, bpk = B(b_)
                hp = slice(2 * b_, 2 * b_ + 2)
                self.cp("act", usb[:, hp, :], ap_[:, :, 0:128], apk, [("usb", b_)])
                self.cp("act", wT[:, hp, :], bp_[:, :, 0:128], bpk, [("wT", b_)])
            ot, otk = otr.next()
            ot3 = ot[:, :].rearrange("p (h d) -> p h d", h=8)
            for b_ in P4:
                ap_, apk = A(b_)
                for s_ in range(2):
                    h = 2 * b_ + s_
                    self.mm(ap_[:, s_, :], wT[:, h, :], Sst[:, h, :], True, True, [("wT", b_), ("S", b_)], apk)
            for b_ in P4:
                ap_, apk = A(b_)
                hp = slice(2 * b_, 2 * b_ + 2)
                self.tt("dve", vnew[:, hp, 0:128], usb[:, hp, :], ap_[:, :, 0:128], ALU.subtract, [("usb", b_)] + apk, [("vn", b_)])
            for b_ in P4:
                ap_, apk = A(b_)
                bp_, bpk = B(b_)
                for s_ in range(2):
                    h = 2 * b_ + s_
                    if lat:
                        self.mm(bp_[:, s_, :], qeT[:, h, :], Sst[:, h, :], True, False, [("qeT", b_), ("S", b_)], bpk)
                        self.mm(bp_[:, s_, :], qkT[:, h, :], vnew[:, h, :], False, True, [("qkT", b_), ("vn", b_)], bpk)
                    self.mm(ap_[:, s_, :], kd[:, h, :], vnew[:, h, :], True, True, [("kd", b_), ("vn", b_)], apk)
            self.tt("pool", Sdec[:, :, :], Sst[:, :, 0:128], egl.unsqueeze(2).to_broadcast([128, 8, 128]), ALU.mult,
                    keys("S") + [gsk], keys("Sdec"))
            for b_ in P4:
                ap_, apk = A(b_)
                bp_, bpk = B(b_)
                hp = slice(2 * b_, 2 * b_ + 2)
                if lat:
                    self.cp("act", ot3[:, hp, :], bp_[:, :, 0:128], bpk, [otk])
                self.tt("dve", Sst[:, hp, 0:128], Sdec[:, hp, :], ap_[:, :, 0:128], ALU.add, [("Sdec", b_)] + apk, [("S", b_)])
            if not lat:
                continue
            if dr == 0:
                self.ld(dd["of_d"][tsl, :], ot[:], [otk], [("of_d", jj)])
            else:
                if self.debug:
                    if not hasattr(self, "ob_d"):
                        self.ob_d = self.nc.dram_tensor("ob_d", [NLAT, D], F32, kind="ExternalOutput").ap()
                    self.ld(self.ob_d[tsl, :], ot[:], [otk], [("ob_d", jj)])
                of, ofk = ofr.next()
                self.ld(of[:], dd["of_d"][tsl, :], [("of_d", jj)], [ofk])
                szt, szk = szr.next()
                self.ld(szt[:], dd["sz_d"][tsl, :], [], [szk])
                self.tt("pool", ot[:], ot[:], of[:], ALU.add, [otk, ofk], [otk])
                self.act(DB[:, :, :].rearrange("p h d -> p (h d)"), ot[:], AF.Square, [otk], ["DB"])
                S.op("dve", lambda e: e.tensor_reduce(out=ssq[:], in_=DB[:, :, :], axis=AX.X, op=ALU.add),
                     ["DB"], ["ssq"])
                self.ts("dve", ssq[:], ssq[:], 1.0 / 128, EPS, ALU.mult, ALU.add, ["ssq"], ["ssq"])
                self.act(ssq[:], ssq[:], AF.Sqrt, ["ssq"], ["ssq"])
                S.op("dve", lambda e: e.reciprocal(out=ssq[:], in_=ssq[:]), ["ssq"], ["ssq"])
                o3 = ot[:, :].rearrange("p (h d) -> p h d", h=8)
                self.tt("dve", o3, o3, ssq[:].unsqueeze(2).to_broadcast([128, 8, 128]), ALU.mult, [otk, "ssq"], [otk])
                self.tt("pool", o3, o3, onb[:].unsqueeze(1).to_broadcast([128, 8, 128]), ALU.mult, [otk, "onb"], [otk])
                self.tt("pool", ot[:], ot[:], szt[:], ALU.mult, [otk, szk], [otk])
                self.ld(dd["of_d"][tsl, :], ot[:], [otk, ofk], [("of_d", jj)])
        sc.close()

    def dn_out(self, of_d):
        nc, S, W = self.nc, self.S, self.W[1]
        sc = Scope(self)
        wo = sc.sb("wo1", [128, 8, D], F32R)
        self.ld(wo[:], W["w_o"].rearrange("(k p) n -> p k n", p=128), [], ["wo1"], q="pool")
        wr = sc.sb("wr1", [128, 8, NE])
        self.ld(wr[:], W["router"].rearrange("(k p) n -> p k n", p=128), [], ["wr"])
        yr = Ring(sc, "oy", [128, D], F32, 2)
        xr = Ring(sc, "ox", [128, D], F32, 2)
        xmr = Ring(sc, "oxm", [128, D], F32, 3)
        ssr = Ring(sc, "oss", [128, 1], F32, 4)
        junk = sc.sb("ojunk", [128, D])
        yT = sc.sb("oyT", [128, 8, 128], F32R)
        h2T = sc.sb("oh2T", [128, 8, 128])
        lg = sc.sb("olg", [128, NE]); mx = sc.sb("omx", [128, 1]); sm = sc.sb("osm", [128, 1])
        pend = [None]
        for i in range(NT_LAT):
            yt, ytk = yr.next()
            self.ld(yt[:], of_d[i * 128:(i + 1) * 128, :], [], [ytk])
            xt, xk = xr.next()
            self.ld(xt[:], self.xres[0][i * 128:(i + 1) * 128, :], [], [xk])
            for kc in range(8):
                self.tr(self.ps[kc // 4][:, (kc % 4) * 128:(kc % 4 + 1) * 128], yt[:, kc * 128:(kc + 1) * 128], [ytk], [self.psk[kc // 4]])
            for b in range(2):
                self.cp("act", yT[:, b * 4:(b + 1) * 4, :], self.ps[b][:, :].rearrange("p (k t) -> p k t", k=4), [self.psk[b]], ["yT"])
            if pend[0] is not None:
                self.moe_prep(*pend[0])
                pend[0] = None
            xm, xmk = xmr.next()
            for dh in range(2):
                pM, pMk = self.ps[2 + dh], self.psk[2 + dh]
                for kc in range(8):
                    self.mm(pM[:, :], yT[:, kc, :], wo[:, kc, dh * 512:(dh + 1) * 512], kc == 0, kc == 7, ["yT", "wo1"], [pMk])
                self.tt("dve", xm[:, dh * 512:(dh + 1) * 512], pM[:, :], self.Gbc[:, 0, 0, dh * 512:(dh + 1) * 512], ALU.mult,
                        [pMk, ("Gbc", 0, 0)], [xmk])
            self.tt("dve", xm[:], xm[:], xt[:], ALU.add, [xmk, xk], [xmk])
            self.ld(self.xres[1][i * 128:(i + 1) * 128, :], xm[:], [xmk], [("xres1", i)])
            pend[0] = (i, 0, xm, xmk, junk, ssr, wr, h2T, lg, mx, sm)
        self.moe_prep(*pend[0])
        sc.close()

    def final_norm(self):
        nc, S = self.nc, self.S
        sc = Scope(self)
        fn = sc.sb("fnb", [128, D])
        self.ld(fn[:], self.inp["final_norm"].partition_broadcast(128), [], ["fnb"])
        xr = Ring(sc, "fx", [128, D], F32, 3)
        ssr = Ring(sc, "fss", [128, 1], F32, 4)
        junk = sc.sb("fjunk", [128, D])
        for i in range(NT_LAT):
            xt, xk = xr.next()
            self.ld(xt[:], self.xres[1][i * 128:(i + 1) * 128, :], [], [xk])
            ss, ssk = ssr.next()
            self.rstd_of(xt[:], xk, junk[:], "fjunk", ss[:], ssk)
            self.stt("dve", xt[:], xt[:], ss[:, 0:1], fn[:], ALU.mult, ALU.mult, [xk, ssk, "fnb"], [xk])
            S.dma("sp", lambda e: e.dma_start(out=self.out[i * 128:(i + 1) * 128, :], in_=xt[:]), [xk], [("out", i)], is_output=True)
        sc.close()

    def moe(self, l, xres, xresk, with_ctx):
        nc, S, W = self.nc, self.S, self.W[l]
        cst = self.cst
        ones = cst[:, C_ONES:C_ONES + 128]
        Umat = cst[:, C_U:C_U + 128]
        iota = cst[:, C_IOTA:C_IOTA + 512]
        sets = [(0, 0, NT_LAT, CAP_LAT)] + ([(1, NT_LAT, NT_CTX, CAP_CTX)] if with_ctx else [])
        sc = Scope(self)
        slot_m = {}; meta = {}
        for (si, j0, nj, cap) in sets:
            slot_m[si] = sc.sb("slotm%d_%d" % (si, l), [128, nj, NE])
            meta[si] = sc.sb("meta%d_%d" % (si, l), [128, nj, NE, 4], F32R)
        scr = Scope(self)
        for (si, j0, nj, cap) in sets:
            sfx = "%d_%d" % (si, l)
            affv = self.aff[:, j0:j0 + nj, :]
            affk = [("aff", j) for j in range(j0, j0 + nj)]
            lo = scr.sb("lo" + sfx, [128, NE]); mid = scr.sb("mid" + sfx, [128, NE])
            cmpt = scr.sb("cmp" + sfx, [128, nj, NE]); cnt = scr.sb("cnt" + sfx, [128, NE])
            tq = scr.sb("tq" + sfx, [128, NE])
            offs = scr.sb("offs" + sfx, [128, nj, NE]); slot = scr.sb("slot" + sfx, [128, nj, NE])
            S.op("dve", lambda e: e.memset(lo[:], 0.0), [], ["lo"])
            S.op("dve", lambda e: e.memset(mid[:], 0.5), [], ["mid"])
            pC, pCk = self.ps[0], self.psk[0]
            for it in range(NBIS):
                w = 2.0 ** -(it + 1)
                self.tt("dve", cmpt[:], affv, mid[:].unsqueeze(1).to_broadcast([128, nj, NE]), ALU.is_ge, affk + ["mid"], ["cmp"])
                S.op("dve", lambda e: e.tensor_reduce(out=cnt[:], in_=cmpt[:, :, :].rearrange("p j e -> p e j"), axis=AX.X, op=ALU.add),
                     ["cmp"], ["cnt"])
                self.mm(pC[:, 0:NE], ones, cnt[:], True, True, ["cst", "cnt"], [pCk])
                self.ts("dve", tq[:], pC[:, 0:NE], cap - 0.5, w, ALU.is_ge, ALU.mult, [pCk], ["tq"])
                self.tt("dve", lo[:], lo[:], tq[:], ALU.add, ["lo", "tq"], ["lo"])
                self.ts("dve", mid[:], lo[:], w * 0.5, None, ALU.add, None, ["lo"], ["mid"])
            self.tt("dve", cmpt[:], affv, lo[:].unsqueeze(1).to_broadcast([128, nj, NE]), ALU.is_ge, affk + ["lo"], ["cmp"])
            pP, pPk = self.ps[1], self.psk[1]
            pT, pTk = self.ps[2], self.psk[2]
            mflat = cmpt[:, :, :].rearrange("p j e -> p (j e)")
            self.mm(pP[:, 0:nj * NE], Umat, mflat, True, True, ["cst", "cmp"], [pPk])
            self.mm(pT[:, 0:nj * NE], ones, mflat, True, True, ["cst", "cmp"], [pTk])
            pTv = pT[:, 0:nj * NE].rearrange("p (j e) -> p j e", e=NE)
            pPv = pP[:, 0:nj * NE].rearrange("p (j e) -> p j e", e=NE)
            S.op("dve", lambda e: e.memset(offs[:, 0, :], 0.0), [], ["offs"])
            for j in range(1, nj):
                self.tt("dve", offs[:, j, :], pTv[:, j - 1, :], offs[:, j - 1, :], ALU.add, [pTk, "offs"], ["offs"])
            self.tt("dve", slot[:], pPv, offs[:], ALU.add, [pPk, "offs"], ["slot"])
            self.ts("dve", cmpt[:], cmpt[:], -1.0e6, 1.0e6, ALU.mult, ALU.add, ["cmp"], ["cmp"])
            self.tt("dve", slot_m[si][:], slot[:], cmpt[:], ALU.add, ["slot", "cmp"], [("slotm", si)])
            mt = meta[si]
            for j in range(nj):
                self.cp("dve", mt[:, j, :, 0:1], cst[:, C_U:C_U + 1].unsqueeze(1).to_broadcast([128, NE, 1]), ["cst"], [("meta", si)])
                self.ts("dve", mt[:, j, :, 0:1], mt[:, j, :, 0:1], float(j0 + j), None, ALU.add, None, [("meta", si)], [("meta", si)])
            mtf = mt[:, :, :, :].rearrange("p j e c -> p (j e) c")
            self.cp("dve", mtf[:, :, 1:2], cst[:, C_PIDX:C_PIDX + 1].unsqueeze(1).to_broadcast([128, nj * NE, 1]), ["cst"], [("meta", si)])
            self.cp("dve", mtf[:, :, 2:3], affv.rearrange("p j e -> p (j e)").unsqueeze(2), affk, [("meta", si)])
            self.cp("dve", mtf[:, :, 3:4], cst[:, C_ONES:C_ONES + 1].unsqueeze(1).to_broadcast([128, nj * NE, 1]), ["cst"], [("meta", si)])
        scr.close()

        NW = 8
        wring = Ring(sc, "wm%d" % l, [128, 8, 256], F32R, NW)
        xsT = sc.sb("xsT%d" % l, [128, 8, 640], F32R)
        hidT = sc.sb("hidT%d" % l, [128, 16, 544], F32R)
        ysb = sc.sb("ysb%d" % l, [128, 5, D])
        xsr = Ring(sc, "xstok%d" % l, [128, D], F32, 2)
        selr = Ring(sc, "sel%d" % l, [128, 512], F32R, 2)
        selc = sc.sb("selc%d" % l, [128, 128], F32R)
        sgr = Ring(sc, "sg%d" % l, [128, 512], F32, 2)
        hcr = Ring(sc, "hc%d" % l, [32, 256], F32, 2)
        idxrow = sc.sb("idxrow%d" % l, [4, 640])
        metac = sc.sb("metac%d" % l, [128, 5, 4])
        tmp5 = sc.sb("tmp5%d" % l, [128, 5]); idxf = sc.sb("idxf%d" % l, [128, 5])
        idur = Ring(sc, "idu%d" % l, [128, 5], U32, 3)
        gcr = Ring(sc, "gc%d" % l, [128, 5], F32, 3)
        self.cp("dve", selc[:], cst[:, C_U:C_U + 1].to_broadcast([128, 128]), ["cst"], ["selc"])
        S.op("dve", lambda e: e.memset(ysb[:], 0.0), [], ["ysb"])
        nk = 5 if with_ctx else 4
        c2 = {0: 0, 1: 1}

        def idx_phase(e):
            pI, pIk = self.ps[7], self.psk[7]
            for (si, j0, nj, cap) in sets:
                ncol = 512 if si == 0 else 128
                for j in range(nj):
                    if si == 0:
                        sel, selk = selr.next()
                        self.ts("dve", sel[:], iota, slot_m[si][:, j, e:e + 1], None, ALU.is_equal, None, ["cst", ("slotm", si)], [selk])
                        rhs = sel[:]
                    else:
                        selk = "selc"
                        self.ts("dve", selc[:, 0:CAP_CTX], iota[:, 0:CAP_CTX], slot_m[si][:, j, e:e + 1], None, ALU.is_equal, None,
                                ["cst", ("slotm", si)], [selk])
                        rhs = selc[:]
                    self.mm(pI[0:4, 0:ncol], meta[si][:, j, e, :], rhs, j == 0, j == nj - 1, [("meta", si), selk], [pIk])
                off = 0 if si == 0 else 512
                self.cp("act", idxrow[0:4, off:off + ncol], pI[0:4, 0:ncol], [pIk], ["idxrow"])
            pX, pXk = self.ps[7], self.psk[7]
            for k in range(nk):
                self.tr(pX[:, k * 4:(k + 1) * 4], idxrow[0:4, k * 128:(k + 1) * 128], ["idxrow"], [pXk], kp=4)
            self.cp("act", metac[:, 0:nk, :], pX[:, 0:nk * 4].rearrange("p (k c) -> p k c", c=4), [pXk], ["metac"])
            idu, iduk = idur.next()
            gc, gck = gcr.next()
            self.stt("dve", idxf[:, 0:nk], metac[:, 0:nk, 0], 128.0, metac[:, 0:nk, 1], ALU.mult, ALU.add, ["metac"], ["idxf"])
            self.ts("dve", tmp5[:, 0:nk], metac[:, 0:nk, 3], -1.0, 1.0, ALU.mult, ALU.add, ["metac"], ["tmp5"])
            self.tt("dve", tmp5[:, 0:nk], tmp5[:, 0:nk], cst[:, C_DMY:C_DMY + nk], ALU.mult, ["tmp5", "cst"], ["tmp5"])
            self.tt("dve", idxf[:, 0:nk], idxf[:, 0:nk], tmp5[:, 0:nk], ALU.add, ["idxf", "tmp5"], ["idxf"])
            self.cp("dve", idu[:, 0:nk], idxf[:, 0:nk], ["idxf"], [iduk])
            self.cp("dve", gc[:, 0:nk], metac[:, 0:nk, 2], ["metac"], [gck])
            return (idu, iduk, gc, gck)

        gt_i = [0]

        def gather_phase(ix):
            idu, iduk, gc, gck = ix
            for k in range(nk):
                xs, xsk = xsr.next()
                S.dma("pool", lambda e: e.indirect_dma_start(out=xs[:], out_offset=None, in_=self.xn2[:, :],
                                                              in_offset=bass.IndirectOffsetOnAxis(ap=idu[:, k:k + 1], axis=0)),
                      [iduk], [xsk])
                c = 1 if k == 4 else 0
                for half in range(2):
                    bnk = gt_i[0] % 4
                    gt_i[0] += 1
                    pt, ptk = self.ps[bnk], self.psk[bnk]
                    for kc in range(half * 4, half * 4 + 4):
                        self.tr(pt[:, (kc % 4) * 128:(kc % 4 + 1) * 128], xs[:, kc * 128:(kc + 1) * 128], [xsk], [ptk])
                    for kc in range(half * 4, half * 4 + 4):
                        self.act(xsT[:, kc, k * 128:(k + 1) * 128], pt[:, (kc % 4) * 128:(kc % 4 + 1) * 128], AF.Identity,
                                 [ptk, ("A", 4), "cols"], ["xsT"], scale=self.A2[:, kc, c:c + 1], bias=self.B2(kc, c))

        def wload(src_ap):
            wt, wk = wring.next()
            self.ld(wt[:], src_ap, [], [wk], q="pool")
            return wt, wk

        gu_i = [0]

        cpend = [None]

        def ctx_tr(hc, hck, fq):
            pt, ptk = self.ps[7], self.psk[7]
            for fc in range(2):
                self.tr(pt[:, fc * 32:(fc + 1) * 32], hc[0:32, fc * 128:(fc + 1) * 128], [hck], [ptk], kp=32)
            self.cp("act", hidT[:, fq * 2:fq * 2 + 2, 512:544], pt[:, 0:64].rearrange("p (a t) -> p a t", a=2), [ptk],
                    [("hidT", fq * 2), ("hidT", fq * 2 + 1)])

        def ffn1(e):
            for fq in range(8):
                wg, wgk = wload(W["w_gate"][e, :, fq * 256:(fq + 1) * 256].rearrange("(k p) n -> p k n", p=128))
                wu, wuk = wload(W["w_up"][e, :, fq * 256:(fq + 1) * 256].rearrange("(k p) n -> p k n", p=128))
                for fc in range(2):
                    fcc = fq * 2 + fc
                    b = gu_i[0] % 2
                    gu_i[0] += 1
                    pG, pGk = self.ps[2 * b], self.psk[2 * b]
                    pU, pUk = self.ps[2 * b + 1], self.psk[2 * b + 1]
                    for kc in range(8):
                        self.mm(pG[:, :], wg[:, kc, fc * 128:(fc + 1) * 128], xsT[:, kc, 0:512], kc == 0, kc == 7, [wgk, "xsT"], [pGk])
                    for kc in range(8):
                        self.mm(pU[:, :], wu[:, kc, fc * 128:(fc + 1) * 128], xsT[:, kc, 0:512], kc == 0, kc == 7, [wuk, "xsT"], [pUk])
                    sg, sgk = sgr.next()
                    self.act(sg[:, 0:512], pG[:, :], AF.Silu, [pGk], [sgk])
                    self.tt("dve", hidT[:, fcc, 0:512], sg[:, 0:512], pU[:, :], ALU.mult, [sgk, pUk], [("hidT", fcc)])
                if with_ctx:
                    pc, pck = self.ps[4], self.psk[4]
                    for kc in range(8):
                        self.mm(pc[0:32, 0:256], xsT[:, kc, 512:544], wg[:, kc, :], kc == 0, kc == 7, [wgk, "xsT"], [pck])
                    for kc in range(8):
                        self.mm(pc[0:32, 256:512], xsT[:, kc, 512:544], wu[:, kc, :], kc == 0, kc == 7, [wuk, "xsT"], [pck])
                    hc, hck = hcr.next()
                    self.act(hc[0:32, 0:256], pc[0:32, 0:256], AF.Silu, [pck], [hck])
                    self.tt("dve", hc[0:32, 0:256], hc[0:32, 0:256], pc[0:32, 256:512], ALU.mult, [hck, pck], [hck])
                    if cpend[0] is not None:
                        ctx_tr(*cpend[0])
                    cpend[0] = (hc, hck, fq)
            if cpend[0] is not None:
                ctx_tr(*cpend[0])
                cpend[0] = None

        y_i = [0]

        def ffn2(e, ix):
            idu, iduk, gc, gck = ix
            hk = [("hidT", f) for f in range(16)]
            for dq in range(4):
                wd = []
                for fh in range(2):
                    wd.append(wload(W["w_down"][e, fh * 1024:(fh + 1) * 1024, dq * 256:(dq + 1) * 256].rearrange("(k p) n -> p k n", p=128)))
                for k in range(nk):
                    if k < 4:
                        b = y_i[0] % 2
                        y_i[0] += 1
                        pY, pYk = self.ps[5 + b], self.psk[5 + b]
                        rows = 128
                        lsl = slice(k * 128, (k + 1) * 128)
                    else:
                        pY, pYk = self.ps[4], self.psk[4]
                        rows = 32
                        lsl = slice(512, 544)
                    for fcc in range(16):
                        wt, wk = wd[fcc // 8]
                        self.mm(pY[0:rows, 0:256], hidT[:, fcc, lsl], wt[:, fcc % 8, :], fcc == 0, fcc == 15, [("hidT", fcc), wk], [pYk])
                    c = 1 if k == 4 else 0
                    self.stt("dve", ysb[0:rows, k, dq * 256:(dq + 1) * 256], pY[0:rows, 0:256], gc[0:rows, k:k + 1],
                             self.Gbc[0:rows, 1, c, dq * 256:(dq + 1) * 256], ALU.mult, ALU.mult,
                             [pYk, gck, ("Gbc", 1, c)], [("ysb", k)])

        def scatter_phase(ix):
            idu, iduk, gc, gck = ix
            for k in range(nk):
                S.dma("pool", lambda e: e.indirect_dma_start(out=xres[:, :], out_offset=bass.IndirectOffsetOnAxis(ap=idu[:, k:k + 1], axis=0),
                                                              in_=ysb[:, k, :], in_offset=None, compute_op=ALU.add),
                      [iduk, ("ysb", k), "ysb"], ["xacc"])

        n_exp = NE if self.n_exp is None else self.n_exp
        ixs = {0: idx_phase(0)}
        gather_phase(ixs[0])
        for e in range(n_exp):
            if e + 1 < n_exp:
                ixs[e + 1] = idx_phase(e + 1)
            ffn1(e)
            if e > 0:
                scatter_phase(ixs[e - 1])
            if e + 1 < n_exp:
                gather_phase(ixs[e + 1])
            ffn2(e, ixs[e])
        scatter_phase(ixs[n_exp - 1])
        sc.close()


def _host_consts():
    cp = np.zeros((128, C_END), np.float32)
    cp[:, C_ID:C_ID + 128] = np.eye(128, dtype=np.float32)
    cp[:, C_ONES:C_ONES + 128] = 1.0
    pi = np.arange(128)
    cp[:, C_U:C_U + 128] = (pi[:, None] < pi[None, :]).astype(np.float32)
    mp = (pi[:, None] >= pi[None, :]).astype(np.float32)
    mn = (pi[:, None] <= pi[None, :]).astype(np.float32)
    cp[:, C_MP:C_MP + 512] = np.tile(mp, (1, 4))
    cp[:, C_MN:C_MN + 512] = np.tile(mn, (1, 4))
    cp[:, C_IOTA:C_IOTA + 512] = np.arange(512, dtype=np.float32)[None, :]
    cp[:, C_PIDX] = pi
    for k in range(5):
        cp[:, C_DMY + k] = NTOK + k * 128 + pi
    t = np.arange(NLAT)
    row = (t // 64).astype(np.float32)
    col = (t % 64).astype(np.float32)
    inv = (np.float32(10000.0) ** (-np.arange(16, dtype=np.float32) / np.float32(16))).astype(np.float32)
    ar = (row[:, None] * inv[None, :]).astype(np.float32)
    ac = (col[:, None] * inv[None, :]).astype(np.float32)
    rope = np.concatenate([np.cos(ar), np.cos(ac), np.sin(ar), np.sin(ac)], axis=1).astype(np.float32)
    return cp, rope


def _host_consts2():
    c2 = np.zeros((128, C2_END), np.float32)
    p = np.arange(128)[:, None]
    f = np.arange(128)[None, :]
    c2[:, C2_LF:C2_LF + 128] = (p <= f)
    c2[:, C2_LB:C2_LB + 128] = (p >= f)
    c2[:, C2_LT:C2_LT + 128] = np.where(f < p, 0.0, NEG)
    c2[:, C2_GT:C2_GT + 128] = np.where(f > p, 0.0, NEG)
    c2[:, C2_GE:C2_GE + 128] = np.where(f >= p, 0.0, NEG)
    c2[:, C2_LE:C2_LE + 128] = np.where(f <= p, 0.0, NEG)
    same = ((p // 64) == (f // 64)).astype(np.float32)
    c2[:, C2_B64:C2_B64 + 128] = same
    c2[:, C2_NB64:C2_NB64 + 128] = -same
    c2[:, C2_OFF:C2_OFF + 128] = 1.0 - same
    return c2


def _colT(v, n):
    return np.ascontiguousarray(np.asarray(v, np.float32).reshape(n, 128).T)


ALL_INPUTS = (
    "x", "c", "ctx", "c_ctx",
    "l0_ada_w", "l0_ada_b", "l0_norm_mix", "l0_w_qkv", "l0_sink", "l0_w_o", "l0_norm_ffn",
    "l0_router", "l0_w_gate", "l0_w_up", "l0_w_down",
    "l1_ada_w", "l1_ada_b", "l1_norm_mix", "l1_w_in", "l1_conv", "l1_a_log", "l1_dt_bias", "l1_o_norm", "l1_w_o",
    "l1_norm_ffn", "l1_router", "l1_w_gate", "l1_w_up", "l1_w_down",
    "final_norm",
)


def make_in_maps(inputs, cores):
    missing = [n for n in ALL_INPUTS if n not in inputs]
    assert not missing, missing
    cp, rope = _host_consts()
    shared = {"cpack": cp, "rope": rope}
    for l in (0, 1):
        p = "l%d_" % l
        shared[p + "ada_w"] = np.asarray(inputs[p + "ada_w"], np.float32)
        shared[p + "ada_b"] = np.asarray(inputs[p + "ada_b"], np.float32)
        shared[p + "ada_bT"] = _colT(inputs[p + "ada_b"], 48)
        shared[p + "nmixT"] = _colT(inputs[p + "norm_mix"], 8)
        shared[p + "nffnT"] = _colT(inputs[p + "norm_ffn"], 8)
        for n in ("router", "w_gate", "w_up", "w_down"):
            shared[p + n] = np.asarray(inputs[p + n], np.float32)
    for n in ("l0_sink", "l0_w_o", "l1_w_in", "l1_o_norm", "l1_w_o", "final_norm"):
        shared[n] = np.asarray(inputs[n], np.float32)
    shared["l1_a_log"] = np.asarray(inputs["l1_a_log"], np.float32).reshape(16)
    shared["l1_dt_bias"] = np.asarray(inputs["l1_dt_bias"], np.float32).reshape(16)
    cv = np.asarray(inputs["l1_conv"], np.float32)
    shared["l1_convT"] = np.ascontiguousarray(cv.reshape(3, 24, 128).transpose(2, 1, 0))
    shared["cpack2"] = _host_consts2()
    wq = np.asarray(inputs["l0_w_qkv"], np.float32)
    perm = [pr * 8 + s_ * 4 + g for pr in range(2) for g in range(4) for s_ in range(2)]
    cols = np.concatenate([np.arange(h * 64, (h + 1) * 64) for h in perm] + [np.arange(1024, 1536)])
    shared["l0_w_qkv"] = np.ascontiguousarray(wq[:, cols])
    maps = []
    for b in cores:
        m = dict(shared)
        m["x"] = np.ascontiguousarray(inputs["x"][b], dtype=np.float32)
        m["ctx"] = np.ascontiguousarray(inputs["ctx"][b], dtype=np.float32)
        cv = np.stack([np.asarray(inputs["c"][b], np.float32), np.asarray(inputs["c_ctx"], np.float32)], axis=1)
        m["cvecT"] = np.ascontiguousarray(cv.reshape(8, 128, 2).transpose(1, 0, 2))
        maps.append(m)
    return maps


def kernel(**inputs):
    b = Builder()
    nc = b.build()
    maps = make_in_maps(inputs, list(range(8)))
    maps = [{k: v for k, v in m.items() if k in b.inp} for m in maps]
    res = run_bass_kernel_spmd(nc, maps, core_ids=list(range(8)))
    return np.stack([r["out"] for r in res.results], axis=0).astype(np.float32)
```

```python
import numpy as np
import concourse.bass as bass
import concourse.mybir as mybir
from concourse.bass_utils import run_bass_kernel_spmd

F32 = mybir.dt.float32
F32R = mybir.dt.float32r
U32 = mybir.dt.uint32
ALU = mybir.AluOpType
AF = mybir.ActivationFunctionType
AX = mybir.AxisListType

D = 1024
NLAT = 4096
NCTX = 256
NT_LAT = 32
NT_CTX = 2
NT = 34
NTOK = NLAT + NCTX
NE = 16
FF = 2048
CAP_LAT = 512
CAP_CTX = 32
NDUMMY = 640
EPS = 1e-6
NBIS = 30

C_ID, C_ONES, C_U, C_MP, C_MN, C_IOTA, C_PIDX, C_DMY, C_END = 0, 128, 256, 384, 896, 1408, 1920, 1921, 1926


C2_LF, C2_LB, C2_LT, C2_GT, C2_GE, C2_LE, C2_B64, C2_NB64, C2_OFF, C2_END = 0, 128, 256, 384, 512, 640, 768, 896, 1024, 1152
NEG = -30000.0


class Sched:
    def __init__(self, nc, n_dma_sems=24):
        self.nc = nc
        self.engs = {"pe": nc.tensor, "act": nc.scalar, "dve": nc.vector,
                     "pool": nc.gpsimd, "sp": nc.sync}
        self.csem = {e: nc.alloc_semaphore("c_" + e) for e in ("pe", "act", "dve", "pool")}
        self.ccnt = {e: 0 for e in self.csem}
        self.known = {e: {} for e in self.engs}
        self.dsems = [nc.alloc_semaphore("d%d" % i) for i in range(2 * n_dma_sems)]
        self.dcnt = [0] * (2 * n_dma_sems)
        self.dpool = {"sp": list(range(0, n_dma_sems)), "pool": list(range(n_dma_sems, 2 * n_dma_sems))}
        self.dnext = {"sp": 0, "pool": 0}
        self.state = {}
        self.out_events = []
        self.n_wait = 0
        self.n_inst = 0

    def _need(self, eng, ev):
        sem, val = ev
        k = self.known[eng]
        if k.get(sem.num, 0) >= val:
            return
        self.engs[eng].wait_ge(sem, val)
        self.n_wait += 1
        k[sem.num] = val

    def _deps(self, eng, reads, writes, skip_self=False):
        evs = {}

        def add(ev):
            if ev is None:
                return
            sem, val = ev
            if skip_self and sem.num == self.csem[eng].num:
                return
            if evs.get(sem.num, (None, 0))[1] < val:
                evs[sem.num] = ev

        own = self.csem[eng].num if eng in self.csem else -1
        for k in reads:
            st = self.state.get(k)
            if st:
                add(st["w"])
                if isinstance(k, tuple) and k[0] == "ps":
                    for r in st["r"]:
                        if r[0].num != own:
                            add(r)
        for k in writes:
            st = self.state.get(k)
            if st:
                add(st["w"])
                for r in st["r"]:
                    add(r)
        for ev in evs.values():
            self._need(eng, ev)

    def _commit(self, ev, reads, writes):
        for k in reads:
            st = self.state.setdefault(k, {"w": None, "r": []})
            st["r"] = [r for r in st["r"] if r[0].num != ev[0].num] + [ev]
        for k in writes:
            self.state[k] = {"w": ev, "r": []}

    def op(self, eng, fn, reads=(), writes=()):
        self._deps(eng, reads, writes, skip_self=(eng == "pe"))
        ins = fn(self.engs[eng])
        self.ccnt[eng] += 1
        ins.then_inc(self.csem[eng], 1)
        ev = (self.csem[eng], self.ccnt[eng])
        self._commit(ev, reads, writes)
        self.n_inst += 1
        return ev

    def dma(self, q, fn, reads=(), writes=(), is_output=False):
        self._deps(q, reads, writes)
        pool = self.dpool[q]
        i = pool[self.dnext[q]]
        self.dnext[q] = (self.dnext[q] + 1) % len(pool)
        sem = self.dsems[i]
        if self.dcnt[i] > 0:
            self._need(q, (sem, 16 * self.dcnt[i]))
        ins = fn(self.engs[q])
        self.dcnt[i] += 1
        ins.then_inc(sem, 16)
        ev = (sem, 16 * self.dcnt[i])
        self._commit(ev, reads, writes)
        if is_output:
            self.out_events.append(ev)
        self.n_inst += 1
        return ev

    def barrier(self):
        for eng in self.engs:
            for i, sem in enumerate(self.dsems):
                if self.dcnt[i] > 0:
                    self._need(eng, (sem, 16 * self.dcnt[i]))
            for e, sem in self.csem.items():
                if self.ccnt[e] > 0:
                    self._need(eng, (sem, self.ccnt[e]))

    def finish(self, eng="sp"):
        for i, sem in enumerate(self.dsems):
            if self.dcnt[i] > 0:
                self._need(eng, (sem, 16 * self.dcnt[i]))
        for e, sem in self.csem.items():
            if self.ccnt[e] > 0:
                self._need(eng, (sem, self.ccnt[e]))


class Scope:
    def __init__(self, builder):
        from contextlib import ExitStack
        self.b = builder
        self.st = ExitStack()

    def sb(self, name, shape, dtype=F32):
        return self.st.enter_context(self.b.nc.sbuf_tensor(name, list(shape), dtype))

    def close(self):
        self.b.S.barrier()
        self.st.close()


class Ring:
    def __init__(self, sc, name, shape, dtype, n):
        self.t = [sc.sb("%s%d" % (name, i), shape, dtype) for i in range(n)]
        self.k = [("%s" % name, i) for i in range(n)]
        self.i = 0

    def next(self):
        r = (self.t[self.i], self.k[self.i])
        self.i = (self.i + 1) % len(self.t)
        return r


class Builder:
    def __init__(self, stage=99, debug=False, n_exp=None, start_layer=0, n_tiles=None):
        self.n_exp = n_exp
        self.start_layer = start_layer
        self.n_tiles = n_tiles
        self.stage = stage
        self.debug = debug
        nc = bass.Bass("TRN2", target_bir_lowering=False)
        self.nc = nc
        self.S = Sched(nc)
        self.inp = {}
        self.ps = [nc.alloc_psum_tensor("psb%d" % i, [128, 512], F32) for i in range(8)]
        self.psk = [("ps", i) for i in range(8)]

    def din(self, name, shape, dtype=F32):
        t = self.nc.dram_tensor(name, list(shape), dtype, kind="ExternalInput").ap()
        self.inp[name] = t
        return t

    def dscratch(self, name, shape, dtype=F32, out=False):
        kind = "ExternalOutput" if (out or self.debug) else "Internal"
        return self.nc.dram_tensor(name, list(shape), dtype, kind=kind).ap()

    def sb(self, name, shape, dtype=F32):
        return self.nc.alloc_sbuf_tensor(name, list(shape), dtype)

    def mm(self, out, lhsT, rhs, start, stop, reads, writes):
        return self.S.op("pe", lambda e: e.matmul(out, lhsT=lhsT, rhs=rhs, start=start, stop=stop),
                         reads, writes)

    def tr(self, out, in_, reads, writes, kp=128):
        ident = self.cst[0:kp, C_ID:C_ID + kp]
        return self.S.op("pe", lambda e: e.transpose(out, in_, ident), list(reads) + ["cst"], writes)

    def act(self, out, in_, func, reads, writes, **kw):
        return self.S.op("act", lambda e: e.activation(out=out, in_=in_, func=func, **kw), reads, writes)

    def tt(self, eng, out, in0, in1, op, reads, writes):
        return self.S.op(eng, lambda e: e.tensor_tensor(out=out, in0=in0, in1=in1, op=op), reads, writes)

    def ts(self, eng, out, in0, s1, s2, op0, op1, reads, writes, **kw):
        if s2 is None:
            return self.S.op(eng, lambda e: e.tensor_scalar(out=out, in0=in0, scalar1=s1, scalar2=None,
                                                            op0=op0, **kw), reads, writes)
        return self.S.op(eng, lambda e: e.tensor_scalar(out=out, in0=in0, scalar1=s1, scalar2=s2,
                                                        op0=op0, op1=op1, **kw), reads, writes)

    def stt(self, eng, out, in0, scalar, in1, op0, op1, reads, writes):
        return self.S.op(eng, lambda e: e.scalar_tensor_tensor(out=out, in0=in0, scalar=scalar, in1=in1,
                                                               op0=op0, op1=op1), reads, writes)

    def cp(self, eng, out, in_, reads, writes):
        if eng == "act":
            return self.act(out, in_, AF.Copy, reads, writes)
        return self.S.op(eng, lambda e: e.tensor_copy(out=out, in_=in_), reads, writes)

    def ld(self, out, in_, reads, writes, q="sp"):
        return self.S.dma(q, lambda e: e.dma_start(out=out, in_=in_), reads, writes)

    def rstd_of(self, x_ap, xk, junk, junkk, ss, ssk):
        self.act(junk, x_ap, AF.Square, [xk], [junkk, ssk], accum_out=ss)
        self.ts("dve", ss, ss, 1.0 / D, EPS, ALU.mult, ALU.add, [ssk], [ssk])
        self.act(ss, ss, AF.Sqrt, [ssk], [ssk])
        self.S.op("dve", lambda e: e.reciprocal(out=ss, in_=ss), [ssk], [ssk])

    def build(self):
        nc, S = self.nc, self.S
        st, sl = self.stage, self.start_layer
        x = self.din("x", [NLAT, D]) if sl == 0 else None
        ctx = self.din("ctx", [NCTX, D]) if sl == 0 else None
        cvecT = self.din("cvecT", [128, 8, 2])
        cpack = self.din("cpack", [128, C_END])
        rope = self.din("rope", [NLAT, 64]) if sl == 0 else None
        W = {}
        for l in (0, 1):
            if l == 0 and sl > 0:
                continue
            if l == 1 and st < 30:
                continue
            W[l] = dict(
                ada_w=self.din("l%d_ada_w" % l, [D, 6 * D]),
                ada_b=self.din("l%d_ada_b" % l, [6 * D]),
                ada_bT=self.din("l%d_ada_bT" % l, [128, 48]),
                nmixT=self.din("l%d_nmixT" % l, [128, 8]),
                nffnT=self.din("l%d_nffnT" % l, [128, 8]),
                router=self.din("l%d_router" % l, [D, NE]),
            )
            if (l == 0 and st >= 2) or (l == 1 and st >= 40):
                W[l].update(w_gate=self.din("l%d_w_gate" % l, [NE, D, FF]),
                            w_up=self.din("l%d_w_up" % l, [NE, D, FF]),
                            w_down=self.din("l%d_w_down" % l, [NE, FF, D]))
        if 0 in W:
            W[0].update(w_qkv=self.din("l0_w_qkv", [D, 1536]), sink=self.din("l0_sink", [16]),
                        w_o=self.din("l0_w_o", [D, D]))
        if 1 in W:
            W[1].update(w_in=self.din("l1_w_in", [D, 4128]), convT=self.din("l1_convT", [128, 24, 3]),
                        a_log=self.din("l1_a_log", [16]), dt_bias=self.din("l1_dt_bias", [16]),
                        o_norm=self.din("l1_o_norm", [128]), w_o=self.din("l1_w_o", [D, D]))
            cpack2 = self.din("cpack2", [128, C2_END])
        if st >= 50:
            self.din("final_norm", [D])
        self.W = W
        self.x, self.ctx, self.rope = x, ctx, rope
        self.out = self.nc.dram_tensor("out", [NLAT, D], F32, kind="ExternalOutput").ap()
        self.qs = self.dscratch("qs", [NTOK, D])
        if sl == 0:
            xa = self.dscratch("xresA", [NTOK + NDUMMY, D])
        else:
            xa = self.din("xresA_in", [NTOK + NDUMMY, D])
        self.xres = [xa, self.dscratch("xresB", [NTOK + NDUMMY, D])]
        self.xn2 = self.dscratch("xn2", [NTOK + NDUMMY, D])

        self.cst = self.sb("cst", [128, C_END])
        self.ld(self.cst[:], cpack, [], ["cst"])
        self.scT = self.sb("scT", [128, 8, 2], F32R)
        self.cols = self.sb("cols", [128, 48, 2])
        self.A1 = self.sb("A1", [128, 8, 2]); self.A2 = self.sb("A2", [128, 8, 2])
        self.Gbc = self.sb("Gbc", [128, 2, 2, D])
        self.aff = self.sb("aff", [128, NT, NE])
        if self.debug:
            S.op("dve", lambda e: e.memset(self.aff[:], 0.0), [], [("aff", i) for i in range(NT)])
        sc0 = Scope(self)
        zero_t = sc0.sb("zero_t", [128, D])
        S.op("dve", lambda e: e.memset(zero_t[:], 0.0), [], ["zero_t"])
        bufs = [(self.xres[1], "xres1"), (self.xn2, "xn2")] + ([(self.xres[0], "xres0")] if sl == 0 else [])
        for buf, k in bufs:
            for r in range(NDUMMY // 128):
                self.ld(buf[NTOK + r * 128: NTOK + (r + 1) * 128, :], zero_t[:], ["zero_t"], [(k, "dummy", r)])
        sc0.close()
        if sl == 0:
            self.modulation(0)
            if st >= 1:
                self.attention_layer()
            if st >= 2:
                self.moe(0, self.xres[0], "xres0", with_ctx=True)
        if st >= 30:
            self.cst2 = self.sb("cst2", [128, C2_END])
            self.ld(self.cst2[:], cpack2, [], ["cst2"])
            self.modulation(1)
            self.deltanet_layer()
        if st >= 40:
            self.moe(1, self.xres[1], "xres1", with_ctx=False)
        if st >= 50:
            self.final_norm()
        if self.debug:
            d_aff = self.nc.dram_tensor("d_aff", [128, NT * NE], F32, kind="ExternalOutput").ap()
            self.ld(d_aff, self.aff[:, :, :].rearrange("p j e -> p (j e)"), [("aff", i) for i in range(NT)], ["d_aff"])
            d_cols = self.nc.dram_tensor("d_cols", [128, 96], F32, kind="ExternalOutput").ap()
            self.ld(d_cols, self.cols[:, :, :].rearrange("p c t -> p (c t)"), ["cols"], ["d_cols"])
            d_g = self.nc.dram_tensor("d_g", [128, 4 * D], F32, kind="ExternalOutput").ap()
            self.ld(d_g, self.Gbc[:, :, :, :].rearrange("p a b d -> p (a b d)"),
                    [("Gbc", a, b) for a in range(2) for b in range(2)], ["d_g"])
        S.finish()
        return nc

    def modulation(self, l):
        nc, S, W = self.nc, self.S, self.W[l]
        sfx = "m%d" % l
        sc = Scope(self)
        self.wring = Ring(sc, "wst" + sfx, [128, 8, 512], F32R, 4)
        cv = sc.sb("cv" + sfx, [128, 8, 2])
        self.ld(cv[:], self.inp["cvecT"], [], ["cv"])
        self.act(self.scT[:], cv[:], AF.Silu, ["cv"], ["scT"])
        screp = [sc.sb("screp%d%s" % (c, sfx), [128, 8, 128], F32R) for c in range(2)]
        for c in range(2):
            self.cp("dve", screp[c][:], self.scT[:, :, c:c + 1].to_broadcast([128, 8, 128]), ["scT"], [("screp", c)])
        abT = sc.sb("abT" + sfx, [128, 48])
        self.ld(abT[:], W["ada_bT"], [], ["abT"])
        nmix = sc.sb("nmix" + sfx, [128, 8]); nffn = sc.sb("nffn" + sfx, [128, 8])
        self.ld(nmix[:], W["nmixT"], [], ["nmix"])
        self.ld(nffn[:], W["nffnT"], [], ["nffn"])
        abbc = sc.sb("abbc" + sfx, [128, 2, D])
        self.ld(abbc[:, 0, :], W["ada_b"][2 * D:3 * D].partition_broadcast(128), [], [("abbc", 0)])
        self.ld(abbc[:, 1, :], W["ada_b"][5 * D:6 * D].partition_broadcast(128), [], [("abbc", 1)])
        pcol = self.ps[0]
        pcv = pcol[:, 0:96].rearrange("p (c t) -> p c t", t=2)
        for cg in range(12):
            wt, wk = self.wring.next()
            self.ld(wt[:], W["ada_w"][:, cg * 512:(cg + 1) * 512].rearrange("(k p) n -> p k n", p=128),
                    [], [wk], q="pool")
            for c4 in range(4):
                cc = cg * 4 + c4
                for kc in range(8):
                    self.mm(pcv[:, cc, :], wt[:, kc, c4 * 128:(c4 + 1) * 128], self.scT[:, kc, :],
                            kc == 0, kc == 7, [wk, "scT"], [self.psk[0]])
            if cg in (4, 5, 10, 11):
                gi = 0 if cg < 6 else 1
                half = cg % 2
                for c in range(2):
                    pb, pbk = self.ps[1 + c], self.psk[1 + c]
                    for kc in range(8):
                        self.mm(pb[:, :], screp[c][:, kc, :], wt[:, kc, :], kc == 0, kc == 7,
                                [wk, ("screp", c)], [pbk])
                    self.tt("dve", self.Gbc[:, gi, c, half * 512:(half + 1) * 512], pb[:, :],
                            abbc[:, gi, half * 512:(half + 1) * 512], ALU.add,
                            [pbk, ("abbc", gi)], [("Gbc", gi, c)])
        self.tt("dve", self.cols[:], pcv, abT[:].unsqueeze(2).to_broadcast([128, 48, 2]), ALU.add,
                [self.psk[0], "abT"], ["cols"])
        for (A, nrm, nk, v) in ((self.A1, nmix, "nmix", 1), (self.A2, nffn, "nffn", 4)):
            self.stt("dve", A[:], self.cols[:, v * 8:(v + 1) * 8, :], 1.0,
                     nrm[:].unsqueeze(2).to_broadcast([128, 8, 2]), ALU.add, ALU.mult,
                     ["cols", nk], [("A", v)])
        sc.close()

    def B1(self, kc, c):
        return self.cols[:, 0 + kc, c:c + 1]

    def B2(self, kc, c):
        return self.cols[:, 24 + kc, c:c + 1]

    def attention_layer(self):
        nc, S, W = self.nc, self.S, self.W[0]
        cst = self.cst
        sca = Scope(self)
        KT = sca.sb("KT", [128, 2, NTOK], F32R)
        V = sca.sb("Vaug", [128, NT, 4, 66], F32R)
        esink = sca.sb("esink", [128, 16])
        wr = sca.sb("wr", [128, 8, NE])
        xr = Ring(sca, "xt", [128, D], F32, 2)
        xnr = Ring(sca, "xnb", [128, D], F32, 3)
        ssr = Ring(sca, "ss", [128, 1], F32, 6)
        junk = sca.sb("junk", [128, D])
        sc1 = Scope(self)
        wbig = sc1.sb("wbig", [128, 8, 1536], F32R)
        hTr = Ring(sc1, "hT", [128, 8, 128], F32R, 3)
        qkr = Ring(sc1, "qk", [128, 1280], F32, 2)
        csr = Ring(sc1, "cs", [128, 64], F32, 6)
        tmpr = Ring(sc1, "rt", [128, 4, 256], F32, 1)
        for cg in range(3):
            self.ld(wbig[:, :, cg * 512:(cg + 1) * 512],
                    W["w_qkv"][:, cg * 512:(cg + 1) * 512].rearrange("(k p) n -> p k n", p=128),
                    [], [("wbig", cg)], q="pool")
        Vf = V[:, :, :, :].rearrange("p j h c -> p (j h) c")
        self.cp("dve", Vf[:, :, 64:65], self.cst[:, C_ONES:C_ONES + 1].unsqueeze(1).to_broadcast([128, NT * 4, 1]), ["cst"], [("V1",)])
        self.cp("dve", Vf[:, :, 65:66], self.cst[:, C_U:C_U + 1].unsqueeze(1).to_broadcast([128, NT * 4, 1]), ["cst"], [("V0",)])
        self.ld(esink[:], W["sink"].partition_broadcast(128), [], ["esink"])
        self.act(esink[:], esink[:], AF.Exp, ["esink"], ["esink"])
        self.ld(wr[:], W["router"].rearrange("(k p) n -> p k n", p=128), [], ["wr"])

        def src_rows(j):
            if j < NT_LAT:
                return self.x[j * 128:(j + 1) * 128, :]
            return self.ctx[(j - NT_LAT) * 128:(j - NT_LAT + 1) * 128, :]

        cx = [dict() for _ in range(NT)]

        def p1_s0(j):
            c = cx[j]
            c["c"] = 0 if j < NT_LAT else 1
            xt, xk = xr.next()
            self.ld(xt[:], src_rows(j), [], [xk])
            if c["c"] == 0:
                c["cs"], c["ck"] = csr.next()
                self.ld(c["cs"][:], self.rope[j * 128:(j + 1) * 128, :], [], [c["ck"]])
            ss, ssk = ssr.next()
            self.rstd_of(xt[:], xk, junk[:], "junk", ss[:], ssk)
            c["xn"], c["xnk"] = xnr.next()
            self.ts("dve", c["xn"][:], xt[:], ss[:, 0:1], None, ALU.mult, None, [xk, ssk], [c["xnk"]])

        def p1_s1(j):
            c = cx[j]
            xn, xnk, cc = c["xn"], c["xnk"], c["c"]
            c["hT"], c["hk"] = hTr.next()
            hT, hk = c["hT"], c["hk"]
            for kc in range(8):
                pt, ptk = self.ps[kc // 4], self.psk[kc // 4]
                self.tr(pt[:, (kc % 4) * 128:(kc % 4 + 1) * 128], xn[:, kc * 128:(kc + 1) * 128], [xnk], [ptk])
            for kc in range(8):
                pt, ptk = self.ps[kc // 4], self.psk[kc // 4]
                self.act(hT[:, kc, :], pt[:, (kc % 4) * 128:(kc % 4 + 1) * 128], AF.Identity,
                         [ptk, ("A", 1), "cols"], [hk], scale=self.A1[:, kc, cc:cc + 1], bias=self.B1(kc, cc))

        def p1_s2(j):
            c = cx[j]
            hT, hk = c["hT"], c["hk"]
            c["pb"] = 2 + 3 * (j % 2)
            for cg in range(3):
                pq, pqk = self.ps[c["pb"] + cg], self.psk[c["pb"] + cg]
                for kc in range(8):
                    self.mm(pq[:, :], hT[:, kc, :], wbig[:, kc, cg * 512:(cg + 1) * 512], kc == 0, kc == 7,
                            [hk, ("wbig", cg)], [pqk])

        def p1_s3(j):
            c = cx[j]
            pb = c["pb"]
            c["qk"], c["qkk"] = qkr.next()
            qk, qkk = c["qk"], c["qkk"]
            if c["c"] == 0:
                cs, ck = c["cs"], c["ck"]
                cosb = lambda nh: cs[:, 0:32].rearrange("p (a f) -> p a f", a=2).unsqueeze(1).to_broadcast([128, nh, 2, 16])
                sinb = lambda nh: cs[:, 32:64].rearrange("p (a f) -> p a f", a=2).unsqueeze(1).to_broadcast([128, nh, 2, 16])
                for cg in range(3):
                    nh = 8 if cg < 2 else 4
                    pq, pqk = self.ps[pb + cg], self.psk[pb + cg]
                    pv = pq[:, 0:nh * 64].rearrange("p (h a b f) -> p h a b f", h=nh, a=2, b=2, f=16)
                    ov = qk[:, cg * 512:cg * 512 + nh * 64].rearrange("p (h a b f) -> p h a b f", h=nh, a=2, b=2, f=16)
                    x1, x2 = pv[:, :, :, 0, :], pv[:, :, :, 1, :]
                    tm, tmk = tmpr.next()
                    t = [tm[:, i, 0:nh * 32].rearrange("p (h a f) -> p h a f", h=nh, a=2, f=16) for i in range(4)]
                    self.tt("dve", t[0], x1, cosb(nh), ALU.mult, [pqk, ck], [tmk])
                    self.tt("dve", t[1], x2, sinb(nh), ALU.mult, [pqk, ck], [tmk])
                    self.tt("dve", t[2], x2, cosb(nh), ALU.mult, [pqk, ck], [tmk])
                    self.tt("dve", t[3], x1, sinb(nh), ALU.mult, [pqk, ck], [tmk])
                    self.tt("pool", ov[:, :, :, 0, :], t[0], t[1], ALU.subtract, [tmk], [qkk])
                    self.tt("pool", ov[:, :, :, 1, :], t[2], t[3], ALU.add, [tmk], [qkk])
            else:
                for cg in range(3):
                    ncol = 512 if cg < 2 else 256
                    self.cp("act", qk[:, cg * 512:cg * 512 + ncol], self.ps[pb + cg][:, 0:ncol], [self.psk[pb + cg]], [qkk])
            self.cp("act", V[:, j, :, 0:64], self.ps[pb + 2][:, 256:512].rearrange("p (h d) -> p h d", h=4),
                    [self.psk[pb + 2]], [("V", j)])
            self.ld(self.qs[j * 128:(j + 1) * 128, :], qk[:, 0:1024], [qkk], [("qs", j)])

        def p1_s4(j):
            c = cx[j]
            qk, qkk = c["qk"], c["qkk"]
            for pr in range(2):
                self.tr(self.ps[1][:, pr * 128:(pr + 1) * 128], qk[:, 1024 + pr * 128:1024 + (pr + 1) * 128], [qkk], [self.psk[1]])
            self.cp("act", KT[:, :, j * 128:(j + 1) * 128], self.ps[1][:, 0:256].rearrange("p (a t) -> p a t", a=2),
                    [self.psk[1]], [("KT", j)])

        stages = [p1_s0, p1_s1, p1_s2, p1_s3, p1_s4]
        for t_ in range(NT + len(stages) - 1):
            for st_ in range(len(stages) - 1, -1, -1):
                i_ = t_ - st_
                if 0 <= i_ < NT:
                    stages[st_](i_)

        sc1.close()
        sca_outer, sca = sca, Scope(self)
        wo = sca.sb("wo", [128, 8, 1024], F32R)
        self.ld(wo[:], W["w_o"].rearrange("(k p) n -> p k n", p=128), [], ["wo"], q="pool")
        wok = ["wo"]
        QTr = Ring(sca, "QT", [128, 2, 4, 128], F32R, 2)
        PTr = Ring(sca, "PT", [128, 5, 512], F32R, 2)
        osb = sca.sb("osb", [128, 16, 64])
        otsr = Ring(sca, "ots", [66, 512], F32, 1)
        oT = sca.sb("oT", [128, 8, 128], F32R)
        den = sca.sb("den", [128, 16])
        xmr = Ring(sca, "xm", [128, D], F32, 3)
        h2T = sca.sb("h2T", [128, 8, 128])
        lg = sca.sb("lg", [128, NE]); mx = sca.sb("mx", [128, 1]); sm = sca.sb("sm", [128, 1])
        pend = [None]

        def p2_loads(i_):
            qt_, qtk_ = xr.next()
            self.ld(qt_[:], self.qs[i_ * 128:(i_ + 1) * 128, :], [("qs", i_)], [qtk_])
            xt_, xk_ = xnr.next()
            self.ld(xt_[:], src_rows(i_), [], [xk_])
            return qt_, qtk_, xt_, xk_

        nxt = p2_loads(0)
        for i in range(NT):
            c = 0 if i < NT_LAT else 1
            qt, qtk, xt, xk = nxt
            if i + 1 < NT:
                nxt = p2_loads(i + 1)
            QT, QTk = QTr.next()
            for pr in range(2):
                for g in range(4):
                    self.tr(self.ps[pr][:, g * 128:(g + 1) * 128], qt[:, (pr * 4 + g) * 128:(pr * 4 + g + 1) * 128], [qtk], [self.psk[pr]])
                self.cp("act", QT[:, pr, :, :], self.ps[pr][:, :].rearrange("p (g t) -> p g t", g=4), [self.psk[pr]], [QTk])
            if c == 0:
                kbs = ([i - 1] if i > 0 else []) + [i] + ([i + 1] if i < NT_LAT - 1 else []) + [32, 33]
                kmask = ([C_MP] if i > 0 else []) + [None] + ([C_MN] if i < NT_LAT - 1 else []) + [None, None]
            else:
                kbs = [32, 33]
                kmask = [None, None]
            if pend[0] is not None:
                self.moe_prep_a(*pend[0], skip0=True)

            def st_phase(kvh):
                pr, base = kvh // 2, (kvh % 2) * 64
                PT, PTk = PTr.next()
                for kbi, kb in enumerate(kbs):
                    pS, pSk = self.ps[2 + kbi % 2], self.psk[2 + kbi % 2]
                    self.mm(pS[:, :], KT[base:base + 64, pr, kb * 128:(kb + 1) * 128],
                            QT[base:base + 64, pr, :, :], True, True, [("KT", kb), QTk], [pSk])
                    self.act(PT[:, kbi, :], pS[:, :], AF.Exp, [pSk], [PTk], scale=0.125)
                    if kmask[kbi] is not None:
                        mo = kmask[kbi]
                        self.tt("pool", PT[:, kbi, :], PT[:, kbi, :], cst[:, mo:mo + 512], ALU.mult, [PTk, "cst"], [PTk])
                return PT, PTk

            def pv_phase(kvh, PT, PTk):
                pO, pOk = self.ps[4 + kvh], self.psk[4 + kvh]
                for kbi, kb in enumerate(kbs):
                    self.mm(pO[0:66, :], V[:, kb, kvh, :], PT[:, kbi, :], kbi == 0, kbi == len(kbs) - 1,
                            [PTk, ("V", kb), ("V1",), ("V0",)], [pOk])
                ots, otsk = otsr.next()
                self.cp("act", ots[0:66, :], pO[0:66, :], [pOk], [otsk])
                for g in range(4):
                    self.tr(pO[:, g * 128:g * 128 + 66], ots[0:66, g * 128:(g + 1) * 128], [otsk], [pOk], kp=66)

            pts = {0: st_phase(0)}
            for kvh in range(4):
                if kvh + 1 < 4:
                    pts[kvh + 1] = st_phase(kvh + 1)
                if kvh == 1 and pend[0] is not None:
                    self.moe_prep_b(*pend[0])
                    pend[0] = None
                pv_phase(kvh, *pts[kvh])
            for kvh in range(4):
                pOv = self.ps[4 + kvh][:, :].rearrange("p (g t) -> p g t", g=4)
                self.tt("dve", den[:, kvh * 4:(kvh + 1) * 4], pOv[:, :, 64], esink[:, kvh * 4:(kvh + 1) * 4], ALU.add,
                        [self.psk[4 + kvh], "esink"], ["den"])
            self.S.op("dve", lambda e: e.reciprocal(out=den[:], in_=den[:]), ["den"], ["den"])
            for kvh in range(4):
                pOv = self.ps[4 + kvh][:, :].rearrange("p (g t) -> p g t", g=4)
                self.tt("dve", osb[:, kvh * 4:(kvh + 1) * 4, :], pOv[:, :, 0:64],
                        den[:, kvh * 4:(kvh + 1) * 4].unsqueeze(2).to_broadcast([128, 4, 64]), ALU.mult,
                        [self.psk[4 + kvh], "den"], ["osb"])
            osf = osb[:, :, :].rearrange("p h d -> p (h d)")
            for kc in range(8):
                self.tr(self.ps[kc // 4][:, (kc % 4) * 128:(kc % 4 + 1) * 128], osf[:, kc * 128:(kc + 1) * 128], ["osb"], [self.psk[kc // 4]])
            for b in range(2):
                self.cp("act", oT[:, b * 4:(b + 1) * 4, :], self.ps[b][:, :].rearrange("p (k t) -> p k t", k=4), [self.psk[b]], ["oT"])
            xm, xmk = xmr.next()
            for dh in range(2):
                pM, pMk = self.ps[2 + dh], self.psk[2 + dh]
                for kc in range(8):
                    self.mm(pM[:, :], oT[:, kc, :], wo[:, kc, dh * 512:(dh + 1) * 512], kc == 0, kc == 7, ["oT"] + wok, [pMk])
                self.tt("dve", xm[:, dh * 512:(dh + 1) * 512], pM[:, :], self.Gbc[:, 0, c, dh * 512:(dh + 1) * 512], ALU.mult,
                        [pMk, ("Gbc", 0, c)], [xmk])
            self.tt("dve", xm[:], xm[:], xt[:], ALU.add, [xmk, xk], [xmk])
            self.ld(self.xres[0][i * 128:(i + 1) * 128, :], xm[:], [xmk], [("xres0", i)])
            pend[0] = (i, c, xm, xmk, junk, ssr, wr, h2T, lg, mx, sm)
            self.moe_prep_a0(*pend[0])
        self.moe_prep_a(*pend[0], skip0=True)
        self.moe_prep_b(*pend[0])
        sca.close()
        sca_outer.close()

    def moe_prep_a0(self, i, c, xm, xmk, junk, ssr, wr, h2T, lg, mx, sm):
        ss, ssk = ssr.next()
        self.rstd_of(xm[:], xmk, junk[:], "junk", ss[:], ssk)
        self.ts("dve", junk[:], xm[:], ss[:, 0:1], None, ALU.mult, None, [xmk, ssk], ["junk"])
        self.ld(self.xn2[i * 128:(i + 1) * 128, :], junk[:], ["junk"], [("xn2", i)])

    def moe_prep_a(self, i, c, xm, xmk, junk, ssr, wr, h2T, lg, mx, sm, skip0=False):
        if not skip0:
            self.moe_prep_a0(i, c, xm, xmk, junk, ssr, wr, h2T, lg, mx, sm)
        for kc in range(8):
            self.tr(self.ps[kc // 4][:, (kc % 4) * 128:(kc % 4 + 1) * 128], junk[:, kc * 128:(kc + 1) * 128], ["junk"], [self.psk[kc // 4]])
        for kc in range(8):
            self.act(h2T[:, kc, :], self.ps[kc // 4][:, (kc % 4) * 128:(kc % 4 + 1) * 128], AF.Identity,
                     [self.psk[kc // 4], ("A", 4), "cols"], ["h2T"], scale=self.A2[:, kc, c:c + 1], bias=self.B2(kc, c))

    def moe_prep_b(self, i, c, xm, xmk, junk, ssr, wr, h2T, lg, mx, sm):
        pL, pLk = self.ps[1], self.psk[1]
        for kc in range(8):
            self.mm(pL[:, 0:NE], h2T[:, kc, :], wr[:, kc, :], kc == 0, kc == 7, ["h2T", "wr"], [pLk])
        self.S.op("dve", lambda e: e.reduce_max(out=mx[:], in_=pL[:, 0:NE], axis=AX.X), [pLk], ["mx"])
        self.ts("dve", mx[:], mx[:], -1.0, None, ALU.mult, None, ["mx"], ["mx"])
        self.act(lg[:], pL[:, 0:NE], AF.Exp, [pLk, "mx"], ["lg", "sm"], bias=mx[:, 0:1], accum_out=sm[:])
        self.S.op("dve", lambda e: e.reciprocal(out=sm[:], in_=sm[:]), ["sm"], ["sm"])
        self.ts("dve", self.aff[:, i, :], lg[:], sm[:, 0:1], None, ALU.mult, None, ["lg", "sm"], [("aff", i)])

    def moe_prep(self, *args):
        self.moe_prep_a(*args)
        self.moe_prep_b(*args)

    def pk(self, b, h):
        return ("ps", b)

    def deltanet_layer(self):
        nc, S, W = self.nc, self.S, self.W[1]
        cst, c2 = self.cst, self.cst2
        xin = self.xres[0]
        qT_d = self.dscratch("qT_d", [D, NTOK]); kT_d = self.dscratch("kT_d", [D, NTOK])
        ktok_d = self.dscratch("ktok_d", [NTOK, D]); vtok_d = self.dscratch("vtok_d", [NTOK, D])
        sz_d = self.dscratch("sz_d", [NTOK, D]); of_d = self.dscratch("of_d", [NLAT, D])
        self.dn_dbg = dict(qT_d=qT_d, kT_d=kT_d, ktok_d=ktok_d, vtok_d=vtok_d, sz_d=sz_d, of_d=of_d)
        ident = cst[:, C_ID:C_ID + 128]
        ones = cst[:, C_ONES:C_ONES + 128]
        zcol = cst[:, C_U:C_U + 1]
        scL = Scope(self)
        g_all = scL.sb("g_all", [128, NT, 16]); beta_all = scL.sb("beta_all", [128, NT, 16])
        lnb_all = scL.sb("lnb_all", [128, NT, 16]); ab_all = scL.sb("ab_all", [128, NT, 32])
        onesR = scL.sb("onesR", [128, 128], F32R)
        self.cp("dve", onesR[:], ones, ["cst"], ["onesR"])
        convT = scL.sb("convT", [128, 24, 3])
        self.ld(convT[:], W["convT"], [], ["convT"])
        allps = [self.pk(b, h) for b in range(8) for h in range(2)]

        def bankk(b):
            return [self.pk(b, 0), self.pk(b, 1)]

        scp = Scope(self)
        win = scp.sb("winqkv", [128, 8, 3072], F32R)
        wink = [("win", i) for i in range(6)]
        for i in range(6):
            self.ld(win[:, :, i * 512:(i + 1) * 512], W["w_in"][:, i * 512:(i + 1) * 512].rearrange("(k p) n -> p k n", p=128),
                    [], [wink[i]], q="pool")
        wnd = [scp.sb("wnd%d" % i, [128, 8, 258], F32R) for i in range(2)]
        xr = Ring(scp, "pxt", [128, D], F32, 4)
        ssr = Ring(scp, "pss", [128, 1], F32, 4)
        junk = scp.sb("pjunk", [128, D])
        c1r = Ring(scp, "pc1", [128, 256], F32, 3)
        sr = Ring(scp, "psl", [128, 256], F32, 8)
        sqr = Ring(scp, "psq", [128, 256], F32R, 3)
        rnr = Ring(scp, "prn", [128, 256], F32, 4)
        qnr = Ring(scp, "pqn", [128, 256], F32, 4)
        tkr = Ring(scp, "ptk", [128, 128], F32, 6)
        groups = [[32, 33]] + [[2 * g, 2 * g + 1] for g in range(16)]

        def xload(jj, xr_):
            xt, xk = xr_.next()
            self.ld(xt[:], xin[jj * 128:(jj + 1) * 128, :], [], [xk])
            return xt, xk

        def tile_hT(jj, dst_fn, xr_, ssr_, junk_, pre=None):
            c = 0 if jj < NT_LAT else 1
            xt, xk = pre if pre is not None else xload(jj, xr_)
            ss, ssk = ssr_.next()
            self.rstd_of(xt[:], xk, junk_[:], "pjunk", ss[:], ssk)
            self.ts("dve", xt[:], xt[:], ss[:, 0:1], None, ALU.mult, None, [xk, ssk], [xk])
            for kc in range(8):
                b = kc // 4
                self.tr(self.ps[b][:, (kc % 4) * 128:(kc % 4 + 1) * 128], xt[:, kc * 128:(kc + 1) * 128], [xk], bankk(b))
            for kc in range(8):
                b = kc // 4
                dst, dk = dst_fn(kc)
                self.act(dst, self.ps[b][:, (kc % 4) * 128:(kc % 4 + 1) * 128], AF.Identity,
                         bankk(b) + [("A", 1), "cols"], [dk], scale=self.A1[:, kc, c:c + 1], bias=self.B1(kc, c))

        pw_pre = {}

        def pw_loads(gi):
            if gi < len(groups):
                pw_pre[gi] = [xload(jj, xr) for jj in groups[gi]]

        pw_loads(0)

        def prep_window(gi):
            buf = gi % 2
            pw_loads(gi + 1)
            for ti, jj in enumerate(groups[gi]):
                tile_hT(jj, lambda kc: (wnd[buf][:, kc, 1 + 128 * ti:1 + 128 * (ti + 1)], ("wnd", buf)), xr, ssr, junk,
                        pre=pw_pre[gi][ti])
            same_prev = gi >= 2
            if same_prev:
                self.cp("dve", wnd[buf][:, :, 0:1], wnd[1 - buf][:, :, 256:257], [("wnd", 1 - buf)], [("wnd", buf)])
                self.cp("dve", wnd[1 - buf][:, :, 257:258], wnd[buf][:, :, 1:2], [("wnd", buf)], [("wnd", 1 - buf)])
            else:
                self.cp("dve", wnd[buf][:, :, 0:1], zcol.unsqueeze(1).to_broadcast([128, 8, 1]), ["cst"], [("wnd", buf)])
                if gi >= 1:
                    self.cp("dve", wnd[1 - buf][:, :, 257:258], zcol.unsqueeze(1).to_broadcast([128, 8, 1]), ["cst"], [("wnd", 1 - buf)])

        pb_i = [0]
        pn_i = [0]

        def skew(n_items, stages):
            k = len(stages)
            for t in range(n_items + k - 1):
                for st_ in range(k - 1, -1, -1):
                    i_ = t - st_
                    if 0 <= i_ < n_items:
                        stages[st_](i_)

        def project(gi):
            buf = gi % 2
            wv, wvk = wnd[buf], ("wnd", buf)
            tok0 = groups[gi][0] * 128
            ctxs = [dict() for _ in range(24)]

            def s0(ch):
                c = ctxs[ch]
                b = pb_i[0] % 3
                pb_i[0] += 1
                c["pb"] = 2 + b
                pP = self.ps[2 + b]
                for kc in range(8):
                    self.mm(pP[:, 0:258], win[:, kc, ch * 128:(ch + 1) * 128], wv[:, kc, 0:258], kc == 0, kc == 7,
                            [wink[ch // 4], wvk], bankk(2 + b))

            def s1(ch):
                c = ctxs[ch]
                pb = c["pb"]
                pP = self.ps[pb]
                c1, c1k = c1r.next()
                self.ts("dve", c1[:], pP[:, 0:256], convT[:, ch, 0:1], None, ALU.mult, None, bankk(pb) + ["convT"], [c1k])
                self.stt("dve", c1[:], pP[:, 1:257], convT[:, ch, 1:2], c1[:], ALU.mult, ALU.add, bankk(pb) + ["convT", c1k], [c1k])
                self.stt("dve", c1[:], pP[:, 2:258], convT[:, ch, 2:3], c1[:], ALU.mult, ALU.add, bankk(pb) + ["convT", c1k], [c1k])
                c["sl"], c["slk"] = sr.next()
                self.act(c["sl"][:], c1[:], AF.Silu, [c1k], [c["slk"]])

            def s2(ch):
                c = ctxs[ch]
                if ch // 8 < 2:
                    sq, sqk = sqr.next()
                    self.act(sq[:], c["sl"][:], AF.Square, [c["slk"]], [sqk])
                    nb = 5 if (pn_i[0] % 2 == 0) else 7
                    pn_i[0] += 1
                    c["nb"] = nb
                    self.mm(self.ps[nb][:, 0:256], onesR[:], sq[:], True, True, ["onesR", sqk], bankk(nb))

            def s3(ch):
                c = ctxs[ch]
                if ch // 8 < 2:
                    c["rn"], c["rnk"] = rnr.next()
                    rn, rnk = c["rn"], c["rnk"]
                    self.ts("dve", rn[:], self.ps[c["nb"]][:, 0:256], EPS, None, ALU.add, None, bankk(c["nb"]), [rnk])
                    self.act(rn[:], rn[:], AF.Sqrt, [rnk], [rnk])

            def s4(ch):
                c = ctxs[ch]
                kind, h = ch // 8, ch % 8
                if kind < 2:
                    rn, rnk = c["rn"], c["rnk"]
                    S.op("dve", lambda e: e.reciprocal(out=rn[:], in_=rn[:]), [rnk], [rnk])
                    qn, qnk = qnr.next()
                    self.stt("dve", qn[:], c["sl"][:], (128.0 ** -0.5) if kind == 0 else 1.0, rn[:], ALU.mult, ALU.mult, [c["slk"], rnk], [qnk])
                    dst = qT_d if kind == 0 else kT_d
                    self.ld(dst[h * 128:(h + 1) * 128, tok0:tok0 + 256], qn[:], [qnk], [("qkT_d", kind, gi, h)])
                    c["src"], c["srck"] = qn, qnk
                else:
                    c["src"], c["srck"] = c["sl"], c["slk"]

            def s5(ch):
                c = ctxs[ch]
                if ch // 8 >= 1:
                    for ti in range(2):
                        self.tr(self.ps[6][:, ti * 128:(ti + 1) * 128], c["src"][:, ti * 128:(ti + 1) * 128], [c["srck"]], bankk(6))

            def s6(ch):
                c = ctxs[ch]
                kind, h = ch // 8, ch % 8
                if kind >= 1:
                    dstd = ktok_d if kind == 1 else vtok_d
                    for ti in range(2):
                        tk, tkk = tkr.next()
                        self.cp("act", tk[:], self.ps[6][:, ti * 128:(ti + 1) * 128], bankk(6), [tkk])
                        self.ld(dstd[tok0 + ti * 128:tok0 + (ti + 1) * 128, h * 128:(h + 1) * 128], tk[:], [tkk], [("tok_d", kind, gi, h, ti)])

            skew(24, [s0, s1, s2, s3, s4, s5, s6])

        for gi in range(len(groups)):
            prep_window(gi)
            if gi >= 1:
                project(gi - 1)
        lastb = (len(groups) - 1) % 2
        self.cp("dve", wnd[lastb][:, :, 257:258], zcol.unsqueeze(1).to_broadcast([128, 8, 1]), ["cst"], [("wnd", lastb)])
        project(len(groups) - 1)
        scp.close()

        scz = Scope(self)
        wz = scz.sb("winz", [128, 8, 1056], F32R)
        self.ld(wz[:, :, 0:528], W["w_in"][:, 3072:3600].rearrange("(k p) n -> p k n", p=128), [], [("wz", 0)], q="pool")
        self.ld(wz[:, :, 528:1056], W["w_in"][:, 3600:4128].rearrange("(k p) n -> p k n", p=128), [], [("wz", 1)], q="pool")
        wzk = [("wz", 0), ("wz", 1)]
        xr = Ring(scz, "zxt", [128, D], F32, 2)
        ssr = Ring(scz, "zss", [128, 1], F32, 4)
        junk = scz.sb("zjunk", [128, D])
        hTr = Ring(scz, "zhT", [128, 8, 128], F32R, 2)
        zr = Ring(scz, "zst", [128, D], F32, 2)
        zpre = xload(0, xr)
        for jj in range(NT):
            hT, hk = hTr.next()
            zcur = zpre
            if jj + 1 < NT:
                zpre = xload(jj + 1, xr)
            tile_hT(jj, lambda kc: (hT[:, kc, :], hk), xr, ssr, junk, pre=zcur)
            for zh in range(2):
                for kc in range(8):
                    self.mm(self.ps[2 + zh][:, :], hT[:, kc, :], wz[:, kc, zh * 512:(zh + 1) * 512], kc == 0, kc == 7, [hk] + wzk, bankk(2 + zh))
            for kc in range(8):
                self.mm(self.ps[4][:, 0:32], hT[:, kc, :], wz[:, kc, 1024:1056], kc == 0, kc == 7, [hk] + wzk, bankk(4))
            zt, ztk = zr.next()
            for zh in range(2):
                self.act(zt[:, zh * 512:(zh + 1) * 512], self.ps[2 + zh][:, :], AF.Silu, bankk(2 + zh), [ztk])
            self.ld(sz_d[jj * 128:(jj + 1) * 128, :], zt[:], [ztk], [("sz_d", jj)])
            self.cp("dve", ab_all[:, jj, :], self.ps[4][:, 0:32], bankk(4), [("ab", jj)])
        abk = [("ab", jj) for jj in range(NT)]
        dtb = scz.sb("dtb", [128, 16]); nea = scz.sb("nea", [128, 16])
        self.ld(dtb[:], W["dt_bias"].partition_broadcast(128), [], ["dtb"])
        self.ld(nea[:], W["a_log"].partition_broadcast(128), [], ["nea"])
        self.act(nea[:], nea[:], AF.Exp, ["nea"], ["nea"])
        self.ts("dve", nea[:], nea[:], -1.0, None, ALU.mult, None, ["nea"], ["nea"])
        self.tt("dve", g_all[:], ab_all[:, :, 0:16], dtb[:].unsqueeze(1).to_broadcast([128, NT, 16]), ALU.add, abk + ["dtb"], ["g_all"])
        uu = scz.sb("sp_u", [128, NT, 16]); la = scz.sb("sp_la", [128, NT, 16])
        qq = scz.sb("sp_q", [128, NT, 16]); mk = scz.sb("sp_mk", [128, NT, 16])
        self.act(uu[:], g_all[:], AF.Exp, ["g_all"], ["sp_u"])
        self.act(la[:], uu[:], AF.Ln, ["sp_u"], ["sp_la"], bias=1.0)
        self.ts("dve", qq[:], uu[:], 1.0 / 7, None, ALU.mult, None, ["sp_u"], ["sp_q"])
        for cc_ in (-1.0 / 6, 1.0 / 5, -1.0 / 4, 1.0 / 3, -1.0 / 2, 1.0):
            self.stt("dve", qq[:], qq[:], cc_, uu[:], ALU.add, ALU.mult, ["sp_q", "sp_u"], ["sp_q"])
        self.ts("dve", mk[:], uu[:], 0.25, None, ALU.is_lt, None, ["sp_u"], ["sp_mk"])
        self.tt("dve", qq[:], qq[:], la[:], ALU.subtract, ["sp_q", "sp_la"], ["sp_q"])
        self.tt("dve", qq[:], qq[:], mk[:], ALU.mult, ["sp_q", "sp_mk"], ["sp_q"])
        self.tt("dve", g_all[:], la[:], qq[:], ALU.add, ["sp_la", "sp_q"], ["g_all"])
        self.tt("dve", g_all[:], g_all[:], nea[:].unsqueeze(1).to_broadcast([128, NT, 16]), ALU.mult, ["g_all", "nea"], ["g_all"])
        self.act(beta_all[:], ab_all[:, :, 16:32], AF.Sigmoid, abk, ["beta_all"])
        self.act(lnb_all[:], beta_all[:], AF.Ln, ["beta_all"], ["lnb_all"])
        scz.close()
        if self.stage >= 31:
            self.dn_scan(0, dict(qT_d=qT_d, kT_d=kT_d, ktok_d=ktok_d, vtok_d=vtok_d, sz_d=sz_d, of_d=of_d), g_all, beta_all, lnb_all)
        if self.stage >= 32:
            self.dn_scan(1, dict(qT_d=qT_d, kT_d=kT_d, ktok_d=ktok_d, vtok_d=vtok_d, sz_d=sz_d, of_d=of_d), g_all, beta_all, lnb_all)
        if self.debug:
            d_gates = self.nc.dram_tensor("d_gates", [128, 3, NT * 16], F32, kind="ExternalOutput").ap()
            for i, (t, k) in enumerate(((g_all, "g_all"), (beta_all, "beta_all"), (lnb_all, "lnb_all"))):
                self.ld(d_gates[:, i, :], t[:, :, :].rearrange("p j e -> p (j e)"), [k], [("d_gates", i)])
        scL.close()
        if self.stage >= 33:
            self.dn_out(of_d)

    def dn_scan(self, dr, dd, g_all, beta_all, lnb_all):
        nc, S, W = self.nc, self.S, self.W[1]
        cst, c2 = self.cst, self.cst2
        ident = cst[:, C_ID:C_ID + 128]
        ones = cst[:, C_ONES:C_ONES + 128]
        zcol = cst[:, C_U:C_U + 1]
        Ltri = c2[:, C2_LF:C2_LF + 128] if dr == 0 else c2[:, C2_LB:C2_LB + 128]
        LT, GT = c2[:, C2_LT:C2_LT + 128], c2[:, C2_GT:C2_GT + 128]
        GE, LE = c2[:, C2_GE:C2_GE + 128], c2[:, C2_LE:C2_LE + 128]
        m_db, m_dbt, m_dt = (LT, GT, GE) if dr == 0 else (GT, LT, LE)
        order = ([32, 33] + list(range(32))) if dr == 0 else ([33, 32] + list(range(31, -1, -1)))
        if self.n_tiles is not None:
            order = order[:self.n_tiles]
        sc = Scope(self)
        sfx = "_%d" % dr

        def bankk(b):
            return [self.pk(b, 0), self.pk(b, 1)]

        def zfill(t, k):
            sh = list(t.shape)
            self.cp("dve", t[:], zcol.to_broadcast(sh) if len(sh) == 2 else zcol.unsqueeze(1).to_broadcast(sh), ["cst"], [k])


        def z3(name, w, dt=F32R, fill=True):
            t = sc.sb(name + sfx, [128, 8, w], dt)
            if fill:
                for b_ in range(4):
                    self.cp("dve", t[:, 2 * b_:2 * b_ + 2, :], zcol.unsqueeze(1).to_broadcast([128, 2, w]), ["cst"], [(name, b_)])
            return t

        Sst = z3("S", 256)
        Xb = [z3("X0", 256)]
        qkT = z3("qkT", 128, fill=False)
        vb = z3("vb", 256); kbe = z3("kbe", 128, fill=False); kd = z3("kd", 128, fill=False)
        qeT = z3("qeT", 128, fill=False); usb = z3("usb", 128, F32, fill=False)
        wT = z3("wT", 128, fill=False); vnew = z3("vn", 256); Sdec = z3("Sdec", 128, F32, fill=False)
        Mf = z3("Mf", 128, F32, fill=False); Ao = z3("Ao", 128, F32, fill=False)
        Xp = [z3("Xa", 128, F32, fill=False), z3("Xb2", 128, F32, fill=False)]
        XPN = ["Xa", "Xb2"]
        ETf = z3("ETf", 128, F32, fill=False)
        Td = z3("Td", 128, F32, fill=False); Uf = z3("Uf", 128, F32, fill=False)
        negIf = sc.sb("negIf" + sfx, [128, 128])
        self.ts("dve", negIf[:], ident, -1.0, None, ALU.mult, None, ["cst"], ["negIf"])
        m64b = c2[:, C2_B64:C2_B64 + 128].unsqueeze(1).to_broadcast([128, 8, 128])
        nm64b = c2[:, C2_NB64:C2_NB64 + 128].unsqueeze(1).to_broadcast([128, 8, 128])
        offb = c2[:, C2_OFF:C2_OFF + 128].unsqueeze(1).to_broadcast([128, 8, 128])
        m64b2 = c2[:, C2_B64:C2_B64 + 128].unsqueeze(1).to_broadcast([128, 2, 128])
        nm64b2 = c2[:, C2_NB64:C2_NB64 + 128].unsqueeze(1).to_broadcast([128, 2, 128])
        offb2 = c2[:, C2_OFF:C2_OFF + 128].unsqueeze(1).to_broadcast([128, 2, 128])
        kqr = Ring(sc, "kq" + sfx, [128, 8, 256], F32R, 2)
        ktr = Ring(sc, "kt" + sfx, [128, D], F32, 1)
        vtr = Ring(sc, "vt" + sfx, [128, D], F32, 1)
        otr = Ring(sc, "ot" + sfx, [128, D], F32, 1)
        Dg2 = [sc.sb("Dg%d" % i + sfx, [128, 2, 8, 128]) for i in range(2)]
        gsm2 = [sc.sb("gsm%d" % i + sfx, [128, 6, 8]) for i in range(2)]
        DB = sc.sb("DB" + sfx, [128, 8, 128]); DBT = sc.sb("DBT" + sfx, [128, 8, 128])
        DT = sc.sb("DT" + sfx, [128, 8, 128]); Ec = sc.sb("Ec" + sfx, [128, 8, 128])
        if dr == 1:
            ofr = Ring(sc, "of" + sfx, [128, D], F32, 1)
            szr = Ring(sc, "sz" + sfx, [128, D], F32, 1)
            ssq = sc.sb("ssq", [128, 8])
            onb = sc.sb("onb", [128, 128])
            self.ld(onb[:], W["o_norm"].partition_broadcast(128), [], ["onb"])
        NIT = 5
        P4 = range(4)

        def A(b_):
            return self.ps[b_][:, :].rearrange("p (s c) -> p s c", s=2), [("ps", b_)]

        def B(b_):
            return self.ps[4 + b_][:, :].rearrange("p (s c) -> p s c", s=2), [("ps", 4 + b_)]

        def keys(name):
            return [(name, b_) for b_ in P4]

        idb8 = ident.unsqueeze(1).to_broadcast([128, 8, 128])
        idb2 = ident.unsqueeze(1).to_broadcast([128, 2, 128])
        gk = ["g_all", "beta_all", "lnb_all"]

        def gates(jj_, sl_):
            gj_ = g_all[:, jj_, dr * 8:(dr + 1) * 8]
            lbj_ = lnb_all[:, jj_, dr * 8:(dr + 1) * 8]
            gs_ = gsm2[sl_]
            gsk = ("gsm", sl_)
            pg = self.ps[7]
            self.mm(pg[:, 0:8], Ltri, gj_, True, True, ["cst2"] + gk, bankk(7))
            self.mm(pg[:, 8:16], ones, gj_, True, True, ["cst"] + gk, bankk(7))
            gc_, gb_, ebg_, ekd_, egl_, tmpg_ = (gs_[:, i, :] for i in range(6))
            self.cp("act", gc_, pg[:, 0:8], bankk(7), [gsk])
            self.tt("dve", gb_, gc_, lbj_, ALU.add, [gsk] + gk, [gsk])
            self.act(ebg_, gb_, AF.Exp, [gsk], [gsk])
            self.tt("dve", tmpg_, pg[:, 8:16], gc_, ALU.subtract, bankk(7) + [gsk], [gsk])
            self.act(ekd_, tmpg_, AF.Exp, [gsk], [gsk])
            self.act(egl_, pg[:, 8:16], AF.Exp, bankk(7), [gsk])
            self.tt("pool", Dg2[sl_][:, 0, :, :], idb8, gc_.unsqueeze(2).to_broadcast([128, 8, 128]), ALU.mult, ["cst", gsk], [("Dg0", sl_)])
            self.tt("pool", Dg2[sl_][:, 1, :, :], idb8, gb_.unsqueeze(2).to_broadcast([128, 8, 128]), ALU.mult, ["cst", gsk], [("Dg1", sl_)])

        def kq_load(jj_):
            tsl_ = slice(jj_ * 128, (jj_ + 1) * 128)
            kq_, kqk_ = kqr.next()
            self.ld(kq_[:, :, 0:128], dd["kT_d"][:, tsl_].rearrange("(h p) t -> p h t", p=128), [], [kqk_ + ("k",)], q="pool")
            keys_ = [kqk_ + ("k",)]
            if jj_ < NT_LAT:
                self.ld(kq_[:, :, 128:256], dd["qT_d"][:, tsl_].rearrange("(h p) t -> p h t", p=128), [], [kqk_ + ("q",)], q="pool")
                keys_.append(kqk_ + ("q",))
            return kq_, kqk_, keys_

        gates(order[0], 0)
        kq_nxt = kq_load(order[0])
        for oi, jj in enumerate(order):
            sl = oi % 2
            gsk = ("gsm", sl)
            Dg = Dg2[sl]
            lat = jj < NT_LAT
            tsl = slice(jj * 128, (jj + 1) * 128)
            kq, kqk, kqkeys = kq_nxt
            if oi + 1 < len(order):
                kq_nxt = kq_load(order[oi + 1])
            kt, ktk = ktr.next()
            self.ld(kt[:], dd["ktok_d"][tsl, :], [], [ktk])
            vt, vtk = vtr.next()
            self.ld(vt[:], dd["vtok_d"][tsl, :], [], [vtk])
            kt3 = kt[:, :].rearrange("p (h d) -> p h d", h=8)
            vt3 = vt[:, :].rearrange("p (h d) -> p h d", h=8)
            if dr == 1 and lat:
                of, ofk = ofr.next()
                self.ld(of[:], dd["of_d"][tsl, :], [("of_d", jj)], [ofk])
                szt, szk = szr.next()
                self.ld(szt[:], dd["sz_d"][tsl, :], [], [szk])
            bj = beta_all[:, jj, dr * 8:(dr + 1) * 8]
            gc, gb, ebg, ekd, egl, tmpg = (gsm2[sl][:, i, :] for i in range(6))
            for half in range(2):
                self.mm(self.ps[4 + half][:, :], ones, Dg[:, 0, half * 4:(half + 1) * 4, :], True, True, ["cst", ("Dg0", sl)], bankk(4 + half))
                self.mm(self.ps[6 + half][:, :], ones, Dg[:, 1, half * 4:(half + 1) * 4, :], True, True, ["cst", ("Dg1", sl)], bankk(6 + half))
            ncol = 256 if lat else 128
            for b_ in P4:
                ap_, apk = A(b_)
                for s_ in range(2):
                    h = 2 * b_ + s_
                    self.mm(ap_[:, s_, 0:ncol], kq[:, h, 0:128], kq[:, h, 0:ncol], True, True, kqkeys, apk)
            if oi + 1 < len(order):
                gates_next = (order[oi + 1], 1 - sl)
            else:
                gates_next = None
            for half in range(2):
                hs = slice(half * 4, half * 4 + 4)
                pRc = self.ps[4 + half][:, :].rearrange("p (h f) -> p h f", h=4)
                pRb = self.ps[6 + half][:, :].rearrange("p (h f) -> p h f", h=4)
                gbb = gb[:, hs].unsqueeze(2).to_broadcast([128, 4, 128])
                gcb = gc[:, hs].unsqueeze(2).to_broadcast([128, 4, 128])
                self.tt("dve", DB[:, hs, :], gbb, pRc, ALU.subtract, [gsk] + bankk(4 + half), ["DB"])
                self.tt("pool", DB[:, hs, :], DB[:, hs, :], m_db.unsqueeze(1).to_broadcast([128, 4, 128]), ALU.add, ["DB", "cst2"], ["DB"])
                self.tt("dve", DBT[:, hs, :], pRb, gcb, ALU.subtract, [gsk] + bankk(6 + half), ["DBT"])
                self.tt("pool", DBT[:, hs, :], DBT[:, hs, :], m_dbt.unsqueeze(1).to_broadcast([128, 4, 128]), ALU.add, ["DBT", "cst2"], ["DBT"])
                if lat:
                    self.tt("dve", DT[:, hs, :], pRc, gcb, ALU.subtract, [gsk] + bankk(4 + half), ["DT"])
                    self.tt("pool", DT[:, hs, :], DT[:, hs, :], m_dt.unsqueeze(1).to_broadcast([128, 4, 128]), ALU.add, ["DT", "cst2"], ["DT"])
                    self.act(Ec[:, hs, :], pRc, AF.Exp, bankk(4 + half), ["Ec"])
            self.act(DB[:], DB[:], AF.Exp, ["DB"], ["DB"])
            self.act(DBT[:], DBT[:], AF.Exp, ["DBT"], ["DBT"])
            if lat:
                self.act(DT[:], DT[:], AF.Exp, ["DT"], ["DT"])
            for b_ in P4:
                ap_, apk = A(b_)
                hp = slice(2 * b_, 2 * b_ + 2)
                self.tt("dve", Mf[:, hp, :], ap_[:, :, 0:128], DB[:, hp, :], ALU.mult, apk + ["DB"], [("Mf", b_)])
                self.tt("dve", Xp[0][:, hp, :], ap_[:, :, 0:128], DBT[:, hp, :], ALU.mult, apk + ["DBT"], [("Xa", b_)])
                if lat:
                    self.tt("dve", qkT[:, hp, :], ap_[:, :, 128:256], DT[:, hp, :], ALU.mult, apk + ["DT"], [("qkT", b_)])
            for b_ in P4:
                hp = slice(2 * b_, 2 * b_ + 2)
                self.tt("pool", Ao[:, hp, :], Mf[:, hp, :], offb2, ALU.mult, [("Mf", b_), "cst2"], [("Ao", b_)])
                self.tt("pool", Mf[:, hp, :], Mf[:, hp, :], m64b2, ALU.mult, [("Mf", b_), ("Ao", b_), "cst2"], [("Mf", b_)])
                self.tt("pool", Mf[:, hp, :], Mf[:, hp, :], idb2, ALU.add, [("Mf", b_), "cst"], [("Mf", b_)])
                self.tt("pool", Xp[0][:, hp, :], Xp[0][:, hp, :], nm64b2, ALU.mult, [("Xa", b_), "cst2"], [("Xa", b_)])
                self.tt("pool", Xp[0][:, hp, :], Xp[0][:, hp, :], idb2, ALU.add, [("Xa", b_), "cst"], [("Xa", b_)])
            if gates_next is not None:
                gates(*gates_next)
            cur = 0
            for it in range(NIT):
                src, srck = Xp[cur], XPN[cur]
                dst, dstk = Xp[1 - cur], XPN[1 - cur]
                for b_ in P4:
                    ap_, apk = A(b_)
                    for s_ in range(2):
                        h = 2 * b_ + s_
                        self.mm(ap_[:, s_, 0:128], src[:, h, :], Mf[:, h, :], True, True, [(srck, b_), ("Mf", b_)], apk)
                for b_ in P4:
                    ap_, apk = A(b_)
                    self.stt("dve", ETf[:, 2 * b_:2 * b_ + 2, :], ap_[:, :, 0:128], -1.0, idb2, ALU.mult, ALU.add, apk + ["cst"], [("ETf", b_)])
                for b_ in P4:
                    bp_, bpk = B(b_)
                    for s_ in range(2):
                        h = 2 * b_ + s_
                        self.mm(bp_[:, s_, 0:128], ETf[:, h, :], src[:, h, :], True, True, [("ETf", b_), (srck, b_)], bpk)
                for b_ in P4:
                    bp_, bpk = B(b_)
                    hp = slice(2 * b_, 2 * b_ + 2)
                    self.tt("dve", dst[:, hp, :], src[:, hp, :], bp_[:, :, 0:128], ALU.add, [(srck, b_)] + bpk, [(dstk, b_)])
                cur = 1 - cur
            Xd, Xdk = Xp[cur], XPN[cur]
            for b_ in P4:
                ap_, apk = A(b_)
                bp_, bpk = B(b_)
                for s_ in range(2):
                    h = 2 * b_ + s_
                    self.tr(ap_[:, s_, 0:128], Xd[:, h, :], [(Xdk, b_)], apk)
                    self.mm(bp_[:, s_, 0:128], Ao[:, h, :], Xd[:, h, :], True, True, [("Ao", b_), (Xdk, b_)], bpk)
            for b_ in P4:
                ap_, apk = A(b_)
                bp_, bpk = B(b_)
                hp = slice(2 * b_, 2 * b_ + 2)
                self.cp("act", Td[:, hp, :], ap_[:, :, 0:128], apk, [("Td", b_)])
                self.cp("act", Uf[:, hp, :], bp_[:, :, 0:128], bpk, [("Uf", b_)])
            for b_ in P4:
                ap_, apk = A(b_)
                for s_ in range(2):
                    h = 2 * b_ + s_
                    self.mm(ap_[:, s_, 0:128], Td[:, h, :], Uf[:, h, :], True, True, [("Td", b_), ("Uf", b_)], apk)
            for b_ in P4:
                ap_, apk = A(b_)
                hp = slice(2 * b_, 2 * b_ + 2)
                self.tt("dve", Xb[0][:, hp, 0:128], Xd[:, hp, :], ap_[:, :, 0:128], ALU.subtract, [(Xdk, b_)] + apk, [("X0", b_)])
            cur = 0
            XN = ["X0"]
            Xf, kf_ = Xb[cur], XN[cur]
            self.tt("dve", vb[:, :, 0:128], vt3, bj.unsqueeze(2).to_broadcast([128, 8, 128]), ALU.mult, [vtk] + gk, keys("vb"))
            self.tt("pool", kbe[:, :, :], kt3, ebg.unsqueeze(2).to_broadcast([128, 8, 128]), ALU.mult, [ktk, gsk], keys("kbe"))
            self.tt("pool", kd[:, :, :], kt3, ekd.unsqueeze(2).to_broadcast([128, 8, 128]), ALU.mult, [ktk, gsk], keys("kd"))
            if lat:
                self.tt("pool", qeT[:, :, :], kq[:, :, 128:256], Ec[:, :, :], ALU.mult, kqkeys + ["Ec"], keys("qeT"))
            for b_ in P4:
                ap_, apk = A(b_)
                bp_, bpk = B(b_)
                for s_ in range(2):
                    h = 2 * b_ + s_
                    self.mm(ap_[:, s_, :], Xf[:, h, 0:128], vb[:, h, :], True, True, [(kf_, b_), ("vb", b_)], apk)
                    self.mm(bp_[:, s_, :], kbe[:, h, :], Xf[:, h, :], True, True, [(kf_, b_), ("kbe", b_)], bpk)
            for b_ in P4:
                ap_, apk = A(b_)
                bp_, bpk = B(b_)
                hp = slice(2 * b_, 2 * b_ + 2)
                self.cp("act", usb[:, hp, :], ap_[:, :, 0:128], apk, [("usb", b_)])
                self.cp("act", wT[:, hp, :], bp_[:, :, 0:128], bpk, [("wT", b_)])
            ot, otk = otr.next()
            ot3 = ot[:, :].rearrange("p (h d) -> p h d", h=8)
            for b_ in P4:
                ap_, apk = A(b_)
                for s_ in range(2):
                    h = 2 * b_ + s_
                    self.mm(ap_[:, s_, :], wT[:, h, :], Sst[:, h, :], True, True, [("wT", b_), ("S", b_)], apk)
            for b_ in P4:
                ap_, apk = A(b_)
                hp = slice(2 * b_, 2 * b_ + 2)
                self.tt("dve", vnew[:, hp, 0:128], usb[:, hp, :], ap_[:, :, 0:128], ALU.subtract, [("usb", b_)] + apk, [("vn", b_)])
            for b_ in P4:
                ap_, apk = A(b_)
                bp_, bpk = B(b_)
                for s_ in range(2):
                    h = 2 * b_ + s_
                    if lat:
                        self.mm(bp_[:, s_, :], qeT[:, h, :], Sst[:, h, :], True, False, [("qeT", b_), ("S", b_)], bpk)
                        self.mm(bp_[:, s_, :], qkT[:, h, :], vnew[:, h, :], False, True, [("qkT", b_), ("vn", b_)], bpk)
                    self.mm(ap_[:, s_, :], kd[:, h, :], vnew[:, h, :], True, True, [("kd", b_), ("vn", b_)], apk)
            self.tt("pool", Sdec[:, :, :], Sst[:, :, 0:128], egl.unsqueeze(2).to_broadcast([128, 8, 128]), ALU.mult,
                    keys("S") + [gsk], keys("Sdec"))
            for b_ in P4:
                ap_, apk = A(b_)
                bp_, bpk = B(b_)
                hp = slice(2 * b_, 2 * b_ + 2)
                if lat:
                    self.cp("act", ot3[:, hp, :], bp_[:, :, 0:128], bpk, [otk])
                self.tt("dve", Sst[:, hp, 0:128], Sdec[:, hp, :], ap_[:, :, 0:128], ALU.add, [("Sdec", b_)] + apk, [("S", b_)])
            if not lat:
                continue
            if dr == 0:
                self.ld(dd["of_d"][tsl, :], ot[:], [otk], [("of_d", jj)])
            else:
                if self.debug:
                    if not hasattr(self, "ob_d"):
                        self.ob_d = self.nc.dram_tensor("ob_d", [NLAT, D], F32, kind="ExternalOutput").ap()
                    self.ld(self.ob_d[tsl, :], ot[:], [otk], [("ob_d", jj)])
                self.tt("pool", ot[:], ot[:], of[:], ALU.add, [otk, ofk], [otk])
                self.act(DB[:, :, :].rearrange("p h d -> p (h d)"), ot[:], AF.Square, [otk], ["DB"])
                S.op("dve", lambda e: e.tensor_reduce(out=ssq[:], in_=DB[:, :, :], axis=AX.X, op=ALU.add),
                     ["DB"], ["ssq"])
                self.ts("dve", ssq[:], ssq[:], 1.0 / 128, EPS, ALU.mult, ALU.add, ["ssq"], ["ssq"])
                self.act(ssq[:], ssq[:], AF.Sqrt, ["ssq"], ["ssq"])
                S.op("dve", lambda e: e.reciprocal(out=ssq[:], in_=ssq[:]), ["ssq"], ["ssq"])
                o3 = ot[:, :].rearrange("p (h d) -> p h d", h=8)
                self.tt("dve", o3, o3, ssq[:].unsqueeze(2).to_broadcast([128, 8, 128]), ALU.mult, [otk, "ssq"], [otk])
                self.tt("pool", o3, o3, onb[:].unsqueeze(1).to_broadcast([128, 8, 128]), ALU.mult, [otk, "onb"], [otk])
                self.tt("pool", ot[:], ot[:], szt[:], ALU.mult, [otk, szk], [otk])
                self.ld(dd["of_d"][tsl, :], ot[:], [otk, ofk], [("of_d", jj)])
        sc.close()

    def dn_out(self, of_d):
        nc, S, W = self.nc, self.S, self.W[1]
        sc = Scope(self)
        wo = sc.sb("wo1", [128, 8, D], F32R)
        self.ld(wo[:], W["w_o"].rearrange("(k p) n -> p k n", p=128), [], ["wo1"], q="pool")
        wr = sc.sb("wr1", [128, 8, NE])
        self.ld(wr[:], W["router"].rearrange("(k p) n -> p k n", p=128), [], ["wr"])
        yr = Ring(sc, "oy", [128, D], F32, 2)
        xr = Ring(sc, "ox", [128, D], F32, 2)
        xmr = Ring(sc, "oxm", [128, D], F32, 3)
        ssr = Ring(sc, "oss", [128, 1], F32, 4)
        junk = sc.sb("ojunk", [128, D])
        yT = sc.sb("oyT", [128, 8, 128], F32R)
        h2T = sc.sb("oh2T", [128, 8, 128])
        lg = sc.sb("olg", [128, NE]); mx = sc.sb("omx", [128, 1]); sm = sc.sb("osm", [128, 1])
        pend = [None]

        def o_loads(i_):
            yt_, ytk_ = yr.next()
            self.ld(yt_[:], of_d[i_ * 128:(i_ + 1) * 128, :], [], [ytk_])
            xt_, xk_ = xr.next()
            self.ld(xt_[:], self.xres[0][i_ * 128:(i_ + 1) * 128, :], [], [xk_])
            return yt_, ytk_, xt_, xk_

        onxt = o_loads(0)
        for i in range(NT_LAT):
            yt, ytk, xt, xk = onxt
            if i + 1 < NT_LAT:
                onxt = o_loads(i + 1)
            for kc in range(8):
                self.tr(self.ps[kc // 4][:, (kc % 4) * 128:(kc % 4 + 1) * 128], yt[:, kc * 128:(kc + 1) * 128], [ytk], [self.psk[kc // 4]])
            for b in range(2):
                self.cp("act", yT[:, b * 4:(b + 1) * 4, :], self.ps[b][:, :].rearrange("p (k t) -> p k t", k=4), [self.psk[b]], ["yT"])
            if pend[0] is not None:
                self.moe_prep(*pend[0])
                pend[0] = None
            xm, xmk = xmr.next()
            for dh in range(2):
                pM, pMk = self.ps[2 + dh], self.psk[2 + dh]
                for kc in range(8):
                    self.mm(pM[:, :], yT[:, kc, :], wo[:, kc, dh * 512:(dh + 1) * 512], kc == 0, kc == 7, ["yT", "wo1"], [pMk])
                self.tt("dve", xm[:, dh * 512:(dh + 1) * 512], pM[:, :], self.Gbc[:, 0, 0, dh * 512:(dh + 1) * 512], ALU.mult,
                        [pMk, ("Gbc", 0, 0)], [xmk])
            self.tt("dve", xm[:], xm[:], xt[:], ALU.add, [xmk, xk], [xmk])
            self.ld(self.xres[1][i * 128:(i + 1) * 128, :], xm[:], [xmk], [("xres1", i)])
            pend[0] = (i, 0, xm, xmk, junk, ssr, wr, h2T, lg, mx, sm)
        self.moe_prep(*pend[0])
        sc.close()

    def final_norm(self):
        nc, S = self.nc, self.S
        sc = Scope(self)
        fn = sc.sb("fnb", [128, D])
        self.ld(fn[:], self.inp["final_norm"].partition_broadcast(128), [], ["fnb"])
        xr = Ring(sc, "fx", [128, D], F32, 4)
        ssr = Ring(sc, "fss", [128, 1], F32, 4)
        junk = sc.sb("fjunk", [128, D])
        def f_load(i_):
            xt_, xk_ = xr.next()
            self.ld(xt_[:], self.xres[1][i_ * 128:(i_ + 1) * 128, :], [], [xk_])
            return xt_, xk_

        fq_ = [f_load(0), f_load(1)]
        for i in range(NT_LAT):
            xt, xk = fq_.pop(0)
            if i + 2 < NT_LAT:
                fq_.append(f_load(i + 2))
            ss, ssk = ssr.next()
            self.rstd_of(xt[:], xk, junk[:], "fjunk", ss[:], ssk)
            self.stt("dve", xt[:], xt[:], ss[:, 0:1], fn[:], ALU.mult, ALU.mult, [xk, ssk, "fnb"], [xk])
            S.dma("sp", lambda e: e.dma_start(out=self.out[i * 128:(i + 1) * 128, :], in_=xt[:]), [xk], [("out", i)], is_output=True)
        sc.close()

    def moe(self, l, xres, xresk, with_ctx):
        nc, S, W = self.nc, self.S, self.W[l]
        cst = self.cst
        ones = cst[:, C_ONES:C_ONES + 128]
        Umat = cst[:, C_U:C_U + 128]
        iota = cst[:, C_IOTA:C_IOTA + 512]
        sets = [(0, 0, NT_LAT, CAP_LAT)] + ([(1, NT_LAT, NT_CTX, CAP_CTX)] if with_ctx else [])
        sc = Scope(self)
        slot_m = {}; meta = {}
        for (si, j0, nj, cap) in sets:
            slot_m[si] = sc.sb("slotm%d_%d" % (si, l), [128, nj, NE])
            meta[si] = sc.sb("meta%d_%d" % (si, l), [128, nj, NE, 4], F32R)
        scr = Scope(self)
        for (si, j0, nj, cap) in sets:
            sfx = "%d_%d" % (si, l)
            affv = self.aff[:, j0:j0 + nj, :]
            affk = [("aff", j) for j in range(j0, j0 + nj)]
            lo = scr.sb("lo" + sfx, [128, NE]); mid = scr.sb("mid" + sfx, [128, NE])
            cmpt = scr.sb("cmp" + sfx, [128, nj, NE]); cnt = scr.sb("cnt" + sfx, [128, NE])
            tq = scr.sb("tq" + sfx, [128, NE])
            offs = scr.sb("offs" + sfx, [128, nj, NE]); slot = scr.sb("slot" + sfx, [128, nj, NE])
            S.op("dve", lambda e: e.memset(lo[:], 0.0), [], ["lo"])
            S.op("dve", lambda e: e.memset(mid[:], 0.5), [], ["mid"])
            pC, pCk = self.ps[0], self.psk[0]
            for it in range(NBIS):
                w = 2.0 ** -(it + 1)
                self.tt("dve", cmpt[:], affv, mid[:].unsqueeze(1).to_broadcast([128, nj, NE]), ALU.is_ge, affk + ["mid"], ["cmp"])
                S.op("dve", lambda e: e.tensor_reduce(out=cnt[:], in_=cmpt[:, :, :].rearrange("p j e -> p e j"), axis=AX.X, op=ALU.add),
                     ["cmp"], ["cnt"])
                self.mm(pC[:, 0:NE], ones, cnt[:], True, True, ["cst", "cnt"], [pCk])
                self.ts("dve", tq[:], pC[:, 0:NE], cap - 0.5, w, ALU.is_ge, ALU.mult, [pCk], ["tq"])
                self.tt("dve", lo[:], lo[:], tq[:], ALU.add, ["lo", "tq"], ["lo"])
                self.ts("dve", mid[:], lo[:], w * 0.5, None, ALU.add, None, ["lo"], ["mid"])
            self.tt("dve", cmpt[:], affv, lo[:].unsqueeze(1).to_broadcast([128, nj, NE]), ALU.is_ge, affk + ["lo"], ["cmp"])
            pP, pPk = self.ps[1], self.psk[1]
            pT, pTk = self.ps[2], self.psk[2]
            mflat = cmpt[:, :, :].rearrange("p j e -> p (j e)")
            self.mm(pP[:, 0:nj * NE], Umat, mflat, True, True, ["cst", "cmp"], [pPk])
            self.mm(pT[:, 0:nj * NE], ones, mflat, True, True, ["cst", "cmp"], [pTk])
            pTv = pT[:, 0:nj * NE].rearrange("p (j e) -> p j e", e=NE)
            pPv = pP[:, 0:nj * NE].rearrange("p (j e) -> p j e", e=NE)
            S.op("dve", lambda e: e.memset(offs[:, 0, :], 0.0), [], ["offs"])
            for j in range(1, nj):
                self.tt("dve", offs[:, j, :], pTv[:, j - 1, :], offs[:, j - 1, :], ALU.add, [pTk, "offs"], ["offs"])
            self.tt("dve", slot[:], pPv, offs[:], ALU.add, [pPk, "offs"], ["slot"])
            self.ts("dve", cmpt[:], cmpt[:], -1.0e6, 1.0e6, ALU.mult, ALU.add, ["cmp"], ["cmp"])
            self.tt("dve", slot_m[si][:], slot[:], cmpt[:], ALU.add, ["slot", "cmp"], [("slotm", si)])
            mt = meta[si]
            for j in range(nj):
                self.cp("dve", mt[:, j, :, 0:1], cst[:, C_U:C_U + 1].unsqueeze(1).to_broadcast([128, NE, 1]), ["cst"], [("meta", si)])
                self.ts("dve", mt[:, j, :, 0:1], mt[:, j, :, 0:1], float(j0 + j), None, ALU.add, None, [("meta", si)], [("meta", si)])
            mtf = mt[:, :, :, :].rearrange("p j e c -> p (j e) c")
            self.cp("dve", mtf[:, :, 1:2], cst[:, C_PIDX:C_PIDX + 1].unsqueeze(1).to_broadcast([128, nj * NE, 1]), ["cst"], [("meta", si)])
            self.cp("dve", mtf[:, :, 2:3], affv.rearrange("p j e -> p (j e)").unsqueeze(2), affk, [("meta", si)])
            self.cp("dve", mtf[:, :, 3:4], cst[:, C_ONES:C_ONES + 1].unsqueeze(1).to_broadcast([128, nj * NE, 1]), ["cst"], [("meta", si)])
        scr.close()

        NW = 8
        wring = Ring(sc, "wm%d" % l, [128, 8, 256], F32R, NW)
        xsT = sc.sb("xsT%d" % l, [128, 8, 640], F32R)
        hidT = sc.sb("hidT%d" % l, [128, 16, 544], F32R)
        ysb = sc.sb("ysb%d" % l, [128, 5, D])
        xsr = Ring(sc, "xstok%d" % l, [128, D], F32, 2)
        selr = Ring(sc, "sel%d" % l, [128, 512], F32R, 2)
        selc = sc.sb("selc%d" % l, [128, 128], F32R)
        sgr = Ring(sc, "sg%d" % l, [128, 512], F32, 2)
        hcr = Ring(sc, "hc%d" % l, [32, 256], F32, 2)
        idxrow = sc.sb("idxrow%d" % l, [4, 640])
        metac = sc.sb("metac%d" % l, [128, 5, 4])
        tmp5 = sc.sb("tmp5%d" % l, [128, 5]); idxf = sc.sb("idxf%d" % l, [128, 5])
        idur = Ring(sc, "idu%d" % l, [128, 5], U32, 3)
        gcr = Ring(sc, "gc%d" % l, [128, 5], F32, 3)
        self.cp("dve", selc[:], cst[:, C_U:C_U + 1].to_broadcast([128, 128]), ["cst"], ["selc"])
        S.op("dve", lambda e: e.memset(ysb[:], 0.0), [], ["ysb"])
        nk = 5 if with_ctx else 4
        c2 = {0: 0, 1: 1}

        def idx_phase(e):
            pI, pIk = self.ps[7], self.psk[7]
            for (si, j0, nj, cap) in sets:
                ncol = 512 if si == 0 else 128
                for j in range(nj):
                    if si == 0:
                        sel, selk = selr.next()
                        self.ts("dve", sel[:], iota, slot_m[si][:, j, e:e + 1], None, ALU.is_equal, None, ["cst", ("slotm", si)], [selk])
                        rhs = sel[:]
                    else:
                        selk = "selc"
                        self.ts("dve", selc[:, 0:CAP_CTX], iota[:, 0:CAP_CTX], slot_m[si][:, j, e:e + 1], None, ALU.is_equal, None,
                                ["cst", ("slotm", si)], [selk])
                        rhs = selc[:]
                    self.mm(pI[0:4, 0:ncol], meta[si][:, j, e, :], rhs, j == 0, j == nj - 1, [("meta", si), selk], [pIk])
                off = 0 if si == 0 else 512
                self.cp("act", idxrow[0:4, off:off + ncol], pI[0:4, 0:ncol], [pIk], ["idxrow"])
            pX, pXk = self.ps[7], self.psk[7]
            for k in range(nk):
                self.tr(pX[:, k * 4:(k + 1) * 4], idxrow[0:4, k * 128:(k + 1) * 128], ["idxrow"], [pXk], kp=4)
            self.cp("act", metac[:, 0:nk, :], pX[:, 0:nk * 4].rearrange("p (k c) -> p k c", c=4), [pXk], ["metac"])
            idu, iduk = idur.next()
            gc, gck = gcr.next()
            self.stt("dve", idxf[:, 0:nk], metac[:, 0:nk, 0], 128.0, metac[:, 0:nk, 1], ALU.mult, ALU.add, ["metac"], ["idxf"])
            self.ts("dve", tmp5[:, 0:nk], metac[:, 0:nk, 3], -1.0, 1.0, ALU.mult, ALU.add, ["metac"], ["tmp5"])
            self.tt("dve", tmp5[:, 0:nk], tmp5[:, 0:nk], cst[:, C_DMY:C_DMY + nk], ALU.mult, ["tmp5", "cst"], ["tmp5"])
            self.tt("dve", idxf[:, 0:nk], idxf[:, 0:nk], tmp5[:, 0:nk], ALU.add, ["idxf", "tmp5"], ["idxf"])
            self.cp("dve", idu[:, 0:nk], idxf[:, 0:nk], ["idxf"], [iduk])
            self.cp("dve", gc[:, 0:nk], metac[:, 0:nk, 2], ["metac"], [gck])
            return (idu, iduk, gc, gck)

        gt_i = [0]

        def gather_phase(ix):
            idu, iduk, gc, gck = ix
            for k in range(nk):
                xs, xsk = xsr.next()
                S.dma("pool", lambda e: e.indirect_dma_start(out=xs[:], out_offset=None, in_=self.xn2[:, :],
                                                              in_offset=bass.IndirectOffsetOnAxis(ap=idu[:, k:k + 1], axis=0)),
                      [iduk], [xsk])
                c = 1 if k == 4 else 0
                for half in range(2):
                    bnk = gt_i[0] % 4
                    gt_i[0] += 1
                    pt, ptk = self.ps[bnk], self.psk[bnk]
                    for kc in range(half * 4, half * 4 + 4):
                        self.tr(pt[:, (kc % 4) * 128:(kc % 4 + 1) * 128], xs[:, kc * 128:(kc + 1) * 128], [xsk], [ptk])
                    for kc in range(half * 4, half * 4 + 4):
                        self.act(xsT[:, kc, k * 128:(k + 1) * 128], pt[:, (kc % 4) * 128:(kc % 4 + 1) * 128], AF.Identity,
                                 [ptk, ("A", 4), "cols"], ["xsT"], scale=self.A2[:, kc, c:c + 1], bias=self.B2(kc, c))

        def wload(src_ap):
            wt, wk = wring.next()
            self.ld(wt[:], src_ap, [], [wk], q="pool")
            return wt, wk

        gu_i = [0]

        cpend = [None]

        def ctx_tr(hc, hck, fq):
            pt, ptk = self.ps[7], self.psk[7]
            for fc in range(2):
                self.tr(pt[:, fc * 32:(fc + 1) * 32], hc[0:32, fc * 128:(fc + 1) * 128], [hck], [ptk], kp=32)
            self.cp("act", hidT[:, fq * 2:fq * 2 + 2, 512:544], pt[:, 0:64].rearrange("p (a t) -> p a t", a=2), [ptk],
                    [("hidT", fq * 2), ("hidT", fq * 2 + 1)])

        def ffn1(e):
            for fq in range(8):
                wg, wgk = wload(W["w_gate"][e, :, fq * 256:(fq + 1) * 256].rearrange("(k p) n -> p k n", p=128))
                wu, wuk = wload(W["w_up"][e, :, fq * 256:(fq + 1) * 256].rearrange("(k p) n -> p k n", p=128))
                for fc in range(2):
                    fcc = fq * 2 + fc
                    b = gu_i[0] % 2
                    gu_i[0] += 1
                    pG, pGk = self.ps[2 * b], self.psk[2 * b]
                    pU, pUk = self.ps[2 * b + 1], self.psk[2 * b + 1]
                    for kc in range(8):
                        self.mm(pG[:, :], wg[:, kc, fc * 128:(fc + 1) * 128], xsT[:, kc, 0:512], kc == 0, kc == 7, [wgk, "xsT"], [pGk])
                    for kc in range(8):
                        self.mm(pU[:, :], wu[:, kc, fc * 128:(fc + 1) * 128], xsT[:, kc, 0:512], kc == 0, kc == 7, [wuk, "xsT"], [pUk])
                    sg, sgk = sgr.next()
                    self.act(sg[:, 0:512], pG[:, :], AF.Silu, [pGk], [sgk])
                    self.tt("dve", hidT[:, fcc, 0:512], sg[:, 0:512], pU[:, :], ALU.mult, [sgk, pUk], [("hidT", fcc)])
                if with_ctx:
                    pc, pck = self.ps[4], self.psk[4]
                    for kc in range(8):
                        self.mm(pc[0:32, 0:256], xsT[:, kc, 512:544], wg[:, kc, :], kc == 0, kc == 7, [wgk, "xsT"], [pck])
                    for kc in range(8):
                        self.mm(pc[0:32, 256:512], xsT[:, kc, 512:544], wu[:, kc, :], kc == 0, kc == 7, [wuk, "xsT"], [pck])
                    hc, hck = hcr.next()
                    self.act(hc[0:32, 0:256], pc[0:32, 0:256], AF.Silu, [pck], [hck])
                    self.tt("dve", hc[0:32, 0:256], hc[0:32, 0:256], pc[0:32, 256:512], ALU.mult, [hck, pck], [hck])
                    if cpend[0] is not None:
                        ctx_tr(*cpend[0])
                    cpend[0] = (hc, hck, fq)
            if cpend[0] is not None:
                ctx_tr(*cpend[0])
                cpend[0] = None

        y_i = [0]

        def ffn2(e, ix):
            idu, iduk, gc, gck = ix
            hk = [("hidT", f) for f in range(16)]
            for dq in range(4):
                wd = []
                for fh in range(2):
                    wd.append(wload(W["w_down"][e, fh * 1024:(fh + 1) * 1024, dq * 256:(dq + 1) * 256].rearrange("(k p) n -> p k n", p=128)))
                for k in range(nk):
                    if k < 4:
                        b = y_i[0] % 2
                        y_i[0] += 1
                        pY, pYk = self.ps[5 + b], self.psk[5 + b]
                        rows = 128
                        lsl = slice(k * 128, (k + 1) * 128)
                    else:
                        pY, pYk = self.ps[4], self.psk[4]
                        rows = 32
                        lsl = slice(512, 544)
                    for fcc in range(16):
                        wt, wk = wd[fcc // 8]
                        self.mm(pY[0:rows, 0:256], hidT[:, fcc, lsl], wt[:, fcc % 8, :], fcc == 0, fcc == 15, [("hidT", fcc), wk], [pYk])
                    c = 1 if k == 4 else 0
                    self.stt("dve", ysb[0:rows, k, dq * 256:(dq + 1) * 256], pY[0:rows, 0:256], gc[0:rows, k:k + 1],
                             self.Gbc[0:rows, 1, c, dq * 256:(dq + 1) * 256], ALU.mult, ALU.mult,
                             [pYk, gck, ("Gbc", 1, c)], [("ysb", k)])

        def scatter_phase(ix):
            idu, iduk, gc, gck = ix
            for k in range(nk):
                S.dma("pool", lambda e: e.indirect_dma_start(out=xres[:, :], out_offset=bass.IndirectOffsetOnAxis(ap=idu[:, k:k + 1], axis=0),
                                                              in_=ysb[:, k, :], in_offset=None, compute_op=ALU.add),
                      [iduk, ("ysb", k), "ysb"], ["xacc"])

        n_exp = NE if self.n_exp is None else self.n_exp
        ixs = {0: idx_phase(0)}
        gather_phase(ixs[0])
        for e in range(n_exp):
            if e + 1 < n_exp:
                ixs[e + 1] = idx_phase(e + 1)
            ffn1(e)
            if e > 0:
                scatter_phase(ixs[e - 1])
            if e + 1 < n_exp:
                gather_phase(ixs[e + 1])
            ffn2(e, ixs[e])
        scatter_phase(ixs[n_exp - 1])
        sc.close()


def _host_consts():
    cp = np.zeros((128, C_END), np.float32)
    cp[:, C_ID:C_ID + 128] = np.eye(128, dtype=np.float32)
    cp[:, C_ONES:C_ONES + 128] = 1.0
    pi = np.arange(128)
    cp[:, C_U:C_U + 128] = (pi[:, None] < pi[None, :]).astype(np.float32)
    mp = (pi[:, None] >= pi[None, :]).astype(np.float32)
    mn = (pi[:, None] <= pi[None, :]).astype(np.float32)
    cp[:, C_MP:C_MP + 512] = np.tile(mp, (1, 4))
    cp[:, C_MN:C_MN + 512] = np.tile(mn, (1, 4))
    cp[:, C_IOTA:C_IOTA + 512] = np.arange(512, dtype=np.float32)[None, :]
    cp[:, C_PIDX] = pi
    for k in range(5):
        cp[:, C_DMY + k] = NTOK + k * 128 + pi
    t = np.arange(NLAT)
    row = (t // 64).astype(np.float32)
    col = (t % 64).astype(np.float32)
    inv = (np.float32(10000.0) ** (-np.arange(16, dtype=np.float32) / np.float32(16))).astype(np.float32)
    ar = (row[:, None] * inv[None, :]).astype(np.float32)
    ac = (col[:, None] * inv[None, :]).astype(np.float32)
    rope = np.concatenate([np.cos(ar), np.cos(ac), np.sin(ar), np.sin(ac)], axis=1).astype(np.float32)
    return cp, rope


def _host_consts2():
    c2 = np.zeros((128, C2_END), np.float32)
    p = np.arange(128)[:, None]
    f = np.arange(128)[None, :]
    c2[:, C2_LF:C2_LF + 128] = (p <= f)
    c2[:, C2_LB:C2_LB + 128] = (p >= f)
    c2[:, C2_LT:C2_LT + 128] = np.where(f < p, 0.0, NEG)
    c2[:, C2_GT:C2_GT + 128] = np.where(f > p, 0.0, NEG)
    c2[:, C2_GE:C2_GE + 128] = np.where(f >= p, 0.0, NEG)
    c2[:, C2_LE:C2_LE + 128] = np.where(f <= p, 0.0, NEG)
    same = ((p // 64) == (f // 64)).astype(np.float32)
    c2[:, C2_B64:C2_B64 + 128] = same
    c2[:, C2_NB64:C2_NB64 + 128] = -same
    c2[:, C2_OFF:C2_OFF + 128] = 1.0 - same
    return c2


def _colT(v, n):
    return np.ascontiguousarray(np.asarray(v, np.float32).reshape(n, 128).T)


ALL_INPUTS = (
    "x", "c", "ctx", "c_ctx",
    "l0_ada_w", "l0_ada_b", "l0_norm_mix", "l0_w_qkv", "l0_sink", "l0_w_o", "l0_norm_ffn",
    "l0_router", "l0_w_gate", "l0_w_up", "l0_w_down",
    "l1_ada_w", "l1_ada_b", "l1_norm_mix", "l1_w_in", "l1_conv", "l1_a_log", "l1_dt_bias", "l1_o_norm", "l1_w_o",
    "l1_norm_ffn", "l1_router", "l1_w_gate", "l1_w_up", "l1_w_down",
    "final_norm",
)


def make_in_maps(inputs, cores):
    missing = [n for n in ALL_INPUTS if n not in inputs]
    assert not missing, missing
    cp, rope = _host_consts()
    shared = {"cpack": cp, "rope": rope}
    for l in (0, 1):
        p = "l%d_" % l
        shared[p + "ada_w"] = np.asarray(inputs[p + "ada_w"], np.float32)
        shared[p + "ada_b"] = np.asarray(inputs[p + "ada_b"], np.float32)
        shared[p + "ada_bT"] = _colT(inputs[p + "ada_b"], 48)
        shared[p + "nmixT"] = _colT(inputs[p + "norm_mix"], 8)
        shared[p + "nffnT"] = _colT(inputs[p + "norm_ffn"], 8)
        for n in ("router", "w_gate", "w_up", "w_down"):
            shared[p + n] = np.asarray(inputs[p + n], np.float32)
    for n in ("l0_sink", "l0_w_o", "l1_w_in", "l1_o_norm", "l1_w_o", "final_norm"):
        shared[n] = np.asarray(inputs[n], np.float32)
    shared["l1_a_log"] = np.asarray(inputs["l1_a_log"], np.float32).reshape(16)
    shared["l1_dt_bias"] = np.asarray(inputs["l1_dt_bias"], np.float32).reshape(16)
    cv = np.asarray(inputs["l1_conv"], np.float32)
    shared["l1_convT"] = np.ascontiguousarray(cv.reshape(3, 24, 128).transpose(2, 1, 0))
    shared["cpack2"] = _host_consts2()
    wq = np.asarray(inputs["l0_w_qkv"], np.float32)
    perm = [pr * 8 + s_ * 4 + g for pr in range(2) for g in range(4) for s_ in range(2)]
    cols = np.concatenate([np.arange(h * 64, (h + 1) * 64) for h in perm] + [np.arange(1024, 1536)])
    shared["l0_w_qkv"] = np.ascontiguousarray(wq[:, cols])
    maps = []
    for b in cores:
        m = dict(shared)
        m["x"] = np.ascontiguousarray(inputs["x"][b], dtype=np.float32)
        m["ctx"] = np.ascontiguousarray(inputs["ctx"][b], dtype=np.float32)
        cv = np.stack([np.asarray(inputs["c"][b], np.float32), np.asarray(inputs["c_ctx"], np.float32)], axis=1)
        m["cvecT"] = np.ascontiguousarray(cv.reshape(8, 128, 2).transpose(1, 0, 2))
        maps.append(m)
    return maps


def kernel(**inputs):
    b = Builder()
    nc = b.build()
    maps = make_in_maps(inputs, list(range(8)))
    maps = [{k: v for k, v in m.items() if k in b.inp} for m in maps]
    res = run_bass_kernel_spmd(nc, maps, core_ids=list(range(8)))
    return np.stack([r["out"] for r in res.results], axis=0).astype(np.float32)
```

```python
import numpy as np
import concourse.bass as bass
import concourse.mybir as mybir
from concourse.bass_utils import run_bass_kernel_spmd

F32 = mybir.dt.float32
F32R = mybir.dt.float32r
U32 = mybir.dt.uint32
ALU = mybir.AluOpType
AF = mybir.ActivationFunctionType
AX = mybir.AxisListType

D = 1024
NLAT = 4096
NCTX = 256
NT_LAT = 32
NT_CTX = 2
NT = 34
NTOK = NLAT + NCTX
NE = 16
FF = 2048
CAP_LAT = 512
CAP_CTX = 32
NDUMMY = 640
EPS = 1e-6
NBIS = 30

C_ID, C_ONES, C_U, C_MP, C_MN, C_IOTA, C_PIDX, C_DMY, C_END = 0, 128, 256, 384, 896, 1408, 1920, 1921, 1926


C2_LF, C2_LB, C2_LT, C2_GT, C2_GE, C2_LE, C2_B64, C2_NB64, C2_OFF, C2_END = 0, 128, 256, 384, 512, 640, 768, 896, 1024, 1152
NEG = -30000.0


class Sched:
    def __init__(self, nc, n_dma_sems=24):
        self.nc = nc
        self.engs = {"pe": nc.tensor, "act": nc.scalar, "dve": nc.vector,
                     "pool": nc.gpsimd, "sp": nc.sync}
        self.csem = {e: nc.alloc_semaphore("c_" + e) for e in ("pe", "act", "dve", "pool")}
        self.ccnt = {e: 0 for e in self.csem}
        self.known = {e: {} for e in self.engs}
        self.dsems = [nc.alloc_semaphore("d%d" % i) for i in range(2 * n_dma_sems)]
        self.dcnt = [0] * (2 * n_dma_sems)
        self.dpool = {"sp": list(range(0, n_dma_sems)), "pool": list(range(n_dma_sems, 2 * n_dma_sems))}
        self.dnext = {"sp": 0, "pool": 0}
        self.state = {}
        self.out_events = []
        self.n_wait = 0
        self.n_inst = 0

    def _need(self, eng, ev):
        sem, val = ev
        k = self.known[eng]
        if k.get(sem.num, 0) >= val:
            return
        self.engs[eng].wait_ge(sem, val)
        self.n_wait += 1
        k[sem.num] = val

    def _deps(self, eng, reads, writes, skip_self=False):
        evs = {}

        def add(ev):
            if ev is None:
                return
            sem, val = ev
            if skip_self and sem.num == self.csem[eng].num:
                return
            if evs.get(sem.num, (None, 0))[1] < val:
                evs[sem.num] = ev

        own = self.csem[eng].num if eng in self.csem else -1
        for k in reads:
            st = self.state.get(k)
            if st:
                add(st["w"])
                if isinstance(k, tuple) and k[0] == "ps":
                    for r in st["r"]:
                        if r[0].num != own:
                            add(r)
        for k in writes:
            st = self.state.get(k)
            if st:
                add(st["w"])
                for r in st["r"]:
                    add(r)
        for ev in evs.values():
            self._need(eng, ev)

    def _commit(self, ev, reads, writes):
        for k in reads:
            st = self.state.setdefault(k, {"w": None, "r": []})
            st["r"] = [r for r in st["r"] if r[0].num != ev[0].num] + [ev]
        for k in writes:
            self.state[k] = {"w": ev, "r": []}

    def op(self, eng, fn, reads=(), writes=()):
        self._deps(eng, reads, writes, skip_self=(eng == "pe"))
        ins = fn(self.engs[eng])
        self.ccnt[eng] += 1
        ins.then_inc(self.csem[eng], 1)
        ev = (self.csem[eng], self.ccnt[eng])
        self._commit(ev, reads, writes)
        self.n_inst += 1
        return ev

    def dma(self, q, fn, reads=(), writes=(), is_output=False):
        self._deps(q, reads, writes)
        pool = self.dpool[q]
        i = pool[self.dnext[q]]
        self.dnext[q] = (self.dnext[q] + 1) % len(pool)
        sem = self.dsems[i]
        if self.dcnt[i] > 0:
            self._need(q, (sem, 16 * self.dcnt[i]))
        ins = fn(self.engs[q])
        self.dcnt[i] += 1
        ins.then_inc(sem, 16)
        ev = (sem, 16 * self.dcnt[i])
        self._commit(ev, reads, writes)
        if is_output:
            self.out_events.append(ev)
        self.n_inst += 1
        return ev

    def barrier(self):
        for eng in self.engs:
            for i, sem in enumerate(self.dsems):
                if self.dcnt[i] > 0:
                    self._need(eng, (sem, 16 * self.dcnt[i]))
            for e, sem in self.csem.items():
                if self.ccnt[e] > 0:
                    self._need(eng, (sem, self.ccnt[e]))

    def finish(self, eng="sp"):
        for i, sem in enumerate(self.dsems):
            if self.dcnt[i] > 0:
                self._need(eng, (sem, 16 * self.dcnt[i]))
        for e, sem in self.csem.items():
            if self.ccnt[e] > 0:
                self._need(eng, (sem, self.ccnt[e]))


class Scope:
    def __init__(self, builder):
        from contextlib import ExitStack
        self.b = builder
        self.st = ExitStack()

    def sb(self, name, shape, dtype=F32):
        return self.st.enter_context(self.b.nc.sbuf_tensor(name, list(shape), dtype))

    def close(self):
        self.b.S.barrier()
        self.st.close()


class Ring:
    def __init__(self, sc, name, shape, dtype, n):
        self.t = [sc.sb("%s%d" % (name, i), shape, dtype) for i in range(n)]
        self.k = [("%s" % name, i) for i in range(n)]
        self.i = 0

    def next(self):
        r = (self.t[self.i], self.k[self.i])
        self.i = (self.i + 1) % len(self.t)
        return r


class Builder:
    def __init__(self, stage=99, debug=False, n_exp=None, start_layer=0, n_tiles=None):
        self.n_exp = n_exp
        self.start_layer = start_layer
        self.n_tiles = n_tiles
        self.stage = stage
        self.debug = debug
        nc = bass.Bass("TRN2", target_bir_lowering=False)
        self.nc = nc
        self.S = Sched(nc)
        self.inp = {}
        self.ps = [nc.alloc_psum_tensor("psb%d" % i, [128, 512], F32) for i in range(8)]
        self.psk = [("ps", i) for i in range(8)]

    def din(self, name, shape, dtype=F32):
        t = self.nc.dram_tensor(name, list(shape), dtype, kind="ExternalInput").ap()
        self.inp[name] = t
        return t

    def dscratch(self, name, shape, dtype=F32, out=False):
        kind = "ExternalOutput" if (out or self.debug) else "Internal"
        return self.nc.dram_tensor(name, list(shape), dtype, kind=kind).ap()

    def sb(self, name, shape, dtype=F32):
        return self.nc.alloc_sbuf_tensor(name, list(shape), dtype)

    def mm(self, out, lhsT, rhs, start, stop, reads, writes):
        return self.S.op("pe", lambda e: e.matmul(out, lhsT=lhsT, rhs=rhs, start=start, stop=stop),
                         reads, writes)

    def tr(self, out, in_, reads, writes, kp=128):
        ident = self.cst[0:kp, C_ID:C_ID + kp]
        return self.S.op("pe", lambda e: e.transpose(out, in_, ident), list(reads) + ["cst"], writes)

    def act(self, out, in_, func, reads, writes, **kw):
        return self.S.op("act", lambda e: e.activation(out=out, in_=in_, func=func, **kw), reads, writes)

    def tt(self, eng, out, in0, in1, op, reads, writes):
        return self.S.op(eng, lambda e: e.tensor_tensor(out=out, in0=in0, in1=in1, op=op), reads, writes)

    def ts(self, eng, out, in0, s1, s2, op0, op1, reads, writes, **kw):
        if s2 is None:
            return self.S.op(eng, lambda e: e.tensor_scalar(out=out, in0=in0, scalar1=s1, scalar2=None,
                                                            op0=op0, **kw), reads, writes)
        return self.S.op(eng, lambda e: e.tensor_scalar(out=out, in0=in0, scalar1=s1, scalar2=s2,
                                                        op0=op0, op1=op1, **kw), reads, writes)

    def stt(self, eng, out, in0, scalar, in1, op0, op1, reads, writes):
        return self.S.op(eng, lambda e: e.scalar_tensor_tensor(out=out, in0=in0, scalar=scalar, in1=in1,
                                                               op0=op0, op1=op1), reads, writes)

    def cp(self, eng, out, in_, reads, writes):
        if eng == "act":
            return self.act(out, in_, AF.Copy, reads, writes)
        return self.S.op(eng, lambda e: e.tensor_copy(out=out, in_=in_), reads, writes)

    def ld(self, out, in_, reads, writes, q="sp"):
        return self.S.dma(q, lambda e: e.dma_start(out=out, in_=in_), reads, writes)

    def rstd_of(self, x_ap, xk, junk, junkk, ss, ssk):
        self.act(junk, x_ap, AF.Square, [xk], [junkk, ssk], accum_out=ss)
        self.ts("dve", ss, ss, 1.0 / D, EPS, ALU.mult, ALU.add, [ssk], [ssk])
        self.act(ss, ss, AF.Sqrt, [ssk], [ssk])
        self.S.op("dve", lambda e: e.reciprocal(out=ss, in_=ss), [ssk], [ssk])

    def build(self):
        nc, S = self.nc, self.S
        st, sl = self.stage, self.start_layer
        x = self.din("x", [NLAT, D]) if sl == 0 else None
        ctx = self.din("ctx", [NCTX, D]) if sl == 0 else None
        cvecT = self.din("cvecT", [128, 8, 2])
        cpack = self.din("cpack", [128, C_END])
        rope = self.din("rope", [NLAT, 64]) if sl == 0 else None
        W = {}
        for l in (0, 1):
            if l == 0 and sl > 0:
                continue
            if l == 1 and st < 30:
                continue
            W[l] = dict(
                ada_w=self.din("l%d_ada_w" % l, [D, 6 * D]),
                ada_b=self.din("l%d_ada_b" % l, [6 * D]),
                ada_bT=self.din("l%d_ada_bT" % l, [128, 48]),
                nmixT=self.din("l%d_nmixT" % l, [128, 8]),
                nffnT=self.din("l%d_nffnT" % l, [128, 8]),
                router=self.din("l%d_router" % l, [D, NE]),
            )
            if (l == 0 and st >= 2) or (l == 1 and st >= 40):
                W[l].update(w_gate=self.din("l%d_w_gate" % l, [NE, D, FF]),
                            w_up=self.din("l%d_w_up" % l, [NE, D, FF]),
                            w_down=self.din("l%d_w_down" % l, [NE, FF, D]))
        if 0 in W:
            W[0].update(w_qkv=self.din("l0_w_qkv", [D, 1536]), sink=self.din("l0_sink", [16]),
                        w_o=self.din("l0_w_o", [D, D]))
        if 1 in W:
            W[1].update(w_in=self.din("l1_w_in", [D, 4128]), convT=self.din("l1_convT", [128, 24, 3]),
                        a_log=self.din("l1_a_log", [16]), dt_bias=self.din("l1_dt_bias", [16]),
                        o_norm=self.din("l1_o_norm", [128]), w_o=self.din("l1_w_o", [D, D]))
            cpack2 = self.din("cpack2", [128, C2_END])
        if st >= 50:
            self.din("final_norm", [D])
        self.W = W
        self.x, self.ctx, self.rope = x, ctx, rope
        self.out = self.nc.dram_tensor("out", [NLAT, D], F32, kind="ExternalOutput").ap()
        self.qs = self.dscratch("qs", [NTOK, D])
        if sl == 0:
            xa = self.dscratch("xresA", [NTOK + NDUMMY, D])
        else:
            xa = self.din("xresA_in", [NTOK + NDUMMY, D])
        self.xres = [xa, self.dscratch("xresB", [NTOK + NDUMMY, D])]
        self.xn2 = self.dscratch("xn2", [NTOK + NDUMMY, D])

        self.cst = self.sb("cst", [128, C_END])
        self.ld(self.cst[:], cpack, [], ["cst"])
        self.scT = self.sb("scT", [128, 8, 2], F32R)
        self.cols = self.sb("cols", [128, 48, 2])
        self.A1 = self.sb("A1", [128, 8, 2]); self.A2 = self.sb("A2", [128, 8, 2])
        self.Gbc = self.sb("Gbc", [128, 2, 2, D])
        self.aff = self.sb("aff", [128, NT, NE])
        if self.debug:
            S.op("dve", lambda e: e.memset(self.aff[:], 0.0), [], [("aff", i) for i in range(NT)])
        sc0 = Scope(self)
        zero_t = sc0.sb("zero_t", [128, D])
        S.op("dve", lambda e: e.memset(zero_t[:], 0.0), [], ["zero_t"])
        bufs = [(self.xres[1], "xres1"), (self.xn2, "xn2")] + ([(self.xres[0], "xres0")] if sl == 0 else [])
        for buf, k in bufs:
            for r in range(NDUMMY // 128):
                self.ld(buf[NTOK + r * 128: NTOK + (r + 1) * 128, :], zero_t[:], ["zero_t"], [(k, "dummy", r)])
        sc0.close()
        if sl == 0:
            self.modulation(0)
            if st >= 1:
                self.attention_layer()
            if st >= 2:
                self.moe(0, self.xres[0], "xres0", with_ctx=True)
        if st >= 30:
            self.cst2 = self.sb("cst2", [128, C2_END])
            self.ld(self.cst2[:], cpack2, [], ["cst2"])
            self.modulation(1)
            self.deltanet_layer()
        if st >= 40:
            self.moe(1, self.xres[1], "xres1", with_ctx=False)
        if st >= 50:
            self.final_norm()
        if self.debug:
            d_aff = self.nc.dram_tensor("d_aff", [128, NT * NE], F32, kind="ExternalOutput").ap()
            self.ld(d_aff, self.aff[:, :, :].rearrange("p j e -> p (j e)"), [("aff", i) for i in range(NT)], ["d_aff"])
            d_cols = self.nc.dram_tensor("d_cols", [128, 96], F32, kind="ExternalOutput").ap()
            self.ld(d_cols, self.cols[:, :, :].rearrange("p c t -> p (c t)"), ["cols"], ["d_cols"])
            d_g = self.nc.dram_tensor("d_g", [128, 4 * D], F32, kind="ExternalOutput").ap()
            self.ld(d_g, self.Gbc[:, :, :, :].rearrange("p a b d -> p (a b d)"),
                    [("Gbc", a, b) for a in range(2) for b in range(2)], ["d_g"])
        S.finish()
        return nc

    def modulation(self, l):
        nc, S, W = self.nc, self.S, self.W[l]
        sfx = "m%d" % l
        sc = Scope(self)
        self.wring = Ring(sc, "wst" + sfx, [128, 8, 512], F32R, 4)
        cv = sc.sb("cv" + sfx, [128, 8, 2])
        self.ld(cv[:], self.inp["cvecT"], [], ["cv"])
        self.act(self.scT[:], cv[:], AF.Silu, ["cv"], ["scT"])
        screp = [sc.sb("screp%d%s" % (c, sfx), [128, 8, 128], F32R) for c in range(2)]
        for c in range(2):
            self.cp("dve", screp[c][:], self.scT[:, :, c:c + 1].to_broadcast([128, 8, 128]), ["scT"], [("screp", c)])
        abT = sc.sb("abT" + sfx, [128, 48])
        self.ld(abT[:], W["ada_bT"], [], ["abT"])
        nmix = sc.sb("nmix" + sfx, [128, 8]); nffn = sc.sb("nffn" + sfx, [128, 8])
        self.ld(nmix[:], W["nmixT"], [], ["nmix"])
        self.ld(nffn[:], W["nffnT"], [], ["nffn"])
        abbc = sc.sb("abbc" + sfx, [128, 2, D])
        self.ld(abbc[:, 0, :], W["ada_b"][2 * D:3 * D].partition_broadcast(128), [], [("abbc", 0)])
        self.ld(abbc[:, 1, :], W["ada_b"][5 * D:6 * D].partition_broadcast(128), [], [("abbc", 1)])
        pcol = self.ps[0]
        pcv = pcol[:, 0:96].rearrange("p (c t) -> p c t", t=2)
        for cg in range(12):
            wt, wk = self.wring.next()
            self.ld(wt[:], W["ada_w"][:, cg * 512:(cg + 1) * 512].rearrange("(k p) n -> p k n", p=128),
                    [], [wk], q="pool")
            for c4 in range(4):
                cc = cg * 4 + c4
                for kc in range(8):
                    self.mm(pcv[:, cc, :], wt[:, kc, c4 * 128:(c4 + 1) * 128], self.scT[:, kc, :],
                            kc == 0, kc == 7, [wk, "scT"], [self.psk[0]])
            if cg in (4, 5, 10, 11):
                gi = 0 if cg < 6 else 1
                half = cg % 2
                for c in range(2):
                    pb, pbk = self.ps[1 + c], self.psk[1 + c]
                    for kc in range(8):
                        self.mm(pb[:, :], screp[c][:, kc, :], wt[:, kc, :], kc == 0, kc == 7,
                                [wk, ("screp", c)], [pbk])
                    self.tt("dve", self.Gbc[:, gi, c, half * 512:(half + 1) * 512], pb[:, :],
                            abbc[:, gi, half * 512:(half + 1) * 512], ALU.add,
                            [pbk, ("abbc", gi)], [("Gbc", gi, c)])
        self.tt("dve", self.cols[:], pcv, abT[:].unsqueeze(2).to_broadcast([128, 48, 2]), ALU.add,
                [self.psk[0], "abT"], ["cols"])
        for (A, nrm, nk, v) in ((self.A1, nmix, "nmix", 1), (self.A2, nffn, "nffn", 4)):
            self.stt("dve", A[:], self.cols[:, v * 8:(v + 1) * 8, :], 1.0,
                     nrm[:].unsqueeze(2).to_broadcast([128, 8, 2]), ALU.add, ALU.mult,
                     ["cols", nk], [("A", v)])
        sc.close()

    def B1(self, kc, c):
        return self.cols[:, 0 + kc, c:c + 1]

    def B2(self, kc, c):
        return self.cols[:, 24 + kc, c:c + 1]

    def attention_layer(self):
        nc, S, W = self.nc, self.S, self.W[0]
        cst = self.cst
        sca = Scope(self)
        KT = sca.sb("KT", [128, 2, NTOK], F32R)
        V = sca.sb("Vaug", [128, NT, 4, 66], F32R)
        esink = sca.sb("esink", [128, 16])
        wr = sca.sb("wr", [128, 8, NE])
        xr = Ring(sca, "xt", [128, D], F32, 2)
        xnr = Ring(sca, "xnb", [128, D], F32, 3)
        ssr = Ring(sca, "ss", [128, 1], F32, 6)
        junk = sca.sb("junk", [128, D])
        sc1 = Scope(self)
        wbig = sc1.sb("wbig", [128, 8, 1536], F32R)
        hTr = Ring(sc1, "hT", [128, 8, 128], F32R, 3)
        qkr = Ring(sc1, "qk", [128, 1280], F32, 2)
        csr = Ring(sc1, "cs", [128, 64], F32, 6)
        tmpr = Ring(sc1, "rt", [128, 4, 256], F32, 1)
        for cg in range(3):
            self.ld(wbig[:, :, cg * 512:(cg + 1) * 512],
                    W["w_qkv"][:, cg * 512:(cg + 1) * 512].rearrange("(k p) n -> p k n", p=128),
                    [], [("wbig", cg)], q="pool")
        Vf = V[:, :, :, :].rearrange("p j h c -> p (j h) c")
        self.cp("dve", Vf[:, :, 64:65], self.cst[:, C_ONES:C_ONES + 1].unsqueeze(1).to_broadcast([128, NT * 4, 1]), ["cst"], [("V1",)])
        self.cp("dve", Vf[:, :, 65:66], self.cst[:, C_U:C_U + 1].unsqueeze(1).to_broadcast([128, NT * 4, 1]), ["cst"], [("V0",)])
        self.ld(esink[:], W["sink"].partition_broadcast(128), [], ["esink"])
        self.act(esink[:], esink[:], AF.Exp, ["esink"], ["esink"])
        self.ld(wr[:], W["router"].rearrange("(k p) n -> p k n", p=128), [], ["wr"])

        def src_rows(j):
            if j < NT_LAT:
                return self.x[j * 128:(j + 1) * 128, :]
            return self.ctx[(j - NT_LAT) * 128:(j - NT_LAT + 1) * 128, :]

        cx = [dict() for _ in range(NT)]

        def p1_s0(j):
            c = cx[j]
            c["c"] = 0 if j < NT_LAT else 1
            xt, xk = xr.next()
            self.ld(xt[:], src_rows(j), [], [xk])
            if c["c"] == 0:
                c["cs"], c["ck"] = csr.next()
                self.ld(c["cs"][:], self.rope[j * 128:(j + 1) * 128, :], [], [c["ck"]])
            ss, ssk = ssr.next()
            self.rstd_of(xt[:], xk, junk[:], "junk", ss[:], ssk)
            c["xn"], c["xnk"] = xnr.next()
            self.ts("dve", c["xn"][:], xt[:], ss[:, 0:1], None, ALU.mult, None, [xk, ssk], [c["xnk"]])

        def p1_s1(j):
            c = cx[j]
            xn, xnk, cc = c["xn"], c["xnk"], c["c"]
            c["hT"], c["hk"] = hTr.next()
            hT, hk = c["hT"], c["hk"]
            for kc in range(8):
                pt, ptk = self.ps[kc // 4], self.psk[kc // 4]
                self.tr(pt[:, (kc % 4) * 128:(kc % 4 + 1) * 128], xn[:, kc * 128:(kc + 1) * 128], [xnk], [ptk])
            for kc in range(8):
                pt, ptk = self.ps[kc // 4], self.psk[kc // 4]
                self.act(hT[:, kc, :], pt[:, (kc % 4) * 128:(kc % 4 + 1) * 128], AF.Identity,
                         [ptk, ("A", 1), "cols"], [hk], scale=self.A1[:, kc, cc:cc + 1], bias=self.B1(kc, cc))

        def p1_s2(j):
            c = cx[j]
            hT, hk = c["hT"], c["hk"]
            c["pb"] = 2 + 3 * (j % 2)
            for cg in range(3):
                pq, pqk = self.ps[c["pb"] + cg], self.psk[c["pb"] + cg]
                for kc in range(8):
                    self.mm(pq[:, :], hT[:, kc, :], wbig[:, kc, cg * 512:(cg + 1) * 512], kc == 0, kc == 7,
                            [hk, ("wbig", cg)], [pqk])

        def p1_s3(j):
            c = cx[j]
            pb = c["pb"]
            c["qk"], c["qkk"] = qkr.next()
            qk, qkk = c["qk"], c["qkk"]
            if c["c"] == 0:
                cs, ck = c["cs"], c["ck"]
                cosb = lambda nh: cs[:, 0:32].rearrange("p (a f) -> p a f", a=2).unsqueeze(1).to_broadcast([128, nh, 2, 16])
                sinb = lambda nh: cs[:, 32:64].rearrange("p (a f) -> p a f", a=2).unsqueeze(1).to_broadcast([128, nh, 2, 16])
                for cg in range(3):
                    nh = 8 if cg < 2 else 4
                    pq, pqk = self.ps[pb + cg], self.psk[pb + cg]
                    pv = pq[:, 0:nh * 64].rearrange("p (h a b f) -> p h a b f", h=nh, a=2, b=2, f=16)
                    ov = qk[:, cg * 512:cg * 512 + nh * 64].rearrange("p (h a b f) -> p h a b f", h=nh, a=2, b=2, f=16)
                    x1, x2 = pv[:, :, :, 0, :], pv[:, :, :, 1, :]
                    tm, tmk = tmpr.next()
                    t = [tm[:, i, 0:nh * 32].rearrange("p (h a f) -> p h a f", h=nh, a=2, f=16) for i in range(4)]
                    self.tt("dve", t[0], x1, cosb(nh), ALU.mult, [pqk, ck], [tmk])
                    self.tt("dve", t[1], x2, sinb(nh), ALU.mult, [pqk, ck], [tmk])
                    self.tt("dve", t[2], x2, cosb(nh), ALU.mult, [pqk, ck], [tmk])
                    self.tt("dve", t[3], x1, sinb(nh), ALU.mult, [pqk, ck], [tmk])
                    self.tt("pool", ov[:, :, :, 0, :], t[0], t[1], ALU.subtract, [tmk], [qkk])
                    self.tt("pool", ov[:, :, :, 1, :], t[2], t[3], ALU.add, [tmk], [qkk])
            else:
                for cg in range(3):
                    ncol = 512 if cg < 2 else 256
                    self.cp("act", qk[:, cg * 512:cg * 512 + ncol], self.ps[pb + cg][:, 0:ncol], [self.psk[pb + cg]], [qkk])
            self.cp("act", V[:, j, :, 0:64], self.ps[pb + 2][:, 256:512].rearrange("p (h d) -> p h d", h=4),
                    [self.psk[pb + 2]], [("V", j)])
            self.ld(self.qs[j * 128:(j + 1) * 128, :], qk[:, 0:1024], [qkk], [("qs", j)])

        def p1_s4(j):
            c = cx[j]
            qk, qkk = c["qk"], c["qkk"]
            for pr in range(2):
                self.tr(self.ps[1][:, pr * 128:(pr + 1) * 128], qk[:, 1024 + pr * 128:1024 + (pr + 1) * 128], [qkk], [self.psk[1]])
            self.cp("act", KT[:, :, j * 128:(j + 1) * 128], self.ps[1][:, 0:256].rearrange("p (a t) -> p a t", a=2),
                    [self.psk[1]], [("KT", j)])

        stages = [p1_s0, p1_s1, p1_s2, p1_s3, p1_s4]
        for t_ in range(NT + len(stages) - 1):
            for st_ in range(len(stages) - 1, -1, -1):
                i_ = t_ - st_
                if 0 <= i_ < NT:
                    stages[st_](i_)

        sc1.close()
        sca_outer, sca = sca, Scope(self)
        wo = sca.sb("wo", [128, 8, 1024], F32R)
        self.ld(wo[:], W["w_o"].rearrange("(k p) n -> p k n", p=128), [], ["wo"], q="pool")
        wok = ["wo"]
        QTr = Ring(sca, "QT", [128, 2, 4, 128], F32R, 2)
        PTr = Ring(sca, "PT", [128, 5, 512], F32R, 2)
        osb = sca.sb("osb", [128, 16, 64])
        otsr = Ring(sca, "ots", [66, 512], F32, 1)
        oT = sca.sb("oT", [128, 8, 128], F32R)
        den = sca.sb("den", [128, 16])
        xmr = Ring(sca, "xm", [128, D], F32, 3)
        h2T = sca.sb("h2T", [128, 8, 128])
        lg = sca.sb("lg", [128, NE]); mx = sca.sb("mx", [128, 1]); sm = sca.sb("sm", [128, 1])
        pend = [None]
        ps_i = [0]

        def p2_loads(i_):
            qt_, qtk_ = xr.next()
            self.ld(qt_[:], self.qs[i_ * 128:(i_ + 1) * 128, :], [("qs", i_)], [qtk_])
            xt_, xk_ = xnr.next()
            self.ld(xt_[:], src_rows(i_), [], [xk_])
            return qt_, qtk_, xt_, xk_

        nxt = p2_loads(0)
        for i in range(NT):
            c = 0 if i < NT_LAT else 1
            qt, qtk, xt, xk = nxt
            if i + 1 < NT:
                nxt = p2_loads(i + 1)
            QT, QTk = QTr.next()
            for pr in range(2):
                for g in range(4):
                    self.tr(self.ps[pr][:, g * 128:(g + 1) * 128], qt[:, (pr * 4 + g) * 128:(pr * 4 + g + 1) * 128], [qtk], [self.psk[pr]])
                self.cp("act", QT[:, pr, :, :], self.ps[pr][:, :].rearrange("p (g t) -> p g t", g=4), [self.psk[pr]], [QTk])
            if c == 0:
                kbs = ([i - 1] if i > 0 else []) + [i] + ([i + 1] if i < NT_LAT - 1 else []) + [32, 33]
                kmask = ([C_MP] if i > 0 else []) + [None] + ([C_MN] if i < NT_LAT - 1 else []) + [None, None]
            else:
                kbs = [32, 33]
                kmask = [None, None]
            if pend[0] is not None:
                self.moe_prep_a(*pend[0], skip0=True)

            def st_phase(kvh):
                pr, base = kvh // 2, (kvh % 2) * 64
                PT, PTk = PTr.next()
                for kbi, kb in enumerate(kbs):
                    sbk = (2, 3, 6, 7)[ps_i[0] % 4]
                    ps_i[0] += 1
                    pS, pSk = self.ps[sbk], self.psk[sbk]
                    self.mm(pS[:, :], KT[base:base + 64, pr, kb * 128:(kb + 1) * 128],
                            QT[base:base + 64, pr, :, :], True, True, [("KT", kb), QTk], [pSk])
                    self.act(PT[:, kbi, :], pS[:, :], AF.Exp, [pSk], [PTk], scale=0.125)
                    if kmask[kbi] is not None:
                        mo = kmask[kbi]
                        self.tt("pool", PT[:, kbi, :], PT[:, kbi, :], cst[:, mo:mo + 512], ALU.mult, [PTk, "cst"], [PTk])
                return PT, PTk

            def pv_phase(kvh, PT, PTk):
                pO, pOk = self.ps[4 + kvh % 2], self.psk[4 + kvh % 2]
                for kbi, kb in enumerate(kbs):
                    self.mm(pO[0:66, :], V[:, kb, kvh, :], PT[:, kbi, :], kbi == 0, kbi == len(kbs) - 1,
                            [PTk, ("V", kb), ("V1",), ("V0",)], [pOk])
                ots, otsk = otsr.next()
                self.cp("act", ots[0:66, :], pO[0:66, :], [pOk], [otsk])
                for g in range(4):
                    self.tr(pO[:, g * 128:g * 128 + 66], ots[0:66, g * 128:(g + 1) * 128], [otsk], [pOk], kp=66)
                pOv = pO[:, :].rearrange("p (g t) -> p g t", g=4)
                dk_ = ("den", kvh)
                self.tt("dve", den[:, kvh * 4:(kvh + 1) * 4], pOv[:, :, 64], esink[:, kvh * 4:(kvh + 1) * 4], ALU.add,
                        [pOk, "esink"], [dk_])
                self.S.op("dve", lambda e: e.reciprocal(out=den[:, kvh * 4:(kvh + 1) * 4], in_=den[:, kvh * 4:(kvh + 1) * 4]), [dk_], [dk_])
                self.tt("dve", osb[:, kvh * 4:(kvh + 1) * 4, :], pOv[:, :, 0:64],
                        den[:, kvh * 4:(kvh + 1) * 4].unsqueeze(2).to_broadcast([128, 4, 64]), ALU.mult,
                        [pOk, dk_], [("osb", kvh)])

            pts = {0: st_phase(0)}
            for kvh in range(4):
                if kvh + 1 < 4:
                    pts[kvh + 1] = st_phase(kvh + 1)
                if kvh == 1 and pend[0] is not None:
                    self.moe_prep_b(*pend[0])
                    pend[0] = None
                pv_phase(kvh, *pts[kvh])
            osf = osb[:, :, :].rearrange("p h d -> p (h d)")
            for kc in range(8):
                self.tr(self.ps[kc // 4][:, (kc % 4) * 128:(kc % 4 + 1) * 128], osf[:, kc * 128:(kc + 1) * 128], [("osb", k_) for k_ in range(4)], [self.psk[kc // 4]])
            for b in range(2):
                self.cp("act", oT[:, b * 4:(b + 1) * 4, :], self.ps[b][:, :].rearrange("p (k t) -> p k t", k=4), [self.psk[b]], ["oT"])
            xm, xmk = xmr.next()
            for dh in range(2):
                pM, pMk = self.ps[2 + dh], self.psk[2 + dh]
                for kc in range(8):
                    self.mm(pM[:, :], oT[:, kc, :], wo[:, kc, dh * 512:(dh + 1) * 512], kc == 0, kc == 7, ["oT"] + wok, [pMk])
                self.tt("dve", xm[:, dh * 512:(dh + 1) * 512], pM[:, :], self.Gbc[:, 0, c, dh * 512:(dh + 1) * 512], ALU.mult,
                        [pMk, ("Gbc", 0, c)], [xmk])
            self.tt("dve", xm[:], xm[:], xt[:], ALU.add, [xmk, xk], [xmk])
            self.ld(self.xres[0][i * 128:(i + 1) * 128, :], xm[:], [xmk], [("xres0", i)])
            pend[0] = (i, c, xm, xmk, junk, ssr, wr, h2T, lg, mx, sm)
            self.moe_prep_a0(*pend[0])
        self.moe_prep_a(*pend[0], skip0=True)
        self.moe_prep_b(*pend[0])
        sca.close()
        sca_outer.close()

    def moe_prep_a0(self, i, c, xm, xmk, junk, ssr, wr, h2T, lg, mx, sm):
        ss, ssk = ssr.next()
        self.rstd_of(xm[:], xmk, junk[:], "junk", ss[:], ssk)
        self.ts("dve", junk[:], xm[:], ss[:, 0:1], None, ALU.mult, None, [xmk, ssk], ["junk"])
        self.ld(self.xn2[i * 128:(i + 1) * 128, :], junk[:], ["junk"], [("xn2", i)])

    def moe_prep_a(self, i, c, xm, xmk, junk, ssr, wr, h2T, lg, mx, sm, skip0=False):
        if not skip0:
            self.moe_prep_a0(i, c, xm, xmk, junk, ssr, wr, h2T, lg, mx, sm)
        for kc in range(8):
            self.tr(self.ps[kc // 4][:, (kc % 4) * 128:(kc % 4 + 1) * 128], junk[:, kc * 128:(kc + 1) * 128], ["junk"], [self.psk[kc // 4]])
        for kc in range(8):
            self.act(h2T[:, kc, :], self.ps[kc // 4][:, (kc % 4) * 128:(kc % 4 + 1) * 128], AF.Identity,
                     [self.psk[kc // 4], ("A", 4), "cols"], ["h2T"], scale=self.A2[:, kc, c:c + 1], bias=self.B2(kc, c))

    def moe_prep_b(self, i, c, xm, xmk, junk, ssr, wr, h2T, lg, mx, sm):
        pL, pLk = self.ps[1], self.psk[1]
        for kc in range(8):
            self.mm(pL[:, 0:NE], h2T[:, kc, :], wr[:, kc, :], kc == 0, kc == 7, ["h2T", "wr"], [pLk])
        self.S.op("dve", lambda e: e.reduce_max(out=mx[:], in_=pL[:, 0:NE], axis=AX.X), [pLk], ["mx"])
        self.ts("dve", mx[:], mx[:], -1.0, None, ALU.mult, None, ["mx"], ["mx"])
        self.act(lg[:], pL[:, 0:NE], AF.Exp, [pLk, "mx"], ["lg", "sm"], bias=mx[:, 0:1], accum_out=sm[:])
        self.S.op("dve", lambda e: e.reciprocal(out=sm[:], in_=sm[:]), ["sm"], ["sm"])
        self.ts("dve", self.aff[:, i, :], lg[:], sm[:, 0:1], None, ALU.mult, None, ["lg", "sm"], [("aff", i)])

    def moe_prep(self, *args):
        self.moe_prep_a(*args)
        self.moe_prep_b(*args)

    def pk(self, b, h):
        return ("ps", b)

    def deltanet_layer(self):
        nc, S, W = self.nc, self.S, self.W[1]
        cst, c2 = self.cst, self.cst2
        xin = self.xres[0]
        qT_d = self.dscratch("qT_d", [D, NTOK]); kT_d = self.dscratch("kT_d", [D, NTOK])
        ktok_d = self.dscratch("ktok_d", [NTOK, D]); vtok_d = self.dscratch("vtok_d", [NTOK, D])
        sz_d = self.dscratch("sz_d", [NTOK, D]); of_d = self.dscratch("of_d", [NLAT, D])
        self.dn_dbg = dict(qT_d=qT_d, kT_d=kT_d, ktok_d=ktok_d, vtok_d=vtok_d, sz_d=sz_d, of_d=of_d)
        ident = cst[:, C_ID:C_ID + 128]
        ones = cst[:, C_ONES:C_ONES + 128]
        zcol = cst[:, C_U:C_U + 1]
        scL = Scope(self)
        g_all = scL.sb("g_all", [128, NT, 16]); beta_all = scL.sb("beta_all", [128, NT, 16])
        lnb_all = scL.sb("lnb_all", [128, NT, 16]); ab_all = scL.sb("ab_all", [128, NT, 32])
        onesR = scL.sb("onesR", [128, 128], F32R)
        self.cp("dve", onesR[:], ones, ["cst"], ["onesR"])
        convT = scL.sb("convT", [128, 24, 3])
        self.ld(convT[:], W["convT"], [], ["convT"])
        allps = [self.pk(b, h) for b in range(8) for h in range(2)]

        def bankk(b):
            return [self.pk(b, 0), self.pk(b, 1)]

        scp = Scope(self)
        win = scp.sb("winqkv", [128, 8, 3072], F32R)
        wink = [("win", i) for i in range(6)]
        for i in range(6):
            self.ld(win[:, :, i * 512:(i + 1) * 512], W["w_in"][:, i * 512:(i + 1) * 512].rearrange("(k p) n -> p k n", p=128),
                    [], [wink[i]], q="pool")
        wnd = [scp.sb("wnd%d" % i, [128, 8, 258], F32R) for i in range(2)]
        xr = Ring(scp, "pxt", [128, D], F32, 4)
        ssr = Ring(scp, "pss", [128, 1], F32, 4)
        junk = scp.sb("pjunk", [128, D])
        c1r = Ring(scp, "pc1", [128, 256], F32, 3)
        sr = Ring(scp, "psl", [128, 256], F32, 8)
        sqr = Ring(scp, "psq", [128, 256], F32R, 3)
        rnr = Ring(scp, "prn", [128, 256], F32, 4)
        qnr = Ring(scp, "pqn", [128, 256], F32, 4)
        tkr = Ring(scp, "ptk", [128, 128], F32, 6)
        groups = [[32, 33]] + [[2 * g, 2 * g + 1] for g in range(16)]

        def xload(jj, xr_):
            xt, xk = xr_.next()
            self.ld(xt[:], xin[jj * 128:(jj + 1) * 128, :], [], [xk])
            return xt, xk

        def tile_hT(jj, dst_fn, xr_, ssr_, junk_, pre=None):
            c = 0 if jj < NT_LAT else 1
            xt, xk = pre if pre is not None else xload(jj, xr_)
            ss, ssk = ssr_.next()
            self.rstd_of(xt[:], xk, junk_[:], "pjunk", ss[:], ssk)
            self.ts("dve", xt[:], xt[:], ss[:, 0:1], None, ALU.mult, None, [xk, ssk], [xk])
            for kc in range(8):
                b = kc // 4
                self.tr(self.ps[b][:, (kc % 4) * 128:(kc % 4 + 1) * 128], xt[:, kc * 128:(kc + 1) * 128], [xk], bankk(b))
            for kc in range(8):
                b = kc // 4
                dst, dk = dst_fn(kc)
                self.act(dst, self.ps[b][:, (kc % 4) * 128:(kc % 4 + 1) * 128], AF.Identity,
                         bankk(b) + [("A", 1), "cols"], [dk], scale=self.A1[:, kc, c:c + 1], bias=self.B1(kc, c))

        pw_pre = {}

        def pw_loads(gi):
            if gi < len(groups):
                pw_pre[gi] = [xload(jj, xr) for jj in groups[gi]]

        pw_loads(0)

        def prep_window(gi):
            buf = gi % 2
            pw_loads(gi + 1)
            for ti, jj in enumerate(groups[gi]):
                tile_hT(jj, lambda kc: (wnd[buf][:, kc, 1 + 128 * ti:1 + 128 * (ti + 1)], ("wnd", buf)), xr, ssr, junk,
                        pre=pw_pre[gi][ti])
            same_prev = gi >= 2
            if same_prev:
                self.cp("dve", wnd[buf][:, :, 0:1], wnd[1 - buf][:, :, 256:257], [("wnd", 1 - buf)], [("wnd", buf)])
                self.cp("dve", wnd[1 - buf][:, :, 257:258], wnd[buf][:, :, 1:2], [("wnd", buf)], [("wnd", 1 - buf)])
            else:
                self.cp("dve", wnd[buf][:, :, 0:1], zcol.unsqueeze(1).to_broadcast([128, 8, 1]), ["cst"], [("wnd", buf)])
                if gi >= 1:
                    self.cp("dve", wnd[1 - buf][:, :, 257:258], zcol.unsqueeze(1).to_broadcast([128, 8, 1]), ["cst"], [("wnd", 1 - buf)])

        pb_i = [0]
        pn_i = [0]

        def skew(n_items, stages):
            k = len(stages)
            for t in range(n_items + k - 1):
                for st_ in range(k - 1, -1, -1):
                    i_ = t - st_
                    if 0 <= i_ < n_items:
                        stages[st_](i_)

        def project(gi):
            buf = gi % 2
            wv, wvk = wnd[buf], ("wnd", buf)
            tok0 = groups[gi][0] * 128
            ctxs = [dict() for _ in range(24)]

            def s0(ch):
                c = ctxs[ch]
                b = pb_i[0] % 3
                pb_i[0] += 1
                c["pb"] = 2 + b
                pP = self.ps[2 + b]
                for kc in range(8):
                    self.mm(pP[:, 0:258], win[:, kc, ch * 128:(ch + 1) * 128], wv[:, kc, 0:258], kc == 0, kc == 7,
                            [wink[ch // 4], wvk], bankk(2 + b))

            def s1(ch):
                c = ctxs[ch]
                pb = c["pb"]
                pP = self.ps[pb]
                c1, c1k = c1r.next()
                self.ts("dve", c1[:], pP[:, 0:256], convT[:, ch, 0:1], None, ALU.mult, None, bankk(pb) + ["convT"], [c1k])
                self.stt("dve", c1[:], pP[:, 1:257], convT[:, ch, 1:2], c1[:], ALU.mult, ALU.add, bankk(pb) + ["convT", c1k], [c1k])
                self.stt("dve", c1[:], pP[:, 2:258], convT[:, ch, 2:3], c1[:], ALU.mult, ALU.add, bankk(pb) + ["convT", c1k], [c1k])
                c["sl"], c["slk"] = sr.next()
                self.act(c["sl"][:], c1[:], AF.Silu, [c1k], [c["slk"]])

            def s2(ch):
                c = ctxs[ch]
                if ch // 8 < 2:
                    sq, sqk = sqr.next()
                    self.act(sq[:], c["sl"][:], AF.Square, [c["slk"]], [sqk])
                    nb = 5 if (pn_i[0] % 2 == 0) else 7
                    pn_i[0] += 1
                    c["nb"] = nb
                    self.mm(self.ps[nb][:, 0:256], onesR[:], sq[:], True, True, ["onesR", sqk], bankk(nb))

            def s3(ch):
                c = ctxs[ch]
                if ch // 8 < 2:
                    c["rn"], c["rnk"] = rnr.next()
                    rn, rnk = c["rn"], c["rnk"]
                    self.ts("dve", rn[:], self.ps[c["nb"]][:, 0:256], EPS, None, ALU.add, None, bankk(c["nb"]), [rnk])
                    self.act(rn[:], rn[:], AF.Sqrt, [rnk], [rnk])

            def s4(ch):
                c = ctxs[ch]
                kind, h = ch // 8, ch % 8
                if kind < 2:
                    rn, rnk = c["rn"], c["rnk"]
                    S.op("dve", lambda e: e.reciprocal(out=rn[:], in_=rn[:]), [rnk], [rnk])
                    qn, qnk = qnr.next()
                    self.stt("dve", qn[:], c["sl"][:], (128.0 ** -0.5) if kind == 0 else 1.0, rn[:], ALU.mult, ALU.mult, [c["slk"], rnk], [qnk])
                    dst = qT_d if kind == 0 else kT_d
                    self.ld(dst[h * 128:(h + 1) * 128, tok0:tok0 + 256], qn[:], [qnk], [("qkT_d", kind, gi, h)])
                    c["src"], c["srck"] = qn, qnk
                else:
                    c["src"], c["srck"] = c["sl"], c["slk"]

            def s5(ch):
                c = ctxs[ch]
                if ch // 8 >= 1:
                    for ti in range(2):
                        self.tr(self.ps[6][:, ti * 128:(ti + 1) * 128], c["src"][:, ti * 128:(ti + 1) * 128], [c["srck"]], bankk(6))

            def s6(ch):
                c = ctxs[ch]
                kind, h = ch // 8, ch % 8
                if kind >= 1:
                    dstd = ktok_d if kind == 1 else vtok_d
                    for ti in range(2):
                        tk, tkk = tkr.next()
                        self.cp("act", tk[:], self.ps[6][:, ti * 128:(ti + 1) * 128], bankk(6), [tkk])
                        self.ld(dstd[tok0 + ti * 128:tok0 + (ti + 1) * 128, h * 128:(h + 1) * 128], tk[:], [tkk], [("tok_d", kind, gi, h, ti)])

            skew(24, [s0, s1, s2, s3, s4, s5, s6])

        for gi in range(len(groups)):
            prep_window(gi)
            if gi >= 1:
                project(gi - 1)
        lastb = (len(groups) - 1) % 2
        self.cp("dve", wnd[lastb][:, :, 257:258], zcol.unsqueeze(1).to_broadcast([128, 8, 1]), ["cst"], [("wnd", lastb)])
        project(len(groups) - 1)
        scp.close()

        scz = Scope(self)
        wz = scz.sb("winz", [128, 8, 1056], F32R)
        self.ld(wz[:, :, 0:528], W["w_in"][:, 3072:3600].rearrange("(k p) n -> p k n", p=128), [], [("wz", 0)], q="pool")
        self.ld(wz[:, :, 528:1056], W["w_in"][:, 3600:4128].rearrange("(k p) n -> p k n", p=128), [], [("wz", 1)], q="pool")
        wzk = [("wz", 0), ("wz", 1)]
        xr = Ring(scz, "zxt", [128, D], F32, 2)
        ssr = Ring(scz, "zss", [128, 1], F32, 4)
        junk = scz.sb("zjunk", [128, D])
        hTr = Ring(scz, "zhT", [128, 8, 128], F32R, 2)
        zr = Ring(scz, "zst", [128, D], F32, 2)
        zpre = xload(0, xr)
        for jj in range(NT):
            hT, hk = hTr.next()
            zcur = zpre
            if jj + 1 < NT:
                zpre = xload(jj + 1, xr)
            tile_hT(jj, lambda kc: (hT[:, kc, :], hk), xr, ssr, junk, pre=zcur)
            for zh in range(2):
                for kc in range(8):
                    self.mm(self.ps[2 + zh][:, :], hT[:, kc, :], wz[:, kc, zh * 512:(zh + 1) * 512], kc == 0, kc == 7, [hk] + wzk, bankk(2 + zh))
            for kc in range(8):
                self.mm(self.ps[4][:, 0:32], hT[:, kc, :], wz[:, kc, 1024:1056], kc == 0, kc == 7, [hk] + wzk, bankk(4))
            zt, ztk = zr.next()
            for zh in range(2):
                self.act(zt[:, zh * 512:(zh + 1) * 512], self.ps[2 + zh][:, :], AF.Silu, bankk(2 + zh), [ztk])
            self.ld(sz_d[jj * 128:(jj + 1) * 128, :], zt[:], [ztk], [("sz_d", jj)])
            self.cp("dve", ab_all[:, jj, :], self.ps[4][:, 0:32], bankk(4), [("ab", jj)])
        abk = [("ab", jj) for jj in range(NT)]
        dtb = scz.sb("dtb", [128, 16]); nea = scz.sb("nea", [128, 16])
        self.ld(dtb[:], W["dt_bias"].partition_broadcast(128), [], ["dtb"])
        self.ld(nea[:], W["a_log"].partition_broadcast(128), [], ["nea"])
        self.act(nea[:], nea[:], AF.Exp, ["nea"], ["nea"])
        self.ts("dve", nea[:], nea[:], -1.0, None, ALU.mult, None, ["nea"], ["nea"])
        self.tt("dve", g_all[:], ab_all[:, :, 0:16], dtb[:].unsqueeze(1).to_broadcast([128, NT, 16]), ALU.add, abk + ["dtb"], ["g_all"])
        uu = scz.sb("sp_u", [128, NT, 16]); la = scz.sb("sp_la", [128, NT, 16])
        qq = scz.sb("sp_q", [128, NT, 16]); mk = scz.sb("sp_mk", [128, NT, 16])
        self.act(uu[:], g_all[:], AF.Exp, ["g_all"], ["sp_u"])
        self.act(la[:], uu[:], AF.Ln, ["sp_u"], ["sp_la"], bias=1.0)
        self.ts("dve", qq[:], uu[:], 1.0 / 7, None, ALU.mult, None, ["sp_u"], ["sp_q"])
        for cc_ in (-1.0 / 6, 1.0 / 5, -1.0 / 4, 1.0 / 3, -1.0 / 2, 1.0):
            self.stt("dve", qq[:], qq[:], cc_, uu[:], ALU.add, ALU.mult, ["sp_q", "sp_u"], ["sp_q"])
        self.ts("dve", mk[:], uu[:], 0.25, None, ALU.is_lt, None, ["sp_u"], ["sp_mk"])
        self.tt("dve", qq[:], qq[:], la[:], ALU.subtract, ["sp_q", "sp_la"], ["sp_q"])
        self.tt("dve", qq[:], qq[:], mk[:], ALU.mult, ["sp_q", "sp_mk"], ["sp_q"])
        self.tt("dve", g_all[:], la[:], qq[:], ALU.add, ["sp_la", "sp_q"], ["g_all"])
        self.tt("dve", g_all[:], g_all[:], nea[:].unsqueeze(1).to_broadcast([128, NT, 16]), ALU.mult, ["g_all", "nea"], ["g_all"])
        self.act(beta_all[:], ab_all[:, :, 16:32], AF.Sigmoid, abk, ["beta_all"])
        self.act(lnb_all[:], beta_all[:], AF.Ln, ["beta_all"], ["lnb_all"])
        scz.close()
        if self.stage >= 31:
            self.dn_scan(0, dict(qT_d=qT_d, kT_d=kT_d, ktok_d=ktok_d, vtok_d=vtok_d, sz_d=sz_d, of_d=of_d), g_all, beta_all, lnb_all)
        if self.stage >= 32:
            self.dn_scan(1, dict(qT_d=qT_d, kT_d=kT_d, ktok_d=ktok_d, vtok_d=vtok_d, sz_d=sz_d, of_d=of_d), g_all, beta_all, lnb_all)
        if self.debug:
            d_gates = self.nc.dram_tensor("d_gates", [128, 3, NT * 16], F32, kind="ExternalOutput").ap()
            for i, (t, k) in enumerate(((g_all, "g_all"), (beta_all, "beta_all"), (lnb_all, "lnb_all"))):
                self.ld(d_gates[:, i, :], t[:, :, :].rearrange("p j e -> p (j e)"), [k], [("d_gates", i)])
        scL.close()
        if self.stage >= 33:
            self.dn_out(of_d)

    def dn_scan(self, dr, dd, g_all, beta_all, lnb_all):
        nc, S, W = self.nc, self.S, self.W[1]
        cst, c2 = self.cst, self.cst2
        ident = cst[:, C_ID:C_ID + 128]
        ones = cst[:, C_ONES:C_ONES + 128]
        zcol = cst[:, C_U:C_U + 1]
        Ltri = c2[:, C2_LF:C2_LF + 128] if dr == 0 else c2[:, C2_LB:C2_LB + 128]
        LT, GT = c2[:, C2_LT:C2_LT + 128], c2[:, C2_GT:C2_GT + 128]
        GE, LE = c2[:, C2_GE:C2_GE + 128], c2[:, C2_LE:C2_LE + 128]
        m_db, m_dbt, m_dt = (LT, GT, GE) if dr == 0 else (GT, LT, LE)
        order = ([32, 33] + list(range(32))) if dr == 0 else ([33, 32] + list(range(31, -1, -1)))
        if self.n_tiles is not None:
            order = order[:self.n_tiles]
        sc = Scope(self)
        sfx = "_%d" % dr

        def bankk(b):
            return [self.pk(b, 0), self.pk(b, 1)]

        def zfill(t, k):
            sh = list(t.shape)
            self.cp("dve", t[:], zcol.to_broadcast(sh) if len(sh) == 2 else zcol.unsqueeze(1).to_broadcast(sh), ["cst"], [k])


        def z3(name, w, dt=F32R, fill=True):
            t = sc.sb(name + sfx, [128, 8, w], dt)
            if fill:
                for b_ in range(4):
                    self.cp("dve", t[:, 2 * b_:2 * b_ + 2, :], zcol.unsqueeze(1).to_broadcast([128, 2, w]), ["cst"], [(name, b_)])
            return t

        Sst = z3("S", 256)
        Xb = [z3("X0", 256)]
        qkT = z3("qkT", 128, fill=False)
        vb = z3("vb", 256); kbe = z3("kbe", 128, fill=False); kd = z3("kd", 128, fill=False)
        qeT = z3("qeT", 128, fill=False); usb = z3("usb", 128, F32, fill=False)
        wT = z3("wT", 128, fill=False); vnew = z3("vn", 256); Sdec = z3("Sdec", 128, F32, fill=False)
        Mf = z3("Mf", 128, F32, fill=False); Ao = z3("Ao", 128, F32, fill=False)
        Xp = [z3("Xa", 128, F32, fill=False), z3("Xb2", 128, F32, fill=False)]
        XPN = ["Xa", "Xb2"]
        ETf = z3("ETf", 128, F32, fill=False)
        Td = z3("Td", 128, F32, fill=False); Uf = z3("Uf", 128, F32, fill=False)
        negIf = sc.sb("negIf" + sfx, [128, 128])
        self.ts("dve", negIf[:], ident, -1.0, None, ALU.mult, None, ["cst"], ["negIf"])
        m64b = c2[:, C2_B64:C2_B64 + 128].unsqueeze(1).to_broadcast([128, 8, 128])
        nm64b = c2[:, C2_NB64:C2_NB64 + 128].unsqueeze(1).to_broadcast([128, 8, 128])
        offb = c2[:, C2_OFF:C2_OFF + 128].unsqueeze(1).to_broadcast([128, 8, 128])
        m64b2 = c2[:, C2_B64:C2_B64 + 128].unsqueeze(1).to_broadcast([128, 2, 128])
        nm64b2 = c2[:, C2_NB64:C2_NB64 + 128].unsqueeze(1).to_broadcast([128, 2, 128])
        offb2 = c2[:, C2_OFF:C2_OFF + 128].unsqueeze(1).to_broadcast([128, 2, 128])
        kqr = Ring(sc, "kq" + sfx, [128, 8, 256], F32R, 2)
        ktr = Ring(sc, "kt" + sfx, [128, D], F32, 1)
        vtr = Ring(sc, "vt" + sfx, [128, D], F32, 1)
        otr = Ring(sc, "ot" + sfx, [128, D], F32, 1)
        Dg2 = [sc.sb("Dg%d" % i + sfx, [128, 2, 8, 128]) for i in range(2)]
        gsm2 = [sc.sb("gsm%d" % i + sfx, [128, 6, 8]) for i in range(2)]
        DB = sc.sb("DB" + sfx, [128, 8, 128]); DBT = sc.sb("DBT" + sfx, [128, 8, 128])
        DT = sc.sb("DT" + sfx, [128, 8, 128]); Ec = sc.sb("Ec" + sfx, [128, 8, 128])
        if dr == 1:
            ofr = Ring(sc, "of" + sfx, [128, D], F32, 1)
            szr = Ring(sc, "sz" + sfx, [128, D], F32, 1)
            ssq = sc.sb("ssq", [128, 8])
            onb = sc.sb("onb", [128, 128])
            self.ld(onb[:], W["o_norm"].partition_broadcast(128), [], ["onb"])
        NIT = 5
        P4 = range(4)

        def A(b_):
            return self.ps[b_][:, :].rearrange("p (s c) -> p s c", s=2), [("ps", b_)]

        def B(b_):
            return self.ps[4 + b_][:, :].rearrange("p (s c) -> p s c", s=2), [("ps", 4 + b_)]

        def keys(name):
            return [(name, b_) for b_ in P4]

        idb8 = ident.unsqueeze(1).to_broadcast([128, 8, 128])
        idb2 = ident.unsqueeze(1).to_broadcast([128, 2, 128])
        gk = ["g_all", "beta_all", "lnb_all"]

        def gates(jj_, sl_):
            gj_ = g_all[:, jj_, dr * 8:(dr + 1) * 8]
            lbj_ = lnb_all[:, jj_, dr * 8:(dr + 1) * 8]
            gs_ = gsm2[sl_]
            gsk = ("gsm", sl_)
            pg = self.ps[7]
            self.mm(pg[:, 0:8], Ltri, gj_, True, True, ["cst2"] + gk, bankk(7))
            self.mm(pg[:, 8:16], ones, gj_, True, True, ["cst"] + gk, bankk(7))
            gc_, gb_, ebg_, ekd_, egl_, tmpg_ = (gs_[:, i, :] for i in range(6))
            self.cp("act", gc_, pg[:, 0:8], bankk(7), [gsk])
            self.tt("dve", gb_, gc_, lbj_, ALU.add, [gsk] + gk, [gsk])
            self.act(ebg_, gb_, AF.Exp, [gsk], [gsk])
            self.tt("dve", tmpg_, pg[:, 8:16], gc_, ALU.subtract, bankk(7) + [gsk], [gsk])
            self.act(ekd_, tmpg_, AF.Exp, [gsk], [gsk])
            self.act(egl_, pg[:, 8:16], AF.Exp, bankk(7), [gsk])
            self.tt("pool", Dg2[sl_][:, 0, :, :], idb8, gc_.unsqueeze(2).to_broadcast([128, 8, 128]), ALU.mult, ["cst", gsk], [("Dg0", sl_)])
            self.tt("pool", Dg2[sl_][:, 1, :, :], idb8, gb_.unsqueeze(2).to_broadcast([128, 8, 128]), ALU.mult, ["cst", gsk], [("Dg1", sl_)])

        def kq_load(jj_):
            tsl_ = slice(jj_ * 128, (jj_ + 1) * 128)
            kq_, kqk_ = kqr.next()
            self.ld(kq_[:, :, 0:128], dd["kT_d"][:, tsl_].rearrange("(h p) t -> p h t", p=128), [], [kqk_ + ("k",)], q="pool")
            keys_ = [kqk_ + ("k",)]
            if jj_ < NT_LAT:
                self.ld(kq_[:, :, 128:256], dd["qT_d"][:, tsl_].rearrange("(h p) t -> p h t", p=128), [], [kqk_ + ("q",)], q="pool")
                keys_.append(kqk_ + ("q",))
            return kq_, kqk_, keys_

        gates(order[0], 0)
        kq_nxt = kq_load(order[0])
        for oi, jj in enumerate(order):
            sl = oi % 2
            gsk = ("gsm", sl)
            Dg = Dg2[sl]
            lat = jj < NT_LAT
            tsl = slice(jj * 128, (jj + 1) * 128)
            kq, kqk, kqkeys = kq_nxt
            if oi + 1 < len(order):
                kq_nxt = kq_load(order[oi + 1])
            kt, ktk = ktr.next()
            self.ld(kt[:], dd["ktok_d"][tsl, :], [], [ktk])
            vt, vtk = vtr.next()
            self.ld(vt[:], dd["vtok_d"][tsl, :], [], [vtk])
            kt3 = kt[:, :].rearrange("p (h d) -> p h d", h=8)
            vt3 = vt[:, :].rearrange("p (h d) -> p h d", h=8)
            if dr == 1 and lat:
                of, ofk = ofr.next()
                self.ld(of[:], dd["of_d"][tsl, :], [("of_d", jj)], [ofk])
                szt, szk = szr.next()
                self.ld(szt[:], dd["sz_d"][tsl, :], [], [szk])
            bj = beta_all[:, jj, dr * 8:(dr + 1) * 8]
            gc, gb, ebg, ekd, egl, tmpg = (gsm2[sl][:, i, :] for i in range(6))
            for half in range(2):
                self.mm(self.ps[4 + half][:, :], ones, Dg[:, 0, half * 4:(half + 1) * 4, :], True, True, ["cst", ("Dg0", sl)], bankk(4 + half))
                self.mm(self.ps[6 + half][:, :], ones, Dg[:, 1, half * 4:(half + 1) * 4, :], True, True, ["cst", ("Dg1", sl)], bankk(6 + half))
            ncol = 256 if lat else 128
            for b_ in P4:
                ap_, apk = A(b_)
                for s_ in range(2):
                    h = 2 * b_ + s_
                    self.mm(ap_[:, s_, 0:ncol], kq[:, h, 0:128], kq[:, h, 0:ncol], True, True, kqkeys, apk)
            if oi + 1 < len(order):
                gates_next = (order[oi + 1], 1 - sl)
            else:
                gates_next = None
            for half in range(2):
                hs = slice(half * 4, half * 4 + 4)
                pRc = self.ps[4 + half][:, :].rearrange("p (h f) -> p h f", h=4)
                pRb = self.ps[6 + half][:, :].rearrange("p (h f) -> p h f", h=4)
                gbb = gb[:, hs].unsqueeze(2).to_broadcast([128, 4, 128])
                gcb = gc[:, hs].unsqueeze(2).to_broadcast([128, 4, 128])
                self.tt("dve", DB[:, hs, :], gbb, pRc, ALU.subtract, [gsk] + bankk(4 + half), ["DB"])
                self.tt("pool", DB[:, hs, :], DB[:, hs, :], m_db.unsqueeze(1).to_broadcast([128, 4, 128]), ALU.add, ["DB", "cst2"], ["DB"])
                self.tt("dve", DBT[:, hs, :], pRb, gcb, ALU.subtract, [gsk] + bankk(6 + half), ["DBT"])
                self.tt("pool", DBT[:, hs, :], DBT[:, hs, :], m_dbt.unsqueeze(1).to_broadcast([128, 4, 128]), ALU.add, ["DBT", "cst2"], ["DBT"])
                if lat:
                    self.tt("dve", DT[:, hs, :], pRc, gcb, ALU.subtract, [gsk] + bankk(4 + half), ["DT"])
                    self.tt("pool", DT[:, hs, :], DT[:, hs, :], m_dt.unsqueeze(1).to_broadcast([128, 4, 128]), ALU.add, ["DT", "cst2"], ["DT"])
                    self.act(Ec[:, hs, :], pRc, AF.Exp, bankk(4 + half), ["Ec"])
            self.act(DB[:], DB[:], AF.Exp, ["DB"], ["DB"])
            self.act(DBT[:], DBT[:], AF.Exp, ["DBT"], ["DBT"])
            if lat:
                self.act(DT[:], DT[:], AF.Exp, ["DT"], ["DT"])
            for b_ in P4:
                ap_, apk = A(b_)
                hp = slice(2 * b_, 2 * b_ + 2)
                self.tt("dve", Mf[:, hp, :], ap_[:, :, 0:128], DB[:, hp, :], ALU.mult, apk + ["DB"], [("Mf", b_)])
                self.tt("dve", Xp[0][:, hp, :], ap_[:, :, 0:128], DBT[:, hp, :], ALU.mult, apk + ["DBT"], [("Xa", b_)])
                if lat:
                    self.tt("dve", qkT[:, hp, :], ap_[:, :, 128:256], DT[:, hp, :], ALU.mult, apk + ["DT"], [("qkT", b_)])
            for b_ in P4:
                hp = slice(2 * b_, 2 * b_ + 2)
                self.tt("pool", Ao[:, hp, :], Mf[:, hp, :], offb2, ALU.mult, [("Mf", b_), "cst2"], [("Ao", b_)])
                self.tt("pool", Mf[:, hp, :], Mf[:, hp, :], m64b2, ALU.mult, [("Mf", b_), ("Ao", b_), "cst2"], [("Mf", b_)])
                self.tt("pool", Mf[:, hp, :], Mf[:, hp, :], idb2, ALU.add, [("Mf", b_), "cst"], [("Mf", b_)])
                self.tt("pool", Xp[0][:, hp, :], Xp[0][:, hp, :], nm64b2, ALU.mult, [("Xa", b_), "cst2"], [("Xa", b_)])
                self.tt("pool", Xp[0][:, hp, :], Xp[0][:, hp, :], idb2, ALU.add, [("Xa", b_), "cst"], [("Xa", b_)])
            if gates_next is not None:
                gates(*gates_next)
            cur = 0
            for it in range(NIT):
                src, srck = Xp[cur], XPN[cur]
                dst, dstk = Xp[1 - cur], XPN[1 - cur]
                for b_ in P4:
                    ap_, apk = A(b_)
                    for s_ in range(2):
                        h = 2 * b_ + s_
                        self.mm(ap_[:, s_, 0:128], src[:, h, :], Mf[:, h, :], True, True, [(srck, b_), ("Mf", b_)], apk)
                for b_ in P4:
                    ap_, apk = A(b_)
                    self.stt("dve", ETf[:, 2 * b_:2 * b_ + 2, :], ap_[:, :, 0:128], -1.0, idb2, ALU.mult, ALU.add, apk + ["cst"], [("ETf", b_)])
                for b_ in P4:
                    bp_, bpk = B(b_)
                    for s_ in range(2):
                        h = 2 * b_ + s_
                        self.mm(bp_[:, s_, 0:128], ETf[:, h, :], src[:, h, :], True, True, [("ETf", b_), (srck, b_)], bpk)
                for b_ in P4:
                    bp_, bpk = B(b_)
                    hp = slice(2 * b_, 2 * b_ + 2)
                    self.tt("dve", dst[:, hp, :], src[:, hp, :], bp_[:, :, 0:128], ALU.add, [(srck, b_)] + bpk, [(dstk, b_)])
                cur = 1 - cur
            Xd, Xdk = Xp[cur], XPN[cur]
            for b_ in P4:
                ap_, apk = A(b_)
                bp_, bpk = B(b_)
                for s_ in range(2):
                    h = 2 * b_ + s_
                    self.tr(ap_[:, s_, 0:128], Xd[:, h, :], [(Xdk, b_)], apk)
                    self.mm(bp_[:, s_, 0:128], Ao[:, h, :], Xd[:, h, :], True, True, [("Ao", b_), (Xdk, b_)], bpk)
            for b_ in P4:
                ap_, apk = A(b_)
                bp_, bpk = B(b_)
                hp = slice(2 * b_, 2 * b_ + 2)
                self.cp("act", Td[:, hp, :], ap_[:, :, 0:128], apk, [("Td", b_)])
                self.cp("act", Uf[:, hp, :], bp_[:, :, 0:128], bpk, [("Uf", b_)])
            for b_ in P4:
                ap_, apk = A(b_)
                for s_ in range(2):
                    h = 2 * b_ + s_
                    self.mm(ap_[:, s_, 0:128], Td[:, h, :], Uf[:, h, :], True, True, [("Td", b_), ("Uf", b_)], apk)
            for b_ in P4:
                ap_, apk = A(b_)
                hp = slice(2 * b_, 2 * b_ + 2)
                self.tt("dve", Xb[0][:, hp, 0:128], Xd[:, hp, :], ap_[:, :, 0:128], ALU.subtract, [(Xdk, b_)] + apk, [("X0", b_)])
            cur = 0
            XN = ["X0"]
            Xf, kf_ = Xb[cur], XN[cur]
            self.tt("dve", vb[:, :, 0:128], vt3, bj.unsqueeze(2).to_broadcast([128, 8, 128]), ALU.mult, [vtk] + gk, keys("vb"))
            self.tt("pool", kbe[:, :, :], kt3, ebg.unsqueeze(2).to_broadcast([128, 8, 128]), ALU.mult, [ktk, gsk], keys("kbe"))
            self.tt("pool", kd[:, :, :], kt3, ekd.unsqueeze(2).to_broadcast([128, 8, 128]), ALU.mult, [ktk, gsk], keys("kd"))
            if lat:
                self.tt("pool", qeT[:, :, :], kq[:, :, 128:256], Ec[:, :, :], ALU.mult, kqkeys + ["Ec"], keys("qeT"))
            for b_ in P4:
                ap_, apk = A(b_)
                bp_, bpk = B(b_)
                for s_ in range(2):
                    h = 2 * b_ + s_
                    self.mm(ap_[:, s_, :], Xf[:, h, 0:128], vb[:, h, :], True, True, [(kf_, b_), ("vb", b_)], apk)
                    self.mm(bp_[:, s_, :], kbe[:, h, :], Xf[:, h, :], True, True, [(kf_, b_), ("kbe", b_)], bpk)
            for b_ in P4:
                ap_, apk = A(b_)
                bp_, bpk = B(b_)
                hp = slice(2 * b_, 2 * b_ + 2)
                self.cp("act", usb[:, hp, :], ap_[:, :, 0:128], apk, [("usb", b_)])
                self.cp("act", wT[:, hp, :], bp_[:, :, 0:128], bpk, [("wT", b_)])
            ot, otk = otr.next()
            ot3 = ot[:, :].rearrange("p (h d) -> p h d", h=8)
            for b_ in P4:
                ap_, apk = A(b_)
                for s_ in range(2):
                    h = 2 * b_ + s_
                    self.mm(ap_[:, s_, :], wT[:, h, :], Sst[:, h, :], True, True, [("wT", b_), ("S", b_)], apk)
            for b_ in P4:
                ap_, apk = A(b_)
                hp = slice(2 * b_, 2 * b_ + 2)
                self.tt("dve", vnew[:, hp, 0:128], usb[:, hp, :], ap_[:, :, 0:128], ALU.subtract, [("usb", b_)] + apk, [("vn", b_)])
            for b_ in P4:
                ap_, apk = A(b_)
                bp_, bpk = B(b_)
                for s_ in range(2):
                    h = 2 * b_ + s_
                    if lat:
                        self.mm(bp_[:, s_, :], qeT[:, h, :], Sst[:, h, :], True, False, [("qeT", b_), ("S", b_)], bpk)
                        self.mm(bp_[:, s_, :], qkT[:, h, :], vnew[:, h, :], False, True, [("qkT", b_), ("vn", b_)], bpk)
                    self.mm(ap_[:, s_, :], kd[:, h, :], vnew[:, h, :], True, True, [("kd", b_), ("vn", b_)], apk)
            self.tt("pool", Sdec[:, :, :], Sst[:, :, 0:128], egl.unsqueeze(2).to_broadcast([128, 8, 128]), ALU.mult,
                    keys("S") + [gsk], keys("Sdec"))
            for b_ in P4:
                ap_, apk = A(b_)
                bp_, bpk = B(b_)
                hp = slice(2 * b_, 2 * b_ + 2)
                if lat:
                    self.cp("act", ot3[:, hp, :], bp_[:, :, 0:128], bpk, [otk])
                self.tt("dve", Sst[:, hp, 0:128], Sdec[:, hp, :], ap_[:, :, 0:128], ALU.add, [("Sdec", b_)] + apk, [("S", b_)])
            if not lat:
                continue
            if dr == 0:
                self.ld(dd["of_d"][tsl, :], ot[:], [otk], [("of_d", jj)])
            else:
                if self.debug:
                    if not hasattr(self, "ob_d"):
                        self.ob_d = self.nc.dram_tensor("ob_d", [NLAT, D], F32, kind="ExternalOutput").ap()
                    self.ld(self.ob_d[tsl, :], ot[:], [otk], [("ob_d", jj)])
                self.tt("pool", ot[:], ot[:], of[:], ALU.add, [otk, ofk], [otk])
                self.act(DB[:, :, :].rearrange("p h d -> p (h d)"), ot[:], AF.Square, [otk], ["DB"])
                S.op("dve", lambda e: e.tensor_reduce(out=ssq[:], in_=DB[:, :, :], axis=AX.X, op=ALU.add),
                     ["DB"], ["ssq"])
                self.ts("dve", ssq[:], ssq[:], 1.0 / 128, EPS, ALU.mult, ALU.add, ["ssq"], ["ssq"])
                self.act(ssq[:], ssq[:], AF.Sqrt, ["ssq"], ["ssq"])
                S.op("dve", lambda e: e.reciprocal(out=ssq[:], in_=ssq[:]), ["ssq"], ["ssq"])
                o3 = ot[:, :].rearrange("p (h d) -> p h d", h=8)
                self.tt("dve", o3, o3, ssq[:].unsqueeze(2).to_broadcast([128, 8, 128]), ALU.mult, [otk, "ssq"], [otk])
                self.tt("pool", o3, o3, onb[:].unsqueeze(1).to_broadcast([128, 8, 128]), ALU.mult, [otk, "onb"], [otk])
                self.tt("pool", ot[:], ot[:], szt[:], ALU.mult, [otk, szk], [otk])
                self.ld(dd["of_d"][tsl, :], ot[:], [otk, ofk], [("of_d", jj)])
        sc.close()

    def dn_out(self, of_d):
        nc, S, W = self.nc, self.S, self.W[1]
        sc = Scope(self)
        wo = sc.sb("wo1", [128, 8, D], F32R)
        self.ld(wo[:], W["w_o"].rearrange("(k p) n -> p k n", p=128), [], ["wo1"], q="pool")
        wr = sc.sb("wr1", [128, 8, NE])
        self.ld(wr[:], W["router"].rearrange("(k p) n -> p k n", p=128), [], ["wr"])
        yr = Ring(sc, "oy", [128, D], F32, 2)
        xr = Ring(sc, "ox", [128, D], F32, 2)
        xmr = Ring(sc, "oxm", [128, D], F32, 3)
        ssr = Ring(sc, "oss", [128, 1], F32, 4)
        junk = sc.sb("ojunk", [128, D])
        yT = sc.sb("oyT", [128, 8, 128], F32R)
        h2T = sc.sb("oh2T", [128, 8, 128])
        lg = sc.sb("olg", [128, NE]); mx = sc.sb("omx", [128, 1]); sm = sc.sb("osm", [128, 1])
        pend = [None]

        def o_loads(i_):
            yt_, ytk_ = yr.next()
            self.ld(yt_[:], of_d[i_ * 128:(i_ + 1) * 128, :], [], [ytk_])
            xt_, xk_ = xr.next()
            self.ld(xt_[:], self.xres[0][i_ * 128:(i_ + 1) * 128, :], [], [xk_])
            return yt_, ytk_, xt_, xk_

        onxt = o_loads(0)
        for i in range(NT_LAT):
            yt, ytk, xt, xk = onxt
            if i + 1 < NT_LAT:
                onxt = o_loads(i + 1)
            for kc in range(8):
                self.tr(self.ps[kc // 4][:, (kc % 4) * 128:(kc % 4 + 1) * 128], yt[:, kc * 128:(kc + 1) * 128], [ytk], [self.psk[kc // 4]])
            for b in range(2):
                self.cp("act", yT[:, b * 4:(b + 1) * 4, :], self.ps[b][:, :].rearrange("p (k t) -> p k t", k=4), [self.psk[b]], ["yT"])
            if pend[0] is not None:
                self.moe_prep(*pend[0])
                pend[0] = None
            xm, xmk = xmr.next()
            for dh in range(2):
                pM, pMk = self.ps[2 + dh], self.psk[2 + dh]
                for kc in range(8):
                    self.mm(pM[:, :], yT[:, kc, :], wo[:, kc, dh * 512:(dh + 1) * 512], kc == 0, kc == 7, ["yT", "wo1"], [pMk])
                self.tt("dve", xm[:, dh * 512:(dh + 1) * 512], pM[:, :], self.Gbc[:, 0, 0, dh * 512:(dh + 1) * 512], ALU.mult,
                        [pMk, ("Gbc", 0, 0)], [xmk])
            self.tt("dve", xm[:], xm[:], xt[:], ALU.add, [xmk, xk], [xmk])
            self.ld(self.xres[1][i * 128:(i + 1) * 128, :], xm[:], [xmk], [("xres1", i)])
            pend[0] = (i, 0, xm, xmk, junk, ssr, wr, h2T, lg, mx, sm)
        self.moe_prep(*pend[0])
        sc.close()

    def final_norm(self):
        nc, S = self.nc, self.S
        sc = Scope(self)
        fn = sc.sb("fnb", [128, D])
        self.ld(fn[:], self.inp["final_norm"].partition_broadcast(128), [], ["fnb"])
        xr = Ring(sc, "fx", [128, D], F32, 4)
        ssr = Ring(sc, "fss", [128, 1], F32, 4)
        junk = sc.sb("fjunk", [128, D])
        def f_load(i_):
            xt_, xk_ = xr.next()
            self.ld(xt_[:], self.xres[1][i_ * 128:(i_ + 1) * 128, :], [], [xk_])
            return xt_, xk_

        fq_ = [f_load(0), f_load(1)]
        for i in range(NT_LAT):
            xt, xk = fq_.pop(0)
            if i + 2 < NT_LAT:
                fq_.append(f_load(i + 2))
            ss, ssk = ssr.next()
            self.rstd_of(xt[:], xk, junk[:], "fjunk", ss[:], ssk)
            self.stt("dve", xt[:], xt[:], ss[:, 0:1], fn[:], ALU.mult, ALU.mult, [xk, ssk, "fnb"], [xk])
            S.dma("sp", lambda e: e.dma_start(out=self.out[i * 128:(i + 1) * 128, :], in_=xt[:]), [xk], [("out", i)], is_output=True)
        sc.close()

    def moe(self, l, xres, xresk, with_ctx):
        nc, S, W = self.nc, self.S, self.W[l]
        cst = self.cst
        ones = cst[:, C_ONES:C_ONES + 128]
        Umat = cst[:, C_U:C_U + 128]
        iota = cst[:, C_IOTA:C_IOTA + 512]
        sets = [(0, 0, NT_LAT, CAP_LAT)] + ([(1, NT_LAT, NT_CTX, CAP_CTX)] if with_ctx else [])
        sc = Scope(self)
        slot_m = {}; meta = {}
        for (si, j0, nj, cap) in sets:
            slot_m[si] = sc.sb("slotm%d_%d" % (si, l), [128, nj, NE])
            meta[si] = sc.sb("meta%d_%d" % (si, l), [128, nj, NE, 4], F32R)
        scr = Scope(self)
        for (si, j0, nj, cap) in sets:
            sfx = "%d_%d" % (si, l)
            affv = self.aff[:, j0:j0 + nj, :]
            affk = [("aff", j) for j in range(j0, j0 + nj)]
            lo = scr.sb("lo" + sfx, [128, NE]); mid = scr.sb("mid" + sfx, [128, NE])
            cmpt = scr.sb("cmp" + sfx, [128, nj, NE]); cnt = scr.sb("cnt" + sfx, [128, NE])
            tq = scr.sb("tq" + sfx, [128, NE])
            offs = scr.sb("offs" + sfx, [128, nj, NE]); slot = scr.sb("slot" + sfx, [128, nj, NE])
            S.op("dve", lambda e: e.memset(lo[:], 0.0), [], ["lo"])
            S.op("dve", lambda e: e.memset(mid[:], 0.5), [], ["mid"])
            pC, pCk = self.ps[0], self.psk[0]
            for it in range(NBIS):
                w = 2.0 ** -(it + 1)
                self.tt("dve", cmpt[:], affv, mid[:].unsqueeze(1).to_broadcast([128, nj, NE]), ALU.is_ge, affk + ["mid"], ["cmp"])
                S.op("dve", lambda e: e.tensor_reduce(out=cnt[:], in_=cmpt[:, :, :].rearrange("p j e -> p e j"), axis=AX.X, op=ALU.add),
                     ["cmp"], ["cnt"])
                self.mm(pC[:, 0:NE], ones, cnt[:], True, True, ["cst", "cnt"], [pCk])
                self.ts("dve", tq[:], pC[:, 0:NE], cap - 0.5, w, ALU.is_ge, ALU.mult, [pCk], ["tq"])
                self.tt("dve", lo[:], lo[:], tq[:], ALU.add, ["lo", "tq"], ["lo"])
                self.ts("dve", mid[:], lo[:], w * 0.5, None, ALU.add, None, ["lo"], ["mid"])
            self.tt("dve", cmpt[:], affv, lo[:].unsqueeze(1).to_broadcast([128, nj, NE]), ALU.is_ge, affk + ["lo"], ["cmp"])
            pP, pPk = self.ps[1], self.psk[1]
            pT, pTk = self.ps[2], self.psk[2]
            mflat = cmpt[:, :, :].rearrange("p j e -> p (j e)")
            self.mm(pP[:, 0:nj * NE], Umat, mflat, True, True, ["cst", "cmp"], [pPk])
            self.mm(pT[:, 0:nj * NE], ones, mflat, True, True, ["cst", "cmp"], [pTk])
            pTv = pT[:, 0:nj * NE].rearrange("p (j e) -> p j e", e=NE)
            pPv = pP[:, 0:nj * NE].rearrange("p (j e) -> p j e", e=NE)
            S.op("dve", lambda e: e.memset(offs[:, 0, :], 0.0), [], ["offs"])
            for j in range(1, nj):
                self.tt("dve", offs[:, j, :], pTv[:, j - 1, :], offs[:, j - 1, :], ALU.add, [pTk, "offs"], ["offs"])
            self.tt("dve", slot[:], pPv, offs[:], ALU.add, [pPk, "offs"], ["slot"])
            self.ts("dve", cmpt[:], cmpt[:], -1.0e6, 1.0e6, ALU.mult, ALU.add, ["cmp"], ["cmp"])
            self.tt("dve", slot_m[si][:], slot[:], cmpt[:], ALU.add, ["slot", "cmp"], [("slotm", si)])
            mt = meta[si]
            for j in range(nj):
                self.cp("dve", mt[:, j, :, 0:1], cst[:, C_U:C_U + 1].unsqueeze(1).to_broadcast([128, NE, 1]), ["cst"], [("meta", si)])
                self.ts("dve", mt[:, j, :, 0:1], mt[:, j, :, 0:1], float(j0 + j), None, ALU.add, None, [("meta", si)], [("meta", si)])
            mtf = mt[:, :, :, :].rearrange("p j e c -> p (j e) c")
            self.cp("dve", mtf[:, :, 1:2], cst[:, C_PIDX:C_PIDX + 1].unsqueeze(1).to_broadcast([128, nj * NE, 1]), ["cst"], [("meta", si)])
            self.cp("dve", mtf[:, :, 2:3], affv.rearrange("p j e -> p (j e)").unsqueeze(2), affk, [("meta", si)])
            self.cp("dve", mtf[:, :, 3:4], cst[:, C_ONES:C_ONES + 1].unsqueeze(1).to_broadcast([128, nj * NE, 1]), ["cst"], [("meta", si)])
        scr.close()

        NW = 8
        wring = Ring(sc, "wm%d" % l, [128, 8, 256], F32R, NW)
        xsT = sc.sb("xsT%d" % l, [128, 8, 640], F32R)
        hidT = sc.sb("hidT%d" % l, [128, 16, 544], F32R)
        ysb = sc.sb("ysb%d" % l, [128, 5, D])
        xsr = Ring(sc, "xstok%d" % l, [128, D], F32, 2)
        selr = Ring(sc, "sel%d" % l, [128, 512], F32R, 2)
        selc = sc.sb("selc%d" % l, [128, 128], F32R)
        sgr = Ring(sc, "sg%d" % l, [128, 512], F32, 2)
        hcr = Ring(sc, "hc%d" % l, [32, 256], F32, 2)
        idxrow = sc.sb("idxrow%d" % l, [4, 640])
        metac = sc.sb("metac%d" % l, [128, 5, 4])
        tmp5 = sc.sb("tmp5%d" % l, [128, 5]); idxf = sc.sb("idxf%d" % l, [128, 5])
        idur = Ring(sc, "idu%d" % l, [128, 5], U32, 3)
        gcr = Ring(sc, "gc%d" % l, [128, 5], F32, 3)
        self.cp("dve", selc[:], cst[:, C_U:C_U + 1].to_broadcast([128, 128]), ["cst"], ["selc"])
        S.op("dve", lambda e: e.memset(ysb[:], 0.0), [], ["ysb"])
        nk = 5 if with_ctx else 4
        c2 = {0: 0, 1: 1}

        def idx_phase(e):
            pI, pIk = self.ps[7], self.psk[7]
            for (si, j0, nj, cap) in sets:
                ncol = 512 if si == 0 else 128
                for j in range(nj):
                    if si == 0:
                        sel, selk = selr.next()
                        self.ts("dve", sel[:], iota, slot_m[si][:, j, e:e + 1], None, ALU.is_equal, None, ["cst", ("slotm", si)], [selk])
                        rhs = sel[:]
                    else:
                        selk = "selc"
                        self.ts("dve", selc[:, 0:CAP_CTX], iota[:, 0:CAP_CTX], slot_m[si][:, j, e:e + 1], None, ALU.is_equal, None,
                                ["cst", ("slotm", si)], [selk])
                        rhs = selc[:]
                    self.mm(pI[0:4, 0:ncol], meta[si][:, j, e, :], rhs, j == 0, j == nj - 1, [("meta", si), selk], [pIk])
                off = 0 if si == 0 else 512
                self.cp("act", idxrow[0:4, off:off + ncol], pI[0:4, 0:ncol], [pIk], ["idxrow"])
            pX, pXk = self.ps[7], self.psk[7]
            for k in range(nk):
                self.tr(pX[:, k * 4:(k + 1) * 4], idxrow[0:4, k * 128:(k + 1) * 128], ["idxrow"], [pXk], kp=4)
            self.cp("act", metac[:, 0:nk, :], pX[:, 0:nk * 4].rearrange("p (k c) -> p k c", c=4), [pXk], ["metac"])
            idu, iduk = idur.next()
            gc, gck = gcr.next()
            self.stt("dve", idxf[:, 0:nk], metac[:, 0:nk, 0], 128.0, metac[:, 0:nk, 1], ALU.mult, ALU.add, ["metac"], ["idxf"])
            self.ts("dve", tmp5[:, 0:nk], metac[:, 0:nk, 3], -1.0, 1.0, ALU.mult, ALU.add, ["metac"], ["tmp5"])
            self.tt("dve", tmp5[:, 0:nk], tmp5[:, 0:nk], cst[:, C_DMY:C_DMY + nk], ALU.mult, ["tmp5", "cst"], ["tmp5"])
            self.tt("dve", idxf[:, 0:nk], idxf[:, 0:nk], tmp5[:, 0:nk], ALU.add, ["idxf", "tmp5"], ["idxf"])
            self.cp("dve", idu[:, 0:nk], idxf[:, 0:nk], ["idxf"], [iduk])
            self.cp("dve", gc[:, 0:nk], metac[:, 0:nk, 2], ["metac"], [gck])
            return (idu, iduk, gc, gck)

        gt_i = [0]

        def gather_phase(ix):
            idu, iduk, gc, gck = ix
            for k in range(nk):
                xs, xsk = xsr.next()
                S.dma("pool", lambda e: e.indirect_dma_start(out=xs[:], out_offset=None, in_=self.xn2[:, :],
                                                              in_offset=bass.IndirectOffsetOnAxis(ap=idu[:, k:k + 1], axis=0)),
                      [iduk], [xsk])
                c = 1 if k == 4 else 0
                for half in range(2):
                    bnk = gt_i[0] % 4
                    gt_i[0] += 1
                    pt, ptk = self.ps[bnk], self.psk[bnk]
                    for kc in range(half * 4, half * 4 + 4):
                        self.tr(pt[:, (kc % 4) * 128:(kc % 4 + 1) * 128], xs[:, kc * 128:(kc + 1) * 128], [xsk], [ptk])
                    for kc in range(half * 4, half * 4 + 4):
                        self.act(xsT[:, kc, k * 128:(k + 1) * 128], pt[:, (kc % 4) * 128:(kc % 4 + 1) * 128], AF.Identity,
                                 [ptk, ("A", 4), "cols"], ["xsT"], scale=self.A2[:, kc, c:c + 1], bias=self.B2(kc, c))

        def wload(src_ap):
            wt, wk = wring.next()
            self.ld(wt[:], src_ap, [], [wk], q="pool")
            return wt, wk

        gu_i = [0]

        cpend = [None]

        def ctx_tr(hc, hck, fq):
            pt, ptk = self.ps[7], self.psk[7]
            for fc in range(2):
                self.tr(pt[:, fc * 32:(fc + 1) * 32], hc[0:32, fc * 128:(fc + 1) * 128], [hck], [ptk], kp=32)
            self.cp("act", hidT[:, fq * 2:fq * 2 + 2, 512:544], pt[:, 0:64].rearrange("p (a t) -> p a t", a=2), [ptk],
                    [("hidT", fq * 2), ("hidT", fq * 2 + 1)])

        def ffn1(e):
            for fq in range(8):
                wg, wgk = wload(W["w_gate"][e, :, fq * 256:(fq + 1) * 256].rearrange("(k p) n -> p k n", p=128))
                wu, wuk = wload(W["w_up"][e, :, fq * 256:(fq + 1) * 256].rearrange("(k p) n -> p k n", p=128))
                for fc in range(2):
                    fcc = fq * 2 + fc
                    b = gu_i[0] % 2
                    gu_i[0] += 1
                    pG, pGk = self.ps[2 * b], self.psk[2 * b]
                    pU, pUk = self.ps[2 * b + 1], self.psk[2 * b + 1]
                    for kc in range(8):
                        self.mm(pG[:, :], wg[:, kc, fc * 128:(fc + 1) * 128], xsT[:, kc, 0:512], kc == 0, kc == 7, [wgk, "xsT"], [pGk])
                    for kc in range(8):
                        self.mm(pU[:, :], wu[:, kc, fc * 128:(fc + 1) * 128], xsT[:, kc, 0:512], kc == 0, kc == 7, [wuk, "xsT"], [pUk])
                    sg, sgk = sgr.next()
                    self.act(sg[:, 0:512], pG[:, :], AF.Silu, [pGk], [sgk])
                    self.tt("dve", hidT[:, fcc, 0:512], sg[:, 0:512], pU[:, :], ALU.mult, [sgk, pUk], [("hidT", fcc)])
                if with_ctx:
                    pc, pck = self.ps[4], self.psk[4]
                    for kc in range(8):
                        self.mm(pc[0:32, 0:256], xsT[:, kc, 512:544], wg[:, kc, :], kc == 0, kc == 7, [wgk, "xsT"], [pck])
                    for kc in range(8):
                        self.mm(pc[0:32, 256:512], xsT[:, kc, 512:544], wu[:, kc, :], kc == 0, kc == 7, [wuk, "xsT"], [pck])
                    hc, hck = hcr.next()
                    self.act(hc[0:32, 0:256], pc[0:32, 0:256], AF.Silu, [pck], [hck])
                    self.tt("dve", hc[0:32, 0:256], hc[0:32, 0:256], pc[0:32, 256:512], ALU.mult, [hck, pck], [hck])
                    if cpend[0] is not None:
                        ctx_tr(*cpend[0])
                    cpend[0] = (hc, hck, fq)
            if cpend[0] is not None:
                ctx_tr(*cpend[0])
                cpend[0] = None

        y_i = [0]

        def ffn2(e, ix):
            idu, iduk, gc, gck = ix
            hk = [("hidT", f) for f in range(16)]
            for dq in range(4):
                wd = []
                for fh in range(2):
                    wd.append(wload(W["w_down"][e, fh * 1024:(fh + 1) * 1024, dq * 256:(dq + 1) * 256].rearrange("(k p) n -> p k n", p=128)))
                for k in range(nk):
                    if k < 4:
                        b = y_i[0] % 2
                        y_i[0] += 1
                        pY, pYk = self.ps[5 + b], self.psk[5 + b]
                        rows = 128
                        lsl = slice(k * 128, (k + 1) * 128)
                    else:
                        pY, pYk = self.ps[4], self.psk[4]
                        rows = 32
                        lsl = slice(512, 544)
                    for fcc in range(16):
                        wt, wk = wd[fcc // 8]
                        self.mm(pY[0:rows, 0:256], hidT[:, fcc, lsl], wt[:, fcc % 8, :], fcc == 0, fcc == 15, [("hidT", fcc), wk], [pYk])
                    c = 1 if k == 4 else 0
                    self.stt("dve", ysb[0:rows, k, dq * 256:(dq + 1) * 256], pY[0:rows, 0:256], gc[0:rows, k:k + 1],
                             self.Gbc[0:rows, 1, c, dq * 256:(dq + 1) * 256], ALU.mult, ALU.mult,
                             [pYk, gck, ("Gbc", 1, c)], [("ysb", k)])

        def scatter_phase(ix):
            idu, iduk, gc, gck = ix
            for k in range(nk):
                S.dma("pool", lambda e: e.indirect_dma_start(out=xres[:, :], out_offset=bass.IndirectOffsetOnAxis(ap=idu[:, k:k + 1], axis=0),
                                                              in_=ysb[:, k, :], in_offset=None, compute_op=ALU.add),
                      [iduk, ("ysb", k), "ysb"], ["xacc"])

        n_exp = NE if self.n_exp is None else self.n_exp
        ixs = {0: idx_phase(0)}
        gather_phase(ixs[0])
        for e in range(n_exp):
            if e + 1 < n_exp:
                ixs[e + 1] = idx_phase(e + 1)
            ffn1(e)
            if e > 0:
                scatter_phase(ixs[e - 1])
            if e + 1 < n_exp:
                gather_phase(ixs[e + 1])
            ffn2(e, ixs[e])
        scatter_phase(ixs[n_exp - 1])
        sc.close()


def _host_consts():
    cp = np.zeros((128, C_END), np.float32)
    cp[:, C_ID:C_ID + 128] = np.eye(128, dtype=np.float32)
    cp[:, C_ONES:C_ONES + 128] = 1.0
    pi = np.arange(128)
    cp[:, C_U:C_U + 128] = (pi[:, None] < pi[None, :]).astype(np.float32)
    mp = (pi[:, None] >= pi[None, :]).astype(np.float32)
    mn = (pi[:, None] <= pi[None, :]).astype(np.float32)
    cp[:, C_MP:C_MP + 512] = np.tile(mp, (1, 4))
    cp[:, C_MN:C_MN + 512] = np.tile(mn, (1, 4))
    cp[:, C_IOTA:C_IOTA + 512] = np.arange(512, dtype=np.float32)[None, :]
    cp[:, C_PIDX] = pi
    for k in range(5):
        cp[:, C_DMY + k] = NTOK + k * 128 + pi
    t = np.arange(NLAT)
    row = (t // 64).astype(np.float32)
    col = (t % 64).astype(np.float32)
    inv = (np.float32(10000.0) ** (-np.arange(16, dtype=np.float32) / np.float32(16))).astype(np.float32)
    ar = (row[:, None] * inv[None, :]).astype(np.float32)
    ac = (col[:, None] * inv[None, :]).astype(np.float32)
    rope = np.concatenate([np.cos(ar), np.cos(ac), np.sin(ar), np.sin(ac)], axis=1).astype(np.float32)
    return cp, rope


def _host_consts2():
    c2 = np.zeros((128, C2_END), np.float32)
    p = np.arange(128)[:, None]
    f = np.arange(128)[None, :]
    c2[:, C2_LF:C2_LF + 128] = (p <= f)
    c2[:, C2_LB:C2_LB + 128] = (p >= f)
    c2[:, C2_LT:C2_LT + 128] = np.where(f < p, 0.0, NEG)
    c2[:, C2_GT:C2_GT + 128] = np.where(f > p, 0.0, NEG)
    c2[:, C2_GE:C2_GE + 128] = np.where(f >= p, 0.0, NEG)
    c2[:, C2_LE:C2_LE + 128] = np.where(f <= p, 0.0, NEG)
    same = ((p // 64) == (f // 64)).astype(np.float32)
    c2[:, C2_B64:C2_B64 + 128] = same
    c2[:, C2_NB64:C2_NB64 + 128] = -same
    c2[:, C2_OFF:C2_OFF + 128] = 1.0 - same
    return c2


def _colT(v, n):
    return np.ascontiguousarray(np.asarray(v, np.float32).reshape(n, 128).T)


ALL_INPUTS = (
    "x", "c", "ctx", "c_ctx",
    "l0_ada_w", "l0_ada_b", "l0_norm_mix", "l0_w_qkv", "l0_sink", "l0_w_o", "l0_norm_ffn",
    "l0_router", "l0_w_gate", "l0_w_up", "l0_w_down",
    "l1_ada_w", "l1_ada_b", "l1_norm_mix", "l1_w_in", "l1_conv", "l1_a_log", "l1_dt_bias", "l1_o_norm", "l1_w_o",
    "l1_norm_ffn", "l1_router", "l1_w_gate", "l1_w_up", "l1_w_down",
    "final_norm",
)


def make_in_maps(inputs, cores):
    missing = [n for n in ALL_INPUTS if n not in inputs]
    assert not missing, missing
    cp, rope = _host_consts()
    shared = {"cpack": cp, "rope": rope}
    for l in (0, 1):
        p = "l%d_" % l
        shared[p + "ada_w"] = np.asarray(inputs[p + "ada_w"], np.float32)
        shared[p + "ada_b"] = np.asarray(inputs[p + "ada_b"], np.float32)
        shared[p + "ada_bT"] = _colT(inputs[p + "ada_b"], 48)
        shared[p + "nmixT"] = _colT(inputs[p + "norm_mix"], 8)
        shared[p + "nffnT"] = _colT(inputs[p + "norm_ffn"], 8)
        for n in ("router", "w_gate", "w_up", "w_down"):
            shared[p + n] = np.asarray(inputs[p + n], np.float32)
    for n in ("l0_sink", "l0_w_o", "l1_w_in", "l1_o_norm", "l1_w_o", "final_norm"):
        shared[n] = np.asarray(inputs[n], np.float32)
    shared["l1_a_log"] = np.asarray(inputs["l1_a_log"], np.float32).reshape(16)
    shared["l1_dt_bias"] = np.asarray(inputs["l1_dt_bias"], np.float32).reshape(16)
    cv = np.asarray(inputs["l1_conv"], np.float32)
    shared["l1_convT"] = np.ascontiguousarray(cv.reshape(3, 24, 128).transpose(2, 1, 0))
    shared["cpack2"] = _host_consts2()
    wq = np.asarray(inputs["l0_w_qkv"], np.float32)
    perm = [pr * 8 + s_ * 4 + g for pr in range(2) for g in range(4) for s_ in range(2)]
    cols = np.concatenate([np.arange(h * 64, (h + 1) * 64) for h in perm] + [np.arange(1024, 1536)])
    shared["l0_w_qkv"] = np.ascontiguousarray(wq[:, cols])
    maps = []
    for b in cores:
        m = dict(shared)
        m["x"] = np.ascontiguousarray(inputs["x"][b], dtype=np.float32)
        m["ctx"] = np.ascontiguousarray(inputs["ctx"][b], dtype=np.float32)
        cv = np.stack([np.asarray(inputs["c"][b], np.float32), np.asarray(inputs["c_ctx"], np.float32)], axis=1)
        m["cvecT"] = np.ascontiguousarray(cv.reshape(8, 128, 2).transpose(1, 0, 2))
        maps.append(m)
    return maps


def kernel(**inputs):
    b = Builder()
    nc = b.build()
    maps = make_in_maps(inputs, list(range(8)))
    maps = [{k: v for k, v in m.items() if k in b.inp} for m in maps]
    res = run_bass_kernel_spmd(nc, maps, core_ids=list(range(8)))
    return np.stack([r["out"] for r in res.results], axis=0).astype(np.float32)
```

```python
import numpy as np
import concourse.bass as bass
import concourse.mybir as mybir
from concourse.bass_utils import run_bass_kernel_spmd

F32 = mybir.dt.float32
F32R = mybir.dt.float32r
U32 = mybir.dt.uint32
ALU = mybir.AluOpType
AF = mybir.ActivationFunctionType
AX = mybir.AxisListType

D = 1024
NLAT = 4096
NCTX = 256
NT_LAT = 32
NT_CTX = 2
NT = 34
NTOK = NLAT + NCTX
NE = 16
FF = 2048
CAP_LAT = 512
CAP_CTX = 32
NDUMMY = 640
EPS = 1e-6
NBIS = 30

C_ID, C_ONES, C_U, C_MP, C_MN, C_IOTA, C_PIDX, C_DMY, C_END = 0, 128, 256, 384, 896, 1408, 1920, 1921, 1926


C2_LF, C2_LB, C2_LT, C2_GT, C2_GE, C2_LE, C2_B64, C2_NB64, C2_OFF, C2_END = 0, 128, 256, 384, 512, 640, 768, 896, 1024, 1152
NEG = -30000.0


class Sched:
    def __init__(self, nc, n_dma_sems=24):
        self.nc = nc
        self.engs = {"pe": nc.tensor, "act": nc.scalar, "dve": nc.vector,
                     "pool": nc.gpsimd, "sp": nc.sync}
        self.csem = {e: nc.alloc_semaphore("c_" + e) for e in ("pe", "act", "dve", "pool")}
        self.ccnt = {e: 0 for e in self.csem}
        self.known = {e: {} for e in self.engs}
        self.dsems = [nc.alloc_semaphore("d%d" % i) for i in range(2 * n_dma_sems)]
        self.dcnt = [0] * (2 * n_dma_sems)
        self.dpool = {"sp": list(range(0, n_dma_sems)), "pool": list(range(n_dma_sems, 2 * n_dma_sems))}
        self.dnext = {"sp": 0, "pool": 0}
        self.state = {}
        self.out_events = []
        self.n_wait = 0
        self.n_inst = 0

    def _need(self, eng, ev):
        sem, val = ev
        k = self.known[eng]
        if k.get(sem.num, 0) >= val:
            return
        self.engs[eng].wait_ge(sem, val)
        self.n_wait += 1
        k[sem.num] = val

    def _deps(self, eng, reads, writes, skip_self=False):
        evs = {}

        def add(ev):
            if ev is None:
                return
            sem, val = ev
            if skip_self and sem.num == self.csem[eng].num:
                return
            if evs.get(sem.num, (None, 0))[1] < val:
                evs[sem.num] = ev

        own = self.csem[eng].num if eng in self.csem else -1
        for k in reads:
            st = self.state.get(k)
            if st:
                add(st["w"])
                if isinstance(k, tuple) and k[0] == "ps":
                    for r in st["r"]:
                        if r[0].num != own:
                            add(r)
        for k in writes:
            st = self.state.get(k)
            if st:
                add(st["w"])
                for r in st["r"]:
                    add(r)
        for ev in evs.values():
            self._need(eng, ev)

    def _commit(self, ev, reads, writes):
        for k in reads:
            st = self.state.setdefault(k, {"w": None, "r": []})
            st["r"] = [r for r in st["r"] if r[0].num != ev[0].num] + [ev]
        for k in writes:
            self.state[k] = {"w": ev, "r": []}

    def op(self, eng, fn, reads=(), writes=()):
        self._deps(eng, reads, writes, skip_self=(eng == "pe"))
        ins = fn(self.engs[eng])
        self.ccnt[eng] += 1
        ins.then_inc(self.csem[eng], 1)
        ev = (self.csem[eng], self.ccnt[eng])
        self._commit(ev, reads, writes)
        self.n_inst += 1
        return ev

    def dma(self, q, fn, reads=(), writes=(), is_output=False):
        self._deps(q, reads, writes)
        pool = self.dpool[q]
        i = pool[self.dnext[q]]
        self.dnext[q] = (self.dnext[q] + 1) % len(pool)
        sem = self.dsems[i]
        if self.dcnt[i] > 0:
            self._need(q, (sem, 16 * self.dcnt[i]))
        ins = fn(self.engs[q])
        self.dcnt[i] += 1
        ins.then_inc(sem, 16)
        ev = (sem, 16 * self.dcnt[i])
        self._commit(ev, reads, writes)
        if is_output:
            self.out_events.append(ev)
        self.n_inst += 1
        return ev

    def barrier(self):
        for eng in self.engs:
            for i, sem in enumerate(self.dsems):
                if self.dcnt[i] > 0:
                    self._need(eng, (sem, 16 * self.dcnt[i]))
            for e, sem in self.csem.items():
                if self.ccnt[e] > 0:
                    self._need(eng, (sem, self.ccnt[e]))

    def finish(self, eng="sp"):
        for i, sem in enumerate(self.dsems):
            if self.dcnt[i] > 0:
                self._need(eng, (sem, 16 * self.dcnt[i]))
        for e, sem in self.csem.items():
            if self.ccnt[e] > 0:
                self._need(eng, (sem, self.ccnt[e]))


class Scope:
    def __init__(self, builder):
        from contextlib import ExitStack
        self.b = builder
        self.st = ExitStack()

    def sb(self, name, shape, dtype=F32):
        return self.st.enter_context(self.b.nc.sbuf_tensor(name, list(shape), dtype))

    def close(self):
        self.b.S.barrier()
        self.st.close()


class Ring:
    def __init__(self, sc, name, shape, dtype, n):
        self.t = [sc.sb("%s%d" % (name, i), shape, dtype) for i in range(n)]
        self.k = [("%s" % name, i) for i in range(n)]
        self.i = 0

    def next(self):
        r = (self.t[self.i], self.k[self.i])
        self.i = (self.i + 1) % len(self.t)
        return r


class Builder:
    def __init__(self, stage=99, debug=False, n_exp=None, start_layer=0, n_tiles=None):
        self.n_exp = n_exp
        self.start_layer = start_layer
        self.n_tiles = n_tiles
        self.stage = stage
        self.debug = debug
        nc = bass.Bass("TRN2", target_bir_lowering=False)
        self.nc = nc
        self.S = Sched(nc)
        self.inp = {}
        self.ps = [nc.alloc_psum_tensor("psb%d" % i, [128, 512], F32) for i in range(8)]
        self.psk = [("ps", i) for i in range(8)]

    def din(self, name, shape, dtype=F32):
        t = self.nc.dram_tensor(name, list(shape), dtype, kind="ExternalInput").ap()
        self.inp[name] = t
        return t

    def dscratch(self, name, shape, dtype=F32, out=False):
        kind = "ExternalOutput" if (out or self.debug) else "Internal"
        return self.nc.dram_tensor(name, list(shape), dtype, kind=kind).ap()

    def sb(self, name, shape, dtype=F32):
        return self.nc.alloc_sbuf_tensor(name, list(shape), dtype)

    def mm(self, out, lhsT, rhs, start, stop, reads, writes):
        return self.S.op("pe", lambda e: e.matmul(out, lhsT=lhsT, rhs=rhs, start=start, stop=stop),
                         reads, writes)

    def tr(self, out, in_, reads, writes, kp=128):
        ident = self.cst[0:kp, C_ID:C_ID + kp]
        return self.S.op("pe", lambda e: e.transpose(out, in_, ident), list(reads) + ["cst"], writes)

    def act(self, out, in_, func, reads, writes, **kw):
        return self.S.op("act", lambda e: e.activation(out=out, in_=in_, func=func, **kw), reads, writes)

    def tt(self, eng, out, in0, in1, op, reads, writes):
        return self.S.op(eng, lambda e: e.tensor_tensor(out=out, in0=in0, in1=in1, op=op), reads, writes)

    def ts(self, eng, out, in0, s1, s2, op0, op1, reads, writes, **kw):
        if s2 is None:
            return self.S.op(eng, lambda e: e.tensor_scalar(out=out, in0=in0, scalar1=s1, scalar2=None,
                                                            op0=op0, **kw), reads, writes)
        return self.S.op(eng, lambda e: e.tensor_scalar(out=out, in0=in0, scalar1=s1, scalar2=s2,
                                                        op0=op0, op1=op1, **kw), reads, writes)

    def stt(self, eng, out, in0, scalar, in1, op0, op1, reads, writes):
        return self.S.op(eng, lambda e: e.scalar_tensor_tensor(out=out, in0=in0, scalar=scalar, in1=in1,
                                                               op0=op0, op1=op1), reads, writes)

    def cp(self, eng, out, in_, reads, writes):
        if eng == "act":
            return self.act(out, in_, AF.Copy, reads, writes)
        return self.S.op(eng, lambda e: e.tensor_copy(out=out, in_=in_), reads, writes)

    def ld(self, out, in_, reads, writes, q="sp"):
        return self.S.dma(q, lambda e: e.dma_start(out=out, in_=in_), reads, writes)

    def rstd_of(self, x_ap, xk, junk, junkk, ss, ssk):
        self.act(junk, x_ap, AF.Square, [xk], [junkk, ssk], accum_out=ss)
        self.ts("dve", ss, ss, 1.0 / D, EPS, ALU.mult, ALU.add, [ssk], [ssk])
        self.act(ss, ss, AF.Sqrt, [ssk], [ssk])
        self.S.op("dve", lambda e: e.reciprocal(out=ss, in_=ss), [ssk], [ssk])

    def build(self):
        nc, S = self.nc, self.S
        st, sl = self.stage, self.start_layer
        x = self.din("x", [NLAT, D]) if sl == 0 else None
        ctx = self.din("ctx", [NCTX, D]) if sl == 0 else None
        cvecT = self.din("cvecT", [128, 8, 2])
        cpack = self.din("cpack", [128, C_END])
        rope = self.din("rope", [NLAT, 64]) if sl == 0 else None
        W = {}
        for l in (0, 1):
            if l == 0 and sl > 0:
                continue
            if l == 1 and st < 30:
                continue
            W[l] = dict(
                ada_w=self.din("l%d_ada_w" % l, [D, 6 * D]),
                ada_b=self.din("l%d_ada_b" % l, [6 * D]),
                ada_bT=self.din("l%d_ada_bT" % l, [128, 48]),
                nmixT=self.din("l%d_nmixT" % l, [128, 8]),
                nffnT=self.din("l%d_nffnT" % l, [128, 8]),
                router=self.din("l%d_router" % l, [D, NE]),
            )
            if (l == 0 and st >= 2) or (l == 1 and st >= 40):
                W[l].update(w_gate=self.din("l%d_w_gate" % l, [NE, D, FF]),
                            w_up=self.din("l%d_w_up" % l, [NE, D, FF]),
                            w_down=self.din("l%d_w_down" % l, [NE, FF, D]))
        if 0 in W:
            W[0].update(w_qkv=self.din("l0_w_qkv", [D, 1536]), sink=self.din("l0_sink", [16]),
                        w_o=self.din("l0_w_o", [D, D]))
        if 1 in W:
            W[1].update(w_in=self.din("l1_w_in", [D, 4128]), convT=self.din("l1_convT", [128, 24, 3]),
                        a_log=self.din("l1_a_log", [16]), dt_bias=self.din("l1_dt_bias", [16]),
                        o_norm=self.din("l1_o_norm", [128]), w_o=self.din("l1_w_o", [D, D]))
            cpack2 = self.din("cpack2", [128, C2_END])
        if st >= 50:
            self.din("final_norm", [D])
        self.W = W
        self.x, self.ctx, self.rope = x, ctx, rope
        self.out = self.nc.dram_tensor("out", [NLAT, D], F32, kind="ExternalOutput").ap()
        self.qs = self.dscratch("qs", [NTOK, D])
        if sl == 0:
            xa = self.dscratch("xresA", [NTOK + NDUMMY, D])
        else:
            xa = self.din("xresA_in", [NTOK + NDUMMY, D])
        self.xres = [xa, self.dscratch("xresB", [NTOK + NDUMMY, D])]
        self.xn2 = self.dscratch("xn2", [NTOK + NDUMMY, D])

        self.cst = self.sb("cst", [128, C_END])
        self.ld(self.cst[:], cpack, [], ["cst"])
        self.scT = self.sb("scT", [128, 8, 2], F32R)
        self.cols = self.sb("cols", [128, 48, 2])
        self.A1 = self.sb("A1", [128, 8, 2]); self.A2 = self.sb("A2", [128, 8, 2])
        self.Gbc = self.sb("Gbc", [128, 2, 2, D])
        self.aff = self.sb("aff", [128, NT, NE])
        if self.debug:
            S.op("dve", lambda e: e.memset(self.aff[:], 0.0), [], [("aff", i) for i in range(NT)])
        sc0 = Scope(self)
        zero_t = sc0.sb("zero_t", [128, D])
        S.op("dve", lambda e: e.memset(zero_t[:], 0.0), [], ["zero_t"])
        bufs = [(self.xres[1], "xres1"), (self.xn2, "xn2")] + ([(self.xres[0], "xres0")] if sl == 0 else [])
        for buf, k in bufs:
            for r in range(NDUMMY // 128):
                self.ld(buf[NTOK + r * 128: NTOK + (r + 1) * 128, :], zero_t[:], ["zero_t"], [(k, "dummy", r)])
        sc0.close()
        if sl == 0:
            self.modulation(0)
            if st >= 1:
                self.attention_layer()
            if st >= 2:
                self.moe(0, self.xres[0], "xres0", with_ctx=True)
        if st >= 30:
            self.cst2 = self.sb("cst2", [128, C2_END])
            self.ld(self.cst2[:], cpack2, [], ["cst2"])
            self.modulation(1)
            self.deltanet_layer()
        if st >= 40:
            self.moe(1, self.xres[1], "xres1", with_ctx=False)
        if st >= 50:
            self.final_norm()
        if self.debug:
            d_aff = self.nc.dram_tensor("d_aff", [128, NT * NE], F32, kind="ExternalOutput").ap()
            self.ld(d_aff, self.aff[:, :, :].rearrange("p j e -> p (j e)"), [("aff", i) for i in range(NT)], ["d_aff"])
            d_cols = self.nc.dram_tensor("d_cols", [128, 96], F32, kind="ExternalOutput").ap()
            self.ld(d_cols, self.cols[:, :, :].rearrange("p c t -> p (c t)"), ["cols"], ["d_cols"])
            d_g = self.nc.dram_tensor("d_g", [128, 4 * D], F32, kind="ExternalOutput").ap()
            self.ld(d_g, self.Gbc[:, :, :, :].rearrange("p a b d -> p (a b d)"),
                    [("Gbc", a, b) for a in range(2) for b in range(2)], ["d_g"])
        S.finish()
        return nc

    def modulation(self, l):
        nc, S, W = self.nc, self.S, self.W[l]
        sfx = "m%d" % l
        sc = Scope(self)
        self.wring = Ring(sc, "wst" + sfx, [128, 8, 512], F32R, 4)
        cv = sc.sb("cv" + sfx, [128, 8, 2])
        self.ld(cv[:], self.inp["cvecT"], [], ["cv"])
        self.act(self.scT[:], cv[:], AF.Silu, ["cv"], ["scT"])
        screp = [sc.sb("screp%d%s" % (c, sfx), [128, 8, 128], F32R) for c in range(2)]
        for c in range(2):
            self.cp("dve", screp[c][:], self.scT[:, :, c:c + 1].to_broadcast([128, 8, 128]), ["scT"], [("screp", c)])
        abT = sc.sb("abT" + sfx, [128, 48])
        self.ld(abT[:], W["ada_bT"], [], ["abT"])
        nmix = sc.sb("nmix" + sfx, [128, 8]); nffn = sc.sb("nffn" + sfx, [128, 8])
        self.ld(nmix[:], W["nmixT"], [], ["nmix"])
        self.ld(nffn[:], W["nffnT"], [], ["nffn"])
        abbc = sc.sb("abbc" + sfx, [128, 2, D])
        self.ld(abbc[:, 0, :], W["ada_b"][2 * D:3 * D].partition_broadcast(128), [], [("abbc", 0)])
        self.ld(abbc[:, 1, :], W["ada_b"][5 * D:6 * D].partition_broadcast(128), [], [("abbc", 1)])
        pcol = self.ps[0]
        pcv = pcol[:, 0:96].rearrange("p (c t) -> p c t", t=2)
        for cg in range(12):
            wt, wk = self.wring.next()
            self.ld(wt[:], W["ada_w"][:, cg * 512:(cg + 1) * 512].rearrange("(k p) n -> p k n", p=128),
                    [], [wk], q="pool")
            for c4 in range(4):
                cc = cg * 4 + c4
                for kc in range(8):
                    self.mm(pcv[:, cc, :], wt[:, kc, c4 * 128:(c4 + 1) * 128], self.scT[:, kc, :],
                            kc == 0, kc == 7, [wk, "scT"], [self.psk[0]])
            if cg in (4, 5, 10, 11):
                gi = 0 if cg < 6 else 1
                half = cg % 2
                for c in range(2):
                    pb, pbk = self.ps[1 + c], self.psk[1 + c]
                    for kc in range(8):
                        self.mm(pb[:, :], screp[c][:, kc, :], wt[:, kc, :], kc == 0, kc == 7,
                                [wk, ("screp", c)], [pbk])
                    self.tt("dve", self.Gbc[:, gi, c, half * 512:(half + 1) * 512], pb[:, :],
                            abbc[:, gi, half * 512:(half + 1) * 512], ALU.add,
                            [pbk, ("abbc", gi)], [("Gbc", gi, c)])
        self.tt("dve", self.cols[:], pcv, abT[:].unsqueeze(2).to_broadcast([128, 48, 2]), ALU.add,
                [self.psk[0], "abT"], ["cols"])
        for (A, nrm, nk, v) in ((self.A1, nmix, "nmix", 1), (self.A2, nffn, "nffn", 4)):
            self.stt("dve", A[:], self.cols[:, v * 8:(v + 1) * 8, :], 1.0,
                     nrm[:].unsqueeze(2).to_broadcast([128, 8, 2]), ALU.add, ALU.mult,
                     ["cols", nk], [("A", v)])
        sc.close()

    def B1(self, kc, c):
        return self.cols[:, 0 + kc, c:c + 1]

    def B2(self, kc, c):
        return self.cols[:, 24 + kc, c:c + 1]

    def attention_layer(self):
        nc, S, W = self.nc, self.S, self.W[0]
        cst = self.cst
        sca = Scope(self)
        KT = sca.sb("KT", [128, 2, NTOK], F32R)
        V = sca.sb("Vaug", [128, NT, 4, 66], F32R)
        esink = sca.sb("esink", [128, 16])
        wr = sca.sb("wr", [128, 8, NE])
        xr = Ring(sca, "xt", [128, D], F32, 2)
        xnr = Ring(sca, "xnb", [128, D], F32, 3)
        ssr = Ring(sca, "ss", [128, 1], F32, 6)
        junk = sca.sb("junk", [128, D])
        sc1 = Scope(self)
        wbig = sc1.sb("wbig", [128, 8, 1536], F32R)
        hTr = Ring(sc1, "hT", [128, 8, 128], F32R, 3)
        qkr = Ring(sc1, "qk", [128, 1280], F32, 2)
        csr = Ring(sc1, "cs", [128, 64], F32, 6)
        tmpr = Ring(sc1, "rt", [128, 4, 256], F32, 1)
        for cg in range(3):
            self.ld(wbig[:, :, cg * 512:(cg + 1) * 512],
                    W["w_qkv"][:, cg * 512:(cg + 1) * 512].rearrange("(k p) n -> p k n", p=128),
                    [], [("wbig", cg)], q="pool")
        Vf = V[:, :, :, :].rearrange("p j h c -> p (j h) c")
        self.cp("dve", Vf[:, :, 64:65], self.cst[:, C_ONES:C_ONES + 1].unsqueeze(1).to_broadcast([128, NT * 4, 1]), ["cst"], [("V1",)])
        self.cp("dve", Vf[:, :, 65:66], self.cst[:, C_U:C_U + 1].unsqueeze(1).to_broadcast([128, NT * 4, 1]), ["cst"], [("V0",)])
        self.ld(esink[:], W["sink"].partition_broadcast(128), [], ["esink"])
        self.act(esink[:], esink[:], AF.Exp, ["esink"], ["esink"])
        self.ld(wr[:], W["router"].rearrange("(k p) n -> p k n", p=128), [], ["wr"])

        def src_rows(j):
            if j < NT_LAT:
                return self.x[j * 128:(j + 1) * 128, :]
            return self.ctx[(j - NT_LAT) * 128:(j - NT_LAT + 1) * 128, :]

        cx = [dict() for _ in range(NT)]

        def p1_s0(j):
            c = cx[j]
            c["c"] = 0 if j < NT_LAT else 1
            xt, xk = xr.next()
            self.ld(xt[:], src_rows(j), [], [xk])
            if c["c"] == 0:
                c["cs"], c["ck"] = csr.next()
                self.ld(c["cs"][:], self.rope[j * 128:(j + 1) * 128, :], [], [c["ck"]])
            ss, ssk = ssr.next()
            self.rstd_of(xt[:], xk, junk[:], "junk", ss[:], ssk)
            c["xn"], c["xnk"] = xnr.next()
            self.ts("dve", c["xn"][:], xt[:], ss[:, 0:1], None, ALU.mult, None, [xk, ssk], [c["xnk"]])

        def p1_s1(j):
            c = cx[j]
            xn, xnk, cc = c["xn"], c["xnk"], c["c"]
            c["hT"], c["hk"] = hTr.next()
            hT, hk = c["hT"], c["hk"]
            for kc in range(8):
                pt, ptk = self.ps[kc // 4], self.psk[kc // 4]
                self.tr(pt[:, (kc % 4) * 128:(kc % 4 + 1) * 128], xn[:, kc * 128:(kc + 1) * 128], [xnk], [ptk])
            for kc in range(8):
                pt, ptk = self.ps[kc // 4], self.psk[kc // 4]
                self.act(hT[:, kc, :], pt[:, (kc % 4) * 128:(kc % 4 + 1) * 128], AF.Identity,
                         [ptk, ("A", 1), "cols"], [hk], scale=self.A1[:, kc, cc:cc + 1], bias=self.B1(kc, cc))

        def p1_s2(j):
            c = cx[j]
            hT, hk = c["hT"], c["hk"]
            c["pb"] = 2 + 3 * (j % 2)
            for cg in range(3):
                pq, pqk = self.ps[c["pb"] + cg], self.psk[c["pb"] + cg]
                for kc in range(8):
                    self.mm(pq[:, :], hT[:, kc, :], wbig[:, kc, cg * 512:(cg + 1) * 512], kc == 0, kc == 7,
                            [hk, ("wbig", cg)], [pqk])

        def p1_s3(j):
            c = cx[j]
            pb = c["pb"]
            c["qk"], c["qkk"] = qkr.next()
            qk, qkk = c["qk"], c["qkk"]
            if c["c"] == 0:
                cs, ck = c["cs"], c["ck"]
                cosb = lambda nh: cs[:, 0:32].rearrange("p (a f) -> p a f", a=2).unsqueeze(1).to_broadcast([128, nh, 2, 16])
                sinb = lambda nh: cs[:, 32:64].rearrange("p (a f) -> p a f", a=2).unsqueeze(1).to_broadcast([128, nh, 2, 16])
                for cg in range(3):
                    nh = 8 if cg < 2 else 4
                    pq, pqk = self.ps[pb + cg], self.psk[pb + cg]
                    pv = pq[:, 0:nh * 64].rearrange("p (h a b f) -> p h a b f", h=nh, a=2, b=2, f=16)
                    ov = qk[:, cg * 512:cg * 512 + nh * 64].rearrange("p (h a b f) -> p h a b f", h=nh, a=2, b=2, f=16)
                    x1, x2 = pv[:, :, :, 0, :], pv[:, :, :, 1, :]
                    tm, tmk = tmpr.next()
                    t = [tm[:, i, 0:nh * 32].rearrange("p (h a f) -> p h a f", h=nh, a=2, f=16) for i in range(4)]
                    self.tt("dve", t[0], x1, cosb(nh), ALU.mult, [pqk, ck], [tmk])
                    self.tt("dve", t[1], x2, sinb(nh), ALU.mult, [pqk, ck], [tmk])
                    self.tt("dve", t[2], x2, cosb(nh), ALU.mult, [pqk, ck], [tmk])
                    self.tt("dve", t[3], x1, sinb(nh), ALU.mult, [pqk, ck], [tmk])
                    self.tt("pool", ov[:, :, :, 0, :], t[0], t[1], ALU.subtract, [tmk], [qkk])
                    self.tt("pool", ov[:, :, :, 1, :], t[2], t[3], ALU.add, [tmk], [qkk])
            else:
                for cg in range(3):
                    ncol = 512 if cg < 2 else 256
                    self.cp("act", qk[:, cg * 512:cg * 512 + ncol], self.ps[pb + cg][:, 0:ncol], [self.psk[pb + cg]], [qkk])
            self.cp("act", V[:, j, :, 0:64], self.ps[pb + 2][:, 256:512].rearrange("p (h d) -> p h d", h=4),
                    [self.psk[pb + 2]], [("V", j)])
            self.ld(self.qs[j * 128:(j + 1) * 128, :], qk[:, 0:1024], [qkk], [("qs", j)])

        def p1_s4(j):
            c = cx[j]
            qk, qkk = c["qk"], c["qkk"]
            for pr in range(2):
                self.tr(self.ps[1][:, pr * 128:(pr + 1) * 128], qk[:, 1024 + pr * 128:1024 + (pr + 1) * 128], [qkk], [self.psk[1]])
            self.cp("act", KT[:, :, j * 128:(j + 1) * 128], self.ps[1][:, 0:256].rearrange("p (a t) -> p a t", a=2),
                    [self.psk[1]], [("KT", j)])

        stages = [p1_s0, p1_s1, p1_s2, p1_s3, p1_s4]
        for t_ in range(NT + len(stages) - 1):
            for st_ in range(len(stages) - 1, -1, -1):
                i_ = t_ - st_
                if 0 <= i_ < NT:
                    stages[st_](i_)

        sc1.close()
        sca_outer, sca = sca, Scope(self)
        wo = sca.sb("wo", [128, 8, 1024], F32R)
        self.ld(wo[:], W["w_o"].rearrange("(k p) n -> p k n", p=128), [], ["wo"], q="pool")
        wok = ["wo"]
        QTr = Ring(sca, "QT", [128, 2, 4, 128], F32R, 2)
        PTr = Ring(sca, "PT", [128, 5, 512], F32R, 2)
        osb = sca.sb("osb", [128, 16, 64])
        otsr = Ring(sca, "ots", [66, 512], F32, 1)
        oT = sca.sb("oT", [128, 8, 128], F32R)
        den = sca.sb("den", [128, 16])
        xmr = Ring(sca, "xm", [128, D], F32, 3)
        h2T = sca.sb("h2T", [128, 8, 128])
        lg = sca.sb("lg", [128, NE]); mx = sca.sb("mx", [128, 1]); sm = sca.sb("sm", [128, 1])
        pend = [None]
        ps_i = [0]

        def p2_loads(i_):
            qt_, qtk_ = xr.next()
            self.ld(qt_[:], self.qs[i_ * 128:(i_ + 1) * 128, :], [("qs", i_)], [qtk_])
            xt_, xk_ = xnr.next()
            self.ld(xt_[:], src_rows(i_), [], [xk_])
            return qt_, qtk_, xt_, xk_

        nxt = p2_loads(0)
        for i in range(NT):
            c = 0 if i < NT_LAT else 1
            qt, qtk, xt, xk = nxt
            if i + 1 < NT:
                nxt = p2_loads(i + 1)
            QT, QTk = QTr.next()
            for pr in range(2):
                for g in range(4):
                    self.tr(self.ps[pr][:, g * 128:(g + 1) * 128], qt[:, (pr * 4 + g) * 128:(pr * 4 + g + 1) * 128], [qtk], [self.psk[pr]])
                self.cp("act", QT[:, pr, :, :], self.ps[pr][:, :].rearrange("p (g t) -> p g t", g=4), [self.psk[pr]], [QTk])
            if c == 0:
                kbs = ([i - 1] if i > 0 else []) + [i] + ([i + 1] if i < NT_LAT - 1 else []) + [32, 33]
                kmask = ([C_MP] if i > 0 else []) + [None] + ([C_MN] if i < NT_LAT - 1 else []) + [None, None]
            else:
                kbs = [32, 33]
                kmask = [None, None]
            if pend[0] is not None:
                self.moe_prep_a(*pend[0], skip0=True)

            def st_phase(kvh):
                pr, base = kvh // 2, (kvh % 2) * 64
                PT, PTk = PTr.next()
                for kbi, kb in enumerate(kbs):
                    sbk = (2, 3, 6, 7)[ps_i[0] % 4]
                    ps_i[0] += 1
                    pS, pSk = self.ps[sbk], self.psk[sbk]
                    self.mm(pS[:, :], KT[base:base + 64, pr, kb * 128:(kb + 1) * 128],
                            QT[base:base + 64, pr, :, :], True, True, [("KT", kb), QTk], [pSk])
                    self.act(PT[:, kbi, :], pS[:, :], AF.Exp, [pSk], [PTk], scale=0.125)
                    if kmask[kbi] is not None:
                        mo = kmask[kbi]
                        self.tt("pool", PT[:, kbi, :], PT[:, kbi, :], cst[:, mo:mo + 512], ALU.mult, [PTk, "cst"], [PTk])
                return PT, PTk

            def pv_phase(kvh, PT, PTk):
                pO, pOk = self.ps[4 + kvh % 2], self.psk[4 + kvh % 2]
                for kbi, kb in enumerate(kbs):
                    self.mm(pO[0:66, :], V[:, kb, kvh, :], PT[:, kbi, :], kbi == 0, kbi == len(kbs) - 1,
                            [PTk, ("V", kb), ("V1",), ("V0",)], [pOk])
                ots, otsk = otsr.next()
                self.cp("act", ots[0:66, :], pO[0:66, :], [pOk], [otsk])
                for g in range(4):
                    self.tr(pO[:, g * 128:g * 128 + 66], ots[0:66, g * 128:(g + 1) * 128], [otsk], [pOk], kp=66)
                pOv = pO[:, :].rearrange("p (g t) -> p g t", g=4)
                dk_ = ("den", kvh)
                self.tt("dve", den[:, kvh * 4:(kvh + 1) * 4], pOv[:, :, 64], esink[:, kvh * 4:(kvh + 1) * 4], ALU.add,
                        [pOk, "esink"], [dk_])
                self.S.op("dve", lambda e: e.reciprocal(out=den[:, kvh * 4:(kvh + 1) * 4], in_=den[:, kvh * 4:(kvh + 1) * 4]), [dk_], [dk_])
                self.tt("dve", osb[:, kvh * 4:(kvh + 1) * 4, :], pOv[:, :, 0:64],
                        den[:, kvh * 4:(kvh + 1) * 4].unsqueeze(2).to_broadcast([128, 4, 64]), ALU.mult,
                        [pOk, dk_], [("osb", kvh)])

            pts = {0: st_phase(0)}
            for kvh in range(4):
                if kvh + 1 < 4:
                    pts[kvh + 1] = st_phase(kvh + 1)
                if kvh == 1 and pend[0] is not None:
                    self.moe_prep_b(*pend[0])
                    pend[0] = None
                pv_phase(kvh, *pts[kvh])
            osf = osb[:, :, :].rearrange("p h d -> p (h d)")
            for kc in range(8):
                self.tr(self.ps[kc // 4][:, (kc % 4) * 128:(kc % 4 + 1) * 128], osf[:, kc * 128:(kc + 1) * 128], [("osb", k_) for k_ in range(4)], [self.psk[kc // 4]])
            for b in range(2):
                self.cp("act", oT[:, b * 4:(b + 1) * 4, :], self.ps[b][:, :].rearrange("p (k t) -> p k t", k=4), [self.psk[b]], ["oT"])
            xm, xmk = xmr.next()
            for dh in range(2):
                pM, pMk = self.ps[2 + dh], self.psk[2 + dh]
                for kc in range(8):
                    self.mm(pM[:, :], oT[:, kc, :], wo[:, kc, dh * 512:(dh + 1) * 512], kc == 0, kc == 7, ["oT"] + wok, [pMk])
                self.tt("dve", xm[:, dh * 512:(dh + 1) * 512], pM[:, :], self.Gbc[:, 0, c, dh * 512:(dh + 1) * 512], ALU.mult,
                        [pMk, ("Gbc", 0, c)], [xmk])
            self.tt("dve", xm[:], xm[:], xt[:], ALU.add, [xmk, xk], [xmk])
            self.ld(self.xres[0][i * 128:(i + 1) * 128, :], xm[:], [xmk], [("xres0", i)])
            pend[0] = (i, c, xm, xmk, junk, ssr, wr, h2T, lg, mx, sm)
            self.moe_prep_a0(*pend[0])
        self.moe_prep_a(*pend[0], skip0=True)
        self.moe_prep_b(*pend[0])
        sca.close()
        sca_outer.close()

    def moe_prep_a0(self, i, c, xm, xmk, junk, ssr, wr, h2T, lg, mx, sm):
        ss, ssk = ssr.next()
        self.rstd_of(xm[:], xmk, junk[:], "junk", ss[:], ssk)
        self.ts("dve", junk[:], xm[:], ss[:, 0:1], None, ALU.mult, None, [xmk, ssk], ["junk"])
        self.ld(self.xn2[i * 128:(i + 1) * 128, :], junk[:], ["junk"], [("xn2", i)])

    def moe_prep_a(self, i, c, xm, xmk, junk, ssr, wr, h2T, lg, mx, sm, skip0=False):
        if not skip0:
            self.moe_prep_a0(i, c, xm, xmk, junk, ssr, wr, h2T, lg, mx, sm)
        for kc in range(8):
            self.tr(self.ps[kc // 4][:, (kc % 4) * 128:(kc % 4 + 1) * 128], junk[:, kc * 128:(kc + 1) * 128], ["junk"], [self.psk[kc // 4]])
        for kc in range(8):
            self.act(h2T[:, kc, :], self.ps[kc // 4][:, (kc % 4) * 128:(kc % 4 + 1) * 128], AF.Identity,
                     [self.psk[kc // 4], ("A", 4), "cols"], ["h2T"], scale=self.A2[:, kc, c:c + 1], bias=self.B2(kc, c))

    def moe_prep_b(self, i, c, xm, xmk, junk, ssr, wr, h2T, lg, mx, sm):
        pL, pLk = self.ps[1], self.psk[1]
        for kc in range(8):
            self.mm(pL[:, 0:NE], h2T[:, kc, :], wr[:, kc, :], kc == 0, kc == 7, ["h2T", "wr"], [pLk])
        self.S.op("dve", lambda e: e.reduce_max(out=mx[:], in_=pL[:, 0:NE], axis=AX.X), [pLk], ["mx"])
        self.ts("dve", mx[:], mx[:], -1.0, None, ALU.mult, None, ["mx"], ["mx"])
        self.act(lg[:], pL[:, 0:NE], AF.Exp, [pLk, "mx"], ["lg", "sm"], bias=mx[:, 0:1], accum_out=sm[:])
        self.S.op("dve", lambda e: e.reciprocal(out=sm[:], in_=sm[:]), ["sm"], ["sm"])
        self.ts("dve", self.aff[:, i, :], lg[:], sm[:, 0:1], None, ALU.mult, None, ["lg", "sm"], [("aff", i)])

    def moe_prep(self, *args):
        self.moe_prep_a(*args)
        self.moe_prep_b(*args)

    def pk(self, b, h):
        return ("ps", b)

    def deltanet_layer(self):
        nc, S, W = self.nc, self.S, self.W[1]
        cst, c2 = self.cst, self.cst2
        xin = self.xres[0]
        qT_d = self.dscratch("qT_d", [D, NTOK]); kT_d = self.dscratch("kT_d", [D, NTOK])
        ktok_d = self.dscratch("ktok_d", [NTOK, D]); vtok_d = self.dscratch("vtok_d", [NTOK, D])
        sz_d = self.dscratch("sz_d", [NTOK, D]); of_d = self.dscratch("of_d", [NLAT, D])
        self.dn_dbg = dict(qT_d=qT_d, kT_d=kT_d, ktok_d=ktok_d, vtok_d=vtok_d, sz_d=sz_d, of_d=of_d)
        ident = cst[:, C_ID:C_ID + 128]
        ones = cst[:, C_ONES:C_ONES + 128]
        zcol = cst[:, C_U:C_U + 1]
        scL = Scope(self)
        g_all = scL.sb("g_all", [128, NT, 16]); beta_all = scL.sb("beta_all", [128, NT, 16])
        lnb_all = scL.sb("lnb_all", [128, NT, 16]); ab_all = scL.sb("ab_all", [128, NT, 32])
        onesR = scL.sb("onesR", [128, 128], F32R)
        self.cp("dve", onesR[:], ones, ["cst"], ["onesR"])
        convT = scL.sb("convT", [128, 24, 3])
        self.ld(convT[:], W["convT"], [], ["convT"])
        allps = [self.pk(b, h) for b in range(8) for h in range(2)]

        def bankk(b):
            return [self.pk(b, 0), self.pk(b, 1)]

        scp = Scope(self)
        win = scp.sb("winqkv", [128, 8, 3072], F32R)
        wink = [("win", i) for i in range(6)]
        for i in range(6):
            self.ld(win[:, :, i * 512:(i + 1) * 512], W["w_in"][:, i * 512:(i + 1) * 512].rearrange("(k p) n -> p k n", p=128),
                    [], [wink[i]], q="pool")
        wnd = [scp.sb("wnd%d" % i, [128, 8, 258], F32R) for i in range(2)]
        xr = Ring(scp, "pxt", [128, D], F32, 4)
        ssr = Ring(scp, "pss", [128, 1], F32, 4)
        junk = scp.sb("pjunk", [128, D])
        c1r = Ring(scp, "pc1", [128, 256], F32, 3)
        sr = Ring(scp, "psl", [128, 256], F32, 8)
        sqr = Ring(scp, "psq", [128, 256], F32R, 3)
        rnr = Ring(scp, "prn", [128, 256], F32, 4)
        qnr = Ring(scp, "pqn", [128, 256], F32, 4)
        tkr = Ring(scp, "ptk", [128, 128], F32, 6)
        groups = [[32, 33]] + [[2 * g, 2 * g + 1] for g in range(16)]

        def xload(jj, xr_):
            xt, xk = xr_.next()
            self.ld(xt[:], xin[jj * 128:(jj + 1) * 128, :], [], [xk])
            return xt, xk

        def tile_hT(jj, dst_fn, xr_, ssr_, junk_, pre=None):
            c = 0 if jj < NT_LAT else 1
            xt, xk = pre if pre is not None else xload(jj, xr_)
            ss, ssk = ssr_.next()
            self.rstd_of(xt[:], xk, junk_[:], "pjunk", ss[:], ssk)
            self.ts("dve", xt[:], xt[:], ss[:, 0:1], None, ALU.mult, None, [xk, ssk], [xk])
            for kc in range(8):
                b = kc // 4
                self.tr(self.ps[b][:, (kc % 4) * 128:(kc % 4 + 1) * 128], xt[:, kc * 128:(kc + 1) * 128], [xk], bankk(b))
            for kc in range(8):
                b = kc // 4
                dst, dk = dst_fn(kc)
                self.act(dst, self.ps[b][:, (kc % 4) * 128:(kc % 4 + 1) * 128], AF.Identity,
                         bankk(b) + [("A", 1), "cols"], [dk], scale=self.A1[:, kc, c:c + 1], bias=self.B1(kc, c))

        pw_pre = {}

        def pw_loads(gi):
            if gi < len(groups):
                pw_pre[gi] = [xload(jj, xr) for jj in groups[gi]]

        pw_loads(0)

        def prep_window(gi):
            buf = gi % 2
            pw_loads(gi + 1)
            for ti, jj in enumerate(groups[gi]):
                tile_hT(jj, lambda kc: (wnd[buf][:, kc, 1 + 128 * ti:1 + 128 * (ti + 1)], ("wnd", buf)), xr, ssr, junk,
                        pre=pw_pre[gi][ti])
            same_prev = gi >= 2
            if same_prev:
                self.cp("dve", wnd[buf][:, :, 0:1], wnd[1 - buf][:, :, 256:257], [("wnd", 1 - buf)], [("wnd", buf)])
                self.cp("dve", wnd[1 - buf][:, :, 257:258], wnd[buf][:, :, 1:2], [("wnd", buf)], [("wnd", 1 - buf)])
            else:
                self.cp("dve", wnd[buf][:, :, 0:1], zcol.unsqueeze(1).to_broadcast([128, 8, 1]), ["cst"], [("wnd", buf)])
                if gi >= 1:
                    self.cp("dve", wnd[1 - buf][:, :, 257:258], zcol.unsqueeze(1).to_broadcast([128, 8, 1]), ["cst"], [("wnd", 1 - buf)])

        pb_i = [0]
        pn_i = [0]

        def skew(n_items, stages):
            k = len(stages)
            for t in range(n_items + k - 1):
                for st_ in range(k - 1, -1, -1):
                    i_ = t - st_
                    if 0 <= i_ < n_items:
                        stages[st_](i_)

        def project(gi):
            buf = gi % 2
            wv, wvk = wnd[buf], ("wnd", buf)
            tok0 = groups[gi][0] * 128
            ctxs = [dict() for _ in range(24)]

            def s0(ch):
                c = ctxs[ch]
                b = pb_i[0] % 3
                pb_i[0] += 1
                c["pb"] = 2 + b
                pP = self.ps[2 + b]
                for kc in range(8):
                    self.mm(pP[:, 0:258], win[:, kc, ch * 128:(ch + 1) * 128], wv[:, kc, 0:258], kc == 0, kc == 7,
                            [wink[ch // 4], wvk], bankk(2 + b))

            def s1(ch):
                c = ctxs[ch]
                pb = c["pb"]
                pP = self.ps[pb]
                c1, c1k = c1r.next()
                self.ts("dve", c1[:], pP[:, 0:256], convT[:, ch, 0:1], None, ALU.mult, None, bankk(pb) + ["convT"], [c1k])
                self.stt("dve", c1[:], pP[:, 1:257], convT[:, ch, 1:2], c1[:], ALU.mult, ALU.add, bankk(pb) + ["convT", c1k], [c1k])
                self.stt("dve", c1[:], pP[:, 2:258], convT[:, ch, 2:3], c1[:], ALU.mult, ALU.add, bankk(pb) + ["convT", c1k], [c1k])
                c["sl"], c["slk"] = sr.next()
                self.act(c["sl"][:], c1[:], AF.Silu, [c1k], [c["slk"]])

            def s2(ch):
                c = ctxs[ch]
                if ch // 8 < 2:
                    sq, sqk = sqr.next()
                    self.act(sq[:], c["sl"][:], AF.Square, [c["slk"]], [sqk])
                    nb = 5 if (pn_i[0] % 2 == 0) else 7
                    pn_i[0] += 1
                    c["nb"] = nb
                    self.mm(self.ps[nb][:, 0:256], onesR[:], sq[:], True, True, ["onesR", sqk], bankk(nb))

            def s3(ch):
                c = ctxs[ch]
                if ch // 8 < 2:
                    c["rn"], c["rnk"] = rnr.next()
                    rn, rnk = c["rn"], c["rnk"]
                    self.ts("dve", rn[:], self.ps[c["nb"]][:, 0:256], EPS, None, ALU.add, None, bankk(c["nb"]), [rnk])
                    self.act(rn[:], rn[:], AF.Sqrt, [rnk], [rnk])

            def s4(ch):
                c = ctxs[ch]
                kind, h = ch // 8, ch % 8
                if kind < 2:
                    rn, rnk = c["rn"], c["rnk"]
                    S.op("dve", lambda e: e.reciprocal(out=rn[:], in_=rn[:]), [rnk], [rnk])
                    qn, qnk = qnr.next()
                    self.stt("dve", qn[:], c["sl"][:], (128.0 ** -0.5) if kind == 0 else 1.0, rn[:], ALU.mult, ALU.mult, [c["slk"], rnk], [qnk])
                    dst = qT_d if kind == 0 else kT_d
                    self.ld(dst[h * 128:(h + 1) * 128, tok0:tok0 + 256], qn[:], [qnk], [("qkT_d", kind, gi, h)])
                    c["src"], c["srck"] = qn, qnk
                else:
                    c["src"], c["srck"] = c["sl"], c["slk"]

            def s5(ch):
                c = ctxs[ch]
                if ch // 8 >= 1:
                    for ti in range(2):
                        self.tr(self.ps[6][:, ti * 128:(ti + 1) * 128], c["src"][:, ti * 128:(ti + 1) * 128], [c["srck"]], bankk(6))

            def s6(ch):
                c = ctxs[ch]
                kind, h = ch // 8, ch % 8
                if kind >= 1:
                    dstd = ktok_d if kind == 1 else vtok_d
                    for ti in range(2):
                        tk, tkk = tkr.next()
                        self.cp("act", tk[:], self.ps[6][:, ti * 128:(ti + 1) * 128], bankk(6), [tkk])
                        self.ld(dstd[tok0 + ti * 128:tok0 + (ti + 1) * 128, h * 128:(h + 1) * 128], tk[:], [tkk], [("tok_d", kind, gi, h, ti)])

            skew(24, [s0, s1, s2, s3, s4, s5, s6])

        for gi in range(len(groups)):
            prep_window(gi)
            if gi >= 1:
                project(gi - 1)
        lastb = (len(groups) - 1) % 2
        self.cp("dve", wnd[lastb][:, :, 257:258], zcol.unsqueeze(1).to_broadcast([128, 8, 1]), ["cst"], [("wnd", lastb)])
        project(len(groups) - 1)
        scp.close()

        scz = Scope(self)
        wz = scz.sb("winz", [128, 8, 1056], F32R)
        self.ld(wz[:, :, 0:528], W["w_in"][:, 3072:3600].rearrange("(k p) n -> p k n", p=128), [], [("wz", 0)], q="pool")
        self.ld(wz[:, :, 528:1056], W["w_in"][:, 3600:4128].rearrange("(k p) n -> p k n", p=128), [], [("wz", 1)], q="pool")
        wzk = [("wz", 0), ("wz", 1)]
        xr = Ring(scz, "zxt", [128, D], F32, 2)
        ssr = Ring(scz, "zss", [128, 1], F32, 4)
        junk = scz.sb("zjunk", [128, D])
        hTr = Ring(scz, "zhT", [128, 8, 128], F32R, 2)
        zr = Ring(scz, "zst", [128, D], F32, 2)
        zpre = xload(0, xr)
        for jj in range(NT):
            hT, hk = hTr.next()
            zcur = zpre
            if jj + 1 < NT:
                zpre = xload(jj + 1, xr)
            tile_hT(jj, lambda kc: (hT[:, kc, :], hk), xr, ssr, junk, pre=zcur)
            for zh in range(2):
                for kc in range(8):
                    self.mm(self.ps[2 + zh][:, :], hT[:, kc, :], wz[:, kc, zh * 512:(zh + 1) * 512], kc == 0, kc == 7, [hk] + wzk, bankk(2 + zh))
            for kc in range(8):
                self.mm(self.ps[4][:, 0:32], hT[:, kc, :], wz[:, kc, 1024:1056], kc == 0, kc == 7, [hk] + wzk, bankk(4))
            zt, ztk = zr.next()
            for zh in range(2):
                self.act(zt[:, zh * 512:(zh + 1) * 512], self.ps[2 + zh][:, :], AF.Silu, bankk(2 + zh), [ztk])
            self.ld(sz_d[jj * 128:(jj + 1) * 128, :], zt[:], [ztk], [("sz_d", jj)])
            self.cp("dve", ab_all[:, jj, :], self.ps[4][:, 0:32], bankk(4), [("ab", jj)])
        abk = [("ab", jj) for jj in range(NT)]
        dtb = scz.sb("dtb", [128, 16]); nea = scz.sb("nea", [128, 16])
        self.ld(dtb[:], W["dt_bias"].partition_broadcast(128), [], ["dtb"])
        self.ld(nea[:], W["a_log"].partition_broadcast(128), [], ["nea"])
        self.act(nea[:], nea[:], AF.Exp, ["nea"], ["nea"])
        self.ts("dve", nea[:], nea[:], -1.0, None, ALU.mult, None, ["nea"], ["nea"])
        self.tt("dve", g_all[:], ab_all[:, :, 0:16], dtb[:].unsqueeze(1).to_broadcast([128, NT, 16]), ALU.add, abk + ["dtb"], ["g_all"])
        uu = scz.sb("sp_u", [128, NT, 16]); la = scz.sb("sp_la", [128, NT, 16])
        qq = scz.sb("sp_q", [128, NT, 16]); mk = scz.sb("sp_mk", [128, NT, 16])
        self.act(uu[:], g_all[:], AF.Exp, ["g_all"], ["sp_u"])
        self.act(la[:], uu[:], AF.Ln, ["sp_u"], ["sp_la"], bias=1.0)
        self.ts("dve", qq[:], uu[:], 1.0 / 7, None, ALU.mult, None, ["sp_u"], ["sp_q"])
        for cc_ in (-1.0 / 6, 1.0 / 5, -1.0 / 4, 1.0 / 3, -1.0 / 2, 1.0):
            self.stt("dve", qq[:], qq[:], cc_, uu[:], ALU.add, ALU.mult, ["sp_q", "sp_u"], ["sp_q"])
        self.ts("dve", mk[:], uu[:], 0.25, None, ALU.is_lt, None, ["sp_u"], ["sp_mk"])
        self.tt("dve", qq[:], qq[:], la[:], ALU.subtract, ["sp_q", "sp_la"], ["sp_q"])
        self.tt("dve", qq[:], qq[:], mk[:], ALU.mult, ["sp_q", "sp_mk"], ["sp_q"])
        self.tt("dve", g_all[:], la[:], qq[:], ALU.add, ["sp_la", "sp_q"], ["g_all"])
        self.tt("dve", g_all[:], g_all[:], nea[:].unsqueeze(1).to_broadcast([128, NT, 16]), ALU.mult, ["g_all", "nea"], ["g_all"])
        self.act(beta_all[:], ab_all[:, :, 16:32], AF.Sigmoid, abk, ["beta_all"])
        self.act(lnb_all[:], beta_all[:], AF.Ln, ["beta_all"], ["lnb_all"])
        scz.close()
        if self.stage >= 31:
            self.dn_scan(0, dict(qT_d=qT_d, kT_d=kT_d, ktok_d=ktok_d, vtok_d=vtok_d, sz_d=sz_d, of_d=of_d), g_all, beta_all, lnb_all)
        if self.stage >= 32:
            self.dn_scan(1, dict(qT_d=qT_d, kT_d=kT_d, ktok_d=ktok_d, vtok_d=vtok_d, sz_d=sz_d, of_d=of_d), g_all, beta_all, lnb_all)
        if self.debug:
            d_gates = self.nc.dram_tensor("d_gates", [128, 3, NT * 16], F32, kind="ExternalOutput").ap()
            for i, (t, k) in enumerate(((g_all, "g_all"), (beta_all, "beta_all"), (lnb_all, "lnb_all"))):
                self.ld(d_gates[:, i, :], t[:, :, :].rearrange("p j e -> p (j e)"), [k], [("d_gates", i)])
        scL.close()
        if self.stage >= 33:
            self.dn_out(of_d)

    def dn_scan(self, dr, dd, g_all, beta_all, lnb_all):
        nc, S, W = self.nc, self.S, self.W[1]
        cst, c2 = self.cst, self.cst2
        ident = cst[:, C_ID:C_ID + 128]
        ones = cst[:, C_ONES:C_ONES + 128]
        zcol = cst[:, C_U:C_U + 1]
        Ltri = c2[:, C2_LF:C2_LF + 128] if dr == 0 else c2[:, C2_LB:C2_LB + 128]
        LT, GT = c2[:, C2_LT:C2_LT + 128], c2[:, C2_GT:C2_GT + 128]
        GE, LE = c2[:, C2_GE:C2_GE + 128], c2[:, C2_LE:C2_LE + 128]
        m_db, m_dbt, m_dt = (LT, GT, GE) if dr == 0 else (GT, LT, LE)
        order = ([32, 33] + list(range(32))) if dr == 0 else ([33, 32] + list(range(31, -1, -1)))
        if self.n_tiles is not None:
            order = order[:self.n_tiles]
        sc = Scope(self)
        sfx = "_%d" % dr

        def bankk(b):
            return [self.pk(b, 0), self.pk(b, 1)]

        def zfill(t, k):
            sh = list(t.shape)
            self.cp("dve", t[:], zcol.to_broadcast(sh) if len(sh) == 2 else zcol.unsqueeze(1).to_broadcast(sh), ["cst"], [k])


        def z3(name, w, dt=F32R, fill=True):
            t = sc.sb(name + sfx, [128, 8, w], dt)
            if fill:
                for b_ in range(4):
                    self.cp("dve", t[:, 2 * b_:2 * b_ + 2, :], zcol.unsqueeze(1).to_broadcast([128, 2, w]), ["cst"], [(name, b_)])
            return t

        Sst = z3("S", 256)
        Xb = [z3("X0", 256)]
        qkT = z3("qkT", 128, fill=False)
        vb = z3("vb", 256); kbe = z3("kbe", 128, fill=False); kd = z3("kd", 128, fill=False)
        qeT = z3("qeT", 128, fill=False); usb = z3("usb", 128, F32, fill=False)
        wT = z3("wT", 128, fill=False); vnew = z3("vn", 256); Sdec = z3("Sdec", 128, F32, fill=False)
        Mf = z3("Mf", 128, F32, fill=False); Ao = z3("Ao", 128, F32, fill=False)
        Xp = [z3("Xa", 128, F32, fill=False), z3("Xb2", 128, F32, fill=False)]
        XPN = ["Xa", "Xb2"]
        ETf = z3("ETf", 128, F32, fill=False)
        Td = z3("Td", 128, F32, fill=False); Uf = z3("Uf", 128, F32, fill=False)
        negIf = sc.sb("negIf" + sfx, [128, 128])
        self.ts("dve", negIf[:], ident, -1.0, None, ALU.mult, None, ["cst"], ["negIf"])
        m64b = c2[:, C2_B64:C2_B64 + 128].unsqueeze(1).to_broadcast([128, 8, 128])
        nm64b = c2[:, C2_NB64:C2_NB64 + 128].unsqueeze(1).to_broadcast([128, 8, 128])
        offb = c2[:, C2_OFF:C2_OFF + 128].unsqueeze(1).to_broadcast([128, 8, 128])
        m64b2 = c2[:, C2_B64:C2_B64 + 128].unsqueeze(1).to_broadcast([128, 2, 128])
        nm64b2 = c2[:, C2_NB64:C2_NB64 + 128].unsqueeze(1).to_broadcast([128, 2, 128])
        offb2 = c2[:, C2_OFF:C2_OFF + 128].unsqueeze(1).to_broadcast([128, 2, 128])
        kqr = Ring(sc, "kq" + sfx, [128, 8, 256], F32R, 2)
        ktr = Ring(sc, "kt" + sfx, [128, D], F32, 1)
        vtr = Ring(sc, "vt" + sfx, [128, D], F32, 1)
        otr = Ring(sc, "ot" + sfx, [128, D], F32, 1)
        Dg2 = [sc.sb("Dg%d" % i + sfx, [128, 2, 8, 128]) for i in range(2)]
        gsm2 = [sc.sb("gsm%d" % i + sfx, [128, 6, 8]) for i in range(2)]
        DB = sc.sb("DB" + sfx, [128, 8, 128]); DBT = sc.sb("DBT" + sfx, [128, 8, 128])
        DT = sc.sb("DT" + sfx, [128, 8, 128]); Ec = sc.sb("Ec" + sfx, [128, 8, 128])
        if dr == 1:
            ofr = Ring(sc, "of" + sfx, [128, D], F32, 1)
            szr = Ring(sc, "sz" + sfx, [128, D], F32, 1)
            ssq = sc.sb("ssq", [128, 8])
            onb = sc.sb("onb", [128, 128])
            self.ld(onb[:], W["o_norm"].partition_broadcast(128), [], ["onb"])
        NIT = 5
        P4 = range(4)

        def A(b_):
            return self.ps[b_][:, :].rearrange("p (s c) -> p s c", s=2), [("ps", b_)]

        def B(b_):
            return self.ps[4 + b_][:, :].rearrange("p (s c) -> p s c", s=2), [("ps", 4 + b_)]

        def keys(name):
            return [(name, b_) for b_ in P4]

        idb8 = ident.unsqueeze(1).to_broadcast([128, 8, 128])
        idb2 = ident.unsqueeze(1).to_broadcast([128, 2, 128])
        gk = ["g_all", "beta_all", "lnb_all"]

        def gates(jj_, sl_):
            gj_ = g_all[:, jj_, dr * 8:(dr + 1) * 8]
            lbj_ = lnb_all[:, jj_, dr * 8:(dr + 1) * 8]
            gs_ = gsm2[sl_]
            gsk = ("gsm", sl_)
            pg = self.ps[7]
            self.mm(pg[:, 0:8], Ltri, gj_, True, True, ["cst2"] + gk, bankk(7))
            self.mm(pg[:, 8:16], ones, gj_, True, True, ["cst"] + gk, bankk(7))
            gc_, gb_, ebg_, ekd_, egl_, tmpg_ = (gs_[:, i, :] for i in range(6))
            self.cp("act", gc_, pg[:, 0:8], bankk(7), [gsk])
            self.tt("dve", gb_, gc_, lbj_, ALU.add, [gsk] + gk, [gsk])
            self.act(ebg_, gb_, AF.Exp, [gsk], [gsk])
            self.tt("dve", tmpg_, pg[:, 8:16], gc_, ALU.subtract, bankk(7) + [gsk], [gsk])
            self.act(ekd_, tmpg_, AF.Exp, [gsk], [gsk])
            self.act(egl_, pg[:, 8:16], AF.Exp, bankk(7), [gsk])
            self.tt("pool", Dg2[sl_][:, 0, :, :], idb8, gc_.unsqueeze(2).to_broadcast([128, 8, 128]), ALU.mult, ["cst", gsk], [("Dg0", sl_)])
            self.tt("pool", Dg2[sl_][:, 1, :, :], idb8, gb_.unsqueeze(2).to_broadcast([128, 8, 128]), ALU.mult, ["cst", gsk], [("Dg1", sl_)])

        def kq_load(jj_):
            tsl_ = slice(jj_ * 128, (jj_ + 1) * 128)
            kq_, kqk_ = kqr.next()
            self.ld(kq_[:, :, 0:128], dd["kT_d"][:, tsl_].rearrange("(h p) t -> p h t", p=128), [], [kqk_ + ("k",)], q="pool")
            keys_ = [kqk_ + ("k",)]
            if jj_ < NT_LAT:
                self.ld(kq_[:, :, 128:256], dd["qT_d"][:, tsl_].rearrange("(h p) t -> p h t", p=128), [], [kqk_ + ("q",)], q="pool")
                keys_.append(kqk_ + ("q",))
            return kq_, kqk_, keys_

        gates(order[0], 0)
        kq_nxt = kq_load(order[0])
        for oi, jj in enumerate(order):
            sl = oi % 2
            gsk = ("gsm", sl)
            Dg = Dg2[sl]
            lat = jj < NT_LAT
            tsl = slice(jj * 128, (jj + 1) * 128)
            kq, kqk, kqkeys = kq_nxt
            if oi + 1 < len(order):
                kq_nxt = kq_load(order[oi + 1])
            kt, ktk = ktr.next()
            self.ld(kt[:], dd["ktok_d"][tsl, :], [], [ktk])
            vt, vtk = vtr.next()
            self.ld(vt[:], dd["vtok_d"][tsl, :], [], [vtk])
            kt3 = kt[:, :].rearrange("p (h d) -> p h d", h=8)
            vt3 = vt[:, :].rearrange("p (h d) -> p h d", h=8)
            if dr == 1 and lat:
                of, ofk = ofr.next()
                self.ld(of[:], dd["of_d"][tsl, :], [("of_d", jj)], [ofk])
                szt, szk = szr.next()
                self.ld(szt[:], dd["sz_d"][tsl, :], [], [szk])
            bj = beta_all[:, jj, dr * 8:(dr + 1) * 8]
            gc, gb, ebg, ekd, egl, tmpg = (gsm2[sl][:, i, :] for i in range(6))
            for half in range(2):
                self.mm(self.ps[4 + half][:, :], ones, Dg[:, 0, half * 4:(half + 1) * 4, :], True, True, ["cst", ("Dg0", sl)], bankk(4 + half))
                self.mm(self.ps[6 + half][:, :], ones, Dg[:, 1, half * 4:(half + 1) * 4, :], True, True, ["cst", ("Dg1", sl)], bankk(6 + half))
            ncol = 256 if lat else 128
            for b_ in P4:
                ap_, apk = A(b_)
                for s_ in range(2):
                    h = 2 * b_ + s_
                    self.mm(ap_[:, s_, 0:ncol], kq[:, h, 0:128], kq[:, h, 0:ncol], True, True, kqkeys, apk)
            if oi + 1 < len(order):
                gates_next = (order[oi + 1], 1 - sl)
            else:
                gates_next = None
            for half in range(2):
                hs = slice(half * 4, half * 4 + 4)
                pRc = self.ps[4 + half][:, :].rearrange("p (h f) -> p h f", h=4)
                pRb = self.ps[6 + half][:, :].rearrange("p (h f) -> p h f", h=4)
                gbb = gb[:, hs].unsqueeze(2).to_broadcast([128, 4, 128])
                gcb = gc[:, hs].unsqueeze(2).to_broadcast([128, 4, 128])
                self.tt("dve", DB[:, hs, :], gbb, pRc, ALU.subtract, [gsk] + bankk(4 + half), ["DB"])
                self.tt("pool", DB[:, hs, :], DB[:, hs, :], m_db.unsqueeze(1).to_broadcast([128, 4, 128]), ALU.add, ["DB", "cst2"], ["DB"])
                self.tt("dve", DBT[:, hs, :], pRb, gcb, ALU.subtract, [gsk] + bankk(6 + half), ["DBT"])
                self.tt("pool", DBT[:, hs, :], DBT[:, hs, :], m_dbt.unsqueeze(1).to_broadcast([128, 4, 128]), ALU.add, ["DBT", "cst2"], ["DBT"])
                if lat:
                    self.tt("dve", DT[:, hs, :], pRc, gcb, ALU.subtract, [gsk] + bankk(4 + half), ["DT"])
                    self.tt("pool", DT[:, hs, :], DT[:, hs, :], m_dt.unsqueeze(1).to_broadcast([128, 4, 128]), ALU.add, ["DT", "cst2"], ["DT"])
                    self.act(Ec[:, hs, :], pRc, AF.Exp, bankk(4 + half), ["Ec"])
            self.act(DB[:], DB[:], AF.Exp, ["DB"], ["DB"])
            self.act(DBT[:], DBT[:], AF.Exp, ["DBT"], ["DBT"])
            if lat:
                self.act(DT[:], DT[:], AF.Exp, ["DT"], ["DT"])
            for b_ in P4:
                ap_, apk = A(b_)
                hp = slice(2 * b_, 2 * b_ + 2)
                self.tt("dve", Mf[:, hp, :], ap_[:, :, 0:128], DB[:, hp, :], ALU.mult, apk + ["DB"], [("Mf", b_)])
                self.tt("dve", Xp[0][:, hp, :], ap_[:, :, 0:128], DBT[:, hp, :], ALU.mult, apk + ["DBT"], [("Xa", b_)])
                if lat:
                    self.tt("dve", qkT[:, hp, :], ap_[:, :, 128:256], DT[:, hp, :], ALU.mult, apk + ["DT"], [("qkT", b_)])
            for b_ in P4:
                hp = slice(2 * b_, 2 * b_ + 2)
                self.tt("pool", Ao[:, hp, :], Mf[:, hp, :], offb2, ALU.mult, [("Mf", b_), "cst2"], [("Ao", b_)])
                self.tt("pool", Mf[:, hp, :], Mf[:, hp, :], m64b2, ALU.mult, [("Mf", b_), ("Ao", b_), "cst2"], [("Mf", b_)])
                self.tt("pool", Mf[:, hp, :], Mf[:, hp, :], idb2, ALU.add, [("Mf", b_), "cst"], [("Mf", b_)])
                self.tt("pool", Xp[0][:, hp, :], Xp[0][:, hp, :], nm64b2, ALU.mult, [("Xa", b_), "cst2"], [("Xa", b_)])
                self.tt("pool", Xp[0][:, hp, :], Xp[0][:, hp, :], idb2, ALU.add, [("Xa", b_), "cst"], [("Xa", b_)])
            if gates_next is not None:
                gates(*gates_next)
            cur = 0
            for it in range(NIT):
                src, srck = Xp[cur], XPN[cur]
                dst, dstk = Xp[1 - cur], XPN[1 - cur]
                for b_ in P4:
                    ap_, apk = A(b_)
                    for s_ in range(2):
                        h = 2 * b_ + s_
                        self.mm(ap_[:, s_, 0:128], src[:, h, :], Mf[:, h, :], True, True, [(srck, b_), ("Mf", b_)], apk)
                for b_ in P4:
                    ap_, apk = A(b_)
                    self.stt("dve", ETf[:, 2 * b_:2 * b_ + 2, :], ap_[:, :, 0:128], -1.0, idb2, ALU.mult, ALU.add, apk + ["cst"], [("ETf", b_)])
                for b_ in P4:
                    bp_, bpk = B(b_)
                    for s_ in range(2):
                        h = 2 * b_ + s_
                        self.mm(bp_[:, s_, 0:128], ETf[:, h, :], src[:, h, :], True, True, [("ETf", b_), (srck, b_)], bpk)
                for b_ in P4:
                    bp_, bpk = B(b_)
                    hp = slice(2 * b_, 2 * b_ + 2)
                    self.tt("dve", dst[:, hp, :], src[:, hp, :], bp_[:, :, 0:128], ALU.add, [(srck, b_)] + bpk, [(dstk, b_)])
                cur = 1 - cur
            Xd, Xdk = Xp[cur], XPN[cur]
            for b_ in P4:
                ap_, apk = A(b_)
                bp_, bpk = B(b_)
                for s_ in range(2):
                    h = 2 * b_ + s_
                    self.tr(ap_[:, s_, 0:128], Xd[:, h, :], [(Xdk, b_)], apk)
                    self.mm(bp_[:, s_, 0:128], Ao[:, h, :], Xd[:, h, :], True, True, [("Ao", b_), (Xdk, b_)], bpk)
            for b_ in P4:
                ap_, apk = A(b_)
                bp_, bpk = B(b_)
                hp = slice(2 * b_, 2 * b_ + 2)
                self.cp("act", Td[:, hp, :], ap_[:, :, 0:128], apk, [("Td", b_)])
                self.cp("act", Uf[:, hp, :], bp_[:, :, 0:128], bpk, [("Uf", b_)])
            for b_ in P4:
                ap_, apk = A(b_)
                for s_ in range(2):
                    h = 2 * b_ + s_
                    self.mm(ap_[:, s_, 0:128], Td[:, h, :], Uf[:, h, :], True, True, [("Td", b_), ("Uf", b_)], apk)
            for b_ in P4:
                ap_, apk = A(b_)
                hp = slice(2 * b_, 2 * b_ + 2)
                self.tt("dve", Xb[0][:, hp, 0:128], Xd[:, hp, :], ap_[:, :, 0:128], ALU.subtract, [(Xdk, b_)] + apk, [("X0", b_)])
            cur = 0
            XN = ["X0"]
            Xf, kf_ = Xb[cur], XN[cur]
            self.tt("dve", vb[:, :, 0:128], vt3, bj.unsqueeze(2).to_broadcast([128, 8, 128]), ALU.mult, [vtk] + gk, keys("vb"))
            self.tt("pool", kbe[:, :, :], kt3, ebg.unsqueeze(2).to_broadcast([128, 8, 128]), ALU.mult, [ktk, gsk], keys("kbe"))
            self.tt("pool", kd[:, :, :], kt3, ekd.unsqueeze(2).to_broadcast([128, 8, 128]), ALU.mult, [ktk, gsk], keys("kd"))
            if lat:
                self.tt("pool", qeT[:, :, :], kq[:, :, 128:256], Ec[:, :, :], ALU.mult, kqkeys + ["Ec"], keys("qeT"))
            for b_ in P4:
                ap_, apk = A(b_)
                bp_, bpk = B(b_)
                for s_ in range(2):
                    h = 2 * b_ + s_
                    self.mm(ap_[:, s_, :], Xf[:, h, 0:128], vb[:, h, :], True, True, [(kf_, b_), ("vb", b_)], apk)
                    self.mm(bp_[:, s_, :], kbe[:, h, :], Xf[:, h, :], True, True, [(kf_, b_), ("kbe", b_)], bpk)
            for b_ in P4:
                ap_, apk = A(b_)
                bp_, bpk = B(b_)
                hp = slice(2 * b_, 2 * b_ + 2)
                self.cp("act", usb[:, hp, :], ap_[:, :, 0:128], apk, [("usb", b_)])
                self.cp("act", wT[:, hp, :], bp_[:, :, 0:128], bpk, [("wT", b_)])
            ot, otk = otr.next()
            ot3 = ot[:, :].rearrange("p (h d) -> p h d", h=8)
            for b_ in P4:
                ap_, apk = A(b_)
                for s_ in range(2):
                    h = 2 * b_ + s_
                    self.mm(ap_[:, s_, :], wT[:, h, :], Sst[:, h, :], True, True, [("wT", b_), ("S", b_)], apk)
            for b_ in P4:
                ap_, apk = A(b_)
                hp = slice(2 * b_, 2 * b_ + 2)
                self.tt("dve", vnew[:, hp, 0:128], usb[:, hp, :], ap_[:, :, 0:128], ALU.subtract, [("usb", b_)] + apk, [("vn", b_)])
            for b_ in P4:
                ap_, apk = A(b_)
                bp_, bpk = B(b_)
                for s_ in range(2):
                    h = 2 * b_ + s_
                    if lat:
                        self.mm(bp_[:, s_, :], qeT[:, h, :], Sst[:, h, :], True, False, [("qeT", b_), ("S", b_)], bpk)
                        self.mm(bp_[:, s_, :], qkT[:, h, :], vnew[:, h, :], False, True, [("qkT", b_), ("vn", b_)], bpk)
                    self.mm(ap_[:, s_, :], kd[:, h, :], vnew[:, h, :], True, True, [("kd", b_), ("vn", b_)], apk)
            self.tt("pool", Sdec[:, :, :], Sst[:, :, 0:128], egl.unsqueeze(2).to_broadcast([128, 8, 128]), ALU.mult,
                    keys("S") + [gsk], keys("Sdec"))
            for b_ in P4:
                ap_, apk = A(b_)
                bp_, bpk = B(b_)
                hp = slice(2 * b_, 2 * b_ + 2)
                if lat:
                    self.cp("act", ot3[:, hp, :], bp_[:, :, 0:128], bpk, [otk])
                self.tt("dve", Sst[:, hp, 0:128], Sdec[:, hp, :], ap_[:, :, 0:128], ALU.add, [("Sdec", b_)] + apk, [("S", b_)])
            if not lat:
                continue
            if dr == 0:
                self.ld(dd["of_d"][tsl, :], ot[:], [otk], [("of_d", jj)])
            else:
                if self.debug:
                    if not hasattr(self, "ob_d"):
                        self.ob_d = self.nc.dram_tensor("ob_d", [NLAT, D], F32, kind="ExternalOutput").ap()
                    self.ld(self.ob_d[tsl, :], ot[:], [otk], [("ob_d", jj)])
                self.tt("pool", ot[:], ot[:], of[:], ALU.add, [otk, ofk], [otk])
                self.act(DB[:, :, :].rearrange("p h d -> p (h d)"), ot[:], AF.Square, [otk], ["DB"])
                S.op("dve", lambda e: e.tensor_reduce(out=ssq[:], in_=DB[:, :, :], axis=AX.X, op=ALU.add),
                     ["DB"], ["ssq"])
                self.ts("dve", ssq[:], ssq[:], 1.0 / 128, EPS, ALU.mult, ALU.add, ["ssq"], ["ssq"])
                self.act(ssq[:], ssq[:], AF.Sqrt, ["ssq"], ["ssq"])
                S.op("dve", lambda e: e.reciprocal(out=ssq[:], in_=ssq[:]), ["ssq"], ["ssq"])
                o3 = ot[:, :].rearrange("p (h d) -> p h d", h=8)
                self.tt("dve", o3, o3, ssq[:].unsqueeze(2).to_broadcast([128, 8, 128]), ALU.mult, [otk, "ssq"], [otk])
                self.tt("pool", o3, o3, onb[:].unsqueeze(1).to_broadcast([128, 8, 128]), ALU.mult, [otk, "onb"], [otk])
                self.tt("pool", ot[:], ot[:], szt[:], ALU.mult, [otk, szk], [otk])
                self.ld(dd["of_d"][tsl, :], ot[:], [otk, ofk], [("of_d", jj)])
        sc.close()

    def dn_out(self, of_d):
        nc, S, W = self.nc, self.S, self.W[1]
        sc = Scope(self)
        wo = sc.sb("wo1", [128, 8, D], F32R)
        self.ld(wo[:], W["w_o"].rearrange("(k p) n -> p k n", p=128), [], ["wo1"], q="pool")
        wr = sc.sb("wr1", [128, 8, NE])
        self.ld(wr[:], W["router"].rearrange("(k p) n -> p k n", p=128), [], ["wr"])
        yr = Ring(sc, "oy", [128, D], F32, 2)
        xr = Ring(sc, "ox", [128, D], F32, 2)
        xmr = Ring(sc, "oxm", [128, D], F32, 3)
        ssr = Ring(sc, "oss", [128, 1], F32, 4)
        junk = sc.sb("ojunk", [128, D])
        yT = sc.sb("oyT", [128, 8, 128], F32R)
        h2T = sc.sb("oh2T", [128, 8, 128])
        lg = sc.sb("olg", [128, NE]); mx = sc.sb("omx", [128, 1]); sm = sc.sb("osm", [128, 1])
        pend = [None]

        def o_loads(i_):
            yt_, ytk_ = yr.next()
            self.ld(yt_[:], of_d[i_ * 128:(i_ + 1) * 128, :], [], [ytk_])
            xt_, xk_ = xr.next()
            self.ld(xt_[:], self.xres[0][i_ * 128:(i_ + 1) * 128, :], [], [xk_])
            return yt_, ytk_, xt_, xk_

        onxt = o_loads(0)
        for i in range(NT_LAT):
            yt, ytk, xt, xk = onxt
            if i + 1 < NT_LAT:
                onxt = o_loads(i + 1)
            for kc in range(8):
                self.tr(self.ps[kc // 4][:, (kc % 4) * 128:(kc % 4 + 1) * 128], yt[:, kc * 128:(kc + 1) * 128], [ytk], [self.psk[kc // 4]])
            for b in range(2):
                self.cp("act", yT[:, b * 4:(b + 1) * 4, :], self.ps[b][:, :].rearrange("p (k t) -> p k t", k=4), [self.psk[b]], ["yT"])
            if pend[0] is not None:
                self.moe_prep(*pend[0])
                pend[0] = None
            xm, xmk = xmr.next()
            for dh in range(2):
                pM, pMk = self.ps[2 + dh], self.psk[2 + dh]
                for kc in range(8):
                    self.mm(pM[:, :], yT[:, kc, :], wo[:, kc, dh * 512:(dh + 1) * 512], kc == 0, kc == 7, ["yT", "wo1"], [pMk])
                self.tt("dve", xm[:, dh * 512:(dh + 1) * 512], pM[:, :], self.Gbc[:, 0, 0, dh * 512:(dh + 1) * 512], ALU.mult,
                        [pMk, ("Gbc", 0, 0)], [xmk])
            self.tt("dve", xm[:], xm[:], xt[:], ALU.add, [xmk, xk], [xmk])
            self.ld(self.xres[1][i * 128:(i + 1) * 128, :], xm[:], [xmk], [("xres1", i)])
            pend[0] = (i, 0, xm, xmk, junk, ssr, wr, h2T, lg, mx, sm)
        self.moe_prep(*pend[0])
        sc.close()

    def final_norm(self):
        nc, S = self.nc, self.S
        sc = Scope(self)
        fn = sc.sb("fnb", [128, D])
        self.ld(fn[:], self.inp["final_norm"].partition_broadcast(128), [], ["fnb"])
        xr = Ring(sc, "fx", [128, D], F32, 4)
        ssr = Ring(sc, "fss", [128, 1], F32, 4)
        junk = sc.sb("fjunk", [128, D])
        def f_load(i_):
            xt_, xk_ = xr.next()
            self.ld(xt_[:], self.xres[1][i_ * 128:(i_ + 1) * 128, :], [], [xk_])
            return xt_, xk_

        fq_ = [f_load(0), f_load(1)]
        for i in range(NT_LAT):
            xt, xk = fq_.pop(0)
            if i + 2 < NT_LAT:
                fq_.append(f_load(i + 2))
            ss, ssk = ssr.next()
            self.rstd_of(xt[:], xk, junk[:], "fjunk", ss[:], ssk)
            self.stt("dve", xt[:], xt[:], ss[:, 0:1], fn[:], ALU.mult, ALU.mult, [xk, ssk, "fnb"], [xk])
            S.dma("sp", lambda e: e.dma_start(out=self.out[i * 128:(i + 1) * 128, :], in_=xt[:]), [xk], [("out", i)], is_output=True)
        sc.close()

    def moe(self, l, xres, xresk, with_ctx):
        nc, S, W = self.nc, self.S, self.W[l]
        cst = self.cst
        ones = cst[:, C_ONES:C_ONES + 128]
        Umat = cst[:, C_U:C_U + 128]
        iota = cst[:, C_IOTA:C_IOTA + 512]
        sets = [(0, 0, NT_LAT, CAP_LAT)] + ([(1, NT_LAT, NT_CTX, CAP_CTX)] if with_ctx else [])
        sc = Scope(self)
        slot_m = {}; meta = {}
        for (si, j0, nj, cap) in sets:
            slot_m[si] = sc.sb("slotm%d_%d" % (si, l), [128, nj, NE])
            meta[si] = sc.sb("meta%d_%d" % (si, l), [128, nj, NE, 4], F32R)
        scr = Scope(self)
        rt = {}
        for (si, j0, nj, cap) in sets:
            sfx = "%d_%d" % (si, l)
            d_ = dict(affv=self.aff[:, j0:j0 + nj, :], affk=[("aff", j) for j in range(j0, j0 + nj)])
            d_["lo"] = scr.sb("lo" + sfx, [128, NE]); d_["mid"] = scr.sb("mid" + sfx, [128, NE])
            d_["cmpt"] = scr.sb("cmp" + sfx, [128, nj, NE]); d_["cnt"] = scr.sb("cnt" + sfx, [128, NE])
            d_["tq"] = scr.sb("tq" + sfx, [128, NE])
            d_["offs"] = scr.sb("offs" + sfx, [128, nj, NE]); d_["slot"] = scr.sb("slot" + sfx, [128, nj, NE])
            lo_, mid_ = d_["lo"], d_["mid"]
            S.op("dve", lambda e: e.memset(lo_[:], 0.0), [], [("lo", si)])
            S.op("dve", lambda e: e.memset(mid_[:], 0.5), [], [("mid", si)])
            rt[si] = d_
        for it in range(NBIS):
            w = 2.0 ** -(it + 1)
            for (si, j0, nj, cap) in sets:
                d_ = rt[si]
                lo, mid, cmpt, cnt, tq, affv, affk = d_["lo"], d_["mid"], d_["cmpt"], d_["cnt"], d_["tq"], d_["affv"], d_["affk"]
                pb_ = 0 if si == 0 else 3
                pC, pCk = self.ps[pb_], self.psk[pb_]
                self.tt("dve", cmpt[:], affv, mid[:].unsqueeze(1).to_broadcast([128, nj, NE]), ALU.is_ge, affk + [("mid", si)], [("cmp", si)])
                S.op("dve", lambda e: e.tensor_reduce(out=cnt[:], in_=cmpt[:, :, :].rearrange("p j e -> p e j"), axis=AX.X, op=ALU.add),
                     [("cmp", si)], [("cnt", si)])
                self.mm(pC[:, 0:NE], ones, cnt[:], True, True, ["cst", ("cnt", si)], [pCk])
                self.ts("dve", tq[:], pC[:, 0:NE], cap - 0.5, w, ALU.is_ge, ALU.mult, [pCk], [("tq", si)])
                self.tt("dve", lo[:], lo[:], tq[:], ALU.add, [("lo", si), ("tq", si)], [("lo", si)])
                self.ts("dve", mid[:], lo[:], w * 0.5, None, ALU.add, None, [("lo", si)], [("mid", si)])
        for (si, j0, nj, cap) in sets:
            d_ = rt[si]
            lo, cmpt, offs, slot, affv, affk = d_["lo"], d_["cmpt"], d_["offs"], d_["slot"], d_["affv"], d_["affk"]
            self.tt("dve", cmpt[:], affv, lo[:].unsqueeze(1).to_broadcast([128, nj, NE]), ALU.is_ge, affk + [("lo", si)], [("cmp", si)])
            pP, pPk = self.ps[1], self.psk[1]
            pT, pTk = self.ps[2], self.psk[2]
            mflat = cmpt[:, :, :].rearrange("p j e -> p (j e)")
            self.mm(pP[:, 0:nj * NE], Umat, mflat, True, True, ["cst", ("cmp", si)], [pPk])
            self.mm(pT[:, 0:nj * NE], ones, mflat, True, True, ["cst", ("cmp", si)], [pTk])
            pTv = pT[:, 0:nj * NE].rearrange("p (j e) -> p j e", e=NE)
            pPv = pP[:, 0:nj * NE].rearrange("p (j e) -> p j e", e=NE)
            S.op("dve", lambda e: e.memset(offs[:, 0, :], 0.0), [], ["offs"])
            for j in range(1, nj):
                self.tt("dve", offs[:, j, :], pTv[:, j - 1, :], offs[:, j - 1, :], ALU.add, [pTk, "offs"], ["offs"])
            self.tt("dve", slot[:], pPv, offs[:], ALU.add, [pPk, "offs"], ["slot"])
            self.ts("dve", cmpt[:], cmpt[:], -1.0e6, 1.0e6, ALU.mult, ALU.add, [("cmp", si)], [("cmp", si)])
            self.tt("dve", slot_m[si][:], slot[:], cmpt[:], ALU.add, ["slot", ("cmp", si)], [("slotm", si)])
            mt = meta[si]
            for j in range(nj):
                self.cp("dve", mt[:, j, :, 0:1], cst[:, C_U:C_U + 1].unsqueeze(1).to_broadcast([128, NE, 1]), ["cst"], [("meta", si)])
                self.ts("dve", mt[:, j, :, 0:1], mt[:, j, :, 0:1], float(j0 + j), None, ALU.add, None, [("meta", si)], [("meta", si)])
            mtf = mt[:, :, :, :].rearrange("p j e c -> p (j e) c")
            self.cp("dve", mtf[:, :, 1:2], cst[:, C_PIDX:C_PIDX + 1].unsqueeze(1).to_broadcast([128, nj * NE, 1]), ["cst"], [("meta", si)])
            self.cp("dve", mtf[:, :, 2:3], affv.rearrange("p j e -> p (j e)").unsqueeze(2), affk, [("meta", si)])
            self.cp("dve", mtf[:, :, 3:4], cst[:, C_ONES:C_ONES + 1].unsqueeze(1).to_broadcast([128, nj * NE, 1]), ["cst"], [("meta", si)])
        scr.close()

        NW = 8
        wring = Ring(sc, "wm%d" % l, [128, 8, 256], F32R, NW)
        xsT = sc.sb("xsT%d" % l, [128, 8, 640], F32R)
        hidT = sc.sb("hidT%d" % l, [128, 16, 544], F32R)
        ysb = sc.sb("ysb%d" % l, [128, 5, D])
        xsr = Ring(sc, "xstok%d" % l, [128, D], F32, 2)
        selr = Ring(sc, "sel%d" % l, [128, 512], F32R, 2)
        selc = sc.sb("selc%d" % l, [128, 128], F32R)
        sgr = Ring(sc, "sg%d" % l, [128, 512], F32, 2)
        hcr = Ring(sc, "hc%d" % l, [32, 256], F32, 2)
        idxrow = sc.sb("idxrow%d" % l, [4, 640])
        metac = sc.sb("metac%d" % l, [128, 5, 4])
        tmp5 = sc.sb("tmp5%d" % l, [128, 5]); idxf = sc.sb("idxf%d" % l, [128, 5])
        idur = Ring(sc, "idu%d" % l, [128, 5], U32, 3)
        gcr = Ring(sc, "gc%d" % l, [128, 5], F32, 3)
        self.cp("dve", selc[:], cst[:, C_U:C_U + 1].to_broadcast([128, 128]), ["cst"], ["selc"])
        S.op("dve", lambda e: e.memset(ysb[:], 0.0), [], ["ysb"])
        nk = 5 if with_ctx else 4
        c2 = {0: 0, 1: 1}

        def idx_phase(e):
            pI, pIk = self.ps[7], self.psk[7]
            for (si, j0, nj, cap) in sets:
                ncol = 512 if si == 0 else 128
                for j in range(nj):
                    if si == 0:
                        sel, selk = selr.next()
                        self.ts("dve", sel[:], iota, slot_m[si][:, j, e:e + 1], None, ALU.is_equal, None, ["cst", ("slotm", si)], [selk])
                        rhs = sel[:]
                    else:
                        selk = "selc"
                        self.ts("dve", selc[:, 0:CAP_CTX], iota[:, 0:CAP_CTX], slot_m[si][:, j, e:e + 1], None, ALU.is_equal, None,
                                ["cst", ("slotm", si)], [selk])
                        rhs = selc[:]
                    self.mm(pI[0:4, 0:ncol], meta[si][:, j, e, :], rhs, j == 0, j == nj - 1, [("meta", si), selk], [pIk])
                off = 0 if si == 0 else 512
                self.cp("act", idxrow[0:4, off:off + ncol], pI[0:4, 0:ncol], [pIk], ["idxrow"])
            pX, pXk = self.ps[7], self.psk[7]
            for k in range(nk):
                self.tr(pX[:, k * 4:(k + 1) * 4], idxrow[0:4, k * 128:(k + 1) * 128], ["idxrow"], [pXk], kp=4)
            self.cp("act", metac[:, 0:nk, :], pX[:, 0:nk * 4].rearrange("p (k c) -> p k c", c=4), [pXk], ["metac"])
            idu, iduk = idur.next()
            gc, gck = gcr.next()
            self.stt("dve", idxf[:, 0:nk], metac[:, 0:nk, 0], 128.0, metac[:, 0:nk, 1], ALU.mult, ALU.add, ["metac"], ["idxf"])
            self.ts("dve", tmp5[:, 0:nk], metac[:, 0:nk, 3], -1.0, 1.0, ALU.mult, ALU.add, ["metac"], ["tmp5"])
            self.tt("dve", tmp5[:, 0:nk], tmp5[:, 0:nk], cst[:, C_DMY:C_DMY + nk], ALU.mult, ["tmp5", "cst"], ["tmp5"])
            self.tt("dve", idxf[:, 0:nk], idxf[:, 0:nk], tmp5[:, 0:nk], ALU.add, ["idxf", "tmp5"], ["idxf"])
            self.cp("dve", idu[:, 0:nk], idxf[:, 0:nk], ["idxf"], [iduk])
            self.cp("dve", gc[:, 0:nk], metac[:, 0:nk, 2], ["metac"], [gck])
            return (idu, iduk, gc, gck)

        gt_i = [0]

        def gather_phase(ix):
            idu, iduk, gc, gck = ix
            for k in range(nk):
                xs, xsk = xsr.next()
                S.dma("pool", lambda e: e.indirect_dma_start(out=xs[:], out_offset=None, in_=self.xn2[:, :],
                                                              in_offset=bass.IndirectOffsetOnAxis(ap=idu[:, k:k + 1], axis=0)),
                      [iduk], [xsk])
                c = 1 if k == 4 else 0
                for half in range(2):
                    bnk = gt_i[0] % 4
                    gt_i[0] += 1
                    pt, ptk = self.ps[bnk], self.psk[bnk]
                    for kc in range(half * 4, half * 4 + 4):
                        self.tr(pt[:, (kc % 4) * 128:(kc % 4 + 1) * 128], xs[:, kc * 128:(kc + 1) * 128], [xsk], [ptk])
                    for kc in range(half * 4, half * 4 + 4):
                        self.act(xsT[:, kc, k * 128:(k + 1) * 128], pt[:, (kc % 4) * 128:(kc % 4 + 1) * 128], AF.Identity,
                                 [ptk, ("A", 4), "cols"], ["xsT"], scale=self.A2[:, kc, c:c + 1], bias=self.B2(kc, c))

        def wload(src_ap):
            wt, wk = wring.next()
            self.ld(wt[:], src_ap, [], [wk], q="pool")
            return wt, wk

        gu_i = [0]

        cpend = [None]

        def ctx_tr(hc, hck, fq):
            pt, ptk = self.ps[7], self.psk[7]
            for fc in range(2):
                self.tr(pt[:, fc * 32:(fc + 1) * 32], hc[0:32, fc * 128:(fc + 1) * 128], [hck], [ptk], kp=32)
            self.cp("act", hidT[:, fq * 2:fq * 2 + 2, 512:544], pt[:, 0:64].rearrange("p (a t) -> p a t", a=2), [ptk],
                    [("hidT", fq * 2), ("hidT", fq * 2 + 1)])

        def ffn1(e):
            for fq in range(8):
                wg, wgk = wload(W["w_gate"][e, :, fq * 256:(fq + 1) * 256].rearrange("(k p) n -> p k n", p=128))
                wu, wuk = wload(W["w_up"][e, :, fq * 256:(fq + 1) * 256].rearrange("(k p) n -> p k n", p=128))
                for fc in range(2):
                    fcc = fq * 2 + fc
                    b = gu_i[0] % 2
                    gu_i[0] += 1
                    pG, pGk = self.ps[2 * b], self.psk[2 * b]
                    pU, pUk = self.ps[2 * b + 1], self.psk[2 * b + 1]
                    for kc in range(8):
                        self.mm(pG[:, :], wg[:, kc, fc * 128:(fc + 1) * 128], xsT[:, kc, 0:512], kc == 0, kc == 7, [wgk, "xsT"], [pGk])
                    for kc in range(8):
                        self.mm(pU[:, :], wu[:, kc, fc * 128:(fc + 1) * 128], xsT[:, kc, 0:512], kc == 0, kc == 7, [wuk, "xsT"], [pUk])
                    sg, sgk = sgr.next()
                    self.act(sg[:, 0:512], pG[:, :], AF.Silu, [pGk], [sgk])
                    self.tt("dve", hidT[:, fcc, 0:512], sg[:, 0:512], pU[:, :], ALU.mult, [sgk, pUk], [("hidT", fcc)])
                if with_ctx:
                    pc, pck = self.ps[4], self.psk[4]
                    for kc in range(8):
                        self.mm(pc[0:32, 0:256], xsT[:, kc, 512:544], wg[:, kc, :], kc == 0, kc == 7, [wgk, "xsT"], [pck])
                    for kc in range(8):
                        self.mm(pc[0:32, 256:512], xsT[:, kc, 512:544], wu[:, kc, :], kc == 0, kc == 7, [wuk, "xsT"], [pck])
                    hc, hck = hcr.next()
                    self.act(hc[0:32, 0:256], pc[0:32, 0:256], AF.Silu, [pck], [hck])
                    self.tt("dve", hc[0:32, 0:256], hc[0:32, 0:256], pc[0:32, 256:512], ALU.mult, [hck, pck], [hck])
                    if cpend[0] is not None:
                        ctx_tr(*cpend[0])
                    cpend[0] = (hc, hck, fq)
            if cpend[0] is not None:
                ctx_tr(*cpend[0])
                cpend[0] = None

        y_i = [0]

        def ffn2(e, ix):
            idu, iduk, gc, gck = ix
            hk = [("hidT", f) for f in range(16)]
            for dq in range(4):
                wd = []
                for fh in range(2):
                    wd.append(wload(W["w_down"][e, fh * 1024:(fh + 1) * 1024, dq * 256:(dq + 1) * 256].rearrange("(k p) n -> p k n", p=128)))
                for k in range(nk):
                    if k < 4:
                        b = y_i[0] % 2
                        y_i[0] += 1
                        pY, pYk = self.ps[5 + b], self.psk[5 + b]
                        rows = 128
                        lsl = slice(k * 128, (k + 1) * 128)
                    else:
                        pY, pYk = self.ps[4], self.psk[4]
                        rows = 32
                        lsl = slice(512, 544)
                    for fcc in range(16):
                        wt, wk = wd[fcc // 8]
                        self.mm(pY[0:rows, 0:256], hidT[:, fcc, lsl], wt[:, fcc % 8, :], fcc == 0, fcc == 15, [("hidT", fcc), wk], [pYk])
                    c = 1 if k == 4 else 0
                    self.stt("dve", ysb[0:rows, k, dq * 256:(dq + 1) * 256], pY[0:rows, 0:256], gc[0:rows, k:k + 1],
                             self.Gbc[0:rows, 1, c, dq * 256:(dq + 1) * 256], ALU.mult, ALU.mult,
                             [pYk, gck, ("Gbc", 1, c)], [("ysb", k)])

        def scatter_phase(ix):
            idu, iduk, gc, gck = ix
            for k in range(nk):
                S.dma("pool", lambda e: e.indirect_dma_start(out=xres[:, :], out_offset=bass.IndirectOffsetOnAxis(ap=idu[:, k:k + 1], axis=0),
                                                              in_=ysb[:, k, :], in_offset=None, compute_op=ALU.add),
                      [iduk, ("ysb", k), "ysb"], ["xacc"])

        n_exp = NE if self.n_exp is None else self.n_exp
        ixs = {0: idx_phase(0)}
        gather_phase(ixs[0])
        for e in range(n_exp):
            if e + 1 < n_exp:
                ixs[e + 1] = idx_phase(e + 1)
            ffn1(e)
            if e > 0:
                scatter_phase(ixs[e - 1])
            if e + 1 < n_exp:
                gather_phase(ixs[e + 1])
            ffn2(e, ixs[e])
        scatter_phase(ixs[n_exp - 1])
        sc.close()


def _host_consts():
    cp = np.zeros((128, C_END), np.float32)
    cp[:, C_ID:C_ID + 128] = np.eye(128, dtype=np.float32)
    cp[:, C_ONES:C_ONES + 128] = 1.0
    pi = np.arange(128)
    cp[:, C_U:C_U + 128] = (pi[:, None] < pi[None, :]).astype(np.float32)
    mp = (pi[:, None] >= pi[None, :]).astype(np.float32)
    mn = (pi[:, None] <= pi[None, :]).astype(np.float32)
    cp[:, C_MP:C_MP + 512] = np.tile(mp, (1, 4))
    cp[:, C_MN:C_MN + 512] = np.tile(mn, (1, 4))
    cp[:, C_IOTA:C_IOTA + 512] = np.arange(512, dtype=np.float32)[None, :]
    cp[:, C_PIDX] = pi
    for k in range(5):
        cp[:, C_DMY + k] = NTOK + k * 128 + pi
    t = np.arange(NLAT)
    row = (t // 64).astype(np.float32)
    col = (t % 64).astype(np.float32)
    inv = (np.float32(10000.0) ** (-np.arange(16, dtype=np.float32) / np.float32(16))).astype(np.float32)
    ar = (row[:, None] * inv[None, :]).astype(np.float32)
    ac = (col[:, None] * inv[None, :]).astype(np.float32)
    rope = np.concatenate([np.cos(ar), np.cos(ac), np.sin(ar), np.sin(ac)], axis=1).astype(np.float32)
    return cp, rope


def _host_consts2():
    c2 = np.zeros((128, C2_END), np.float32)
    p = np.arange(128)[:, None]
    f = np.arange(128)[None, :]
    c2[:, C2_LF:C2_LF + 128] = (p <= f)
    c2[:, C2_LB:C2_LB + 128] = (p >= f)
    c2[:, C2_LT:C2_LT + 128] = np.where(f < p, 0.0, NEG)
    c2[:, C2_GT:C2_GT + 128] = np.where(f > p, 0.0, NEG)
    c2[:, C2_GE:C2_GE + 128] = np.where(f >= p, 0.0, NEG)
    c2[:, C2_LE:C2_LE + 128] = np.where(f <= p, 0.0, NEG)
    same = ((p // 64) == (f // 64)).astype(np.float32)
    c2[:, C2_B64:C2_B64 + 128] = same
    c2[:, C2_NB64:C2_NB64 + 128] = -same
    c2[:, C2_OFF:C2_OFF + 128] = 1.0 - same
    return c2


def _colT(v, n):
    return np.ascontiguousarray(np.asarray(v, np.float32).reshape(n, 128).T)


ALL_INPUTS = (
    "x", "c", "ctx", "c_ctx",
    "l0_ada_w", "l0_ada_b", "l0_norm_mix", "l0_w_qkv", "l0_sink", "l0_w_o", "l0_norm_ffn",
    "l0_router", "l0_w_gate", "l0_w_up", "l0_w_down",
    "l1_ada_w", "l1_ada_b", "l1_norm_mix", "l1_w_in", "l1_conv", "l1_a_log", "l1_dt_bias", "l1_o_norm", "l1_w_o",
    "l1_norm_ffn", "l1_router", "l1_w_gate", "l1_w_up", "l1_w_down",
    "final_norm",
)


def make_in_maps(inputs, cores):
    missing = [n for n in ALL_INPUTS if n not in inputs]
    assert not missing, missing
    cp, rope = _host_consts()
    shared = {"cpack": cp, "rope": rope}
    for l in (0, 1):
        p = "l%d_" % l
        shared[p + "ada_w"] = np.asarray(inputs[p + "ada_w"], np.float32)
        shared[p + "ada_b"] = np.asarray(inputs[p + "ada_b"], np.float32)
        shared[p + "ada_bT"] = _colT(inputs[p + "ada_b"], 48)
        shared[p + "nmixT"] = _colT(inputs[p + "norm_mix"], 8)
        shared[p + "nffnT"] = _colT(inputs[p + "norm_ffn"], 8)
        for n in ("router", "w_gate", "w_up", "w_down"):
            shared[p + n] = np.asarray(inputs[p + n], np.float32)
    for n in ("l0_sink", "l0_w_o", "l1_w_in", "l1_o_norm", "l1_w_o", "final_norm"):
        shared[n] = np.asarray(inputs[n], np.float32)
    shared["l1_a_log"] = np.asarray(inputs["l1_a_log"], np.float32).reshape(16)
    shared["l1_dt_bias"] = np.asarray(inputs["l1_dt_bias"], np.float32).reshape(16)
    cv = np.asarray(inputs["l1_conv"], np.float32)
    shared["l1_convT"] = np.ascontiguousarray(cv.reshape(3, 24, 128).transpose(2, 1, 0))
    shared["cpack2"] = _host_consts2()
    wq = np.asarray(inputs["l0_w_qkv"], np.float32)
    perm = [pr * 8 + s_ * 4 + g for pr in range(2) for g in range(4) for s_ in range(2)]
    cols = np.concatenate([np.arange(h * 64, (h + 1) * 64) for h in perm] + [np.arange(1024, 1536)])
    shared["l0_w_qkv"] = np.ascontiguousarray(wq[:, cols])
    maps = []
    for b in cores:
        m = dict(shared)
        m["x"] = np.ascontiguousarray(inputs["x"][b], dtype=np.float32)
        m["ctx"] = np.ascontiguousarray(inputs["ctx"][b], dtype=np.float32)
        cv = np.stack([np.asarray(inputs["c"][b], np.float32), np.asarray(inputs["c_ctx"], np.float32)], axis=1)
        m["cvecT"] = np.ascontiguousarray(cv.reshape(8, 128, 2).transpose(1, 0, 2))
        maps.append(m)
    return maps


def kernel(**inputs):
    b = Builder()
    nc = b.build()
    maps = make_in_maps(inputs, list(range(8)))
    maps = [{k: v for k, v in m.items() if k in b.inp} for m in maps]
    res = run_bass_kernel_spmd(nc, maps, core_ids=list(range(8)))
    return np.stack([r["out"] for r in res.results], axis=0).astype(np.float32)
```
